# Optimizing a Trainium2 kernel written in Bass

```python
import math
import jax, jax.numpy as jnp
from jax import lax
import numpy as np

D_MODEL = 1024
BATCH = 2
SEQ = 8192
DEPTH = 2

MLA_HEADS = 8
MLA_Q_RANK = 256
MLA_KV_RANK = 128
MLA_NOPE = 64
MLA_ROPE = 32
MLA_V = 64
ROPE_BASE = 10000.0
ATTN_BLOCK = 128
MAX_POS_OFFSET = 4096
MASK_VALUE = -1e30
DN_HEADS = 4
DN_DK = 128
DN_DV = 128
DN_CONV = 4
DN_CHUNK = 64
HG_HEADS = 4
HG_DK = 128
HG_DV = 128
HG_CHUNK = 64
MIN_FORGET = 1e-30
N_GROUPS = 4
EXPERTS_PER_GROUP = 8
N_EXPERTS = N_GROUPS * EXPERTS_PER_GROUP
TOP_K_IN_GROUP = 2
EXPERT_FF = 256
MOE_TOKEN_BLOCK = 128
N_BRANCHES = 3
DEEPNORM_ALPHA = (2 * DEPTH) ** 0.25
DEEPNORM_BETA = (8 * DEPTH) ** -0.25
NORM_EPS = 1e-6

SPLIT_SIZES = (
    MLA_Q_RANK, MLA_KV_RANK, MLA_ROPE,
    DN_HEADS * (2 * DN_DK + DN_DV), DN_HEADS, DN_HEADS,
    DN_HEADS * DN_DV,
    HG_HEADS * HG_DK, HG_HEADS * HG_DK, HG_HEADS * HG_DV,
    HG_HEADS * HG_DV,
    N_BRANCHES * D_MODEL,
)
SPLIT_POINTS = tuple(int(v) for v in np.cumsum(SPLIT_SIZES)[:-1])
IN_COLS = int(sum(SPLIT_SIZES))

kernel_name = "hybrid_mla_gdn_hgrn2_hmoe_deepnorm"


def layer_norm(x, g, b):
    xf = x.astype(jnp.float32)
    mu = jnp.mean(xf, -1, keepdims=True)
    var = jnp.mean(jnp.square(xf - mu), -1, keepdims=True)
    return ((xf - mu) * lax.rsqrt(var + NORM_EPS) * g.astype(jnp.float32)
            + b.astype(jnp.float32)).astype(x.dtype)


def rms_norm(x, g):
    xf = x.astype(jnp.float32)
    y = xf * lax.rsqrt(jnp.mean(jnp.square(xf), -1, keepdims=True) + NORM_EPS)
    return (y * g.astype(jnp.float32)).astype(x.dtype)


def l2norm(t):
    return t * lax.rsqrt(jnp.sum(t * t, -1, keepdims=True) + NORM_EPS)


def masked_exp(mask, t):
    return jnp.where(mask, jnp.exp(jnp.where(mask, t, 0.0)), 0.0)


def rope_tables(positions):
    half = MLA_ROPE // 2
    inv_freq = ROPE_BASE ** (-jnp.arange(half, dtype=jnp.float32) / half)
    ang = positions.astype(jnp.float32)[..., None] * inv_freq
    return jnp.cos(ang), jnp.sin(ang)


def apply_rope(x, cos, sin):
    half = x.shape[-1] // 2
    xf = x.astype(jnp.float32)
    x1, x2 = xf[..., :half], xf[..., half:]
    return jnp.concatenate([x1 * cos - x2 * sin, x2 * cos + x1 * sin], -1).astype(x.dtype)


def mla_branch(c_q, c_kv, k_rope, q_norm, w_uq, kv_norm, w_ukv, cos, sin):
    B, S, _ = c_q.shape
    q = (rms_norm(c_q, q_norm) @ w_uq).reshape(B, S, MLA_HEADS, MLA_NOPE + MLA_ROPE)
    kv = (rms_norm(c_kv, kv_norm) @ w_ukv).reshape(B, S, MLA_HEADS, MLA_NOPE + MLA_V)
    q_nope, q_rope = q[..., :MLA_NOPE], q[..., MLA_NOPE:]
    k_nope, v = kv[..., :MLA_NOPE], kv[..., MLA_NOPE:]
    q_rope = apply_rope(q_rope, cos[:, :, None, :], sin[:, :, None, :])
    k_rope = apply_rope(k_rope, cos, sin)
    q = jnp.concatenate([q_nope, q_rope], -1)
    k = jnp.concatenate(
        [k_nope, jnp.broadcast_to(k_rope[:, :, None, :], (B, S, MLA_HEADS, MLA_ROPE))], -1)
    scale = (MLA_NOPE + MLA_ROPE) ** -0.5
    n_blk = S // ATTN_BLOCK
    qb = (q * scale).reshape(B, n_blk, ATTN_BLOCK, MLA_HEADS, -1).transpose(1, 0, 2, 3, 4)
    key_pos = jnp.arange(S)

    def attend(args):
        q_blk, blk = args
        s = jnp.einsum('bqhd,bkhd->bhqk', q_blk, k).astype(jnp.float32)
        q_pos = blk * ATTN_BLOCK + jnp.arange(ATTN_BLOCK)
        s = jnp.where(key_pos[None, :] <= q_pos[:, None], s, MASK_VALUE)
        p = jax.nn.softmax(s, axis=-1).astype(v.dtype)
        return jnp.einsum('bhqk,bkhd->bqhd', p, v)

    o = lax.map(attend, (qb, jnp.arange(n_blk)))
    return o.transpose(1, 0, 2, 3, 4).reshape(B, S, MLA_HEADS * MLA_V)


def causal_conv_silu(x, w):
    K, C = w.shape
    y = lax.conv_general_dilated(x, w[:, None, :], window_strides=(1,), padding=[(K - 1, 0)],
                                 dimension_numbers=('NWC', 'WIO', 'NWC'), feature_group_count=C)
    return jax.nn.silu(y)


def gated_deltanet_branch(qkv, beta_logit, a_logit, gate, conv_w, a_log, dt_bias, o_norm):
    B, S, _ = qkv.shape
    f32 = jnp.float32
    H, C = DN_HEADS, DN_CHUNK
    N = S // C
    qkv = causal_conv_silu(qkv, conv_w)
    q, k, v = jnp.split(qkv, [H * DN_DK, 2 * H * DN_DK], axis=-1)
    q = l2norm(q.reshape(B, S, H, DN_DK).astype(f32)) * DN_DK ** -0.5
    k = l2norm(k.reshape(B, S, H, DN_DK).astype(f32))
    v = v.reshape(B, S, H, DN_DV).astype(f32)
    beta = jax.nn.sigmoid(beta_logit.astype(f32))
    g = -jnp.exp(a_log.astype(f32)) * jax.nn.softplus(a_logit.astype(f32) + dt_bias.astype(f32))

    def chunks(t):
        return jnp.moveaxis(t.reshape((B, N, C, H) + t.shape[3:]), 3, 1)

    q, k, v, beta, g = chunks(q), chunks(k), chunks(v), chunks(beta), chunks(g)
    g = jnp.cumsum(g, axis=-1)
    tri_incl = jnp.tril(jnp.ones((C, C), bool))
    tri_strict = jnp.tril(jnp.ones((C, C), bool), -1)
    decay = masked_exp(tri_incl, g[..., :, None] - g[..., None, :])
    kb = k * beta[..., None]
    m = jnp.where(tri_strict, jnp.einsum('bhnid,bhnjd->bhnij', kb, k) * decay, 0.0)
    a = m + jnp.eye(C, dtype=f32)
    rhs = jnp.concatenate([v * beta[..., None], kb * jnp.exp(g)[..., None]], -1)
    sol = lax.linalg.triangular_solve(a, rhs, left_side=True, lower=True, unit_diagonal=True)
    u, w = sol[..., :DN_DV], sol[..., DN_DV:]
    qk = jnp.einsum('bhnid,bhnjd->bhnij', q, k) * decay
    q_dec = q * jnp.exp(g)[..., None]
    k_dec = k * jnp.exp(g[..., -1:] - g)[..., None]
    g_last = jnp.exp(g[..., -1])
    xs = tuple(jnp.moveaxis(t, 2, 0) for t in (q_dec, k_dec, u, w, qk, g_last))

    def step(state, inp):
        qd, kd, uc, wc, qkc, gl = inp
        v_new = uc - jnp.einsum('bhcd,bhde->bhce', wc, state)
        o = jnp.einsum('bhcd,bhde->bhce', qd, state) + jnp.einsum('bhij,bhje->bhie', qkc, v_new)
        state = state * gl[..., None, None] + jnp.einsum('bhcd,bhce->bhde', kd, v_new)
        return state, o

    s0 = jnp.zeros((B, H, DN_DK, DN_DV), f32)
    _, o = lax.scan(step, s0, xs)
    o = o.transpose(1, 0, 3, 2, 4).reshape(B, S, H, DN_DV)
    o = rms_norm(o, o_norm) * jax.nn.silu(gate.reshape(B, S, H, DN_DV).astype(f32))
    return o.reshape(B, S, H * DN_DV).astype(qkv.dtype)


def hgrn2_branch(q, f_logit, i_val, gate, lower_bound, o_norm):
    B, S, _ = q.shape
    f32 = jnp.float32
    H, C = HG_HEADS, HG_CHUNK
    N = S // C
    q = jax.nn.silu(q.astype(f32)).reshape(B, S, H, HG_DK)
    lb = lower_bound.reshape(H, HG_DK)
    z = f_logit.astype(f32).reshape(B, S, H, HG_DK)
    f = lb + (1.0 - lb) * jax.nn.sigmoid(z)
    k = (1.0 - lb) * jax.nn.sigmoid(-z)
    log_f = jnp.log(jnp.maximum(f, MIN_FORGET))
    v = i_val.astype(f32).reshape(B, S, H, HG_DV)

    def chunks(t):
        return t.reshape(B, N, C, H, t.shape[-1]).transpose(1, 0, 3, 2, 4)

    qc, kc, vc, bc = chunks(q), chunks(k), chunks(v), chunks(log_f)
    bc = jnp.cumsum(bc, axis=3)
    q_dec = qc * jnp.exp(bc)
    k_dec = kc * jnp.exp(bc[:, :, :, -1:, :] - bc)
    d_last = jnp.exp(bc[:, :, :, -1, :])
    tri_incl = jnp.tril(jnp.ones((C, C), bool))[:, :, None]

    def step(state, inp):
        qs, ks, vs, bs, qd, kd, dl = inp
        dec = masked_exp(tri_incl, bs[:, :, :, None, :] - bs[:, :, None, :, :])
        att = jnp.einsum('bhid,bhjd,bhijd->bhij', qs, ks, dec)
        o = jnp.einsum('bhcd,bhde->bhce', qd, state) + jnp.einsum('bhij,bhje->bhie', att, vs)
        state = state * dl[..., :, None] + jnp.einsum('bhcd,bhce->bhde', kd, vs)
        return state, o

    s0 = jnp.zeros((B, H, HG_DK, HG_DV), f32)
    _, o = lax.scan(step, s0, (qc, kc, vc, bc, q_dec, k_dec, d_last))
    o = o.transpose(1, 0, 3, 2, 4).reshape(B, S, H, HG_DV)
    o = rms_norm(o, o_norm) * jax.nn.silu(gate.reshape(B, S, H, HG_DV).astype(f32))
    return o.reshape(B, S, H * HG_DV).astype(i_val.dtype)


def hier_moe(x, wg, bg, we, be, w_gate, w_up, w_down):
    B, S, D = x.shape
    f32 = jnp.float32
    xt = x.reshape(-1, D)
    group_logits = (xt @ wg + bg).astype(f32)
    p_group = jnp.max(jax.nn.softmax(group_logits, -1), -1, keepdims=True)
    top_group = jnp.argmax(group_logits, -1)
    expert_logits = (xt @ we + be).astype(f32).reshape(-1, N_GROUPS, EXPERTS_PER_GROUP)
    in_group = jnp.einsum('tg,tge->te', jax.nn.one_hot(top_group, N_GROUPS, dtype=f32),
                          expert_logits)
    top_val, top_idx = lax.top_k(in_group, TOP_K_IN_GROUP)
    w_top = jax.nn.softmax(top_val, -1) * p_group
    expert_id = top_group[:, None] * EXPERTS_PER_GROUP + top_idx
    combine = jnp.einsum('tk,tke->te', w_top,
                         jax.nn.one_hot(expert_id, N_EXPERTS, dtype=f32)).astype(x.dtype)
    xb = xt.reshape(-1, MOE_TOKEN_BLOCK, D)
    cb = combine.reshape(-1, MOE_TOKEN_BLOCK, N_EXPERTS)

    def expert_block(args):
        xs, cs = args
        h = jax.nn.silu(jnp.einsum('td,edf->tef', xs, w_gate)) * jnp.einsum('td,edf->tef', xs, w_up)
        return jnp.einsum('tef,efd->td', h * cs[..., None], w_down)

    return lax.map(expert_block, (xb, cb)).reshape(B, S, D)


def setup_inputs(seed: int = 0) -> dict:
    key = jax.random.key(seed)
    keys = jax.random.split(key, 32)
    L, D = DEPTH, D_MODEL
    f32 = jnp.float32

    def nrm(i, shape, scale):
        return jax.random.normal(keys[i], shape, f32) * scale

    x = nrm(0, (BATCH, SEQ, D), 1.0)
    offs = jax.random.randint(keys[1], (BATCH, 1), 0, MAX_POS_OFFSET, dtype=jnp.int32)
    positions = offs + jnp.arange(SEQ, dtype=jnp.int32)[None, :]
    a_init = jax.random.uniform(keys[11], (L, DN_HEADS), f32, 1.0, 16.0)
    dt = jnp.exp(jax.random.uniform(keys[12], (L, DN_HEADS), f32, math.log(1e-3), math.log(1e-1)))
    mla_v_width = MLA_HEADS * MLA_V
    dn_width = DN_HEADS * DN_DV
    hg_width = HG_HEADS * HG_DV
    return {
        "x": x,
        "positions": positions,
        "ln_in_g": 1.0 + nrm(2, (D,), 0.02),
        "ln_in_b": nrm(3, (D,), 0.02),
        "hg_lower_bounds": 1.0 + nrm(4, (L, HG_HEADS * HG_DK), 0.1),
        "w_in": nrm(5, (L, D, IN_COLS), D ** -0.5),
        "mla_q_norm": 1.0 + nrm(6, (L, MLA_Q_RANK), 0.02),
        "mla_w_uq": nrm(7, (L, MLA_Q_RANK, MLA_HEADS * (MLA_NOPE + MLA_ROPE)), MLA_Q_RANK ** -0.5),
        "mla_kv_norm": 1.0 + nrm(8, (L, MLA_KV_RANK), 0.02),
        "mla_w_ukv": nrm(9, (L, MLA_KV_RANK, MLA_HEADS * (MLA_NOPE + MLA_V)), MLA_KV_RANK ** -0.5),
        "dn_conv": nrm(10, (L, DN_CONV, DN_HEADS * (2 * DN_DK + DN_DV)), DN_CONV ** -0.5),
        "dn_a_log": jnp.log(a_init),
        "dn_dt_bias": dt + jnp.log(-jnp.expm1(-dt)),
        "dn_o_norm": 1.0 + nrm(13, (L, DN_DV), 0.02),
        "hg_o_norm": 1.0 + nrm(14, (L, HG_DV), 0.02),
        "w_br_a": nrm(15, (L, mla_v_width, D), mla_v_width ** -0.5),
        "w_br_b": nrm(16, (L, dn_width, D), dn_width ** -0.5),
        "w_br_c": nrm(17, (L, hg_width, D), hg_width ** -0.5),
        "w_out": nrm(18, (L, D, D), D ** -0.5 * DEEPNORM_BETA),
        "ln1_g": 1.0 + nrm(19, (L, D), 0.02),
        "ln1_b": nrm(20, (L, D), 0.02),
        "router_group_w": nrm(21, (L, D, N_GROUPS), D ** -0.5),
        "router_group_b": nrm(22, (L, N_GROUPS), 0.01),
        "router_expert_w": nrm(23, (L, D, N_EXPERTS), D ** -0.5),
        "router_expert_b": nrm(24, (L, N_EXPERTS), 0.01),
        "exp_w_gate": nrm(25, (L, N_EXPERTS, D, EXPERT_FF), D ** -0.5),
        "exp_w_up": nrm(26, (L, N_EXPERTS, D, EXPERT_FF), D ** -0.5),
        "exp_w_down": nrm(27, (L, N_EXPERTS, EXPERT_FF, D), EXPERT_FF ** -0.5 * DEEPNORM_BETA),
        "ln2_g": 1.0 + nrm(28, (L, D), 0.02),
        "ln2_b": nrm(29, (L, D), 0.02),
    }


def reference(x, positions, ln_in_g, ln_in_b, hg_lower_bounds, w_in, mla_q_norm, mla_w_uq,
              mla_kv_norm, mla_w_ukv, dn_conv, dn_a_log, dn_dt_bias, dn_o_norm, hg_o_norm,
              w_br_a, w_br_b, w_br_c, w_out, ln1_g, ln1_b, router_group_w, router_group_b,
              router_expert_w, router_expert_b, exp_w_gate, exp_w_up, exp_w_down, ln2_g, ln2_b):
    cos, sin = rope_tables(positions)
    lb_soft = jax.nn.softmax(hg_lower_bounds.astype(jnp.float32), axis=0)
    lb_all = jnp.cumsum(lb_soft, axis=0) - lb_soft[0]
    h = layer_norm(x, ln_in_g, ln_in_b)
    for l in range(DEPTH):
        proj = h @ w_in[l]
        (c_q, c_kv, k_rope, dn_qkv, dn_beta, dn_a, dn_gate,
         hg_q, hg_f, hg_i, hg_gate, gate_logits) = jnp.split(proj, SPLIT_POINTS, axis=-1)
        y_a = mla_branch(c_q, c_kv, k_rope, mla_q_norm[l], mla_w_uq[l], mla_kv_norm[l],
                         mla_w_ukv[l], cos, sin) @ w_br_a[l]
        y_b = gated_deltanet_branch(dn_qkv, dn_beta, dn_a, dn_gate, dn_conv[l], dn_a_log[l],
                                    dn_dt_bias[l], dn_o_norm[l]) @ w_br_b[l]
        y_c = hgrn2_branch(hg_q, hg_f, hg_i, hg_gate, lb_all[l], hg_o_norm[l]) @ w_br_c[l]
        g_a, g_b, g_c = jnp.split(jax.nn.sigmoid(gate_logits), N_BRANCHES, axis=-1)
        mixed = (g_a * y_a + g_b * y_b + g_c * y_c) @ w_out[l]
        h = layer_norm(DEEPNORM_ALPHA * h + mixed, ln1_g[l], ln1_b[l])
        moe_out = hier_moe(h, router_group_w[l], router_group_b[l], router_expert_w[l],
                           router_expert_b[l], exp_w_gate[l], exp_w_up[l], exp_w_down[l])
        h = layer_norm(DEEPNORM_ALPHA * h + moe_out, ln2_g[l], ln2_b[l])
    return h
```

```python
from contextlib import ExitStack
import numpy as np
import concourse.bass as bass
import concourse.mybir as mybir
from concourse.bass_utils import run_bass_kernel_spmd

F32 = mybir.dt.float32
BF16 = mybir.dt.bfloat16
I32 = mybir.dt.int32
AF = mybir.ActivationFunctionType
ALU = mybir.AluOpType
AX = mybir.AxisListType

NCORES = 8
D = 1024
B = 2
S = 8192
DEPTH = 2
ALPHA = (2 * DEPTH) ** 0.25
EPS = 1e-6
IN_COLS = 7592
NEXP = 32


class Res:
    __slots__ = ("name", "w", "r", "dsem", "dkey", "dcount", "wdma", "excl")

    def __init__(self, name=""):
        self.name = name
        self.w = None
        self.r = {}
        self.dsem = None
        self.dkey = None
        self.dcount = 0
        self.wdma = False
        self.excl = False


class V:
    __slots__ = ("ap", "res")

    def __init__(self, ap, res):
        self.ap = ap
        self.res = res

    def __getitem__(self, key):
        return V(self.ap[key], self.res)

    def f(self, fn):
        return V(fn(self.ap), self.res)

    def bitcast(self, dt):
        return V(self.ap.bitcast(dt), self.res)


class T:
    def __init__(self, handle, name, res=None):
        self.h = handle
        self.res = res if res is not None else Res(name)

    def __getitem__(self, key):
        return V(self.h[key], self.res)

    def v(self, ap):
        return V(ap, self.res)


class _Dummy:
    def then_inc(self, *a, **kw):
        return self


_DUMMY = _Dummy()


class K:
    def __init__(self, nc):
        self.nc = nc
        self.E = {"pe": nc.tensor, "dve": nc.vector, "act": nc.scalar, "pool": nc.gpsimd, "sp": nc.sync}
        self.semobj = {}
        self.tok = {}
        self.seen = {n: {} for n in self.E}
        for n in self.E:
            self.semobj["s_" + n] = nc.alloc_semaphore("s_" + n)
            self.tok[n] = 0
        self.ndsem = 0
        self.dres = []
        self.nuniq = 0
        self.stacks = []
        self.phase_res = []
        self.free_dsems = []
        self.ncoll = 0
        self.coll_tokens = []
        self.dry = None

    def sb(self, name, shape, dt, res=None):
        self.nuniq += 1
        if self.stacks:
            h = self.stacks[-1].enter_context(self.nc.sbuf_tensor(f"{name}_{self.nuniq}", list(shape), dt))
        else:
            h = self.nc.alloc_sbuf_tensor(f"{name}_{self.nuniq}", list(shape), dt)
        t = T(h, name, res=res)
        if self.stacks and res is None:
            self.phase_res[-1].append(t.res)
        return t

    def push(self):
        self.stacks.append(ExitStack())
        self.phase_res.append([])

    def pop(self):
        self.barrier()
        self.stacks.pop().close()
        for r in self.phase_res.pop():
            if r.dsem is not None:
                self.free_dsems.append((r.dsem, r.dkey, r.dcount))
                self.dres.remove(r)
                r.dsem = None

    def ps(self, name, shape, dt=F32):
        self.nuniq += 1
        t = T(self.nc.alloc_psum_tensor(f"{name}_{self.nuniq}", list(shape), dt), name)
        t.res.excl = True
        return t

    def dram(self, name, shape, dt, kind):
        h = self.nc.dram_tensor(name, list(shape), dt, kind=kind)
        return T(h.ap(), name)

    def _gather(self, eng, reads, writes):
        own = "s_" + eng
        deps = {}

        def add(t, raw):
            if t is None:
                return
            k, v = t
            if k == own and (eng == "pe" or not raw):
                return
            if deps.get(k, 0) < v:
                deps[k] = v

        for r in reads:
            add(r.w, True)
            if r.excl:
                for k, v in r.r.items():
                    add((k, v), False)
        for w in writes:
            add(w.w, False)
            for k, v in w.r.items():
                add((k, v), False)
        return deps

    def _emit_waits(self, eng, deps):
        e = self.E[eng]
        seen = self.seen[eng]
        for k, v in deps.items():
            if k.startswith("s_"):
                assert v <= self.tok[k[2:]], f"wait on unrealised token {k} {v} > {self.tok[k[2:]]}"
            if seen.get(k, 0) >= v:
                continue
            e.wait_ge(self.semobj[k], v)
            seen[k] = v

    def op(self, eng, fn, reads, writes, inc=True):
        reads = [r.res if isinstance(r, (V, T)) else r for r in reads if r is not None]
        writes = [w.res if isinstance(w, (V, T)) else w for w in writes if w is not None]
        if self.dry is not None:
            self.dry.append((eng, reads, writes))
            return _DUMMY
        deps = self._gather(eng, reads, writes)
        self._emit_waits(eng, deps)
        ins = fn(self.E[eng])
        key = "s_" + eng
        if inc:
            ins.then_inc(self.semobj[key], 1)
            self.tok[eng] += 1
            t = (key, self.tok[eng])
        else:
            t = (key, self.tok[eng] + 1)
        for w in writes:
            w.w = t
            w.r = {}
            w.wdma = False
        for r in reads:
            if r in writes:
                continue
            if r.r.get(key, 0) < t[1]:
                r.r[key] = t[1]
        return ins

    def collective(self, kind, ins, outs, groups):
        deps = {}

        def add(t):
            if t is None:
                return
            k_, v = t
            if deps.get(k_, 0) < v:
                deps[k_] = v

        for i in ins:
            add(i.res.w)
        for o in outs:
            add(o.res.w)
            for k_, v in o.res.r.items():
                add((k_, v))
        self._emit_waits("pool", deps)
        self.ncoll += 1
        key = f"cc{self.ncoll}"
        sem = self.nc.alloc_semaphore(key)
        self.semobj[key] = sem
        self.E["pool"].collective_compute(kind, ALU.bypass, replica_groups=groups,
                                          ins=[i.ap.opt() for i in ins], outs=[o.ap.opt() for o in outs]).then_inc(sem, 1)
        self.coll_tokens.append((key, 1))
        for o in outs:
            o.res.w = (key, 1)
            o.res.r = {}
            o.res.wdma = False
        for i in ins:
            i.res.r[key] = 1

    def dma(self, q, out, in_, extra_reads=(), **kw):
        w = out.res
        rd = in_.res
        if self.dry is not None:
            self.dry.append(("dma", [rd] + list(extra_reads), [w]))
            return
        own = "s_" + q
        deps = {}

        def add(t):
            if t is None:
                return
            k, v = t
            if deps.get(k, 0) < v:
                deps[k] = v

        add(rd.w)
        for xr in extra_reads:
            add(xr.w)
        if not (w.wdma and not w.r):
            add(w.w)
        for k, v in w.r.items():
            add((k, v))
        self._emit_waits(q, deps)
        if w.dsem is None:
            if self.free_dsems:
                w.dsem, w.dkey, w.dcount = self.free_dsems.pop()
            else:
                self.ndsem += 1
                w.dkey = f"d{self.ndsem}"
                w.dsem = self.nc.alloc_semaphore(w.dkey)
                self.semobj[w.dkey] = w.dsem
                w.dcount = 0
            self.dres.append(w)
        self.E[q].dma_start(out=out.ap, in_=in_.ap, **kw).then_inc(w.dsem, 16)
        w.dcount += 16
        t = (w.dkey, w.dcount)
        w.w = t
        w.r = {}
        w.wdma = True
        if rd.r.get(t[0], 0) < t[1]:
            rd.r[t[0]] = t[1]
        for xr in extra_reads:
            if xr.r.get(t[0], 0) < t[1]:
                xr.r[t[0]] = t[1]

    def barrier(self):
        for eng in self.E:
            deps = {}
            for x in self.E:
                if x != eng and self.tok[x] > 0:
                    deps["s_" + x] = self.tok[x]
            for r in self.dres:
                deps[r.dkey] = max(deps.get(r.dkey, 0), r.dcount)
            for key, v in self.coll_tokens:
                deps[key] = v
            self._emit_waits(eng, deps)

    def finish(self, outs):
        deps = {}
        for o in outs:
            r = o.res if isinstance(o, (V, T)) else o
            deps[r.w[0]] = r.w[1]
        self._emit_waits("sp", deps)

    def mm(self, out, lhsT, rhs, start=True, stop=True, inc=None, extra_reads=()):
        if inc is None:
            inc = stop
        return self.op("pe", lambda e: e.matmul(out.ap, lhsT.ap, rhs.ap, start=start, stop=stop),
                       [lhsT, rhs] + list(extra_reads), [out], inc=inc)

    def transpose(self, out, in_, ident, inc=True):
        return self.op("pe", lambda e: e.transpose(out.ap, in_.ap, ident.ap), [in_, ident], [out], inc=inc)

    def act(self, out, in_, func, bias=None, scale=None, accum_out=None, eng="act"):
        kw = {}
        rd = [in_]
        if bias is not None:
            if isinstance(bias, V):
                kw["bias"] = bias.ap
                rd.append(bias)
            else:
                kw["bias"] = bias
        if scale is not None:
            if isinstance(scale, V):
                kw["scale"] = scale.ap
                rd.append(scale)
            else:
                kw["scale"] = scale
        wr = [out]
        if accum_out is not None:
            kw["accum_out"] = accum_out.ap
            wr.append(accum_out)
        return self.op("act", lambda e: e.activation(out.ap, in_.ap, func, **kw), rd, wr)

    def tt(self, eng, out, in0, in1, op):
        return self.op(eng, lambda e: e.tensor_tensor(out.ap, in0.ap, in1.ap, op), [in0, in1], [out])

    def ts(self, eng, out, in0, s1, s2, op0, op1=None, accum_out=None):
        rd = [in0]
        a1 = s1
        a2 = s2
        if isinstance(s1, V):
            rd.append(s1)
            a1 = s1.ap
        if isinstance(s2, V):
            rd.append(s2)
            a2 = s2.ap
        wr = [out]
        kw = {}
        if op1 is not None:
            kw["op1"] = op1
        if accum_out is not None:
            kw["accum_out"] = accum_out.ap
            wr.append(accum_out)
        return self.op(eng, lambda e: e.tensor_scalar(out.ap, in0.ap, a1, a2, op0, **kw), rd, wr)

    def stt(self, eng, out, in0, scalar, in1, op0, op1):
        rd = [in0, in1]
        a = scalar
        if isinstance(scalar, V):
            rd.append(scalar)
            a = scalar.ap
        return self.op(eng, lambda e: e.scalar_tensor_tensor(out.ap, in0.ap, a, in1.ap, op0, op1), rd, [out])

    def rstd(self, out, in_, scale, eps):
        self.act(out, in_, AF.Sqrt, bias=eps, scale=scale)
        self.op("dve", lambda e: e.reciprocal(out.ap, out.ap), [out], [out])

    def rstd_ln(self, out, in_, scale, eps):
        self.act(out, in_, AF.Ln, bias=eps, scale=scale)
        self.act(out, out, AF.Exp, scale=-0.5)

    def copy(self, eng, out, in_):
        if eng == "act":
            return self.op("act", lambda e: e.copy(out.ap, in_.ap), [in_], [out])
        return self.op(eng, lambda e: e.tensor_copy(out.ap, in_.ap), [in_], [out])

    def memset(self, eng, out, val):
        return self.op(eng, lambda e: e.memset(out.ap, val), [], [out])

    def reduce(self, eng, out, in_, op, axis=AX.X):
        return self.op(eng, lambda e: e.tensor_reduce(out.ap, in_.ap, axis, op), [in_], [out])


def layer_norm_tile(k, x, out, g_b, b_b, tmp, st, pre_scale_res=None, eng_g="pool"):
    k.reduce("dve", st[:, 0:1], x, ALU.add)
    k.ts("dve", st[:, 1:2], st[:, 0:1], -1.0 / D, None, ALU.mult)
    k.act(tmp, x, AF.Square, bias=st[:, 1:2], scale=1.0, accum_out=st[:, 2:3])
    k.rstd_ln(st[:, 3:4], st[:, 2:3], 1.0 / D, EPS)
    k.ts("dve", tmp, x, st[:, 1:2], st[:, 3:4], ALU.add, ALU.mult)
    k.tt(eng_g, tmp, tmp, g_b, ALU.mult)
    k.tt("pool", out, tmp, b_b, ALU.add)


def build_ln0(ntok):
    nc = bass.Bass("TRN2", target_bir_lowering=False)
    k = K(nc)
    x = k.dram("x", [ntok, D], F32, "ExternalInput")
    g = k.dram("g", [1, D], F32, "ExternalInput")
    b = k.dram("b", [1, D], F32, "ExternalInput")
    y = k.dram("y", [ntok, D], F32, "ExternalOutput")
    g_b = k.sb("g_b", [128, D], F32)
    b_b = k.sb("b_b", [128, D], F32)
    k.dma("sp", g_b[:, :], g.v(g.h.partition_broadcast(128)))
    k.dma("sp", b_b[:, :], b.v(b.h.partition_broadcast(128)))
    nt = ntok // 128
    xs = [k.sb(f"x{i}", [128, D], F32) for i in range(2)]
    ys = [k.sb(f"y{i}", [128, D], F32) for i in range(2)]
    tmps = [k.sb(f"t{i}", [128, D], F32) for i in range(2)]
    sts = [k.sb(f"s{i}", [128, 4], F32) for i in range(2)]
    for i in range(nt):
        xt, yt, tt_, st = xs[i % 2], ys[i % 2], tmps[i % 2], sts[i % 2]
        k.dma("sp", xt[:, :], x[i * 128:(i + 1) * 128, :])
        layer_norm_tile(k, xt[:, :], yt[:, :], g_b[:, :], b_b[:, :], tt_[:, :], st[:, :])
        k.dma("sp", y[i * 128:(i + 1) * 128, :], yt[:, :])
    k.finish([y])
    return nc


def run_ln0(x, g, b):
    T_ = x.shape[0] * x.shape[1]
    xs = x.reshape(T_, D)
    per = T_ // NCORES
    nc = build_ln0(per)
    in_maps = [{"x": np.ascontiguousarray(xs[c * per:(c + 1) * per]), "g": g.reshape(1, D), "b": b.reshape(1, D)}
               for c in range(NCORES)]
    res = run_bass_kernel_spmd(nc, in_maps, core_ids=list(range(NCORES)))
    return np.concatenate([r["y"] for r in res.results], axis=0)


BIG = 1.0e30


def bcast_row(t, n=128):
    return t.v(t.h.partition_broadcast(n))


def build_stage_c(ntok, upto=9):
    nc = bass.Bass("TRN2", target_bir_lowering=False)
    k = K(nc)
    NT = ntok // 128
    NB = ntok // 512
    h_d = k.dram("h", [ntok, D], F32, "ExternalInput")
    hT_d = k.dram("hT", [D, ntok], F32, "ExternalInput")
    oT_d = [k.dram(n, [512, ntok], F32, "ExternalInput") for n in ("oaT", "obT", "ocT")]
    wg_d = k.dram("w_gates", [D, 3 * D], F32, "ExternalInput")
    wbr_d = [k.dram(n, [512, D], F32, "ExternalInput") for n in ("w_br_a", "w_br_b", "w_br_c")]
    wout_d = k.dram("w_out", [D, D], F32, "ExternalInput")
    ln1g_d = k.dram("ln1_g", [1, D], F32, "ExternalInput")
    ln1b_d = k.dram("ln1_b", [1, D], F32, "ExternalInput")
    ln2g_d = k.dram("ln2_g", [1, D], F32, "ExternalInput")
    ln2b_d = k.dram("ln2_b", [1, D], F32, "ExternalInput")
    wr_d = k.dram("w_router", [D, 36], F32, "ExternalInput")
    br_d = k.dram("b_router", [1, 36], F32, "ExternalInput")
    ewg_d = k.dram("exp_w_gate", [NEXP, D, 256], F32, "ExternalInput")
    ewu_d = k.dram("exp_w_up", [NEXP, D, 256], F32, "ExternalInput")
    ewd_d = k.dram("exp_w_down", [NEXP, 256, D], F32, "ExternalInput")
    ident_d = k.dram("ident", [128, 128], F32, "ExternalInput")
    out_d = k.dram("out", [ntok, D], F32, "ExternalOutput")

    banks = [k.ps(f"bank{i}", [128, 512], F32) for i in range(8)]

    ident = k.sb("ident", [128, 128], F32)
    k.dma("sp", ident[:, :], ident_d[:, :])
    comb_all = k.sb("comb_all", [128, NT, 32], F32)
    mixT = k.sb("mixT", [128, 8, ntok], BF16)

    k.push()
    wg = k.sb("wg", [128, 8, 3 * D], BF16)
    wbr = k.sb("wbr", [128, 12, D], BF16)
    for kc in range(8):
        k.dma("pool", wg[:, kc, :], wg_d[kc * 128:(kc + 1) * 128, :])
    for br in range(3):
        for kc in range(4):
            k.dma("pool", wbr[:, br * 4 + kc, :], wbr_d[br][kc * 128:(kc + 1) * 128, :])
    hTb = [k.sb(f"hTb{i}", [128, 8, 512], BF16) for i in range(2)]
    oTb = [k.sb(f"oTb{i}", [128, 12, 512], BF16) for i in range(2)]
    sg = [k.sb(f"sg{i}", [128, 512], BF16) for i in range(2)]
    tmx = [k.sb(f"tmx{i}", [128, 512], F32) for i in range(2)]
    mixf = [k.sb(f"mixf{i}", [128, 512], F32) for i in range(2)]
    nb = 0
    for tb in range(NB):
        tsl = slice(tb * 512, (tb + 1) * 512)
        hb, ob = hTb[tb % 2], oTb[tb % 2]
        for kc in range(8):
            k.dma("pool", hb[:, kc, :], hT_d[kc * 128:(kc + 1) * 128, tsl])
        for br in range(3):
            for kc in range(4):
                k.dma("pool", ob[:, br * 4 + kc, :], oT_d[br][kc * 128:(kc + 1) * 128, tsl])
        for r in range(8):
            mf = mixf[r % 2]
            for br in range(3):
                bg = banks[nb % 2]
                by = banks[2 + nb % 2]
                sgt = sg[nb % 2]
                tm = tmx[nb % 2]
                nb += 1
                col = br * D + r * 128
                for kc in range(8):
                    k.mm(bg[:, :], wg[:, kc, col:col + 128], hb[:, kc, :], start=(kc == 0), stop=(kc == 7))
                k.act(sgt[:, :], bg[:, :], AF.Sigmoid)
                for kc in range(4):
                    k.mm(by[:, :], wbr[:, br * 4 + kc, r * 128:(r + 1) * 128], ob[:, br * 4 + kc, :],
                         start=(kc == 0), stop=(kc == 3))
                if br == 0:
                    k.tt("dve", mf[:, :], by[:, :], sgt[:, :], ALU.mult)
                elif br == 1:
                    k.tt("dve", tm[:, :], by[:, :], sgt[:, :], ALU.mult)
                    k.tt("pool", mf[:, :], mf[:, :], tm[:, :], ALU.add)
                else:
                    k.tt("dve", tm[:, :], by[:, :], sgt[:, :], ALU.mult)
                    k.tt("pool", mixT[:, r, tsl], mf[:, :], tm[:, :], ALU.add)
    k.pop()
    if upto == 0:
        dbg = k.dram("dbg", [128, 8 * ntok], BF16, "ExternalOutput")
        k.dma("sp", dbg[:, :], mixT[:, :, :].f(lambda a: a.rearrange("p a b -> p (a b)")))
        k.finish([dbg])
        return nc

    acc = [k.sb(f"acc{i}", [128, D], F32) for i in range(NT)]
    h1T = k.sb("h1T", [128, 8, ntok], BF16)
    k.push()
    wout = k.sb("wout", [128, 8, D], BF16)
    for kc in range(8):
        k.dma("pool", wout[:, kc, :], wout_d[kc * 128:(kc + 1) * 128, :])
    wr = k.sb("wr", [128, 8, 36], F32)
    for kc in range(8):
        k.dma("sp", wr[:, kc, :], wr_d[kc * 128:(kc + 1) * 128, :])
    brb = k.sb("brb", [128, 36], F32)
    k.dma("sp", brb[:, :], bcast_row(br_d))
    g1 = k.sb("g1", [128, D], F32)
    b1 = k.sb("b1", [128, D], F32)
    k.dma("sp", g1[:, :], bcast_row(ln1g_d))
    k.dma("sp", b1[:, :], bcast_row(ln1b_d))
    hts = [k.sb(f"ht{i}", [128, D], F32) for i in range(2)]
    x1s = [k.sb(f"x1{i}", [128, D], F32) for i in range(2)]
    h1s = [k.sb(f"h1{i}", [128, D], F32) for i in range(2)]
    tmps = [k.sb(f"lt{i}", [128, D], F32) for i in range(2)]
    sts = [k.sb(f"ls{i}", [128, 4], F32) for i in range(2)]
    hTf = [k.sb(f"hTf{i}", [128, 8, 128], F32) for i in range(2)]
    rl = [k.sb(f"rl{i}", [128, 36], F32) for i in range(2)]
    rs = [k.sb(f"rs{i}", [128, 16], F32) for i in range(2)]
    elm = [k.sb(f"elm{i}", [128, 32], F32) for i in range(2)]
    elm2 = [k.sb(f"elm2{i}", [128, 32], F32) for i in range(2)]
    oh1 = [k.sb(f"oh1{i}", [128, 32], F32) for i in range(2)]
    oh2 = [k.sb(f"oh2{i}", [128, 32], F32) for i in range(2)]
    for t in range(NT):
        p = t % 2
        tok = slice(t * 128, (t + 1) * 128)
        ht, x1, h1t, tmp, st = hts[p], x1s[p], h1s[p], tmps[p], sts[p]
        k.dma("sp", ht[:, :], h_d[tok, :])
        for half in range(2):
            bk = banks[half + 2 * p]
            for kc in range(8):
                k.mm(bk[:, :], mixT[:, kc, tok], wout[:, kc, half * 512:(half + 1) * 512],
                     start=(kc == 0), stop=(kc == 7))
            k.stt("dve", x1[:, half * 512:(half + 1) * 512], ht[:, half * 512:(half + 1) * 512], ALPHA,
                  bk[:, :], ALU.mult, ALU.add)
        layer_norm_tile(k, x1[:, :], h1t[:, :], g1[:, :], b1[:, :], tmp[:, :], st[:, :])
        k.act(acc[t][:, :], h1t[:, :], AF.Copy, scale=ALPHA)
        hf = hTf[p]
        for q4 in range(2):
            bk = banks[4 + q4 + 2 * p]
            for j in range(4):
                kc = q4 * 4 + j
                k.transpose(bk[:, j * 128:(j + 1) * 128], h1t[:, kc * 128:(kc + 1) * 128], ident[:, :],
                            inc=(j == 3))
            k.copy("dve", hf[:, q4 * 4:(q4 + 1) * 4, :],
                   bk[:, :].f(lambda a: a.rearrange("p (j t) -> p j t", j=4)))
        k.copy("act", h1T[:, :, tok], hf[:, :, :])
        bk = banks[p]
        for kc in range(8):
            k.mm(bk[:, 0:36], hf[:, kc, :], wr[:, kc, :], start=(kc == 0), stop=(kc == 7))
        l, s_, em, em2, o1, o2 = rl[p], rs[p], elm[p], elm2[p], oh1[p], oh2[p]
        cb = comb_all[:, t, :]
        k.tt("dve", l[:, :], bk[:, 0:36], brb[:, :], ALU.add)
        k.reduce("dve", s_[:, 0:1], l[:, 0:4], ALU.max)
        k.ts("dve", s_[:, 1:2], s_[:, 0:1], -1.0, None, ALU.mult)
        k.act(s_[:, 8:12], l[:, 0:4], AF.Exp, bias=s_[:, 1:2], scale=1.0, accum_out=s_[:, 2:3])
        k.op("dve", lambda e: e.reciprocal(s_[:, 3:4].ap, s_[:, 2:3].ap), [s_], [s_])
        k.ts("dve", s_[:, 12:16], l[:, 0:4], s_[:, 0:1], None, ALU.is_equal)
        k.ts("dve", s_[:, 12:16], s_[:, 12:16], BIG, -BIG, ALU.mult, ALU.add)
        k.tt("dve", em[:, :].f(lambda a: a.rearrange("p (g e) -> p g e", g=4)),
             l[:, 4:36].f(lambda a: a.rearrange("p (g e) -> p g e", g=4)),
             s_[:, 12:16].f(lambda a: a.unsqueeze(2).broadcast_to([128, 4, 8])), ALU.add)
        k.reduce("dve", s_[:, 4:5], em[:, :], ALU.max)
        k.ts("dve", o1[:, :], em[:, :], s_[:, 4:5], None, ALU.is_equal)
        k.stt("dve", em2[:, :], o1[:, :], -BIG, em[:, :], ALU.mult, ALU.add)
        k.reduce("dve", s_[:, 5:6], em2[:, :], ALU.max)
        k.ts("dve", o2[:, :], em2[:, :], s_[:, 5:6], None, ALU.is_equal)
        k.tt("dve", s_[:, 6:7], s_[:, 5:6], s_[:, 4:5], ALU.subtract)
        k.act(s_[:, 6:7], s_[:, 6:7], AF.Exp)
        k.ts("dve", s_[:, 7:8], s_[:, 6:7], 1.0, None, ALU.add)
        k.op("dve", lambda e: e.reciprocal(s_[:, 7:8].ap, s_[:, 7:8].ap), [s_], [s_])
        k.tt("dve", s_[:, 7:8], s_[:, 7:8], s_[:, 3:4], ALU.mult)
        k.tt("dve", s_[:, 6:7], s_[:, 6:7], s_[:, 7:8], ALU.mult)
        k.ts("dve", cb, o1[:, :], s_[:, 7:8], None, ALU.mult)
        k.stt("dve", cb, o2[:, :], s_[:, 6:7], cb, ALU.mult, ALU.add)
    k.pop()
    if upto == 1:
        dbg = k.dram("dbg", [128, NT * 32], F32, "ExternalOutput")
        k.dma("sp", dbg[:, :], comb_all[:, :, :].f(lambda a: a.rearrange("p a b -> p (a b)")))
        dbg2 = k.dram("dbg2", [ntok, D], F32, "ExternalOutput")
        for t in range(NT):
            k.dma("sp", dbg2[t * 128:(t + 1) * 128, :], acc[t][:, :])
        k.finish([dbg, dbg2])
        return nc

    k.push()
    NW = 3
    ewg = [k.sb(f"ewg{i}", [128, 8, 256], BF16) for i in range(NW)]
    ewu = [k.sb(f"ewu{i}", [128, 8, 256], BF16) for i in range(NW)]
    ewd = [k.sb(f"ewd{i}", [128, 2, D], BF16) for i in range(NW)]
    sgs = [k.sb(f"sG{i}", [128, 512], BF16) for i in range(2)]
    hcs = [k.sb(f"Hc{i}", [128, 2, 512], BF16) for i in range(2)]
    n1 = 0
    n2 = 0
    for e in range(NEXP):
        wgt, wut, wdt = ewg[e % NW], ewu[e % NW], ewd[e % NW]
        k.dma("pool", wgt[:, :, :], ewg_d.v(ewg_d.h[e].rearrange("(kc p) f -> p kc f", p=128)))
        k.dma("pool", wut[:, :, :], ewu_d.v(ewu_d.h[e].rearrange("(kc p) f -> p kc f", p=128)))
        k.dma("pool", wdt[:, :, :], ewd_d.v(ewd_d.h[e].rearrange("(fc p) d -> p fc d", p=128)))
        for tb in range(NB):
            tsl = slice(tb * 512, (tb + 1) * 512)
            hc = hcs[tb % 2]
            for fc in range(2):
                bG = banks[1 + n1 % 2]
                bU = banks[3 + n1 % 2]
                sgt = sgs[n1 % 2]
                n1 += 1
                for kc in range(8):
                    k.mm(bG[:, :], wgt[:, kc, fc * 128:(fc + 1) * 128], h1T[:, kc, tsl], start=(kc == 0), stop=(kc == 7))
                for kc in range(8):
                    k.mm(bU[:, :], wut[:, kc, fc * 128:(fc + 1) * 128], h1T[:, kc, tsl], start=(kc == 0), stop=(kc == 7))
                k.act(sgt[:, :], bG[:, :], AF.Silu)
                k.tt("dve", hc[:, fc, :], bU[:, :], sgt[:, :], ALU.mult)
            for tt_ in range(4):
                t = tb * 4 + tt_
                for half in range(2):
                    bO = banks[5 + n2 % 3]
                    n2 += 1
                    for fc in range(2):
                        k.mm(bO[:, :], hc[:, fc, tt_ * 128:(tt_ + 1) * 128], wdt[:, fc, half * 512:(half + 1) * 512],
                             start=(fc == 0), stop=(fc == 1))
                    k.stt("dve", acc[t][:, half * 512:(half + 1) * 512], bO[:, :], comb_all[:, t, e:e + 1],
                          acc[t][:, half * 512:(half + 1) * 512], ALU.mult, ALU.add)
    k.pop()

    k.push()
    g2 = k.sb("g2", [128, D], F32)
    b2 = k.sb("b2", [128, D], F32)
    k.dma("sp", g2[:, :], bcast_row(ln2g_d))
    k.dma("sp", b2[:, :], bcast_row(ln2b_d))
    ys = [k.sb(f"y{i}", [128, D], F32) for i in range(2)]
    tmps = [k.sb(f"lt{i}", [128, D], F32) for i in range(2)]
    sts = [k.sb(f"ls{i}", [128, 4], F32) for i in range(2)]
    for t in range(NT):
        p = t % 2
        layer_norm_tile(k, acc[t][:, :], ys[p][:, :], g2[:, :], b2[:, :], tmps[p][:, :], sts[p][:, :])
        k.dma("sp", out_d[t * 128:(t + 1) * 128, :], ys[p][:, :])
    k.finish([out_d])
    k.pop()
    return nc


def stage_c_consts():
    return np.eye(128, dtype=np.float32)


def run_stage_c(h, oa, ob, oc, P, l, upto=9):
    T_ = h.shape[0]
    per = T_ // NCORES
    nc = build_stage_c(per, upto)
    ident = stage_c_consts()
    w_in = P["w_in"][l]
    common = {
        "w_gates": np.ascontiguousarray(w_in[:, 4520:]),
        "w_br_a": P["w_br_a"][l], "w_br_b": P["w_br_b"][l], "w_br_c": P["w_br_c"][l],
        "w_out": P["w_out"][l],
        "ln1_g": P["ln1_g"][l].reshape(1, D), "ln1_b": P["ln1_b"][l].reshape(1, D),
        "ln2_g": P["ln2_g"][l].reshape(1, D), "ln2_b": P["ln2_b"][l].reshape(1, D),
        "w_router": np.ascontiguousarray(np.concatenate([P["router_group_w"][l], P["router_expert_w"][l]], axis=1)),
        "b_router": np.concatenate([P["router_group_b"][l], P["router_expert_b"][l]]).reshape(1, 36),
        "exp_w_gate": P["exp_w_gate"][l], "exp_w_up": P["exp_w_up"][l], "exp_w_down": P["exp_w_down"][l],
        "ident": ident,
    }
    in_maps = []
    for c in range(NCORES):
        sl = slice(c * per, (c + 1) * per)
        m = dict(common)
        m["h"] = np.ascontiguousarray(h[sl])
        m["hT"] = np.ascontiguousarray(h[sl].T)
        m["oaT"] = np.ascontiguousarray(oa[sl].T)
        m["obT"] = np.ascontiguousarray(ob[sl].T)
        m["ocT"] = np.ascontiguousarray(oc[sl].T)
        in_maps.append(m)
    res = run_bass_kernel_spmd(nc, in_maps, core_ids=list(range(NCORES)))
    if upto < 9:
        return res.results
    return np.concatenate([r["out"] for r in res.results], axis=0)


QK_SCALE = 96 ** -0.5
TWO_PI = 2.0 * np.pi


class BankRR:
    def __init__(self, banks):
        self.banks = banks
        self.i = 0

    def __call__(self):
        b = self.banks[self.i % len(self.banks)]
        self.i += 1
        return b


def build_mla(T=S, nblk=None):
    nc = bass.Bass("TRN2", target_bir_lowering=False)
    k = K(nc)
    NBLK = T // 512 if nblk is None else nblk
    hT_d = k.dram("hT", [D, T], F32, "ExternalInput")
    wlat_d = k.dram("w_lat", [D, 448], F32, "ExternalInput")
    gq_d = k.dram("g_q", [128, 2], F32, "ExternalInput")
    gkv_d = k.dram("g_kv", [128, 1], F32, "ExternalInput")
    wuq_d = k.dram("w_uq", [256, 256], F32, "ExternalInput")
    wuk_d = k.dram("w_uk", [128, 128], F32, "ExternalInput")
    wuv_d = k.dram("w_uv", [128, 128], F32, "ExternalInput")
    pos_d = k.dram("pos", [1, T], I32, "ExternalInput")
    frq_d = k.dram("frq", [128, 1], F32, "ExternalInput")
    sgn_d = k.dram("sgn", [128, 1], F32, "ExternalInput")
    tri_d = k.dram("tri", [128, 128], F32, "ExternalInput")
    esel_d = k.dram("esel", [128, 64], F32, "ExternalInput")
    oT_d = k.dram("oT", [128, T], F32, "ExternalOutput")

    banks = [k.ps(f"bank{i}", [128, 512], F32) for i in range(8)]
    nb = BankRR(banks)

    wlat = k.sb("wlat", [128, 8, 448], BF16)
    for kc in range(8):
        k.dma("pool", wlat[:, kc, :], wlat_d[kc * 128:(kc + 1) * 128, :])
    gq = k.sb("gq", [128, 2], F32)
    gkv = k.sb("gkv", [128, 1], F32)
    frq = k.sb("frq", [128, 1], F32)
    sgn = k.sb("sgn", [128, 1], F32)
    esel = k.sb("esel", [128, 64], F32)
    tri = k.sb("tri", [128, 128], BF16)
    k.dma("sp", gq[:, :], gq_d[:, :])
    k.dma("sp", gkv[:, :], gkv_d[:, :])
    k.dma("sp", frq[:, :], frq_d[:, :])
    k.dma("sp", sgn[:, :], sgn_d[:, :])
    k.dma("sp", esel[:, :], esel_d[:, :])
    k.dma("pool", tri[:, :], tri_d[:, :])
    wtmp = k.sb("wtmp", [128, 2, 256], F32)
    wuq = k.sb("wuq", [128, 2, 256], BF16)
    for c in range(2):
        k.dma("sp", wtmp[:, c, :], wuq_d[c * 128:(c + 1) * 128, :])
    for c in range(2):
        k.ts("dve", wuq[:, c, :], wtmp[:, c, :], gq[:, c:c + 1], QK_SCALE, ALU.mult, ALU.mult)
    wtmp2 = k.sb("wtmp2", [128, 2, 128], F32)
    wuk = k.sb("wuk", [128, 128], BF16)
    wuv = k.sb("wuv", [128, 128], BF16)
    k.dma("sp", wtmp2[:, 0, :], wuk_d[:, :])
    k.dma("sp", wtmp2[:, 1, :], wuv_d[:, :])
    k.ts("dve", wuk[:, :], wtmp2[:, 0, :], gkv[:, 0:1], None, ALU.mult)
    k.ts("dve", wuv[:, :], wtmp2[:, 1, :], gkv[:, 0:1], None, ALU.mult)
    ones = k.sb("ones", [128, 128], F32)
    k.memset("dve", ones[:, :], 1.0)

    kT = [k.sb(f"kT{h}", [96, T], BF16) for h in range(2)]
    qT = [k.sb(f"qT{h}", [96, T], BF16) for h in range(2)]
    Vp = k.sb("Vp", [128, 2, T // 128, 128], BF16)
    k.memset("pool", Vp[:, :, :, :], 1.0)
    mx = k.sb("mx", [128, 4], F32)
    k.memset("dve", mx[:, :], 0.0)

    hTb = [k.sb(f"hTb{i}", [128, 8, 512], BF16) for i in range(2)]
    posi = [k.sb(f"posi{i}", [128, 512], I32) for i in range(2)]
    ang = k.sb("ang", [128, 512], F32)
    ang2 = k.sb("ang2", [128, 512], F32)
    ni = k.sb("ni", [128, 512], I32)
    nf = k.sb("nf", [128, 512], F32)
    cs = [k.sb(f"cs{i}", [128, 512], F32) for i in range(2)]
    sn = [k.sb(f"sn{i}", [128, 512], F32) for i in range(2)]
    cq_sb = [k.sb(f"cq_sb{i}", [128, 512], F32) for i in range(2)]
    sq_sb = [k.sb(f"sq_sb{i}", [128, 512], F32) for i in range(2)]
    ckv_sb = k.sb("ckv_sb", [128, 512], F32)
    sqkv = k.sb("sqkv", [128, 512], F32)
    rq = k.sb("rq", [128, 512], F32)
    rkv = k.sb("rkv", [128, 512], F32)
    cqn = [k.sb(f"cqn{i}", [128, 512], BF16) for i in range(2)]
    ckvn = k.sb("ckvn", [128, 512], BF16)
    t1 = k.sb("t1", [128, 512], F32)
    t2 = k.sb("t2", [128, 512], F32)
    nsq = k.sb("nsq", [96, 512], F32)
    mtmp = k.sb("mtmp", [128, 1], F32)

    def sincos(dst, src_ang):
        r6 = slice(64, 96)
        k.ts("dve", nf[r6, :], src_ang[r6, :], 1.0 / TWO_PI, None, ALU.mult)
        k.copy("dve", ni[r6, :], nf[r6, :])
        k.copy("dve", nf[r6, :], ni[r6, :])
        k.stt("dve", nf[r6, :], nf[r6, :], -TWO_PI, src_ang[r6, :], ALU.mult, ALU.add)
        k.ts("dve", nf[r6, :], nf[r6, :], 3.1415925, -3.1415925, ALU.min, ALU.max)
        k.act(dst[r6, :], nf[r6, :], AF.Sin)

    def rope_rows(dsts, bA, bB, cst, snt, col):
        r6 = slice(64, 96)
        k.stt("dve", t1[r6, :], bB[r6, :], sgn[r6, 0:1], snt[r6, :], ALU.mult, ALU.mult)
        k.tt("dve", t2[r6, :], bA[r6, :], cst[r6, :], ALU.mult)
        for i, d_ in enumerate(dsts):
            k.tt("pool", d_[r6, col], t1[r6, :], t2[r6, :], ALU.add)

    def normsq(src, col, slot):
        k.act(nsq[:, :], src[0:96, col], AF.Square)
        bk = nb()
        k.mm(bk[:, :], ones[0:96, :], nsq[:, :])
        k.reduce("dve", mtmp[:, :], bk[:, :], ALU.max)
        k.tt("dve", mx[:, slot:slot + 1], mx[:, slot:slot + 1], mtmp[:, :], ALU.max)

    for blk in range(NBLK):
        col = slice(blk * 512, (blk + 1) * 512)
        hb = hTb[blk % 2]
        pi_ = posi[blk % 2]
        cst, snt = cs[blk % 2], sn[blk % 2]
        for kc in range(8):
            k.dma("pool", hb[:, kc, :], hT_d[kc * 128:(kc + 1) * 128, col])
        k.dma("sp", pi_[:, :], pos_d.v(pos_d.h[:, col].partition_broadcast(128)))
        r6 = slice(64, 96)
        k.copy("dve", ang[r6, :], pi_[r6, :])
        k.ts("dve", ang[r6, :], ang[r6, :], frq[r6, 0:1], None, ALU.mult)
        k.ts("dve", ang2[r6, :], ang[r6, :], float(np.pi / 2), None, ALU.add)
        sincos(snt, ang)
        sincos(cst, ang2)
        for c in range(2):
            bk = nb()
            for kc in range(8):
                k.mm(bk[:, :], wlat[:, kc, c * 128:(c + 1) * 128], hb[:, kc, :], start=(kc == 0), stop=(kc == 7))
            k.copy("act", cq_sb[c][:, :], bk[:, :])
            k.act(sq_sb[c][:, :], bk[:, :], AF.Square)
        bk = nb()
        for kc in range(8):
            k.mm(bk[:, :], wlat[:, kc, 256:384], hb[:, kc, :], start=(kc == 0), stop=(kc == 7))
        k.copy("act", ckv_sb[:, :], bk[:, :])
        k.act(sqkv[:, :], bk[:, :], AF.Square)
        bA = nb()
        for kc in range(8):
            k.mm(bA[0:96, :], wlat[:, kc, 320:416], hb[:, kc, :], start=(kc == 0), stop=(kc == 7))
        bB = nb()
        for kc in range(8):
            k.mm(bB[0:96, :], wlat[:, kc, 352:448], hb[:, kc, :], start=(kc == 0), stop=(kc == 7))
        rope_rows([kT[0], kT[1]], bA, bB, cst, snt, col)
        bk = nb()
        k.mm(bk[:, :], ones[:, :], sq_sb[0][:, :], start=True, stop=False)
        k.mm(bk[:, :], ones[:, :], sq_sb[1][:, :], start=False, stop=True)
        k.rstd(rq[:, :], bk[:, :], 1.0 / 256, EPS)
        bk = nb()
        k.mm(bk[:, :], ones[:, :], sqkv[:, :])
        k.rstd(rkv[:, :], bk[:, :], 1.0 / 128, EPS)
        for c in range(2):
            k.tt("dve", cqn[c][:, :], cq_sb[c][:, :], rq[:, :], ALU.mult)
        k.tt("pool", ckvn[:, :], ckv_sb[:, :], rkv[:, :], ALU.mult)
        for hd in range(2):
            bk = nb()
            k.mm(bk[0:64, :], wuk[:, hd * 64:(hd + 1) * 64], ckvn[:, :])
            k.copy("act", kT[hd][0:64, col], bk[0:64, :])
        bk = nb()
        for tt_ in range(4):
            k.mm(bk[:, tt_ * 128:(tt_ + 1) * 128], ckvn[:, tt_ * 128:(tt_ + 1) * 128], wuv[:, :],
                 start=True, stop=True, inc=(tt_ == 3))
        k.copy("act", Vp[:, :, blk * 4:(blk + 1) * 4, 0:64],
               bk[:, :].f(lambda a: a.rearrange("p (t h d) -> p h t d", t=4, h=2)))
        for hd in range(2):
            bA = nb()
            for c in range(2):
                k.mm(bA[0:96, :], wuq[:, c, hd * 128:hd * 128 + 96], cqn[c][:, :], start=(c == 0), stop=(c == 1))
            bB = nb()
            for c in range(2):
                k.mm(bB[0:96, :], wuq[:, c, hd * 128 + 32:hd * 128 + 128], cqn[c][:, :], start=(c == 0), stop=(c == 1))
            k.copy("act", qT[hd][0:64, col], bA[0:64, :])
            rope_rows([qT[hd]], bA, bB, cst, snt, col)
        for hd in range(2):
            normsq(qT[hd], col, hd)
            normsq(kT[hd], col, 2 + hd)

    negc = k.sb("negc", [128, 2], F32)
    k.tt("dve", negc[:, :], mx[:, 0:2], mx[:, 2:4], ALU.mult)
    k.act(negc[:, :], negc[:, :], AF.Sqrt)
    k.ts("dve", negc[:, :], negc[:, :], -1.0, None, ALU.mult)

    acc_banks = BankRR(banks[0:2])
    s_banks = BankRR(banks[2:7])
    den_bank = banks[7]
    pT = [k.sb(f"pT{i}", [128, 512], BF16) for i in range(4)]
    osb = [k.sb(f"osb{i}", [128, 512], F32) for i in range(2)]
    ores = [k.sb(f"ores{i}", [64, 512], F32) for i in range(2)]
    npt = 0
    no = 0
    for hd in range(2):
        for qi in range(NBLK):
            oacc = acc_banks()
            nkb = 4 * qi + 4
            for kb in range(nkb):
                r = kb - 4 * qi
                c0 = 128 * r if r > 0 else 0
                qcol = slice(qi * 512 + c0, (qi + 1) * 512)
                sb_ = s_banks()
                pt = pT[npt % 4]
                npt += 1
                k.mm(sb_[:, c0:512], kT[hd][:, kb * 128:(kb + 1) * 128], qT[hd][:, qcol])
                k.act(pt[:, c0:512], sb_[:, c0:512], AF.Exp, bias=negc[:, hd:hd + 1], scale=1.0)
                if r >= 0:
                    k.tt("pool", pt[:, c0:c0 + 128], pt[:, c0:c0 + 128], tri[:, :], ALU.mult)
                k.mm(oacc[:, c0:512], Vp[:, hd, kb, :], pt[:, c0:512], start=(kb == 0), stop=(kb == nkb - 1))
            ob = osb[no % 2]
            orr = ores[no % 2]
            no += 1
            k.copy("act", ob[:, :], oacc[:, :])
            k.op("dve", lambda e: e.reciprocal(ob[64:128, :].ap, ob[64:128, :].ap), [ob], [ob])
            k.mm(den_bank[0:64, :], esel[:, :], ob[:, :])
            k.tt("dve", orr[:, :], ob[0:64, :], den_bank[0:64, :], ALU.mult)
            k.dma("sp", oT_d[hd * 64:(hd + 1) * 64, qi * 512:(qi + 1) * 512], orr[:, :])
    k.finish([oT_d])
    return nc


def mla_inputs(hT_b, pos_b, P, l, j, consts_only=False):
    inv_freq = (10000.0 ** (-np.arange(16, dtype=np.float32) / np.float32(16))).astype(np.float32)
    frq = np.zeros((128, 1), np.float32)
    frq[64:80, 0] = inv_freq
    frq[80:96, 0] = inv_freq
    sgn = np.zeros((128, 1), np.float32)
    sgn[64:80] = -1.0
    sgn[80:96] = 1.0
    tri = (np.arange(128)[:, None] <= np.arange(128)[None, :]).astype(np.float32)
    esel = np.zeros((128, 64), np.float32)
    esel[64, :] = 1.0
    if consts_only:
        return {"frq": frq, "sgn": sgn, "tri": tri, "esel": esel}
    w_in = P["w_in"][l]
    kr = w_in[:, 384:416]
    wlat = np.concatenate([w_in[:, 0:384], kr, kr[:, 16:32], kr[:, 0:16]], axis=1)
    wuq = P["mla_w_uq"][l].reshape(256, 8, 96)
    wukv = P["mla_w_ukv"][l].reshape(128, 8, 128)
    heads = (2 * j, 2 * j + 1)
    wuq_c = np.concatenate([np.concatenate([wuq[:, h, 0:64], wuq[:, h, 64:96], wuq[:, h, 80:96], wuq[:, h, 64:80]], axis=1)
                            for h in heads], axis=1)
    wuk_c = np.concatenate([wukv[:, h, 0:64] for h in heads], axis=1)
    wuv_c = np.concatenate([wukv[:, h, 64:128] for h in heads], axis=1)
    inv_freq = (10000.0 ** (-np.arange(16, dtype=np.float32) / np.float32(16))).astype(np.float32)
    frq = np.zeros((128, 1), np.float32)
    frq[64:80, 0] = inv_freq
    frq[80:96, 0] = inv_freq
    sgn = np.zeros((128, 1), np.float32)
    sgn[64:80] = -1.0
    sgn[80:96] = 1.0
    tri = (np.arange(128)[:, None] <= np.arange(128)[None, :]).astype(np.float32)
    esel = np.zeros((128, 64), np.float32)
    esel[64, :] = 1.0
    return {
        "hT": hT_b, "w_lat": np.ascontiguousarray(wlat),
        "g_q": np.ascontiguousarray(P["mla_q_norm"][l].reshape(2, 128).T),
        "g_kv": np.ascontiguousarray(P["mla_kv_norm"][l].reshape(128, 1)),
        "w_uq": np.ascontiguousarray(wuq_c), "w_uk": np.ascontiguousarray(wuk_c), "w_uv": np.ascontiguousarray(wuv_c),
        "pos": np.ascontiguousarray(pos_b.reshape(1, -1).astype(np.int32)),
        "frq": frq, "sgn": sgn, "tri": tri, "esel": esel,
    }


HC = 32


def hg_consts():
    t = np.arange(128)
    ch = t // HC
    same = ch[:, None] == ch[None, :]
    U = (same & (t[:, None] <= t[None, :])).astype(np.float32)
    mid = ch * HC + (HC // 2 - 1)
    Umid = (same & (t[:, None] <= mid[None, :])).astype(np.float32)
    W = (same & (t[:, None] > t[None, :])).astype(np.float32)
    cones = (ch[:, None] == np.arange(4)[None, :]).astype(np.float32)
    maskbd = (same & (t[:, None] <= t[None, :])).astype(np.float32)
    return {"cU": U, "cUrel": (U - Umid).astype(np.float32), "cW": W, "cones": cones, "maskbd": maskbd,
            "rowmask": cones.copy()}


def build_hg(T=S, layer=0):
    nc = bass.Bass("TRN2", target_bir_lowering=False)
    k = K(nc)
    NBLK = T // 512
    hT_d = k.dram("hT", [D, T], F32, "ExternalInput")
    w_d = k.dram("w_hg", [D, 512], F32, "ExternalInput")
    lbr_d = k.dram("lb_rows", [DEPTH, 128], F32, "ExternalInput")
    lbc_d = k.dram("lb_cols", [128, DEPTH], F32, "ExternalInput")
    on_d = k.dram("o_norm", [1, 128], F32, "ExternalInput")
    cU_d = k.dram("cU", [128, 128], F32, "ExternalInput")
    cUrel_d = k.dram("cUrel", [128, 128], F32, "ExternalInput")
    cW_d = k.dram("cW", [128, 128], F32, "ExternalInput")
    cones_d = k.dram("cones", [128, 4], F32, "ExternalInput")
    mbd_d = k.dram("maskbd", [128, 128], F32, "ExternalInput")
    rm_d = k.dram("rowmask", [128, 4], F32, "ExternalInput")
    o_d = k.dram("o", [T, 128], F32, "ExternalOutput")

    banks = [k.ps(f"bank{i}", [128, 512], F32) for i in range(8)]
    nb = BankRR(banks)

    whg = k.sb("whg", [128, 8, 512], BF16)
    for kc in range(8):
        k.dma("pool", whg[:, kc, :], w_d[kc * 128:(kc + 1) * 128, :])
    cU = k.sb("cU", [128, 128], F32)
    cUrel = k.sb("cUrel", [128, 128], F32)
    cW = k.sb("cW", [128, 128], F32)
    cones = k.sb("cones", [128, 4], F32)
    mbd = k.sb("mbd", [128, 128], F32)
    rowm = k.sb("rowm", [128, 4], F32)
    gb = k.sb("gb", [128, 128], F32)
    for t_, d_ in ((cU, cU_d), (cUrel, cUrel_d), (cW, cW_d), (cones, cones_d), (mbd, mbd_d), (rowm, rm_d)):
        k.dma("sp", t_[:, :], d_[:, :])
    k.dma("sp", gb[:, :], bcast_row(on_d))

    def lower_bound(x, n, name):
        m = k.sb(name + "_m", [128, n], F32)
        e = k.sb(name + "_e", [128, DEPTH, n], F32)
        ssum = k.sb(name + "_s", [128, n], F32)
        lb = k.sb(name + "_lb", [128, n], F32)
        oml = k.sb(name + "_oml", [128, n], F32)
        k.copy("dve", m[:, :], x[:, 0, :])
        for i in range(1, DEPTH):
            k.tt("dve", m[:, :], m[:, :], x[:, i, :], ALU.max)
        for i in range(DEPTH):
            k.tt("dve", e[:, i, :], x[:, i, :], m[:, :], ALU.subtract)
        k.act(e[:, :, :], e[:, :, :], AF.Exp)
        k.copy("dve", ssum[:, :], e[:, 0, :])
        for i in range(1, DEPTH):
            k.tt("dve", ssum[:, :], ssum[:, :], e[:, i, :], ALU.add)
        k.op("dve", lambda en: en.reciprocal(ssum[:, :].ap, ssum[:, :].ap), [ssum], [ssum])
        for i in range(DEPTH):
            k.tt("dve", e[:, i, :], e[:, i, :], ssum[:, :], ALU.mult)
        k.copy("dve", lb[:, :], e[:, 0, :])
        for i in range(1, layer + 1):
            k.tt("dve", lb[:, :], lb[:, :], e[:, i, :], ALU.add)
        k.tt("dve", lb[:, :], lb[:, :], e[:, 0, :], ALU.subtract)
        k.ts("dve", oml[:, :], lb[:, :], -1.0, 1.0, ALU.mult, ALU.add)
        return lb, oml

    xr = k.sb("xr", [128, DEPTH, 128], F32)
    for i in range(DEPTH):
        k.dma("sp", xr[:, i, :], lbr_d.v(lbr_d.h[i:i + 1, :].partition_broadcast(128)))
    lb_b, oml_b = lower_bound(xr, 128, "lbr")
    xc = k.sb("xc", [128, DEPTH, 1], F32)
    k.dma("sp", xc[:, :, 0], lbc_d[:, :])
    lb_c, oml_c = lower_bound(xc, 1, "lbc")
    noml_c = k.sb("noml_c", [128, 1], F32)
    k.ts("dve", noml_c[:, :], oml_c[:, :], -1.0, None, ALU.mult)

    NS = 8
    Sf = [k.sb(f"Sf{i}", [128, 128], F32) for i in range(2)]
    Sb = [k.sb(f"Sb{i}", [128, 128], BF16) for i in range(NS)]
    k.memset("dve", Sf[0][:, :], 0.0)
    k.memset("dve", Sb[0][:, :], 0.0)
    Z = [k.sb(f"Z{i}", [128, 4, 128], BF16) for i in range(2)]
    for z in Z:
        k.memset("pool", z[:, :, :], 0.0)
    si = 0

    hTb = [k.sb(f"hTb{i}", [128, 8, 512], BF16) for i in range(2)]
    qTs = [k.sb(f"qTs{i}", [128, 512], F32) for i in range(2)]
    kTs = [k.sb(f"kTs{i}", [128, 512], F32) for i in range(2)]

    def dbl(name, shape, dt, n=2):
        return [k.sb(f"{name}{i}", shape, dt) for i in range(n)]

    sg = dbl("sg", [128, 128], F32)
    uu = dbl("uu", [128, 128], F32)
    ff = dbl("ff", [128, 128], F32)
    ktm = dbl("ktm", [128, 128], F32)
    logf = dbl("logf", [128, 128], F32)
    vbf = dbl("vbf", [128, 128], BF16)
    sgate = dbl("sgate", [128, 128], F32)
    e1 = dbl("e1", [128, 128], F32)
    e2 = dbl("e2", [128, 128], F32)
    e3 = dbl("e3", [128, 128], F32)
    e4 = dbl("e4", [128, 128], F32)
    dl = dbl("dl", [128, 4], F32)
    qpT = dbl("qpT", [128, 128], BF16)
    kpT = dbl("kpT", [128, 128], BF16)
    kdp = dbl("kdp", [128, 4, 128], BF16)
    attm = dbl("attm", [128, 128], BF16)
    osq = dbl("osq", [128, 128], F32)
    ost = dbl("ost", [128, 2], F32)
    y1 = dbl("y1", [128, 128], F32)
    y2 = dbl("y2", [128, 128], F32)

    nt = 0
    for blk in range(NBLK):
        col = slice(blk * 512, (blk + 1) * 512)
        hb = hTb[blk % 2]
        for kc in range(8):
            k.dma("pool", hb[:, kc, :], hT_d[kc * 128:(kc + 1) * 128, col])
        qT_s, kT_s = qTs[blk % 2], kTs[blk % 2]
        bq = nb()
        for kc in range(8):
            k.mm(bq[:, :], whg[:, kc, 0:128], hb[:, kc, :], start=(kc == 0), stop=(kc == 7))
        k.act(qT_s[:, :], bq[:, :], AF.Silu)
        bz = nb()
        for kc in range(8):
            k.mm(bz[:, :], whg[:, kc, 128:256], hb[:, kc, :], start=(kc == 0), stop=(kc == 7))
        k.act(kT_s[:, :], bz[:, :], AF.Sigmoid)
        k.ts("dve", kT_s[:, :], kT_s[:, :], noml_c[:, 0:1], oml_c[:, 0:1], ALU.mult, ALU.add)
        for tt_ in range(4):
            p = nt % 2
            nt += 1
            tcol = slice(tt_ * 128, (tt_ + 1) * 128)
            tok = slice(blk * 512 + tt_ * 128, blk * 512 + (tt_ + 1) * 128)
            btm = nb()
            for kc in range(8):
                k.mm(btm[:, 0:384], hb[:, kc, tcol], whg[:, kc, 128:512], start=(kc == 0), stop=(kc == 7))
            k.act(sg[p][:, :], btm[:, 0:128], AF.Sigmoid)
            k.copy("act", vbf[p][:, :], btm[:, 128:256])
            k.act(sgate[p][:, :], btm[:, 256:384], AF.Silu)
            k.tt("dve", uu[p][:, :], sg[p][:, :], oml_b[:, :], ALU.mult)
            k.tt("pool", ff[p][:, :], uu[p][:, :], lb_b[:, :], ALU.add)
            k.tt("pool", ktm[p][:, :], oml_b[:, :], uu[p][:, :], ALU.subtract)
            k.ts("dve", ff[p][:, :], ff[p][:, :], 1e-30, None, ALU.max)
            k.act(logf[p][:, :], ff[p][:, :], AF.Ln)
            bc = nb()
            k.mm(bc[:, 0:128], logf[p][:, :], cU[:, :], inc=False)
            k.mm(bc[:, 128:256], logf[p][:, :], cUrel[:, :], inc=False)
            k.mm(bc[:, 256:384], cW[:, :], logf[p][:, :], inc=False)
            k.mm(bc[:, 384:388], logf[p][:, :], cones[:, :], inc=True)
            k.act(e1[p][:, :], bc[:, 0:128], AF.Exp)
            k.act(e2[p][:, :], bc[:, 128:256], AF.Exp)
            k.act(e3[p][:, :], bc[:, 128:256], AF.Exp, scale=-1.0)
            k.act(e4[p][:, :], bc[:, 256:384], AF.Exp)
            k.act(dl[p][:, :], bc[:, 384:388], AF.Exp)
            z = Z[p]
            zdiag = z.v(bass.AP(z.h, 0, [[512, 128], [160, 4], [1, 32]]))
            k.tt("dve", zdiag, qT_s[:, tcol].f(lambda a: a.rearrange("p (c x) -> p c x", c=4)),
                 e1[p][:, :].f(lambda a: a.rearrange("p (c x) -> p c x", c=4)), ALU.mult)
            k.tt("pool", qpT[p][:, :], qT_s[:, tcol], e2[p][:, :], ALU.mult)
            k.tt("pool", kpT[p][:, :], kT_s[:, tcol], e3[p][:, :], ALU.mult)
            for c in range(4):
                k.stt("dve" if c % 2 == 0 else "pool", kdp[p][:, c, :], ktm[p][:, :], rowm[:, c:c + 1], e4[p][:, :],
                      ALU.mult, ALU.mult) if c % 2 == 0 else None
            for c in range(4):
                if c % 2 == 1:
                    k.stt("dve", kdp[p][:, c, :], ktm[p][:, :], rowm[:, c:c + 1], e4[p][:, :], ALU.mult, ALU.mult)
            ba = nb()
            k.mm(ba[:, 0:128], kpT[p][:, :], qpT[p][:, :])
            k.tt("dve", attm[p][:, :], ba[:, 0:128], mbd[:, :], ALU.mult)
            bs = nb()
            for c in range(4):
                k.mm(bs[:, c * 128:(c + 1) * 128], kdp[p][:, c, :], vbf[p][:, :], inc=(c == 3))
            bo = nb()
            for c in range(4):
                k.mm(bo[:, 0:128], z[:, c, :], Sb[(si + c) % NS][:, :], start=(c == 0), stop=False, inc=False)
                s_old = Sf[(si + c) % 2]
                s_new = Sf[(si + c + 1) % 2]
                k.stt("dve", s_new[:, :], s_old[:, :], dl[p][:, c:c + 1], bs[:, c * 128:(c + 1) * 128],
                      ALU.mult, ALU.add)
                k.copy("act", Sb[(si + c + 1) % NS][:, :], s_new[:, :])
            k.mm(bo[:, 0:128], attm[p][:, :], vbf[p][:, :], start=False, stop=True, inc=True)
            si += 4
            k.act(osq[p][:, :], bo[:, 0:128], AF.Square, accum_out=ost[p][:, 0:1])
            k.rstd(ost[p][:, 1:2], ost[p][:, 0:1], 1.0 / 128, EPS)
            k.stt("dve", y1[p][:, :], bo[:, 0:128], ost[p][:, 1:2], gb[:, :], ALU.mult, ALU.mult)
            k.tt("pool", y2[p][:, :], y1[p][:, :], sgate[p][:, :], ALU.mult)
            k.dma("sp", o_d[tok, :], y2[p][:, :])
    k.finish([o_d])
    return nc


def hg_inputs(hT_b, P, l, j):
    w_in = P["w_in"][l]
    cols = [2472 + j * 128, 2984 + j * 128, 3496 + j * 128, 4008 + j * 128]
    w = np.concatenate([w_in[:, c:c + 128] for c in cols], axis=1)
    lb = P["hg_lower_bounds"][:, j * 128:(j + 1) * 128]
    m = {"hT": hT_b, "w_hg": np.ascontiguousarray(w), "lb_rows": np.ascontiguousarray(lb),
         "lb_cols": np.ascontiguousarray(lb.T), "o_norm": P["hg_o_norm"][l].reshape(1, 128)}
    m.update(hg_consts())
    return m


MASKV = 30000.0


def dn_consts():
    t = np.arange(128)
    uinc = (t[:, None] <= t[None, :]).astype(np.float32)
    lpos_s = np.where(t[None, :] < t[:, None], 0.0, MASKV).astype(np.float32)
    uneg = np.where(t[:, None] <= t[None, :], 0.0, -MASKV).astype(np.float32)
    return {"uinc": uinc, "lpos_s": lpos_s, "uneg": uneg, "ident": np.eye(128, dtype=np.float32)}


def build_dn(T=S):
    nc = bass.Bass("TRN2", target_bir_lowering=False)
    k = K(nc)
    NBLK = T // 512
    hT_d = k.dram("hT", [D, T], F32, "ExternalInput")
    w_d = k.dram("w_dn", [D, 384 + 130], F32, "ExternalInput")
    cw_d = k.dram("conv_w", [128, 3, 4], F32, "ExternalInput")
    alog_d = k.dram("a_log", [1, 1], F32, "ExternalInput")
    dtb_d = k.dram("dt_bias", [1, 1], F32, "ExternalInput")
    on_d = k.dram("o_norm", [1, 128], F32, "ExternalInput")
    uinc_d = k.dram("uinc", [128, 128], F32, "ExternalInput")
    lpos_d = k.dram("lpos_s", [128, 128], F32, "ExternalInput")
    uneg_d = k.dram("uneg", [128, 128], F32, "ExternalInput")
    ident_d = k.dram("ident", [128, 128], F32, "ExternalInput")
    o_d = k.dram("o", [T, 128], F32, "ExternalOutput")

    banks = [k.ps(f"bank{i}", [128, 512], F32) for i in range(8)]
    nb = BankRR(banks)

    wdn = k.sb("wdn", [128, 8, 514], BF16)
    for kc in range(8):
        k.dma("pool", wdn[:, kc, :], w_d[kc * 128:(kc + 1) * 128, :])
    cw = k.sb("cw", [128, 3, 4], F32)
    k.dma("sp", cw[:, :, :], cw_d[:, :, :])
    uinc = k.sb("uinc", [128, 128], F32)
    lpos = k.sb("lpos", [128, 128], F32)
    uneg = k.sb("uneg", [128, 128], F32)
    ident = k.sb("ident", [128, 128], F32)
    gb = k.sb("gb", [128, 128], F32)
    for t_, d_ in ((uinc, uinc_d), (lpos, lpos_d), (uneg, uneg_d), (ident, ident_d)):
        k.dma("sp", t_[:, :], d_[:, :])
    k.dma("sp", gb[:, :], bcast_row(on_d))
    sc = k.sb("sc", [128, 4], F32)
    k.dma("sp", sc[:, 0:1], bcast_row(alog_d))
    k.dma("sp", sc[:, 1:2], bcast_row(dtb_d))
    k.act(sc[:, 2:3], sc[:, 0:1], AF.Exp)
    k.ts("dve", sc[:, 2:3], sc[:, 2:3], -1.0, None, ALU.mult)
    ones = k.sb("ones", [128, 128], F32)
    k.memset("dve", ones[:, :], 1.0)

    Sf = [k.sb(f"S{i}", [128, 128], F32) for i in range(2)]
    k.memset("dve", Sf[0][:, :], 0.0)
    si = 0

    hTb = [k.sb(f"hTb{i}", [128, 8, 512], BF16) for i in range(2)]
    xh = [[k.sb(f"xh{w}_{i}", [128, 515], F32) for i in range(2)] for w in range(3)]
    for w in range(3):
        k.memset("dve", xh[w][1][:, 512:515], 0.0)
    cv = [k.sb(f"cv{w}", [128, 512], F32) for w in range(3)]
    sq = [k.sb(f"sq{w}", [128, 512], F32) for w in range(2)]
    rs = [k.sb(f"rs{w}", [128, 512], F32) for w in range(2)]

    def per_tile(name, shape, dt=F32):
        return [k.sb(f"{name}{i}", shape, dt) for i in range(4)]

    tmc = per_tile("tmc", [128, 8])
    sgate = per_tile("sgate", [128, 128])
    ktm = per_tile("ktm", [128, 128])
    vb = per_tile("vb", [128, 128])
    gbc = per_tile("gbc", [128, 128])
    xm = per_tile("xm", [128, 128])
    ym = per_tile("ym", [128, 128])
    dec_s = per_tile("dec_s", [128, 128])
    decT = per_tile("decT", [128, 128])
    egb = per_tile("egb", [128, 128])
    Mt = per_tile("M", [128, 128])
    Nt = per_tile("N", [128, 128])
    Pa = per_tile("Pa", [128, 128])
    Pat = per_tile("Pat", [128, 128])
    Pb = per_tile("Pb", [128, 128])
    Pbt = per_tile("Pbt", [128, 128])
    Rr = per_tile("R", [128, 128])
    Rt = per_tile("Rt", [128, 128])
    qkT = per_tile("qkT", [128, 128])
    qdT = per_tile("qdT", [128, 128])
    kbg = per_tile("kbg", [128, 128])
    kdec = per_tile("kdec", [128, 128])
    u_sb = per_tile("u", [128, 128])
    wT_sb = per_tile("wT", [128, 128])
    vnew = per_tile("vnew", [128, 128])
    osq = per_tile("osq", [128, 128])
    ost = per_tile("ost", [128, 2])
    y1 = per_tile("y1", [128, 128])
    y2 = per_tile("y2", [128, 128])

    for blk in range(NBLK):
        col = slice(blk * 512, (blk + 1) * 512)
        hb = hTb[blk % 2]
        for kc in range(8):
            k.dma("pool", hb[:, kc, :], hT_d[kc * 128:(kc + 1) * 128, col])
        for w in range(3):
            bk = nb()
            for kc in range(8):
                k.mm(bk[:, :], wdn[:, kc, w * 128:(w + 1) * 128], hb[:, kc, :], start=(kc == 0), stop=(kc == 7))
            cur, prv = xh[w][blk % 2], xh[w][(blk + 1) % 2]
            k.copy("act", cur[:, 3:515], bk[:, :])
            k.copy("act", cur[:, 0:3], prv[:, 512:515])
            y = cv[w]
            k.ts("dve", y[:, :], cur[:, 0:512], cw[:, w, 0:1], None, ALU.mult)
            for m in range(1, 4):
                k.stt("dve" if m != 2 else "dve", y[:, :], cur[:, m:m + 512], cw[:, w, m:m + 1], y[:, :], ALU.mult, ALU.add)
            k.act(y[:, :], y[:, :], AF.Silu)
        for w in range(2):
            k.act(sq[w][:, :], cv[w][:, :], AF.Square)
            bk = nb()
            k.mm(bk[:, :], ones[:, :], sq[w][:, :])
            k.rstd(rs[w][:, :], bk[:, :], 1.0, EPS)
            if w == 0:
                k.stt("pool", cv[w][:, :], cv[w][:, :], 1.0, rs[w][:, :], ALU.mult, ALU.mult) if False else None
        k.tt("pool", cv[0][:, :], cv[0][:, :], rs[0][:, :], ALU.mult)
        k.tt("pool", cv[1][:, :], cv[1][:, :], rs[1][:, :], ALU.mult)
        k.act(cv[0][:, :], cv[0][:, :], AF.Copy, scale=float(128 ** -0.5))
        qT_, kT_, vT_ = cv

        for t in range(4):
            tc_ = slice(t * 128, (t + 1) * 128)
            c = tmc[t]
            bk = nb()
            for kc in range(8):
                k.mm(bk[:, 0:130], hb[:, kc, tc_], wdn[:, kc, 384:514], start=(kc == 0), stop=(kc == 7))
            k.act(c[:, 0:1], bk[:, 0:1], AF.Sigmoid)
            k.act(c[:, 1:2], bk[:, 1:2], AF.Exp, bias=sc[:, 1:2], scale=1.0)
            k.act(sgate[t][:, :], bk[:, 2:130], AF.Silu)
            k.act(c[:, 1:2], c[:, 1:2], AF.Ln, bias=1.0, scale=1.0)
            k.tt("dve", c[:, 2:3], c[:, 1:2], sc[:, 2:3], ALU.mult)
            bk = nb()
            k.transpose(bk[:, 0:128], kT_[:, tc_], ident[:, :], inc=False)
            k.transpose(bk[:, 128:256], vT_[:, tc_], ident[:, :], inc=True)
            k.copy("act", ktm[t][:, :], bk[:, 0:128])
            k.act(vb[t][:, :], bk[:, 128:256], AF.Copy, scale=c[:, 0:1])
            k.ts("dve", gbc[t][:, :], ones[:, :], c[:, 2:3], None, ALU.mult)
            bk = nb()
            k.mm(bk[:, 0:128], gbc[t][:, :], uinc[:, :], inc=False)
            k.mm(bk[:, 128:129], uinc[:, :], c[:, 2:3], inc=True)
            k.copy("dve", c[:, 3:4], bk[:, 128:129])
            k.copy("dve", c[:, 7:8], bk[:, 127:128])
            k.stt("dve", xm[t][:, :], bk[:, 0:128], c[:, 3:4], lpos[:, :], ALU.subtract, ALU.max)
            k.stt("dve", ym[t][:, :], bk[:, 0:128], c[:, 3:4], uneg[:, :], ALU.subtract, ALU.min)
            k.act(egb[t][:, :], bk[:, 0:128], AF.Exp)
            k.act(dec_s[t][:, :], xm[t][:, :], AF.Exp, scale=-1.0)
            k.act(decT[t][:, :], ym[t][:, :], AF.Exp)
            k.act(c[:, 4:5], c[:, 3:4], AF.Exp)
            k.tt("dve", c[:, 4:5], c[:, 4:5], c[:, 0:1], ALU.mult)
            k.act(c[:, 5:6], c[:, 3:4], AF.Exp, bias=c[:, 7:8], scale=-1.0)
            k.act(c[:, 6:7], c[:, 7:8], AF.Exp)
            k.ts("pool", kbg[t][:, :], ktm[t][:, :], c[:, 4:5], None, ALU.mult) if False else None
            k.ts("dve", kbg[t][:, :], ktm[t][:, :], c[:, 4:5], None, ALU.mult)
            k.ts("dve", kdec[t][:, :], ktm[t][:, :], c[:, 5:6], None, ALU.mult)
            k.tt("pool", qdT[t][:, :], qT_[:, tc_], egb[t][:, :], ALU.mult)
            bk = nb()
            k.mm(bk[:, 0:128], kT_[:, tc_], kT_[:, tc_], inc=False)
            k.mm(bk[:, 128:256], kT_[:, tc_], qT_[:, tc_], inc=True)
            k.stt("dve", Mt[t][:, :], bk[:, 0:128], c[:, 0:1], dec_s[t][:, :], ALU.mult, ALU.mult)
            k.tt("dve", qkT[t][:, :], bk[:, 128:256], decT[t][:, :], ALU.mult)
            bk = nb()
            k.transpose(bk[:, 0:128], Mt[t][:, :], ident[:, :])
            k.copy("act", Nt[t][:, :], bk[:, 0:128])
            k.tt("pool", Rr[t][:, :], ident[:, :], Nt[t][:, :], ALU.subtract)
            k.tt("pool", Rt[t][:, :], ident[:, :], Mt[t][:, :], ALU.subtract)

        P = [Nt[t] for t in range(4)]
        Ptr = [Mt[t] for t in range(4)]
        for lvl in range(6):
            last = lvl == 5
            newP = Pa if lvl % 2 == 0 else Pb
            newPt = Pat if lvl % 2 == 0 else Pbt
            for t in range(4):
                bk = nb()
                k.mm(bk[:, 0:128], Ptr[t][:, :], P[t][:, :], inc=last)
                if not last:
                    k.mm(bk[:, 128:256], P[t][:, :], Ptr[t][:, :], inc=True)
                k.copy("act", newP[t][:, :], bk[:, 0:128])
                if not last:
                    k.copy("act", newPt[t][:, :], bk[:, 128:256])
            for t in range(4):
                bk = nb()
                k.mm(bk[:, 0:128], Rt[t][:, :], newP[t][:, :], inc=last)
                if not last:
                    k.mm(bk[:, 128:256], newP[t][:, :], Rt[t][:, :], inc=True)
                k.tt("dve", Rr[t][:, :], Rr[t][:, :], bk[:, 0:128], ALU.add)
                if not last:
                    k.tt("dve", Rt[t][:, :], Rt[t][:, :], bk[:, 128:256], ALU.add)
            P = [newP[t] for t in range(4)]
            Ptr = [newPt[t] for t in range(4)]

        for t in range(4):
            bk = nb()
            k.mm(bk[:, 0:128], Rr[t][:, :], vb[t][:, :], inc=False)
            k.mm(bk[:, 128:256], kbg[t][:, :], Rr[t][:, :], inc=True)
            k.copy("act", u_sb[t][:, :], bk[:, 0:128])
            k.copy("act", wT_sb[t][:, :], bk[:, 128:256])

        for t in range(4):
            tok = slice(blk * 512 + t * 128, blk * 512 + (t + 1) * 128)
            c = tmc[t]
            s_old = Sf[si % 2]
            s_new = Sf[(si + 1) % 2]
            si += 1
            bk = nb()
            k.mm(bk[:, 0:128], wT_sb[t][:, :], s_old[:, :])
            k.tt("dve", vnew[t][:, :], u_sb[t][:, :], bk[:, 0:128], ALU.subtract)
            bo = nb()
            k.mm(bo[:, 0:128], qdT[t][:, :], s_old[:, :], start=True, stop=False, inc=False)
            k.mm(bo[:, 0:128], qkT[t][:, :], vnew[t][:, :], start=False, stop=True, inc=True)
            bs = nb()
            k.mm(bs[:, 0:128], kdec[t][:, :], vnew[t][:, :])
            k.stt("dve", s_new[:, :], s_old[:, :], c[:, 6:7], bs[:, 0:128], ALU.mult, ALU.add)
            k.act(osq[t][:, :], bo[:, 0:128], AF.Square, accum_out=ost[t][:, 0:1])
            k.rstd(ost[t][:, 1:2], ost[t][:, 0:1], 1.0 / 128, EPS)
            k.stt("dve", y1[t][:, :], bo[:, 0:128], ost[t][:, 1:2], gb[:, :], ALU.mult, ALU.mult)
            k.tt("pool", y2[t][:, :], y1[t][:, :], sgate[t][:, :], ALU.mult)
            k.dma("sp", o_d[tok, :], y2[t][:, :])
    k.finish([o_d])
    return nc


def dn_inputs(hT_b, P, l, j):
    w_in = P["w_in"][l]
    cq, ck, cvv = 416 + j * 128, 416 + 512 + j * 128, 416 + 1024 + j * 128
    w = np.concatenate([w_in[:, cq:cq + 128], w_in[:, ck:ck + 128], w_in[:, cvv:cvv + 128],
                        w_in[:, 1952 + j:1953 + j], w_in[:, 1956 + j:1957 + j],
                        w_in[:, 1960 + j * 128:1960 + (j + 1) * 128]], axis=1)
    conv = P["dn_conv"][l]
    cwm = np.stack([conv[:, cq - 416:cq - 416 + 128], conv[:, ck - 416:ck - 416 + 128],
                    conv[:, cvv - 416:cvv - 416 + 128]], axis=0)
    m = {"hT": hT_b, "w_dn": np.ascontiguousarray(w),
         "conv_w": np.ascontiguousarray(cwm.transpose(2, 0, 1)),
         "a_log": P["dn_a_log"][l][j].reshape(1, 1), "dt_bias": P["dn_dt_bias"][l][j].reshape(1, 1),
         "o_norm": P["dn_o_norm"][l].reshape(1, 128)}
    m.update(dn_consts())
    return m


class Rec:
    _PASS = ("sb", "ps", "dram")

    def __init__(self, k):
        self._k = k
        self.segs = [[]]

    def sb(self, *a, **kw):
        return self._k.sb(*a, **kw)

    def push(self):
        pass

    def pop(self):
        pass

    def mark(self):
        self.segs.append([])

    def __getattr__(self, name):
        def f(*a, **kw):
            self.segs[-1].append((name, a, kw))
        return f


SEM_LAT = 1.2
_GHZ = {"pe": 1.9, "act": 1.2, "dve": 0.96, "pool": 0.6}
_FIX = {"pe": 0.06, "act": 0.2, "dve": 0.1, "pool": 0.25}


def _op_cost(k, rec):
    name, ar, kw = rec
    k.dry = []
    getattr(k, name)(*ar, **kw)
    infos, k.dry = k.dry, None
    n = 128
    out = ar[1] if name in ("tt", "ts", "stt", "copy", "memset", "reduce", "dma") else (ar[0] if ar else None)
    if name == "op":
        out = None
    if isinstance(out, V):
        try:
            n = out.ap.free_size()
        except Exception:
            n = 128
    res = []
    for eng, rd, wr in infos:
        if eng == "dma":
            d = 2.5
        else:
            passes = 1
            if name in ("mm", "transpose") and isinstance(ar[1], V) and ar[1].ap.dtype == F32:
                passes = 4
            d = _FIX[eng] + passes * n / (_GHZ[eng] * 1000.0)
        res.append((eng, rd, wr, d))
    return res


def replay_merged(k, *lists):
    lists = [l for l in lists if l]
    if not lists:
        return
    if len(lists) == 1:
        for name, ar, kw in lists[0]:
            getattr(k, name)(*ar, **kw)
        return
    free = {}
    ready = {}
    rdone = {}
    idx = [0] * len(lists)
    costs = [[None] * len(l) for l in lists]
    total = sum(len(l) for l in lists)

    def start_time(info):
        eng, rd, wr, d = info
        t = free.get(eng, 0.0)
        for r in rd:
            if r in ready:
                tr, pe_ = ready[r]
                t = max(t, tr + (SEM_LAT if pe_ != eng else 0.0))
        for w in wr:
            if w in ready:
                tr, pe_ = ready[w]
                t = max(t, tr + (SEM_LAT if pe_ != eng else 0.0))
            if w in rdone:
                t = max(t, rdone[w] + SEM_LAT)
        return t

    for _ in range(total):
        best, bt = None, None
        for n, l in enumerate(lists):
            if idx[n] < len(l):
                if costs[n][idx[n]] is None:
                    costs[n][idx[n]] = _op_cost(k, l[idx[n]])
                c = costs[n][idx[n]]
                t = start_time(c[0]) if c else 0.0
                key = (t, idx[n] / len(l))
                if bt is None or key < bt:
                    best, bt = n, key
        c = costs[best][idx[best]]
        for info in c:
            eng, rd, wr, d = info
            t = start_time(info)
            free[eng] = t + d
            for r in rd:
                rdone[r] = max(rdone.get(r, 0.0), t + d)
            for w in wr:
                ready[w] = (t + d, eng)
                rdone.pop(w, None)
        name, ar, kw = lists[best][idx[best]]
        idx[best] += 1
        getattr(k, name)(*ar, **kw)


def emit_dn_hg(k, banks, C, W, G, layer):
    NBLK = S // 512
    k.push()
    hTb = [k.sb(f"hTbS{i}", [128, 8, 512], BF16) for i in range(3)]
    ra, rb = Rec(k), Rec(k)
    emit_dn(ra, banks[0:DN_BANKS], C, W, G, hTb=hTb)
    emit_hg(rb, banks[DN_BANKS:8], C, W, G, layer, hTb=hTb)
    assert len(ra.segs) == 2 * NBLK + 1 and len(rb.segs) == NBLK + 1
    front = lambda i: ra.segs[1 + 2 * i] if i < NBLK else []
    back = lambda i: ra.segs[2 + 2 * i]

    def load(blk):
        if blk < NBLK:
            for kc in range(8):
                k.dma("sp", hTb[blk % 3][:, kc, :], G["hT_blk"](blk, kc))

    load(0)
    load(1)
    replay_merged(k, ra.segs[0], rb.segs[0])
    replay_merged(k, front(0))
    for blk in range(NBLK):
        load(blk + 2)
        replay_merged(k, front(blk + 1), back(blk), rb.segs[1 + blk])
        if blk % 4 == 3:
            q = blk // 4
            for br in (1, 2):
                k.collective("AllGather", [G["osrc"][br][q][:, :]], [G["odst"][br][q][:, :]], GROUPS)
    k.pop()


DN_BANKS = 5
DN_BACK_BANKS = 1

def emit_publish_tile(k, banks, ident, y, hTsb, t):
    tok = slice(t * 128, (t + 1) * 128)
    for q4 in range(2):
        bk = banks[4 + q4 + 2 * (t % 2)]
        for j in range(4):
            kc = q4 * 4 + j
            k.transpose(bk[:, j * 128:(j + 1) * 128], y[:, kc * 128:(kc + 1) * 128], ident[:, :], inc=(j == 3))
        k.copy("act", hTsb[:, q4 * 4:(q4 + 1) * 4, tok], bk[:, :].f(lambda a: a.rearrange("p (j t) -> p j t", j=4)))


def emit_allgather_h(k, hTsb, G):
    for kc in range(8):
        k.dma("sp", G["hsrc"][kc // 2][(kc % 2) * 128:(kc % 2 + 1) * 128, :], hTsb[:, kc, :])
    for q in range(4):
        k.collective("AllGather", [G["hsrc"][q][:, :]], [G["hdst"][q][:, :]], GROUPS)


def emit_allgather_o(k, G, br):
    for q in range(4):
        k.collective("AllGather", [G["osrc"][br][q][:, :]], [G["odst"][br][q][:, :]], GROUPS)


def emit_ln0(k, banks, C, G):
    ntok = S * B // NCORES
    k.push()
    x = G["x"]
    g_b = k.sb("g_b", [128, D], F32)
    b_b = k.sb("b_b", [128, D], F32)
    k.dma("sp", g_b[:, :], bcast_row(G["ln_in_g"]))
    k.dma("sp", b_b[:, :], bcast_row(G["ln_in_b"]))
    hTsb = k.sb("hTsb", [128, 8, ntok], BF16)
    xs = [k.sb(f"x{i}", [128, D], F32) for i in range(2)]
    ys = [k.sb(f"y{i}", [128, D], F32) for i in range(2)]
    tmps = [k.sb(f"t{i}", [128, D], F32) for i in range(2)]
    sts = [k.sb(f"s{i}", [128, 4], F32) for i in range(2)]
    NT = ntok // 128
    recs = [Rec(k), Rec(k)]

    def ld(i):
        recs[i % 2].dma("sp", xs[i % 2][:, :], x[i * 128:(i + 1) * 128, :])

    ld(0)
    ld(1)
    for i in range(NT):
        kr = recs[i % 2]
        xt, yt, tt_, st = xs[i % 2], ys[i % 2], tmps[i % 2], sts[i % 2]
        layer_norm_tile(kr, xt[:, :], yt[:, :], g_b[:, :], b_b[:, :], tt_[:, :], st[:, :], eng_g="dve")
        if i + 2 < NT:
            ld(i + 2)
        kr.dma("sp", G["h_cur"][i * 128:(i + 1) * 128, :], yt[:, :])
        emit_publish_tile(kr, banks, C["ident"], yt, hTsb, i)
    replay_merged(k, recs[0].segs[0], recs[1].segs[0])
    emit_allgather_h(k, hTsb, G)
    k.pop()


GROUPS = [[0, 1, 2, 3], [4, 5, 6, 7]]

def emit_mla(k0, banks, C, W, G):
    T = S
    NBLK = T // 512
    wlat_d, gq_d, gkv_d, wuq_d, wuk_d, wuv_d = W["w_lat"], W["g_q"], W["g_kv"], W["w_uq"], W["w_uk"], W["w_uv"]
    pos_d = G["pos"]
    frq, sgn, esel, tri = C["frq"], C["sgn"], C["esel"], C["tri"]
    k = k0
    k.push()

    wlat = k.sb("wlat", [128, 8, 448], BF16)
    for kc in range(8):
        k.dma("pool", wlat[:, kc, :], wlat_d[kc * 128:(kc + 1) * 128, :])
    gq = k.sb("gq", [128, 2], F32)
    gkv = k.sb("gkv", [128, 1], F32)
    k.dma("sp", gq[:, :], gq_d[:, :])
    k.dma("sp", gkv[:, :], gkv_d[:, :])
    wtmp = k.sb("wtmp", [128, 2, 256], F32)
    wuq = k.sb("wuq", [128, 2, 256], BF16)
    for c in range(2):
        k.dma("sp", wtmp[:, c, :], wuq_d[c * 128:(c + 1) * 128, :])
    for c in range(2):
        k.ts("dve", wuq[:, c, :], wtmp[:, c, :], gq[:, c:c + 1], QK_SCALE, ALU.mult, ALU.mult)
    wtmp2 = k.sb("wtmp2", [128, 2, 128], F32)
    wuk = k.sb("wuk", [128, 128], BF16)
    wuv = k.sb("wuv", [128, 128], BF16)
    k.dma("sp", wtmp2[:, 0, :], wuk_d[:, :])
    k.dma("sp", wtmp2[:, 1, :], wuv_d[:, :])
    k.ts("dve", wuk[:, :], wtmp2[:, 0, :], gkv[:, 0:1], None, ALU.mult)
    k.ts("dve", wuv[:, :], wtmp2[:, 1, :], gkv[:, 0:1], None, ALU.mult)
    ones = k.sb("ones", [128, 128], F32)
    k.memset("dve", ones[:, :], 1.0)

    kT = [k.sb(f"kT{h}", [96, T], BF16) for h in range(2)]
    qT = [k.sb(f"qT{h}", [96, T], BF16) for h in range(2)]
    Vp = k.sb("Vp", [128, 2, T // 128, 128], BF16)
    kTv = [[V(kT[h].h[:, b * 512:(b + 1) * 512], Res(f"kT{h}_{b}")) for b in range(NBLK)] for h in range(2)]
    qTv = [[V(qT[h].h[:, b * 512:(b + 1) * 512], Res(f"qT{h}_{b}")) for b in range(NBLK)] for h in range(2)]
    Vpv = [V(Vp.h[:, :, b * 4:(b + 1) * 4, :], Res(f"Vp_{b}")) for b in range(NBLK)]
    for b in range(NBLK):
        k.memset("pool", Vpv[b], 1.0)
    mx = k.sb("mx", [128, 4], F32)
    k.memset("dve", mx[:, :], 0.0)
    negcb = [k.sb(f"negc{b}", [128, 2], F32) for b in range(NBLK)]

    hTb = [k.sb(f"hTb{i}", [128, 8, 512], BF16) for i in range(2)]
    posi = [k.sb(f"posi{i}", [128, 512], I32) for i in range(2)]
    ang = k.sb("ang", [128, 1024], F32)
    ni = k.sb("ni", [128, 1024], I32)
    nf = k.sb("nf", [128, 1024], F32)
    scs = [k.sb(f"scs{i}", [128, 1024], F32) for i in range(2)]
    cq_sb = [k.sb(f"cq_sb{i}", [128, 512], F32) for i in range(2)]
    sq_sb = [k.sb(f"sq_sb{i}", [128, 512], F32) for i in range(2)]
    ckv_sb = k.sb("ckv_sb", [128, 512], F32)
    sqkv = k.sb("sqkv", [128, 512], F32)
    rq = k.sb("rq", [128, 512], F32)
    rkv = k.sb("rkv", [128, 512], F32)
    cqn = [k.sb(f"cqn{i}", [128, 512], BF16) for i in range(2)]
    ckvn = k.sb("ckvn", [128, 512], BF16)
    t1 = k.sb("t1", [128, 512], F32)
    t2 = k.sb("t2", [128, 512], F32)
    nsq = k.sb("nsq", [96, 512], F32)
    mtmp = k.sb("mtmp", [128, 1], F32)
    osb = [k.sb(f"osb{i}", [128, 512], F32) for i in range(2)]
    ores = [k.sb(f"ores{i}", [64, 512], BF16) for i in range(2)]
    pT = [k.sb(f"pTx{i}", [128, 512], BF16) for i in range(7)]

    def load(blk):
        if blk < NBLK:
            col = slice(blk * 512, (blk + 1) * 512)
            for kc in range(8):
                k0.dma("sp", hTb[blk % 2][:, kc, :], G["hT_blk"](blk, kc))
            k0.dma("sp", posi[blk % 2][:, :], pos_d.v(pos_d.h[:, col].partition_broadcast(128)))

    rp = Rec(k0)
    k = rp
    nb = BankRR(banks[6:8])
    r6 = slice(64, 96)

    def rope_rows(dsts, bA, bB, cst, snt):
        k.stt("dve", t1[r6, :], bB[r6, :], sgn[r6, 0:1], snt, ALU.mult, ALU.mult)
        k.tt("dve", t2[r6, :], bA[r6, :], cst, ALU.mult)
        for d_ in dsts:
            k.tt("pool", d_[r6, :], t1[r6, :], t2[r6, :], ALU.add)

    def normsq(src, slot, running):
        k.act(nsq[:, :], src[0:96, :], AF.Square)
        bk = nb()
        k.mm(bk[:, :], ones[0:96, :], nsq[:, :])
        k.reduce("dve", mtmp[:, :], bk[:, :], ALU.max)
        if running:
            k.tt("dve", mx[:, slot:slot + 1], mx[:, slot:slot + 1], mtmp[:, :], ALU.max)
        else:
            k.copy("dve", mx[:, slot:slot + 1], mtmp[:, :])

    for blk in range(NBLK):
        k.mark()
        hb = hTb[blk % 2]
        pi_ = posi[blk % 2]
        sc_ = scs[blk % 2]
        snt, cst = sc_[r6, 0:512], sc_[r6, 512:1024]
        k.copy("dve", ang[r6, 0:512], pi_[r6, :])
        k.ts("dve", ang[r6, 0:512], ang[r6, 0:512], frq[r6, 0:1], None, ALU.mult)
        k.ts("dve", ang[r6, 512:1024], ang[r6, 0:512], float(np.pi / 2), None, ALU.add)
        k.ts("dve", nf[r6, :], ang[r6, :], 1.0 / TWO_PI, None, ALU.mult)
        k.copy("dve", ni[r6, :], nf[r6, :])
        k.copy("dve", nf[r6, :], ni[r6, :])
        k.stt("dve", nf[r6, :], nf[r6, :], -TWO_PI, ang[r6, :], ALU.mult, ALU.add)
        k.ts("dve", nf[r6, :], nf[r6, :], 3.1415925, -3.1415925, ALU.min, ALU.max)
        k.act(sc_[r6, :], nf[r6, :], AF.Sin)
        for c in range(2):
            bk = nb()
            for kc in range(8):
                k.mm(bk[:, :], wlat[:, kc, c * 128:(c + 1) * 128], hb[:, kc, :], start=(kc == 0), stop=(kc == 7))
            k.copy("act", cq_sb[c][:, :], bk[:, :])
            k.act(sq_sb[c][:, :], bk[:, :], AF.Square)
        bk = nb()
        for kc in range(8):
            k.mm(bk[:, :], wlat[:, kc, 256:384], hb[:, kc, :], start=(kc == 0), stop=(kc == 7))
        k.copy("act", ckv_sb[:, :], bk[:, :])
        k.act(sqkv[:, :], bk[:, :], AF.Square)
        bA = nb()
        for kc in range(8):
            k.mm(bA[0:96, :], wlat[:, kc, 320:416], hb[:, kc, :], start=(kc == 0), stop=(kc == 7))
        bB = nb()
        for kc in range(8):
            k.mm(bB[0:96, :], wlat[:, kc, 352:448], hb[:, kc, :], start=(kc == 0), stop=(kc == 7))
        rope_rows([kTv[0][blk], kTv[1][blk]], bA, bB, cst, snt)
        bk = nb()
        k.mm(bk[:, :], ones[:, :], sq_sb[0][:, :], start=True, stop=False)
        k.mm(bk[:, :], ones[:, :], sq_sb[1][:, :], start=False, stop=True)
        k.rstd_ln(rq[:, :], bk[:, :], 1.0 / 256, EPS)
        bk = nb()
        k.mm(bk[:, :], ones[:, :], sqkv[:, :])
        k.rstd_ln(rkv[:, :], bk[:, :], 1.0 / 128, EPS)
        for c in range(2):
            k.tt("dve", cqn[c][:, :], cq_sb[c][:, :], rq[:, :], ALU.mult)
        k.tt("pool", ckvn[:, :], ckv_sb[:, :], rkv[:, :], ALU.mult)
        for hd in range(2):
            bk = nb()
            k.mm(bk[0:64, :], wuk[:, hd * 64:(hd + 1) * 64], ckvn[:, :])
            k.copy("act", kTv[hd][blk][0:64, :], bk[0:64, :])
        bk = nb()
        for tt_ in range(4):
            k.mm(bk[:, tt_ * 128:(tt_ + 1) * 128], ckvn[:, tt_ * 128:(tt_ + 1) * 128], wuv[:, :],
                 start=True, stop=True, inc=(tt_ == 3))
        k.copy("act", Vpv[blk][:, :, :, 0:64],
               bk[:, :].f(lambda a: a.rearrange("p (t h d) -> p h t d", t=4, h=2)))
        for hd in range(2):
            bA = nb()
            for c in range(2):
                k.mm(bA[0:96, :], wuq[:, c, hd * 128:hd * 128 + 96], cqn[c][:, :], start=(c == 0), stop=(c == 1))
            bB = nb()
            for c in range(2):
                k.mm(bB[0:96, :], wuq[:, c, hd * 128 + 32:hd * 128 + 128], cqn[c][:, :], start=(c == 0), stop=(c == 1))
            k.copy("act", qTv[hd][blk][0:64, :], bA[0:64, :])
            rope_rows([qTv[hd][blk]], bA, bB, cst, snt)
        for hd in range(2):
            normsq(qTv[hd][blk], hd, False)
            normsq(kTv[hd][blk], 2 + hd, True)
        ng = negcb[blk]
        k.tt("dve", ng[:, :], mx[:, 0:2], mx[:, 2:4], ALU.mult)
        k.act(ng[:, :], ng[:, :], AF.Ln)
        k.act(ng[:, :], ng[:, :], AF.Exp, scale=0.5)
        k.ts("dve", ng[:, :], ng[:, :], -1.0, None, ALU.mult)

    ra = Rec(k0)
    k = ra
    s_banks = BankRR(banks[2:5])
    den_bank = banks[5]
    blocks = [(hd, qi, kb) for qi in range(NBLK) for hd in range(2) for kb in range(4 * qi + 4)]
    LOOKAHEAD = 4

    def stage1(i):
        hd, qi, kb = blocks[i]
        r = kb - 4 * qi
        c0 = 128 * r if r > 0 else 0
        sb_ = s_banks()
        pt = pT[i % 7]
        kblk, ko = kb // 4, (kb % 4) * 128
        k.mm(sb_[:, c0:512], kTv[hd][kblk][:, ko:ko + 128], qTv[hd][qi][:, c0:512])
        k.act(pt[:, c0:512], sb_[:, c0:512], AF.Exp, bias=negcb[qi][:, hd:hd + 1], scale=1.0)
        if r >= 0:
            k.tt("pool", pt[:, c0:c0 + 128], pt[:, c0:c0 + 128], tri[:, :], ALU.mult)
        return pt, c0

    def stage2(i, pt, c0):
        hd, qi, kb = blocks[i]
        nkb = 4 * qi + 4
        g = qi * 2 + hd
        oacc = banks[g % 2]
        k.mm(oacc[:, c0:512], Vpv[kb // 4][:, hd, kb % 4, :], pt[:, c0:512], start=(kb == 0), stop=(kb == nkb - 1))
        if kb == nkb - 1:
            ob = osb[g % 2]
            orr = ores[g % 2]
            k.copy("act", ob[:, :], oacc[:, :])
            k.op("dve", lambda e, ob=ob: e.reciprocal(ob[64:128, :].ap, ob[64:128, :].ap), [ob], [ob])
            k.mm(den_bank[0:64, :], esel[:, :], ob[:, :])
            k.tt("dve", orr[:, :], ob[0:64, :], den_bank[0:64, :], ALU.mult)
            k.dma("sp", G["osrc"][0][qi // 4][hd * 64:(hd + 1) * 64, (qi % 4) * 512:(qi % 4 + 1) * 512], orr[:, :])

    pend = []
    cur_qi = -1
    for i in range(len(blocks)):
        if blocks[i][1] != cur_qi:
            cur_qi = blocks[i][1]
            k.mark()
        pend.append((i,) + stage1(i))
        if len(pend) > LOOKAHEAD:
            stage2(*pend.pop(0))
    while pend:
        stage2(*pend.pop(0))

    k = k0
    assert len(rp.segs) == NBLK + 1 and len(ra.segs) == NBLK + 1 and not rp.segs[0] and not ra.segs[0]
    load(0)
    load(1)
    replay_merged(k, rp.segs[1])
    for qi in range(NBLK):
        load(qi + 2)
        replay_merged(k, ra.segs[1 + qi], rp.segs[2 + qi] if qi + 1 < NBLK else [])
    k.pop()


def emit_hg(k, banks, C, W, G, layer, hTb=None):
    T = S
    NBLK = T // 512
    w_d, lbr_d, lbc_d, on_d = W["w_hg"], G["lb_rows"], G["lb_cols"], W["hg_o_norm"]
    cU, cUrel, cW, cones, mbd, rowm, ident = C["cU"], C["cUrel"], C["cW"], C["cones"], C["maskbd"], C["rowmask"], C["ident"]
    nb = BankRR(banks)
    k.push()

    whg = k.sb("whg", [128, 8, 512], BF16)
    for kc in range(8):
        k.dma("pool", whg[:, kc, :], w_d[kc * 128:(kc + 1) * 128, :])
    gb = k.sb("gb", [128, 128], F32)
    k.dma("sp", gb[:, :], bcast_row(on_d))
    oTb = [k.sb(f"oTb{i}", [128, 512], BF16) for i in range(2)]

    def lower_bound(x, n, name):
        m = k.sb(name + "_m", [128, n], F32)
        e = k.sb(name + "_e", [128, DEPTH, n], F32)
        ssum = k.sb(name + "_s", [128, n], F32)
        lb = k.sb(name + "_lb", [128, n], F32)
        oml = k.sb(name + "_oml", [128, n], F32)
        k.copy("dve", m[:, :], x[:, 0, :])
        for i in range(1, DEPTH):
            k.tt("dve", m[:, :], m[:, :], x[:, i, :], ALU.max)
        for i in range(DEPTH):
            k.tt("dve", e[:, i, :], x[:, i, :], m[:, :], ALU.subtract)
        k.act(e[:, :, :], e[:, :, :], AF.Exp)
        k.copy("dve", ssum[:, :], e[:, 0, :])
        for i in range(1, DEPTH):
            k.tt("dve", ssum[:, :], ssum[:, :], e[:, i, :], ALU.add)
        k.op("dve", lambda en: en.reciprocal(ssum[:, :].ap, ssum[:, :].ap), [ssum], [ssum])
        for i in range(DEPTH):
            k.tt("dve", e[:, i, :], e[:, i, :], ssum[:, :], ALU.mult)
        k.copy("dve", lb[:, :], e[:, 0, :])
        for i in range(1, layer + 1):
            k.tt("dve", lb[:, :], lb[:, :], e[:, i, :], ALU.add)
        k.tt("dve", lb[:, :], lb[:, :], e[:, 0, :], ALU.subtract)
        k.ts("dve", oml[:, :], lb[:, :], -1.0, 1.0, ALU.mult, ALU.add)
        return lb, oml

    xr = k.sb("xr", [128, DEPTH, 128], F32)
    for i in range(DEPTH):
        k.dma("sp", xr[:, i, :], lbr_d.v(lbr_d.h[i:i + 1, :].partition_broadcast(128)))
    lb_b, oml_b = lower_bound(xr, 128, "lbr")
    xc = k.sb("xc", [128, DEPTH, 1], F32)
    k.dma("sp", xc[:, :, 0], lbc_d[:, :])
    lb_c, oml_c = lower_bound(xc, 1, "lbc")

    NS = 8
    Sf = [k.sb(f"Sf{i}", [128, 128], F32) for i in range(2)]
    Sb = [k.sb(f"Sb{i}", [128, 128], BF16) for i in range(NS)]
    k.memset("dve", Sf[0][:, :], 0.0)
    k.memset("dve", Sb[0][:, :], 0.0)
    Z = [k.sb(f"Z{i}", [128, 4, 128], BF16) for i in range(2)]
    for z in Z:
        k.memset("pool", z[:, :, :], 0.0)
    si = 0

    shared = hTb is not None
    if not shared:
        hTb = [k.sb(f"hTb{i}", [128, 8, 512], BF16) for i in range(2)]
    qTs = [k.sb(f"qTs{i}", [128, 512], F32) for i in range(2)]
    kTs = [k.sb(f"kTs{i}", [128, 512], F32) for i in range(2)]

    def dbl(name, shape, dt, n=2):
        return [k.sb(f"{name}{i}", shape, dt) for i in range(n)]

    sgx = dbl("sgx", [128, 384], F32)
    sg = [s_[:, 0:128] for s_ in sgx]
    uu = dbl("uu", [128, 128], F32)
    ff = dbl("ff", [128, 128], F32)
    ktm = dbl("ktm", [128, 128], F32)
    logf = dbl("logf", [128, 128], F32)
    vbf = dbl("vbf", [128, 128], BF16)
    sgate = dbl("sgate", [128, 128], F32)
    e1 = dbl("e1", [128, 128], F32)
    e2 = dbl("e2", [128, 128], F32)
    e3 = dbl("e3", [128, 128], F32)
    e4 = dbl("e4", [128, 128], F32)
    dl = dbl("dl", [128, 4], F32)
    qpT = dbl("qpT", [128, 128], BF16)
    kpT = dbl("kpT", [128, 128], BF16)
    kdp = dbl("kdp", [128, 4, 128], BF16)
    attm = dbl("attm", [128, 128], BF16)
    osq = dbl("osq", [128, 128], F32)
    ost = dbl("ost", [128, 2], F32)
    y1 = dbl("y1", [128, 128], F32)
    y2 = dbl("y2", [128, 128], F32)

    nt = 0
    for blk in range(NBLK):
        col = slice(blk * 512, (blk + 1) * 512)
        hb = hTb[blk % len(hTb)]
        if shared:
            k.mark()
        else:
            for kc in range(8):
                k.dma("sp", hb[:, kc, :], G["hT_blk"](blk, kc))
        qT_s, kT_s = qTs[blk % 2], kTs[blk % 2]
        bq = nb()
        for kc in range(8):
            k.mm(bq[:, :], whg[:, kc, 0:128], hb[:, kc, :], start=(kc == 0), stop=(kc == 7))
        k.act(qT_s[:, :], bq[:, :], AF.Exp, scale=-1.0)
        k.act(qT_s[:, :], qT_s[:, :], AF.Ln, bias=1.0, scale=1.0)
        k.act(qT_s[:, :], qT_s[:, :], AF.Exp, scale=-1.0)
        k.tt("dve", qT_s[:, :], bq[:, :], qT_s[:, :], ALU.mult)
        bz = nb()
        for kc in range(8):
            k.mm(bz[:, :], whg[:, kc, 128:256], hb[:, kc, :], start=(kc == 0), stop=(kc == 7))
        k.act(kT_s[:, :], bz[:, :], AF.Exp)
        k.act(kT_s[:, :], kT_s[:, :], AF.Ln, bias=1.0, scale=1.0)
        k.act(kT_s[:, :], kT_s[:, :], AF.Exp, scale=-1.0)
        k.ts("dve", kT_s[:, :], kT_s[:, :], oml_c[:, 0:1], None, ALU.mult)
        for tt_ in range(4):
            p = nt % 2
            nt += 1
            tcol = slice(tt_ * 128, (tt_ + 1) * 128)
            tok = slice(blk * 512 + tt_ * 128, blk * 512 + (tt_ + 1) * 128)
            btm = nb()
            for kc in range(8):
                k.mm(btm[:, 0:384], hb[:, kc, tcol], whg[:, kc, 128:512], start=(kc == 0), stop=(kc == 7))
            k.act(sgx[p][:, :], btm[:, 0:384], AF.Exp, scale=-1.0)
            k.copy("act", vbf[p][:, :], btm[:, 128:256])
            k.act(sgx[p][:, :], sgx[p][:, :], AF.Ln, bias=1.0, scale=1.0)
            k.act(sgx[p][:, :], sgx[p][:, :], AF.Exp, scale=-1.0)
            k.tt("dve", sgate[p][:, :], btm[:, 256:384], sgx[p][:, 256:384], ALU.mult)
            k.tt("dve", uu[p][:, :], sg[p][:, :], oml_b[:, :], ALU.mult)
            k.tt("pool", ff[p][:, :], uu[p][:, :], lb_b[:, :], ALU.add)
            k.tt("pool", ktm[p][:, :], oml_b[:, :], uu[p][:, :], ALU.subtract)
            k.ts("dve", ff[p][:, :], ff[p][:, :], 1e-30, None, ALU.max)
            k.act(logf[p][:, :], ff[p][:, :], AF.Ln)
            bc = nb()
            k.mm(bc[:, 0:128], logf[p][:, :], cU[:, :], inc=False)
            k.mm(bc[:, 128:256], logf[p][:, :], cUrel[:, :], inc=False)
            k.mm(bc[:, 256:384], cW[:, :], logf[p][:, :], inc=False)
            k.mm(bc[:, 384:388], logf[p][:, :], cones[:, :], inc=True)
            k.act(e1[p][:, :], bc[:, 0:128], AF.Exp)
            k.act(e2[p][:, :], bc[:, 128:256], AF.Exp)
            k.act(e3[p][:, :], bc[:, 128:256], AF.Exp, scale=-1.0)
            k.act(e4[p][:, :], bc[:, 256:384], AF.Exp)
            k.act(dl[p][:, :], bc[:, 384:388], AF.Exp)
            z = Z[p]
            zdiag = z.v(bass.AP(z.h, 0, [[512, 128], [160, 4], [1, 32]]))
            k.tt("dve", zdiag, qT_s[:, tcol].f(lambda a: a.rearrange("p (c x) -> p c x", c=4)),
                 e1[p][:, :].f(lambda a: a.rearrange("p (c x) -> p c x", c=4)), ALU.mult)
            k.tt("pool", qpT[p][:, :], qT_s[:, tcol], e2[p][:, :], ALU.mult)
            k.tt("pool", kpT[p][:, :], kT_s[:, tcol], e3[p][:, :], ALU.mult)
            for c in range(4):
                k.stt("dve" if c % 2 == 0 else "pool", kdp[p][:, c, :], ktm[p][:, :], rowm[:, c:c + 1], e4[p][:, :],
                      ALU.mult, ALU.mult) if c % 2 == 0 else None
            for c in range(4):
                if c % 2 == 1:
                    k.stt("dve", kdp[p][:, c, :], ktm[p][:, :], rowm[:, c:c + 1], e4[p][:, :], ALU.mult, ALU.mult)
            ba = nb()
            k.mm(ba[:, 0:128], kpT[p][:, :], qpT[p][:, :])
            k.tt("dve", attm[p][:, :], ba[:, 0:128], mbd[:, :], ALU.mult)
            bs = nb()
            for c in range(4):
                k.mm(bs[:, c * 128:(c + 1) * 128], kdp[p][:, c, :], vbf[p][:, :], inc=(c == 3))
            bo = nb()
            for c in range(4):
                k.mm(bo[:, 0:128], z[:, c, :], Sb[(si + c) % NS][:, :], start=(c == 0), stop=False, inc=False)
                s_old = Sf[(si + c) % 2]
                s_new = Sf[(si + c + 1) % 2]
                k.stt("dve", s_new[:, :], s_old[:, :], dl[p][:, c:c + 1], bs[:, c * 128:(c + 1) * 128],
                      ALU.mult, ALU.add)
                k.copy("pool", Sb[(si + c + 1) % NS][:, :], s_new[:, :])
            k.mm(bo[:, 0:128], attm[p][:, :], vbf[p][:, :], start=False, stop=True, inc=True)
            si += 4
            k.act(osq[p][:, :], bo[:, 0:128], AF.Square, accum_out=ost[p][:, 0:1])
            k.rstd_ln(ost[p][:, 1:2], ost[p][:, 0:1], 1.0 / 128, EPS)
            k.stt("dve", y1[p][:, :], bo[:, 0:128], ost[p][:, 1:2], gb[:, :], ALU.mult, ALU.mult)
            k.tt("pool", y2[p][:, :], y1[p][:, :], sgate[p][:, :], ALU.mult)
            bt = nb()
            k.transpose(bt[:, 0:128], y2[p][:, :], ident[:, :])
            k.copy("act", oTb[blk % 2][:, tcol], bt[:, 0:128])
        k.dma("sp", G["osrc"][2][blk // 4][:, (blk % 4) * 512:(blk % 4 + 1) * 512], oTb[blk % 2][:, :])
    k.pop()


def emit_dn(k, banks, C, W, G, hTb=None):
    T = S
    NBLK = T // 512
    w_d, cw_d, alog_d, dtb_d, on_d = W["w_dn"], W["conv_w"], W["a_log"], W["dt_bias"], W["dn_o_norm"]
    uinc, lpos, uneg, ident = C["uinc"], C["lpos_s"], C["uneg"], C["ident"]
    if hTb is not None:
        nb = BankRR(banks[:-DN_BACK_BANKS])
        nbb = BankRR(banks[-DN_BACK_BANKS:])
    else:
        nb = nbb = BankRR(banks)
    k.push()

    wdn = k.sb("wdn", [128, 8, 514], BF16)
    for kc in range(8):
        k.dma("pool", wdn[:, kc, :], w_d[kc * 128:(kc + 1) * 128, :])
    cw = k.sb("cw", [128, 3, 4], F32)
    k.dma("sp", cw[:, :, :], cw_d[:, :, :])
    gb = k.sb("gb", [128, 128], F32)
    k.dma("sp", gb[:, :], bcast_row(on_d))
    oTb = [k.sb(f"oTb{i}", [128, 512], BF16) for i in range(2)]
    sc = k.sb("sc", [128, 4], F32)
    k.dma("sp", sc[:, 0:1], bcast_row(alog_d))
    k.dma("sp", sc[:, 1:2], bcast_row(dtb_d))
    k.act(sc[:, 2:3], sc[:, 0:1], AF.Exp)
    k.ts("dve", sc[:, 2:3], sc[:, 2:3], -1.0, None, ALU.mult)
    ones = k.sb("ones", [128, 128], F32)
    k.memset("dve", ones[:, :], 1.0)
    ey = k.sb("ey", [128, 512], F32)

    Sf = [k.sb(f"S{i}", [128, 128], F32) for i in range(2)]
    k.memset("dve", Sf[0][:, :], 0.0)
    si = 0

    shared = hTb is not None
    if not shared:
        hTb = [k.sb(f"hTb{i}", [128, 8, 512], BF16) for i in range(2)]
    xh = [[k.sb(f"xh{w}_{i}", [128, 515], F32) for i in range(2)] for w in range(3)]
    for w in range(3):
        k.memset("dve", xh[w][1][:, 512:515], 0.0)
    cv = [k.sb(f"cv{w}", [128, 512], F32) for w in range(3)]
    sq = [k.sb(f"sq{w}", [128, 512], F32) for w in range(2)]
    rs = [k.sb(f"rs{w}", [128, 512], F32) for w in range(2)]

    def per_tile(name, shape, dt=F32):
        return [k.sb(f"{name}{i}", shape, dt) for i in range(4)]

    def per_tile2(name, shape, dt=F32):
        return [k.sb(f"{name}{i}", shape, dt) for i in range(8 if shared else 4)]

    tmcA = per_tile2("tmc", [128, 8])
    sgateA = per_tile2("sgate", [128, 128])
    r130 = per_tile("r130", [128, 130])
    ktm = per_tile("ktm", [128, 128])
    vb = per_tile("vb", [128, 128])
    gbc = per_tile("gbc", [128, 128])
    xm = per_tile("xm", [128, 128])
    ym = per_tile("ym", [128, 128])
    dec_s = per_tile("dec_s", [128, 128])
    decT = per_tile("decT", [128, 128])
    egb = per_tile("egb", [128, 128])
    Mt = per_tile("M", [128, 128])
    Nt = per_tile("N", [128, 128])
    Pa = per_tile("Pa", [128, 128])
    Pat = per_tile("Pat", [128, 128])
    Pb = per_tile("Pb", [128, 128])
    Pbt = per_tile("Pbt", [128, 128])
    Rr = per_tile("R", [128, 128])
    Rt = per_tile("Rt", [128, 128])
    qkTA = per_tile2("qkT", [128, 128])
    qdTA = per_tile2("qdT", [128, 128])
    kbg = per_tile("kbg", [128, 128])
    kdecA = per_tile2("kdec", [128, 128])
    u_sbA = per_tile2("u", [128, 128])
    wT_sbA = per_tile2("wT", [128, 128])
    vnew = per_tile("vnew", [128, 128])
    osq = per_tile("osq", [128, 128])
    ost = per_tile("ost", [128, 2])
    y1 = per_tile("y1", [128, 128])
    y2 = per_tile("y2", [128, 128])

    for blk in range(NBLK):
        col = slice(blk * 512, (blk + 1) * 512)
        hb = hTb[blk % len(hTb)]
        if shared:
            k.mark()
        else:
            for kc in range(8):
                k.dma("sp", hb[:, kc, :], G["hT_blk"](blk, kc))
        pb = (blk % 2) * 4 if shared else 0
        tmc, sgate, qkT, qdT, kdec, u_sb, wT_sb = (x[pb:pb + 4] for x in (tmcA, sgateA, qkTA, qdTA, kdecA, u_sbA, wT_sbA))
        for w in range(3):
            bk = nb()
            for kc in range(8):
                k.mm(bk[:, :], wdn[:, kc, w * 128:(w + 1) * 128], hb[:, kc, :], start=(kc == 0), stop=(kc == 7))
            cur, prv = xh[w][blk % 2], xh[w][(blk + 1) % 2]
            k.copy("act", cur[:, 3:515], bk[:, :])
            k.copy("act", cur[:, 0:3], prv[:, 512:515])
            y = cv[w]
            k.ts("dve", y[:, :], cur[:, 0:512], cw[:, w, 0:1], None, ALU.mult)
            for m in range(1, 4):
                k.stt("dve" if m != 2 else "dve", y[:, :], cur[:, m:m + 512], cw[:, w, m:m + 1], y[:, :], ALU.mult, ALU.add)
            k.act(ey[:, :], y[:, :], AF.Exp, scale=-1.0)
            k.act(ey[:, :], ey[:, :], AF.Ln, bias=1.0, scale=1.0)
            k.act(ey[:, :], ey[:, :], AF.Exp, scale=-1.0)
            k.tt("pool", y[:, :], y[:, :], ey[:, :], ALU.mult)
        for w in range(2):
            k.act(sq[w][:, :], cv[w][:, :], AF.Square)
            bk = nb()
            k.mm(bk[:, :], ones[:, :], sq[w][:, :])
            k.rstd_ln(rs[w][:, :], bk[:, :], 1.0, EPS)
            if w == 0:
                k.stt("pool", cv[w][:, :], cv[w][:, :], 1.0, rs[w][:, :], ALU.mult, ALU.mult) if False else None
        k.tt("pool", cv[0][:, :], cv[0][:, :], rs[0][:, :], ALU.mult)
        k.tt("pool", cv[1][:, :], cv[1][:, :], rs[1][:, :], ALU.mult)
        k.act(cv[0][:, :], cv[0][:, :], AF.Copy, scale=float(128 ** -0.5))
        qT_, kT_, vT_ = cv

        for t in range(4):
            tc_ = slice(t * 128, (t + 1) * 128)
            c = tmc[t]
            bk = nb()
            for kc in range(8):
                k.mm(bk[:, 0:130], hb[:, kc, tc_], wdn[:, kc, 384:514], start=(kc == 0), stop=(kc == 7))
            k.act(r130[t][:, :], bk[:, 0:130], AF.Exp, scale=-1.0)
            k.act(c[:, 1:2], bk[:, 1:2], AF.Exp, bias=sc[:, 1:2], scale=1.0)
            k.act(r130[t][:, :], r130[t][:, :], AF.Ln, bias=1.0, scale=1.0)
            k.act(r130[t][:, :], r130[t][:, :], AF.Exp, scale=-1.0)
            k.copy("dve", c[:, 0:1], r130[t][:, 0:1])
            k.tt("dve", sgate[t][:, :], bk[:, 2:130], r130[t][:, 2:130], ALU.mult)
            k.act(c[:, 1:2], c[:, 1:2], AF.Ln, bias=1.0, scale=1.0)
            k.tt("dve", c[:, 2:3], c[:, 1:2], sc[:, 2:3], ALU.mult)
            bk = nb()
            k.transpose(bk[:, 0:128], kT_[:, tc_], ident[:, :], inc=False)
            k.transpose(bk[:, 128:256], vT_[:, tc_], ident[:, :], inc=True)
            k.copy("act", ktm[t][:, :], bk[:, 0:128])
            k.act(vb[t][:, :], bk[:, 128:256], AF.Copy, scale=c[:, 0:1])
            k.ts("dve", gbc[t][:, :], ones[:, :], c[:, 2:3], None, ALU.mult)
            bk = nb()
            k.mm(bk[:, 0:128], gbc[t][:, :], uinc[:, :], inc=False)
            k.mm(bk[:, 128:129], uinc[:, :], c[:, 2:3], inc=True)
            k.copy("dve", c[:, 3:4], bk[:, 128:129])
            k.copy("dve", c[:, 7:8], bk[:, 127:128])
            k.stt("dve", xm[t][:, :], bk[:, 0:128], c[:, 3:4], lpos[:, :], ALU.subtract, ALU.max)
            k.stt("dve", ym[t][:, :], bk[:, 0:128], c[:, 3:4], uneg[:, :], ALU.subtract, ALU.min)
            k.act(egb[t][:, :], bk[:, 0:128], AF.Exp)
            k.act(dec_s[t][:, :], xm[t][:, :], AF.Exp, scale=-1.0)
            k.act(decT[t][:, :], ym[t][:, :], AF.Exp)
            k.act(c[:, 4:5], c[:, 3:4], AF.Exp)
            k.tt("dve", c[:, 4:5], c[:, 4:5], c[:, 0:1], ALU.mult)
            k.act(c[:, 5:6], c[:, 3:4], AF.Exp, bias=c[:, 7:8], scale=-1.0)
            k.act(c[:, 6:7], c[:, 7:8], AF.Exp)
            k.ts("pool", kbg[t][:, :], ktm[t][:, :], c[:, 4:5], None, ALU.mult) if False else None
            k.ts("dve", kbg[t][:, :], ktm[t][:, :], c[:, 4:5], None, ALU.mult)
            k.ts("dve", kdec[t][:, :], ktm[t][:, :], c[:, 5:6], None, ALU.mult)
            k.tt("pool", qdT[t][:, :], qT_[:, tc_], egb[t][:, :], ALU.mult)
            bk = nb()
            k.mm(bk[:, 0:128], kT_[:, tc_], kT_[:, tc_], inc=False)
            k.mm(bk[:, 128:256], kT_[:, tc_], qT_[:, tc_], inc=True)
            k.stt("dve", Mt[t][:, :], bk[:, 0:128], c[:, 0:1], dec_s[t][:, :], ALU.mult, ALU.mult)
            k.tt("dve", qkT[t][:, :], bk[:, 128:256], decT[t][:, :], ALU.mult)
            bk = nb()
            k.transpose(bk[:, 0:128], Mt[t][:, :], ident[:, :])
            k.copy("act", Nt[t][:, :], bk[:, 0:128])
            k.tt("pool", Rr[t][:, :], ident[:, :], Nt[t][:, :], ALU.subtract)
            k.tt("pool", Rt[t][:, :], ident[:, :], Mt[t][:, :], ALU.subtract)

        P = [Nt[t] for t in range(4)]
        Ptr = [Mt[t] for t in range(4)]
        for lvl in range(6):
            last = lvl == 5
            newP = Pa if lvl % 2 == 0 else Pb
            newPt = Pat if lvl % 2 == 0 else Pbt
            for t in range(4):
                bk = nb()
                k.mm(bk[:, 0:128], Ptr[t][:, :], P[t][:, :], inc=last)
                if not last:
                    k.mm(bk[:, 128:256], P[t][:, :], Ptr[t][:, :], inc=True)
                k.copy("act", newP[t][:, :], bk[:, 0:128])
                if not last:
                    k.copy("dve", newPt[t][:, :], bk[:, 128:256])
            for t in range(4):
                bk = nb()
                k.mm(bk[:, 0:128], Rt[t][:, :], newP[t][:, :], inc=last)
                if not last:
                    k.mm(bk[:, 128:256], newP[t][:, :], Rt[t][:, :], inc=True)
                k.tt("dve", Rr[t][:, :], Rr[t][:, :], bk[:, 0:128], ALU.add)
                if not last:
                    k.tt("dve", Rt[t][:, :], Rt[t][:, :], bk[:, 128:256], ALU.add)
            P = [newP[t] for t in range(4)]
            Ptr = [newPt[t] for t in range(4)]

        for t in range(4):
            bk = nb()
            k.mm(bk[:, 0:128], Rr[t][:, :], vb[t][:, :], inc=False)
            k.mm(bk[:, 128:256], kbg[t][:, :], Rr[t][:, :], inc=True)
            k.copy("act", u_sb[t][:, :], bk[:, 0:128])
            k.copy("dve", wT_sb[t][:, :], bk[:, 128:256])

        if shared:
            k.mark()
        for t in range(4):
            tok = slice(blk * 512 + t * 128, blk * 512 + (t + 1) * 128)
            c = tmc[t]
            s_old = Sf[si % 2]
            s_new = Sf[(si + 1) % 2]
            si += 1
            bk = nbb()
            k.mm(bk[:, 0:128], wT_sb[t][:, :], s_old[:, :])
            k.tt("dve", vnew[t][:, :], u_sb[t][:, :], bk[:, 0:128], ALU.subtract)
            bs = nbb()
            k.mm(bs[:, 0:128], kdec[t][:, :], vnew[t][:, :])
            k.stt("dve", s_new[:, :], s_old[:, :], c[:, 6:7], bs[:, 0:128], ALU.mult, ALU.add)
            bo = nbb()
            k.mm(bo[:, 0:128], qdT[t][:, :], s_old[:, :], start=True, stop=False, inc=False)
            k.mm(bo[:, 0:128], qkT[t][:, :], vnew[t][:, :], start=False, stop=True, inc=True)
            k.act(osq[t][:, :], bo[:, 0:128], AF.Square, accum_out=ost[t][:, 0:1])
            k.rstd_ln(ost[t][:, 1:2], ost[t][:, 0:1], 1.0 / 128, EPS)
            k.stt("dve", y1[t][:, :], bo[:, 0:128], ost[t][:, 1:2], gb[:, :], ALU.mult, ALU.mult)
            k.tt("pool", y2[t][:, :], y1[t][:, :], sgate[t][:, :], ALU.mult)
            bt = nbb()
            k.transpose(bt[:, 0:128], y2[t][:, :], ident[:, :])
            k.copy("act", oTb[blk % 2][:, t * 128:(t + 1) * 128], bt[:, 0:128])
        k.dma("sp", G["osrc"][1][blk // 4][:, (blk % 4) * 512:(blk % 4 + 1) * 512], oTb[blk % 2][:, :])
    k.pop()


def emit_stage_c(k, banks, C, W, G, last):
    ntok = S * B // NCORES
    NT = ntok // 128
    NB = ntok // 512
    upto = 9
    h_d = G["h_cur"]
    wg_d, wout_d = W["w_gates"], W["w_out"]
    wbr_d = [W["w_br_a"], W["w_br_b"], W["w_br_c"]]
    ln1g_d, ln1b_d, ln2g_d, ln2b_d = W["ln1_g"], W["ln1_b"], W["ln2_g"], W["ln2_b"]
    wr_d, br_d = W["w_router"], W["b_router"]
    ewg_d, ewu_d, ewd_d = W["exp_w_gate"], W["exp_w_up"], W["exp_w_down"]
    out_d = G["out"]
    ident = C["ident"]
    k.push()

    comb_all = k.sb("comb_all", [128, NT, 32], F32)
    mixT = k.sb("mixT", [128, 8, ntok], BF16)

    k.push()
    wg = k.sb("wg", [128, 8, 3 * D], BF16)
    wbr = k.sb("wbr", [128, 12, D], BF16)
    for kc in range(8):
        k.dma("pool", wg[:, kc, :], wg_d[kc * 128:(kc + 1) * 128, :])
    for br in range(3):
        for kc in range(4):
            k.dma("pool", wbr[:, br * 4 + kc, :], wbr_d[br][kc * 128:(kc + 1) * 128, :])
    hTb = [k.sb(f"hTb{i}", [128, 8, 512], BF16) for i in range(2)]
    oTb = [k.sb(f"oTb{i}", [128, 12, 512], BF16) for i in range(2)]
    sg = [k.sb(f"sg{i}", [128, 512], BF16) for i in range(2)]
    tmx = [k.sb(f"tmx{i}", [128, 512], F32) for i in range(2)]
    mixf = [k.sb(f"mixf{i}", [128, 512], F32) for i in range(2)]
    nb = 0
    for tb in range(NB):
        tsl = slice(tb * 512, (tb + 1) * 512)
        hb, ob = hTb[tb % 2], oTb[tb % 2]
        for kc in range(8):
            k.dma("sp", hb[:, kc, :], G["hsrc"][kc // 2][(kc % 2) * 128:(kc % 2 + 1) * 128, tsl])
        for br in range(3):
            for kc in range(4):
                k.dma("sp", ob[:, br * 4 + kc, :], G["o_own"](br, kc, tsl), extra_reads=G["o_own_res"](br))
        for r in range(8):
            mf = mixf[r % 2]
            for br in range(3):
                bg = banks[nb % 2]
                by = banks[2 + nb % 2]
                sgt = sg[nb % 2]
                tm = tmx[nb % 2]
                nb += 1
                col = br * D + r * 128
                for kc in range(8):
                    k.mm(bg[:, :], wg[:, kc, col:col + 128], hb[:, kc, :], start=(kc == 0), stop=(kc == 7))
                k.act(sgt[:, :], bg[:, :], AF.Sigmoid)
                for kc in range(4):
                    k.mm(by[:, :], wbr[:, br * 4 + kc, r * 128:(r + 1) * 128], ob[:, br * 4 + kc, :],
                         start=(kc == 0), stop=(kc == 3))
                if br == 0:
                    k.tt("dve", mf[:, :], by[:, :], sgt[:, :], ALU.mult)
                elif br == 1:
                    k.tt("dve", tm[:, :], by[:, :], sgt[:, :], ALU.mult)
                    k.tt("pool", mf[:, :], mf[:, :], tm[:, :], ALU.add)
                else:
                    k.tt("dve", tm[:, :], by[:, :], sgt[:, :], ALU.mult)
                    k.tt("pool", mixT[:, r, tsl], mf[:, :], tm[:, :], ALU.add)
    k.pop()

    acc = [k.sb(f"acc{i}", [128, D], F32) for i in range(NT)]
    h1T = k.sb("h1T", [128, 8, ntok], BF16)
    k.push()
    wout = k.sb("wout", [128, 8, D], BF16)
    for kc in range(8):
        k.dma("pool", wout[:, kc, :], wout_d[kc * 128:(kc + 1) * 128, :])
    wr = k.sb("wr", [128, 8, 36], F32)
    for kc in range(8):
        k.dma("sp", wr[:, kc, :], wr_d[kc * 128:(kc + 1) * 128, :])
    brb = k.sb("brb", [128, 36], F32)
    k.dma("sp", brb[:, :], bcast_row(br_d))
    g1 = k.sb("g1", [128, D], F32)
    b1 = k.sb("b1", [128, D], F32)
    k.dma("sp", g1[:, :], bcast_row(ln1g_d))
    k.dma("sp", b1[:, :], bcast_row(ln1b_d))
    hts = [k.sb(f"ht{i}", [128, D], F32) for i in range(2)]
    x1s = [k.sb(f"x1{i}", [128, D], F32) for i in range(2)]
    h1s = [k.sb(f"h1{i}", [128, D], F32) for i in range(2)]
    tmps = [k.sb(f"lt{i}", [128, D], F32) for i in range(2)]
    sts = [k.sb(f"ls{i}", [128, 4], F32) for i in range(2)]
    hTf = [k.sb(f"hTf{i}", [128, 8, 128], F32) for i in range(2)]
    rl = [k.sb(f"rl{i}", [128, 36], F32) for i in range(2)]
    rs = [k.sb(f"rs{i}", [128, 16], F32) for i in range(2)]
    elm = [k.sb(f"elm{i}", [128, 32], F32) for i in range(2)]
    elm2 = [k.sb(f"elm2{i}", [128, 32], F32) for i in range(2)]
    oh1 = [k.sb(f"oh1{i}", [128, 32], F32) for i in range(2)]
    oh2 = [k.sb(f"oh2{i}", [128, 32], F32) for i in range(2)]
    k_main = k
    recs = [Rec(k_main), Rec(k_main)]
    for t in range(NT):
        p = t % 2
        k = recs[p]
        tok = slice(t * 128, (t + 1) * 128)
        ht, x1, h1t, tmp, st = hts[p], x1s[p], h1s[p], tmps[p], sts[p]
        k.dma("sp", ht[:, :], h_d[tok, :])
        for half in range(2):
            bk = banks[half + 2 * p]
            for kc in range(8):
                k.mm(bk[:, :], mixT[:, kc, tok], wout[:, kc, half * 512:(half + 1) * 512],
                     start=(kc == 0), stop=(kc == 7))
            k.stt("dve", x1[:, half * 512:(half + 1) * 512], ht[:, half * 512:(half + 1) * 512], ALPHA,
                  bk[:, :], ALU.mult, ALU.add)
        layer_norm_tile(k, x1[:, :], h1t[:, :], g1[:, :], b1[:, :], tmp[:, :], st[:, :])
        k.act(acc[t][:, :], h1t[:, :], AF.Copy, scale=ALPHA)
        hf = hTf[p]
        for q4 in range(2):
            bk = banks[4 + q4 + 2 * p]
            for j in range(4):
                kc = q4 * 4 + j
                k.transpose(bk[:, j * 128:(j + 1) * 128], h1t[:, kc * 128:(kc + 1) * 128], ident[:, :],
                            inc=(j == 3))
            k.copy("dve", hf[:, q4 * 4:(q4 + 1) * 4, :],
                   bk[:, :].f(lambda a: a.rearrange("p (j t) -> p j t", j=4)))
        k.copy("act", h1T[:, :, tok], hf[:, :, :])
        bk = banks[2 * p]
        for kc in range(8):
            k.mm(bk[:, 0:36], hf[:, kc, :], wr[:, kc, :], start=(kc == 0), stop=(kc == 7))
        l, s_, em, em2, o1, o2 = rl[p], rs[p], elm[p], elm2[p], oh1[p], oh2[p]
        cb = comb_all[:, t, :]
        k.tt("dve", l[:, :], bk[:, 0:36], brb[:, :], ALU.add)
        k.reduce("dve", s_[:, 0:1], l[:, 0:4], ALU.max)
        k.ts("dve", s_[:, 1:2], s_[:, 0:1], -1.0, None, ALU.mult)
        k.act(s_[:, 8:12], l[:, 0:4], AF.Exp, bias=s_[:, 1:2], scale=1.0, accum_out=s_[:, 2:3])
        k.op("dve", lambda e, s_=s_: e.reciprocal(s_[:, 3:4].ap, s_[:, 2:3].ap), [s_], [s_])
        k.ts("dve", s_[:, 12:16], l[:, 0:4], s_[:, 0:1], None, ALU.is_equal)
        k.ts("dve", s_[:, 12:16], s_[:, 12:16], BIG, -BIG, ALU.mult, ALU.add)
        k.tt("dve", em[:, :].f(lambda a: a.rearrange("p (g e) -> p g e", g=4)),
             l[:, 4:36].f(lambda a: a.rearrange("p (g e) -> p g e", g=4)),
             s_[:, 12:16].f(lambda a: a.unsqueeze(2).broadcast_to([128, 4, 8])), ALU.add)
        k.reduce("dve", s_[:, 4:5], em[:, :], ALU.max)
        k.ts("dve", o1[:, :], em[:, :], s_[:, 4:5], None, ALU.is_equal)
        k.stt("dve", em2[:, :], o1[:, :], -BIG, em[:, :], ALU.mult, ALU.add)
        k.reduce("dve", s_[:, 5:6], em2[:, :], ALU.max)
        k.ts("dve", o2[:, :], em2[:, :], s_[:, 5:6], None, ALU.is_equal)
        k.tt("dve", s_[:, 6:7], s_[:, 5:6], s_[:, 4:5], ALU.subtract)
        k.act(s_[:, 6:7], s_[:, 6:7], AF.Exp)
        k.ts("dve", s_[:, 7:8], s_[:, 6:7], 1.0, None, ALU.add)
        k.op("dve", lambda e, s_=s_: e.reciprocal(s_[:, 7:8].ap, s_[:, 7:8].ap), [s_], [s_])
        k.tt("dve", s_[:, 7:8], s_[:, 7:8], s_[:, 3:4], ALU.mult)
        k.tt("dve", s_[:, 6:7], s_[:, 6:7], s_[:, 7:8], ALU.mult)
        k.ts("dve", cb, o1[:, :], s_[:, 7:8], None, ALU.mult)
        k.stt("dve", cb, o2[:, :], s_[:, 6:7], cb, ALU.mult, ALU.add)
    k = k_main
    replay_merged(k, recs[0].segs[0], recs[1].segs[0])
    k.pop()

    k.push()
    NW = 3
    ewg = [k.sb(f"ewg{i}", [128, 8, 256], BF16) for i in range(NW)]
    ewu = [k.sb(f"ewu{i}", [128, 8, 256], BF16) for i in range(NW)]
    ewd = [k.sb(f"ewd{i}", [128, 2, D], BF16) for i in range(NW)]
    sgs = [k.sb(f"sG{i}", [128, 512], BF16) for i in range(2)]
    hcs = [k.sb(f"Hc{i}", [128, 2, 512], BF16) for i in range(2)]
    n1 = 0
    n2 = [0]
    pending = None

    def down(e, tb, hc, wdt):
        for tt_ in range(4):
            t = tb * 4 + tt_
            for half in range(2):
                bO = banks[5 + n2[0] % 3]
                n2[0] += 1
                for fc in range(2):
                    k.mm(bO[:, :], hc[:, fc, tt_ * 128:(tt_ + 1) * 128], wdt[:, fc, half * 512:(half + 1) * 512],
                         start=(fc == 0), stop=(fc == 1))
                k.stt("dve", acc[t][:, half * 512:(half + 1) * 512], bO[:, :], comb_all[:, t, e:e + 1],
                      acc[t][:, half * 512:(half + 1) * 512], ALU.mult, ALU.add)

    for e in range(NEXP):
        wgt, wut, wdt = ewg[e % NW], ewu[e % NW], ewd[e % NW]
        k.dma("pool", wgt[:, :, :], ewg_d.v(ewg_d.h[e].rearrange("(kc p) f -> p kc f", p=128)))
        k.dma("pool", wut[:, :, :], ewu_d.v(ewu_d.h[e].rearrange("(kc p) f -> p kc f", p=128)))
        k.dma("pool", wdt[:, :, :], ewd_d.v(ewd_d.h[e].rearrange("(fc p) d -> p fc d", p=128)))
        for tb in range(NB):
            tsl = slice(tb * 512, (tb + 1) * 512)
            hc = hcs[tb % 2]
            for fc in range(2):
                bG = banks[1 + n1 % 2]
                bU = banks[3 + n1 % 2]
                sgt = sgs[n1 % 2]
                n1 += 1
                for kc in range(8):
                    k.mm(bG[:, :], wgt[:, kc, fc * 128:(fc + 1) * 128], h1T[:, kc, tsl], start=(kc == 0), stop=(kc == 7))
                for kc in range(8):
                    k.mm(bU[:, :], wut[:, kc, fc * 128:(fc + 1) * 128], h1T[:, kc, tsl], start=(kc == 0), stop=(kc == 7))
                k.act(sgt[:, :], bG[:, :], AF.Silu)
                k.tt("dve", hc[:, fc, :], bU[:, :], sgt[:, :], ALU.mult)
            if pending is not None:
                down(*pending)
            pending = (e, tb, hc, wdt)
    down(*pending)
    k.pop()

    k.push()
    g2 = k.sb("g2", [128, D], F32)
    b2 = k.sb("b2", [128, D], F32)
    k.dma("sp", g2[:, :], bcast_row(ln2g_d))
    k.dma("sp", b2[:, :], bcast_row(ln2b_d))
    ys = [k.sb(f"y{i}", [128, D], F32) for i in range(2)]
    tmps = [k.sb(f"lt{i}", [128, D], F32) for i in range(2)]
    sts = [k.sb(f"ls{i}", [128, 4], F32) for i in range(2)]
    if not last:
        hTsb = k.sb("hTsb", [128, 8, ntok], BF16)
    recs = [Rec(k), Rec(k)]
    for t in range(NT):
        p = t % 2
        kr = recs[p]
        layer_norm_tile(kr, acc[t][:, :], ys[p][:, :], g2[:, :], b2[:, :], tmps[p][:, :], sts[p][:, :], eng_g="dve")
        if last:
            kr.dma("sp", out_d[t * 128:(t + 1) * 128, :], ys[p][:, :])
        else:
            kr.dma("sp", h_d[t * 128:(t + 1) * 128, :], ys[p][:, :])
            emit_publish_tile(kr, banks, ident, ys[p], hTsb, t)
    replay_merged(k, recs[0].segs[0], recs[1].segs[0])
    if not last:
        emit_allgather_h(k, hTsb, G)
    k.pop()
    k.pop()


CONST_SHAPES = {"ident": [128, 128], "esel": [128, 64], "frq": [128, 1], "sgn": [128, 1],
                "cU": [128, 128], "cUrel": [128, 128], "cW": [128, 128], "cones": [128, 4], "maskbd": [128, 128],
                "rowmask": [128, 4], "uinc": [128, 128], "lpos_s": [128, 128], "uneg": [128, 128]}

LAYER_SHAPES = {
    "w_lat": [D, 448], "g_q": [128, 2], "g_kv": [128, 1], "w_uq": [256, 256], "w_uk": [128, 128], "w_uv": [128, 128],
    "w_hg": [D, 512], "hg_o_norm": [1, 128],
    "w_dn": [D, 514], "conv_w": [128, 3, 4], "a_log": [1, 1], "dt_bias": [1, 1], "dn_o_norm": [1, 128],
    "w_gates": [D, 3 * D], "w_br_a": [512, D], "w_br_b": [512, D], "w_br_c": [512, D], "w_out": [D, D],
    "ln1_g": [1, D], "ln1_b": [1, D], "ln2_g": [1, D], "ln2_b": [1, D],
    "w_router": [D, 36], "b_router": [1, 36],
    "exp_w_gate": [NEXP, D, 256], "exp_w_up": [NEXP, D, 256], "exp_w_down": [NEXP, 256, D],
}


def build_fused():
    nc = bass.Bass("TRN2", target_bir_lowering=False)
    k = K(nc)
    ntok = S * B // NCORES
    G = {}
    G["x"] = k.dram("x", [ntok, D], F32, "ExternalInput")
    G["ln_in_g"] = k.dram("ln_in_g", [1, D], F32, "ExternalInput")
    G["ln_in_b"] = k.dram("ln_in_b", [1, D], F32, "ExternalInput")
    G["pos"] = k.dram("pos", [1, S], I32, "ExternalInput")
    G["lb_rows"] = k.dram("lb_rows", [DEPTH, 128], F32, "ExternalInput")
    G["lb_cols"] = k.dram("lb_cols", [128, DEPTH], F32, "ExternalInput")
    rank_d = k.dram("rank", [1, 1], I32, "ExternalInput")
    tri_d = k.dram("tri", [128, 128], F32, "ExternalInput")
    G["out"] = k.dram("out", [ntok, D], F32, "ExternalOutput")
    cdram = {n: k.dram("c_" + n, shp, F32, "ExternalInput") for n, shp in CONST_SHAPES.items()}
    Ws = [{n: k.dram(f"L{l}_{n}", shp, F32, "ExternalInput") for n, shp in LAYER_SHAPES.items()} for l in range(DEPTH)]

    G["h_cur"] = k.dram("h_cur", [ntok, D], F32, "Internal")
    G["hsrc"] = [k.dram(f"hsrc{q}", [256, ntok], BF16, "Internal") for q in range(4)]
    G["hdst"] = [k.dram(f"hdst{q}", [4 * 256, ntok], BF16, "Internal") for q in range(4)]
    G["osrc"] = [[k.dram(f"osrc{br}_{q}", [128, ntok], BF16, "Internal") for q in range(4)] for br in range(3)]
    odst_full = [nc.dram_tensor(f"odst{br}", [4, 512, ntok], BF16, kind="Internal").ap() for br in range(3)]
    G["odst"] = [[T(odst_full[br][q], f"odst{br}_{q}") for q in range(4)] for br in range(3)]

    def hT_blk(blk, kc):
        r, t0, q = blk // 4, (blk % 4) * 512, kc // 2
        row0 = r * 256 + (kc % 2) * 128
        return G["hdst"][q][row0:row0 + 128, t0:t0 + 512]

    G["hT_blk"] = hT_blk

    reg = nc.sync.alloc_register("rank")
    nc.sync.reg_load(reg, rank_d.h[0:1, 0:1])
    rank_off = nc.sync.snap(reg, min_val=0, max_val=3)

    def o_own(br, kc, tsl):
        ap = odst_full[br][bass.ds(rank_off, 1), kc * 128:(kc + 1) * 128, tsl].rearrange("o p c -> (o p) c")
        return V(ap, G["odst"][br][0].res)

    G["o_own"] = o_own
    G["o_own_res"] = lambda br: [G["odst"][br][q].res for q in range(1, 4)]

    banks = [k.ps(f"bank{i}", [128, 512], F32) for i in range(8)]

    cres = Res("consts")
    C = {}
    for n, shp in CONST_SHAPES.items():
        C[n] = k.sb("c_" + n, shp, F32, res=cres)
        k.dma("sp", C[n][tuple(slice(None) for _ in shp)], cdram[n][tuple(slice(None) for _ in shp)])
    C["tri"] = k.sb("c_tri", [128, 128], BF16)
    k.dma("pool", C["tri"][:, :], tri_d[:, :])

    emit_ln0(k, banks, C, G)
    for l in range(DEPTH):
        W = Ws[l]
        emit_mla(k, banks, C, W, G)
        emit_allgather_o(k, G, 0)
        emit_dn_hg(k, banks, C, W, G, l)
        emit_stage_c(k, banks, C, W, G, last=(l == DEPTH - 1))
    k.finish([G["out"]])
    assert 5 + k.ndsem + k.ncoll <= 100, (k.ndsem, k.ncoll)
    build_fused.stats = (k.ndsem, k.ncoll, dict(k.tok))
    return nc


def layer_inputs(P, l, j):
    m = {}
    a = mla_inputs(None, np.zeros(1, np.int32), P, l, j)
    for n in ("w_lat", "g_q", "g_kv", "w_uq", "w_uk", "w_uv"):
        m[n] = a[n]
    hgi = hg_inputs(None, P, l, j)
    m["w_hg"] = hgi["w_hg"]
    m["hg_o_norm"] = hgi["o_norm"]
    dni = dn_inputs(None, P, l, j)
    for n in ("w_dn", "conv_w", "a_log", "dt_bias"):
        m[n] = dni[n]
    m["dn_o_norm"] = dni["o_norm"]
    w_in = P["w_in"][l]
    m.update({
        "w_gates": np.ascontiguousarray(w_in[:, 4520:]),
        "w_br_a": P["w_br_a"][l], "w_br_b": P["w_br_b"][l], "w_br_c": P["w_br_c"][l], "w_out": P["w_out"][l],
        "ln1_g": P["ln1_g"][l].reshape(1, D), "ln1_b": P["ln1_b"][l].reshape(1, D),
        "ln2_g": P["ln2_g"][l].reshape(1, D), "ln2_b": P["ln2_b"][l].reshape(1, D),
        "w_router": np.ascontiguousarray(np.concatenate([P["router_group_w"][l], P["router_expert_w"][l]], axis=1)),
        "b_router": np.concatenate([P["router_group_b"][l], P["router_expert_b"][l]]).reshape(1, 36),
        "exp_w_gate": P["exp_w_gate"][l], "exp_w_up": P["exp_w_up"][l], "exp_w_down": P["exp_w_down"][l],
    })
    return {f"L{l}_{n}": np.ascontiguousarray(v, dtype=np.float32) for n, v in m.items()}


def const_inputs():
    c = {}
    c.update(hg_consts())
    c.update(dn_consts())
    a = mla_inputs(None, np.zeros(1, np.int32), None, 0, 0, consts_only=True)
    c.update({n: a[n] for n in ("frq", "sgn", "esel")})
    out = {"c_" + n: np.ascontiguousarray(c[n], dtype=np.float32) for n in CONST_SHAPES}
    out["tri"] = a["tri"]
    return out


def kernel(**inputs):
    P = {k_: np.asarray(v) for k_, v in inputs.items()}
    x = np.asarray(P["x"], dtype=np.float32).reshape(B * S, D)
    pos = P["positions"]
    per = B * S // NCORES
    nc = build_fused()
    consts = const_inputs()
    in_maps = []
    for c in range(NCORES):
        b, j = c // 4, c % 4
        m = dict(consts)
        m["x"] = np.ascontiguousarray(x[c * per:(c + 1) * per])
        m["ln_in_g"] = P["ln_in_g"].reshape(1, D).astype(np.float32)
        m["ln_in_b"] = P["ln_in_b"].reshape(1, D).astype(np.float32)
        m["pos"] = np.ascontiguousarray(pos[b].reshape(1, S).astype(np.int32))
        lb = P["hg_lower_bounds"][:, j * 128:(j + 1) * 128].astype(np.float32)
        m["lb_rows"] = np.ascontiguousarray(lb)
        m["lb_cols"] = np.ascontiguousarray(lb.T)
        m["rank"] = np.array([[j]], np.int32)
        for l in range(DEPTH):
            m.update(layer_inputs(P, l, j))
        in_maps.append(m)
    res = run_bass_kernel_spmd(nc, in_maps, core_ids=list(range(NCORES)))
    out = np.concatenate([r["out"] for r in res.results], axis=0)
    return np.ascontiguousarray(out.reshape(B, S, D).astype(np.float32))
```

```python
from contextlib import ExitStack
import numpy as np
import concourse.bass as bass
import concourse.mybir as mybir
from concourse.bass_utils import run_bass_kernel_spmd

F32 = mybir.dt.float32
BF16 = mybir.dt.bfloat16
I32 = mybir.dt.int32
AF = mybir.ActivationFunctionType
ALU = mybir.AluOpType
AX = mybir.AxisListType

NCORES = 8
D = 1024
B = 2
S = 8192
DEPTH = 2
ALPHA = (2 * DEPTH) ** 0.25
EPS = 1e-6
IN_COLS = 7592
NEXP = 32


class Res:
    __slots__ = ("name", "w", "r", "dsem", "dkey", "dcount", "wdma", "excl")

    def __init__(self, name=""):
        self.name = name
        self.w = None
        self.r = {}
        self.dsem = None
        self.dkey = None
        self.dcount = 0
        self.wdma = False
        self.excl = False


class V:
    __slots__ = ("ap", "res")

    def __init__(self, ap, res):
        self.ap = ap
        self.res = res

    def __getitem__(self, key):
        return V(self.ap[key], self.res)

    def f(self, fn):
        return V(fn(self.ap), self.res)

    def bitcast(self, dt):
        return V(self.ap.bitcast(dt), self.res)


class T:
    def __init__(self, handle, name, res=None):
        self.h = handle
        self.res = res if res is not None else Res(name)

    def __getitem__(self, key):
        return V(self.h[key], self.res)

    def v(self, ap):
        return V(ap, self.res)


class _Dummy:
    def then_inc(self, *a, **kw):
        return self


_DUMMY = _Dummy()


class K:
    def __init__(self, nc):
        self.nc = nc
        self.E = {"pe": nc.tensor, "dve": nc.vector, "act": nc.scalar, "pool": nc.gpsimd, "sp": nc.sync}
        self.semobj = {}
        self.tok = {}
        self.seen = {n: {} for n in self.E}
        for n in self.E:
            self.semobj["s_" + n] = nc.alloc_semaphore("s_" + n)
            self.tok[n] = 0
        self.ndsem = 0
        self.dres = []
        self.nuniq = 0
        self.stacks = []
        self.phase_res = []
        self.free_dsems = []
        self.ncoll = 0
        self.coll_tokens = []
        self.dry = None

    def sb(self, name, shape, dt, res=None):
        self.nuniq += 1
        if self.stacks:
            h = self.stacks[-1].enter_context(self.nc.sbuf_tensor(f"{name}_{self.nuniq}", list(shape), dt))
        else:
            h = self.nc.alloc_sbuf_tensor(f"{name}_{self.nuniq}", list(shape), dt)
        t = T(h, name, res=res)
        if self.stacks and res is None:
            self.phase_res[-1].append(t.res)
        return t

    def push(self):
        self.stacks.append(ExitStack())
        self.phase_res.append([])

    def pop(self):
        self.barrier()
        self.stacks.pop().close()
        for r in self.phase_res.pop():
            if r.dsem is not None:
                self.free_dsems.append((r.dsem, r.dkey, r.dcount))
                self.dres.remove(r)
                r.dsem = None

    def ps(self, name, shape, dt=F32):
        self.nuniq += 1
        t = T(self.nc.alloc_psum_tensor(f"{name}_{self.nuniq}", list(shape), dt), name)
        t.res.excl = True
        return t

    def dram(self, name, shape, dt, kind):
        h = self.nc.dram_tensor(name, list(shape), dt, kind=kind)
        return T(h.ap(), name)

    def _gather(self, eng, reads, writes):
        own = "s_" + eng
        deps = {}

        def add(t, raw):
            if t is None:
                return
            k, v = t
            if k == own and (eng == "pe" or not raw):
                return
            if deps.get(k, 0) < v:
                deps[k] = v

        for r in reads:
            add(r.w, True)
            if r.excl:
                for k, v in r.r.items():
                    add((k, v), False)
        for w in writes:
            add(w.w, False)
            for k, v in w.r.items():
                add((k, v), False)
        return deps

    def _emit_waits(self, eng, deps):
        e = self.E[eng]
        seen = self.seen[eng]
        for k, v in deps.items():
            if k.startswith("s_"):
                assert v <= self.tok[k[2:]], f"wait on unrealised token {k} {v} > {self.tok[k[2:]]}"
            if seen.get(k, 0) >= v:
                continue
            e.wait_ge(self.semobj[k], v)
            seen[k] = v

    def op(self, eng, fn, reads, writes, inc=True):
        reads = [r.res if isinstance(r, (V, T)) else r for r in reads if r is not None]
        writes = [w.res if isinstance(w, (V, T)) else w for w in writes if w is not None]
        if self.dry is not None:
            self.dry.append((eng, reads, writes))
            return _DUMMY
        deps = self._gather(eng, reads, writes)
        self._emit_waits(eng, deps)
        ins = fn(self.E[eng])
        key = "s_" + eng
        if inc:
            ins.then_inc(self.semobj[key], 1)
            self.tok[eng] += 1
            t = (key, self.tok[eng])
        else:
            t = (key, self.tok[eng] + 1)
        for w in writes:
            w.w = t
            w.r = {}
            w.wdma = False
        for r in reads:
            if r in writes:
                continue
            if r.r.get(key, 0) < t[1]:
                r.r[key] = t[1]
        return ins

    def collective(self, kind, ins, outs, groups):
        deps = {}

        def add(t):
            if t is None:
                return
            k_, v = t
            if deps.get(k_, 0) < v:
                deps[k_] = v

        for i in ins:
            add(i.res.w)
        for o in outs:
            add(o.res.w)
            for k_, v in o.res.r.items():
                add((k_, v))
        self._emit_waits("pool", deps)
        self.ncoll += 1
        key = f"cc{self.ncoll}"
        sem = self.nc.alloc_semaphore(key)
        self.semobj[key] = sem
        self.E["pool"].collective_compute(kind, ALU.bypass, replica_groups=groups,
                                          ins=[i.ap.opt() for i in ins], outs=[o.ap.opt() for o in outs]).then_inc(sem, 1)
        self.coll_tokens.append((key, 1))
        for o in outs:
            o.res.w = (key, 1)
            o.res.r = {}
            o.res.wdma = False
        for i in ins:
            i.res.r[key] = 1

    def dma(self, q, out, in_, extra_reads=(), **kw):
        w = out.res
        rd = in_.res
        if self.dry is not None:
            self.dry.append(("dma", [rd] + list(extra_reads), [w]))
            return
        own = "s_" + q
        deps = {}

        def add(t):
            if t is None:
                return
            k, v = t
            if deps.get(k, 0) < v:
                deps[k] = v

        add(rd.w)
        for xr in extra_reads:
            add(xr.w)
        if not (w.wdma and not w.r):
            add(w.w)
        for k, v in w.r.items():
            add((k, v))
        self._emit_waits(q, deps)
        if w.dsem is None:
            if self.free_dsems:
                w.dsem, w.dkey, w.dcount = self.free_dsems.pop()
            else:
                self.ndsem += 1
                w.dkey = f"d{self.ndsem}"
                w.dsem = self.nc.alloc_semaphore(w.dkey)
                self.semobj[w.dkey] = w.dsem
                w.dcount = 0
            self.dres.append(w)
        self.E[q].dma_start(out=out.ap, in_=in_.ap, **kw).then_inc(w.dsem, 16)
        w.dcount += 16
        t = (w.dkey, w.dcount)
        w.w = t
        w.r = {}
        w.wdma = True
        if rd.r.get(t[0], 0) < t[1]:
            rd.r[t[0]] = t[1]
        for xr in extra_reads:
            if xr.r.get(t[0], 0) < t[1]:
                xr.r[t[0]] = t[1]

    def barrier(self):
        for eng in self.E:
            deps = {}
            for x in self.E:
                if x != eng and self.tok[x] > 0:
                    deps["s_" + x] = self.tok[x]
            for r in self.dres:
                deps[r.dkey] = max(deps.get(r.dkey, 0), r.dcount)
            for key, v in self.coll_tokens:
                deps[key] = v
            self._emit_waits(eng, deps)

    def finish(self, outs):
        deps = {}
        for o in outs:
            r = o.res if isinstance(o, (V, T)) else o
            deps[r.w[0]] = r.w[1]
        self._emit_waits("sp", deps)

    def mm(self, out, lhsT, rhs, start=True, stop=True, inc=None, extra_reads=()):
        if inc is None:
            inc = stop
        return self.op("pe", lambda e: e.matmul(out.ap, lhsT.ap, rhs.ap, start=start, stop=stop),
                       [lhsT, rhs] + list(extra_reads), [out], inc=inc)

    def transpose(self, out, in_, ident, inc=True):
        return self.op("pe", lambda e: e.transpose(out.ap, in_.ap, ident.ap), [in_, ident], [out], inc=inc)

    def act(self, out, in_, func, bias=None, scale=None, accum_out=None, eng="act"):
        kw = {}
        rd = [in_]
        if bias is not None:
            if isinstance(bias, V):
                kw["bias"] = bias.ap
                rd.append(bias)
            else:
                kw["bias"] = bias
        if scale is not None:
            if isinstance(scale, V):
                kw["scale"] = scale.ap
                rd.append(scale)
            else:
                kw["scale"] = scale
        wr = [out]
        if accum_out is not None:
            kw["accum_out"] = accum_out.ap
            wr.append(accum_out)
        return self.op("act", lambda e: e.activation(out.ap, in_.ap, func, **kw), rd, wr)

    def tt(self, eng, out, in0, in1, op):
        return self.op(eng, lambda e: e.tensor_tensor(out.ap, in0.ap, in1.ap, op), [in0, in1], [out])

    def ts(self, eng, out, in0, s1, s2, op0, op1=None, accum_out=None):
        rd = [in0]
        a1 = s1
        a2 = s2
        if isinstance(s1, V):
            rd.append(s1)
            a1 = s1.ap
        if isinstance(s2, V):
            rd.append(s2)
            a2 = s2.ap
        wr = [out]
        kw = {}
        if op1 is not None:
            kw["op1"] = op1
        if accum_out is not None:
            kw["accum_out"] = accum_out.ap
            wr.append(accum_out)
        return self.op(eng, lambda e: e.tensor_scalar(out.ap, in0.ap, a1, a2, op0, **kw), rd, wr)

    def stt(self, eng, out, in0, scalar, in1, op0, op1):
        rd = [in0, in1]
        a = scalar
        if isinstance(scalar, V):
            rd.append(scalar)
            a = scalar.ap
        return self.op(eng, lambda e: e.scalar_tensor_tensor(out.ap, in0.ap, a, in1.ap, op0, op1), rd, [out])

    def rstd(self, out, in_, scale, eps):
        self.act(out, in_, AF.Sqrt, bias=eps, scale=scale)
        self.op("dve", lambda e: e.reciprocal(out.ap, out.ap), [out], [out])

    def rstd_ln(self, out, in_, scale, eps):
        self.act(out, in_, AF.Ln, bias=eps, scale=scale)
        self.act(out, out, AF.Exp, scale=-0.5)

    def copy(self, eng, out, in_):
        if eng == "act":
            return self.op("act", lambda e: e.copy(out.ap, in_.ap), [in_], [out])
        return self.op(eng, lambda e: e.tensor_copy(out.ap, in_.ap), [in_], [out])

    def memset(self, eng, out, val):
        return self.op(eng, lambda e: e.memset(out.ap, val), [], [out])

    def reduce(self, eng, out, in_, op, axis=AX.X):
        return self.op(eng, lambda e: e.tensor_reduce(out.ap, in_.ap, axis, op), [in_], [out])


def layer_norm_tile(k, x, out, g_b, b_b, tmp, st, pre_scale_res=None, eng_g="pool"):
    k.reduce("dve", st[:, 0:1], x, ALU.add)
    k.ts("dve", st[:, 1:2], st[:, 0:1], -1.0 / D, None, ALU.mult)
    k.act(tmp, x, AF.Square, bias=st[:, 1:2], scale=1.0, accum_out=st[:, 2:3])
    k.rstd_ln(st[:, 3:4], st[:, 2:3], 1.0 / D, EPS)
    k.ts("dve", tmp, x, st[:, 1:2], st[:, 3:4], ALU.add, ALU.mult)
    k.tt(eng_g, tmp, tmp, g_b, ALU.mult)
    k.tt("pool", out, tmp, b_b, ALU.add)


def build_ln0(ntok):
    nc = bass.Bass("TRN2", target_bir_lowering=False)
    k = K(nc)
    x = k.dram("x", [ntok, D], F32, "ExternalInput")
    g = k.dram("g", [1, D], F32, "ExternalInput")
    b = k.dram("b", [1, D], F32, "ExternalInput")
    y = k.dram("y", [ntok, D], F32, "ExternalOutput")
    g_b = k.sb("g_b", [128, D], F32)
    b_b = k.sb("b_b", [128, D], F32)
    k.dma("sp", g_b[:, :], g.v(g.h.partition_broadcast(128)))
    k.dma("sp", b_b[:, :], b.v(b.h.partition_broadcast(128)))
    nt = ntok // 128
    xs = [k.sb(f"x{i}", [128, D], F32) for i in range(2)]
    ys = [k.sb(f"y{i}", [128, D], F32) for i in range(2)]
    tmps = [k.sb(f"t{i}", [128, D], F32) for i in range(2)]
    sts = [k.sb(f"s{i}", [128, 4], F32) for i in range(2)]
    for i in range(nt):
        xt, yt, tt_, st = xs[i % 2], ys[i % 2], tmps[i % 2], sts[i % 2]
        k.dma("sp", xt[:, :], x[i * 128:(i + 1) * 128, :])
        layer_norm_tile(k, xt[:, :], yt[:, :], g_b[:, :], b_b[:, :], tt_[:, :], st[:, :])
        k.dma("sp", y[i * 128:(i + 1) * 128, :], yt[:, :])
    k.finish([y])
    return nc


def run_ln0(x, g, b):
    T_ = x.shape[0] * x.shape[1]
    xs = x.reshape(T_, D)
    per = T_ // NCORES
    nc = build_ln0(per)
    in_maps = [{"x": np.ascontiguousarray(xs[c * per:(c + 1) * per]), "g": g.reshape(1, D), "b": b.reshape(1, D)}
               for c in range(NCORES)]
    res = run_bass_kernel_spmd(nc, in_maps, core_ids=list(range(NCORES)))
    return np.concatenate([r["y"] for r in res.results], axis=0)


BIG = 1.0e30


def bcast_row(t, n=128):
    return t.v(t.h.partition_broadcast(n))


def build_stage_c(ntok, upto=9):
    nc = bass.Bass("TRN2", target_bir_lowering=False)
    k = K(nc)
    NT = ntok // 128
    NB = ntok // 512
    h_d = k.dram("h", [ntok, D], F32, "ExternalInput")
    hT_d = k.dram("hT", [D, ntok], F32, "ExternalInput")
    oT_d = [k.dram(n, [512, ntok], F32, "ExternalInput") for n in ("oaT", "obT", "ocT")]
    wg_d = k.dram("w_gates", [D, 3 * D], F32, "ExternalInput")
    wbr_d = [k.dram(n, [512, D], F32, "ExternalInput") for n in ("w_br_a", "w_br_b", "w_br_c")]
    wout_d = k.dram("w_out", [D, D], F32, "ExternalInput")
    ln1g_d = k.dram("ln1_g", [1, D], F32, "ExternalInput")
    ln1b_d = k.dram("ln1_b", [1, D], F32, "ExternalInput")
    ln2g_d = k.dram("ln2_g", [1, D], F32, "ExternalInput")
    ln2b_d = k.dram("ln2_b", [1, D], F32, "ExternalInput")
    wr_d = k.dram("w_router", [D, 36], F32, "ExternalInput")
    br_d = k.dram("b_router", [1, 36], F32, "ExternalInput")
    ewg_d = k.dram("exp_w_gate", [NEXP, D, 256], F32, "ExternalInput")
    ewu_d = k.dram("exp_w_up", [NEXP, D, 256], F32, "ExternalInput")
    ewd_d = k.dram("exp_w_down", [NEXP, 256, D], F32, "ExternalInput")
    ident_d = k.dram("ident", [128, 128], F32, "ExternalInput")
    out_d = k.dram("out", [ntok, D], F32, "ExternalOutput")

    banks = [k.ps(f"bank{i}", [128, 512], F32) for i in range(8)]

    ident = k.sb("ident", [128, 128], F32)
    k.dma("sp", ident[:, :], ident_d[:, :])
    comb_all = k.sb("comb_all", [128, NT, 32], F32)
    mixT = k.sb("mixT", [128, 8, ntok], BF16)

    k.push()
    wg = k.sb("wg", [128, 8, 3 * D], BF16)
    wbr = k.sb("wbr", [128, 12, D], BF16)
    for kc in range(8):
        k.dma("pool", wg[:, kc, :], wg_d[kc * 128:(kc + 1) * 128, :])
    for br in range(3):
        for kc in range(4):
            k.dma("pool", wbr[:, br * 4 + kc, :], wbr_d[br][kc * 128:(kc + 1) * 128, :])
    hTb = [k.sb(f"hTb{i}", [128, 8, 512], BF16) for i in range(2)]
    oTb = [k.sb(f"oTb{i}", [128, 12, 512], BF16) for i in range(2)]
    sg = [k.sb(f"sg{i}", [128, 512], BF16) for i in range(2)]
    tmx = [k.sb(f"tmx{i}", [128, 512], F32) for i in range(2)]
    mixf = [k.sb(f"mixf{i}", [128, 512], F32) for i in range(2)]
    nb = 0
    for tb in range(NB):
        tsl = slice(tb * 512, (tb + 1) * 512)
        hb, ob = hTb[tb % 2], oTb[tb % 2]
        for kc in range(8):
            k.dma("pool", hb[:, kc, :], hT_d[kc * 128:(kc + 1) * 128, tsl])
        for br in range(3):
            for kc in range(4):
                k.dma("pool", ob[:, br * 4 + kc, :], oT_d[br][kc * 128:(kc + 1) * 128, tsl])
        for r in range(8):
            mf = mixf[r % 2]
            for br in range(3):
                bg = banks[nb % 2]
                by = banks[2 + nb % 2]
                sgt = sg[nb % 2]
                tm = tmx[nb % 2]
                nb += 1
                col = br * D + r * 128
                for kc in range(8):
                    k.mm(bg[:, :], wg[:, kc, col:col + 128], hb[:, kc, :], start=(kc == 0), stop=(kc == 7))
                k.act(sgt[:, :], bg[:, :], AF.Sigmoid)
                for kc in range(4):
                    k.mm(by[:, :], wbr[:, br * 4 + kc, r * 128:(r + 1) * 128], ob[:, br * 4 + kc, :],
                         start=(kc == 0), stop=(kc == 3))
                if br == 0:
                    k.tt("dve", mf[:, :], by[:, :], sgt[:, :], ALU.mult)
                elif br == 1:
                    k.tt("dve", tm[:, :], by[:, :], sgt[:, :], ALU.mult)
                    k.tt("pool", mf[:, :], mf[:, :], tm[:, :], ALU.add)
                else:
                    k.tt("dve", tm[:, :], by[:, :], sgt[:, :], ALU.mult)
                    k.tt("pool", mixT[:, r, tsl], mf[:, :], tm[:, :], ALU.add)
    k.pop()
    if upto == 0:
        dbg = k.dram("dbg", [128, 8 * ntok], BF16, "ExternalOutput")
        k.dma("sp", dbg[:, :], mixT[:, :, :].f(lambda a: a.rearrange("p a b -> p (a b)")))
        k.finish([dbg])
        return nc

    acc = [k.sb(f"acc{i}", [128, D], F32) for i in range(NT)]
    h1T = k.sb("h1T", [128, 8, ntok], BF16)
    k.push()
    wout = k.sb("wout", [128, 8, D], BF16)
    for kc in range(8):
        k.dma("pool", wout[:, kc, :], wout_d[kc * 128:(kc + 1) * 128, :])
    wr = k.sb("wr", [128, 8, 36], F32)
    for kc in range(8):
        k.dma("sp", wr[:, kc, :], wr_d[kc * 128:(kc + 1) * 128, :])
    brb = k.sb("brb", [128, 36], F32)
    k.dma("sp", brb[:, :], bcast_row(br_d))
    g1 = k.sb("g1", [128, D], F32)
    b1 = k.sb("b1", [128, D], F32)
    k.dma("sp", g1[:, :], bcast_row(ln1g_d))
    k.dma("sp", b1[:, :], bcast_row(ln1b_d))
    hts = [k.sb(f"ht{i}", [128, D], F32) for i in range(2)]
    x1s = [k.sb(f"x1{i}", [128, D], F32) for i in range(2)]
    h1s = [k.sb(f"h1{i}", [128, D], F32) for i in range(2)]
    tmps = [k.sb(f"lt{i}", [128, D], F32) for i in range(2)]
    sts = [k.sb(f"ls{i}", [128, 4], F32) for i in range(2)]
    hTf = [k.sb(f"hTf{i}", [128, 8, 128], F32) for i in range(2)]
    rl = [k.sb(f"rl{i}", [128, 36], F32) for i in range(2)]
    rs = [k.sb(f"rs{i}", [128, 16], F32) for i in range(2)]
    elm = [k.sb(f"elm{i}", [128, 32], F32) for i in range(2)]
    elm2 = [k.sb(f"elm2{i}", [128, 32], F32) for i in range(2)]
    oh1 = [k.sb(f"oh1{i}", [128, 32], F32) for i in range(2)]
    oh2 = [k.sb(f"oh2{i}", [128, 32], F32) for i in range(2)]
    for t in range(NT):
        p = t % 2
        tok = slice(t * 128, (t + 1) * 128)
        ht, x1, h1t, tmp, st = hts[p], x1s[p], h1s[p], tmps[p], sts[p]
        k.dma("sp", ht[:, :], h_d[tok, :])
        for half in range(2):
            bk = banks[half + 2 * p]
            for kc in range(8):
                k.mm(bk[:, :], mixT[:, kc, tok], wout[:, kc, half * 512:(half + 1) * 512],
                     start=(kc == 0), stop=(kc == 7))
            k.stt("dve", x1[:, half * 512:(half + 1) * 512], ht[:, half * 512:(half + 1) * 512], ALPHA,
                  bk[:, :], ALU.mult, ALU.add)
        layer_norm_tile(k, x1[:, :], h1t[:, :], g1[:, :], b1[:, :], tmp[:, :], st[:, :])
        k.act(acc[t][:, :], h1t[:, :], AF.Copy, scale=ALPHA)
        hf = hTf[p]
        for q4 in range(2):
            bk = banks[4 + q4 + 2 * p]
            for j in range(4):
                kc = q4 * 4 + j
                k.transpose(bk[:, j * 128:(j + 1) * 128], h1t[:, kc * 128:(kc + 1) * 128], ident[:, :],
                            inc=(j == 3))
            k.copy("dve", hf[:, q4 * 4:(q4 + 1) * 4, :],
                   bk[:, :].f(lambda a: a.rearrange("p (j t) -> p j t", j=4)))
        k.copy("act", h1T[:, :, tok], hf[:, :, :])
        bk = banks[p]
        for kc in range(8):
            k.mm(bk[:, 0:36], hf[:, kc, :], wr[:, kc, :], start=(kc == 0), stop=(kc == 7))
        l, s_, em, em2, o1, o2 = rl[p], rs[p], elm[p], elm2[p], oh1[p], oh2[p]
        cb = comb_all[:, t, :]
        k.tt("dve", l[:, :], bk[:, 0:36], brb[:, :], ALU.add)
        k.reduce("dve", s_[:, 0:1], l[:, 0:4], ALU.max)
        k.ts("dve", s_[:, 1:2], s_[:, 0:1], -1.0, None, ALU.mult)
        k.act(s_[:, 8:12], l[:, 0:4], AF.Exp, bias=s_[:, 1:2], scale=1.0, accum_out=s_[:, 2:3])
        k.op("dve", lambda e: e.reciprocal(s_[:, 3:4].ap, s_[:, 2:3].ap), [s_], [s_])
        k.ts("dve", s_[:, 12:16], l[:, 0:4], s_[:, 0:1], None, ALU.is_equal)
        k.ts("dve", s_[:, 12:16], s_[:, 12:16], BIG, -BIG, ALU.mult, ALU.add)
        k.tt("dve", em[:, :].f(lambda a: a.rearrange("p (g e) -> p g e", g=4)),
             l[:, 4:36].f(lambda a: a.rearrange("p (g e) -> p g e", g=4)),
             s_[:, 12:16].f(lambda a: a.unsqueeze(2).broadcast_to([128, 4, 8])), ALU.add)
        k.reduce("dve", s_[:, 4:5], em[:, :], ALU.max)
        k.ts("dve", o1[:, :], em[:, :], s_[:, 4:5], None, ALU.is_equal)
        k.stt("dve", em2[:, :], o1[:, :], -BIG, em[:, :], ALU.mult, ALU.add)
        k.reduce("dve", s_[:, 5:6], em2[:, :], ALU.max)
        k.ts("dve", o2[:, :], em2[:, :], s_[:, 5:6], None, ALU.is_equal)
        k.tt("dve", s_[:, 6:7], s_[:, 5:6], s_[:, 4:5], ALU.subtract)
        k.act(s_[:, 6:7], s_[:, 6:7], AF.Exp)
        k.ts("dve", s_[:, 7:8], s_[:, 6:7], 1.0, None, ALU.add)
        k.op("dve", lambda e: e.reciprocal(s_[:, 7:8].ap, s_[:, 7:8].ap), [s_], [s_])
        k.tt("dve", s_[:, 7:8], s_[:, 7:8], s_[:, 3:4], ALU.mult)
        k.tt("dve", s_[:, 6:7], s_[:, 6:7], s_[:, 7:8], ALU.mult)
        k.ts("dve", cb, o1[:, :], s_[:, 7:8], None, ALU.mult)
        k.stt("dve", cb, o2[:, :], s_[:, 6:7], cb, ALU.mult, ALU.add)
    k.pop()
    if upto == 1:
        dbg = k.dram("dbg", [128, NT * 32], F32, "ExternalOutput")
        k.dma("sp", dbg[:, :], comb_all[:, :, :].f(lambda a: a.rearrange("p a b -> p (a b)")))
        dbg2 = k.dram("dbg2", [ntok, D], F32, "ExternalOutput")
        for t in range(NT):
            k.dma("sp", dbg2[t * 128:(t + 1) * 128, :], acc[t][:, :])
        k.finish([dbg, dbg2])
        return nc

    k.push()
    NW = 3
    ewg = [k.sb(f"ewg{i}", [128, 8, 256], BF16) for i in range(NW)]
    ewu = [k.sb(f"ewu{i}", [128, 8, 256], BF16) for i in range(NW)]
    ewd = [k.sb(f"ewd{i}", [128, 2, D], BF16) for i in range(NW)]
    sgs = [k.sb(f"sG{i}", [128, 512], BF16) for i in range(2)]
    hcs = [k.sb(f"Hc{i}", [128, 2, 512], BF16) for i in range(2)]
    n1 = 0
    n2 = 0
    for e in range(NEXP):
        wgt, wut, wdt = ewg[e % NW], ewu[e % NW], ewd[e % NW]
        k.dma("pool", wgt[:, :, :], ewg_d.v(ewg_d.h[e].rearrange("(kc p) f -> p kc f", p=128)))
        k.dma("pool", wut[:, :, :], ewu_d.v(ewu_d.h[e].rearrange("(kc p) f -> p kc f", p=128)))
        k.dma("pool", wdt[:, :, :], ewd_d.v(ewd_d.h[e].rearrange("(fc p) d -> p fc d", p=128)))
        for tb in range(NB):
            tsl = slice(tb * 512, (tb + 1) * 512)
            hc = hcs[tb % 2]
            for fc in range(2):
                bG = banks[1 + n1 % 2]
                bU = banks[3 + n1 % 2]
                sgt = sgs[n1 % 2]
                n1 += 1
                for kc in range(8):
                    k.mm(bG[:, :], wgt[:, kc, fc * 128:(fc + 1) * 128], h1T[:, kc, tsl], start=(kc == 0), stop=(kc == 7))
                for kc in range(8):
                    k.mm(bU[:, :], wut[:, kc, fc * 128:(fc + 1) * 128], h1T[:, kc, tsl], start=(kc == 0), stop=(kc == 7))
                k.act(sgt[:, :], bG[:, :], AF.Silu)
                k.tt("dve", hc[:, fc, :], bU[:, :], sgt[:, :], ALU.mult)
            for tt_ in range(4):
                t = tb * 4 + tt_
                for half in range(2):
                    bO = banks[5 + n2 % 3]
                    n2 += 1
                    for fc in range(2):
                        k.mm(bO[:, :], hc[:, fc, tt_ * 128:(tt_ + 1) * 128], wdt[:, fc, half * 512:(half + 1) * 512],
                             start=(fc == 0), stop=(fc == 1))
                    k.stt("dve", acc[t][:, half * 512:(half + 1) * 512], bO[:, :], comb_all[:, t, e:e + 1],
                          acc[t][:, half * 512:(half + 1) * 512], ALU.mult, ALU.add)
    k.pop()

    k.push()
    g2 = k.sb("g2", [128, D], F32)
    b2 = k.sb("b2", [128, D], F32)
    k.dma("sp", g2[:, :], bcast_row(ln2g_d))
    k.dma("sp", b2[:, :], bcast_row(ln2b_d))
    ys = [k.sb(f"y{i}", [128, D], F32) for i in range(2)]
    tmps = [k.sb(f"lt{i}", [128, D], F32) for i in range(2)]
    sts = [k.sb(f"ls{i}", [128, 4], F32) for i in range(2)]
    for t in range(NT):
        p = t % 2
        layer_norm_tile(k, acc[t][:, :], ys[p][:, :], g2[:, :], b2[:, :], tmps[p][:, :], sts[p][:, :])
        k.dma("sp", out_d[t * 128:(t + 1) * 128, :], ys[p][:, :])
    k.finish([out_d])
    k.pop()
    return nc


def stage_c_consts():
    return np.eye(128, dtype=np.float32)


def run_stage_c(h, oa, ob, oc, P, l, upto=9):
    T_ = h.shape[0]
    per = T_ // NCORES
    nc = build_stage_c(per, upto)
    ident = stage_c_consts()
    w_in = P["w_in"][l]
    common = {
        "w_gates": np.ascontiguousarray(w_in[:, 4520:]),
        "w_br_a": P["w_br_a"][l], "w_br_b": P["w_br_b"][l], "w_br_c": P["w_br_c"][l],
        "w_out": P["w_out"][l],
        "ln1_g": P["ln1_g"][l].reshape(1, D), "ln1_b": P["ln1_b"][l].reshape(1, D),
        "ln2_g": P["ln2_g"][l].reshape(1, D), "ln2_b": P["ln2_b"][l].reshape(1, D),
        "w_router": np.ascontiguousarray(np.concatenate([P["router_group_w"][l], P["router_expert_w"][l]], axis=1)),
        "b_router": np.concatenate([P["router_group_b"][l], P["router_expert_b"][l]]).reshape(1, 36),
        "exp_w_gate": P["exp_w_gate"][l], "exp_w_up": P["exp_w_up"][l], "exp_w_down": P["exp_w_down"][l],
        "ident": ident,
    }
    in_maps = []
    for c in range(NCORES):
        sl = slice(c * per, (c + 1) * per)
        m = dict(common)
        m["h"] = np.ascontiguousarray(h[sl])
        m["hT"] = np.ascontiguousarray(h[sl].T)
        m["oaT"] = np.ascontiguousarray(oa[sl].T)
        m["obT"] = np.ascontiguousarray(ob[sl].T)
        m["ocT"] = np.ascontiguousarray(oc[sl].T)
        in_maps.append(m)
    res = run_bass_kernel_spmd(nc, in_maps, core_ids=list(range(NCORES)))
    if upto < 9:
        return res.results
    return np.concatenate([r["out"] for r in res.results], axis=0)


QK_SCALE = 96 ** -0.5
TWO_PI = 2.0 * np.pi


class BankRR:
    def __init__(self, banks):
        self.banks = banks
        self.i = 0

    def __call__(self):
        b = self.banks[self.i % len(self.banks)]
        self.i += 1
        return b


def build_mla(T=S, nblk=None):
    nc = bass.Bass("TRN2", target_bir_lowering=False)
    k = K(nc)
    NBLK = T // 512 if nblk is None else nblk
    hT_d = k.dram("hT", [D, T], F32, "ExternalInput")
    wlat_d = k.dram("w_lat", [D, 448], F32, "ExternalInput")
    gq_d = k.dram("g_q", [128, 2], F32, "ExternalInput")
    gkv_d = k.dram("g_kv", [128, 1], F32, "ExternalInput")
    wuq_d = k.dram("w_uq", [256, 256], F32, "ExternalInput")
    wuk_d = k.dram("w_uk", [128, 128], F32, "ExternalInput")
    wuv_d = k.dram("w_uv", [128, 128], F32, "ExternalInput")
    pos_d = k.dram("pos", [1, T], I32, "ExternalInput")
    frq_d = k.dram("frq", [128, 1], F32, "ExternalInput")
    sgn_d = k.dram("sgn", [128, 1], F32, "ExternalInput")
    tri_d = k.dram("tri", [128, 128], F32, "ExternalInput")
    esel_d = k.dram("esel", [128, 64], F32, "ExternalInput")
    oT_d = k.dram("oT", [128, T], F32, "ExternalOutput")

    banks = [k.ps(f"bank{i}", [128, 512], F32) for i in range(8)]
    nb = BankRR(banks)

    wlat = k.sb("wlat", [128, 8, 448], BF16)
    for kc in range(8):
        k.dma("pool", wlat[:, kc, :], wlat_d[kc * 128:(kc + 1) * 128, :])
    gq = k.sb("gq", [128, 2], F32)
    gkv = k.sb("gkv", [128, 1], F32)
    frq = k.sb("frq", [128, 1], F32)
    sgn = k.sb("sgn", [128, 1], F32)
    esel = k.sb("esel", [128, 64], F32)
    tri = k.sb("tri", [128, 128], BF16)
    k.dma("sp", gq[:, :], gq_d[:, :])
    k.dma("sp", gkv[:, :], gkv_d[:, :])
    k.dma("sp", frq[:, :], frq_d[:, :])
    k.dma("sp", sgn[:, :], sgn_d[:, :])
    k.dma("sp", esel[:, :], esel_d[:, :])
    k.dma("pool", tri[:, :], tri_d[:, :])
    wtmp = k.sb("wtmp", [128, 2, 256], F32)
    wuq = k.sb("wuq", [128, 2, 256], BF16)
    for c in range(2):
        k.dma("sp", wtmp[:, c, :], wuq_d[c * 128:(c + 1) * 128, :])
    for c in range(2):
        k.ts("dve", wuq[:, c, :], wtmp[:, c, :], gq[:, c:c + 1], QK_SCALE, ALU.mult, ALU.mult)
    wtmp2 = k.sb("wtmp2", [128, 2, 128], F32)
    wuk = k.sb("wuk", [128, 128], BF16)
    wuv = k.sb("wuv", [128, 128], BF16)
    k.dma("sp", wtmp2[:, 0, :], wuk_d[:, :])
    k.dma("sp", wtmp2[:, 1, :], wuv_d[:, :])
    k.ts("dve", wuk[:, :], wtmp2[:, 0, :], gkv[:, 0:1], None, ALU.mult)
    k.ts("dve", wuv[:, :], wtmp2[:, 1, :], gkv[:, 0:1], None, ALU.mult)
    ones = k.sb("ones", [128, 128], F32)
    k.memset("dve", ones[:, :], 1.0)

    kT = [k.sb(f"kT{h}", [96, T], BF16) for h in range(2)]
    qT = [k.sb(f"qT{h}", [96, T], BF16) for h in range(2)]
    Vp = k.sb("Vp", [128, 2, T // 128, 128], BF16)
    k.memset("pool", Vp[:, :, :, :], 1.0)
    mx = k.sb("mx", [128, 4], F32)
    k.memset("dve", mx[:, :], 0.0)

    hTb = [k.sb(f"hTb{i}", [128, 8, 512], BF16) for i in range(2)]
    posi = [k.sb(f"posi{i}", [128, 512], I32) for i in range(2)]
    ang = k.sb("ang", [128, 512], F32)
    ang2 = k.sb("ang2", [128, 512], F32)
    ni = k.sb("ni", [128, 512], I32)
    nf = k.sb("nf", [128, 512], F32)
    cs = [k.sb(f"cs{i}", [128, 512], F32) for i in range(2)]
    sn = [k.sb(f"sn{i}", [128, 512], F32) for i in range(2)]
    cq_sb = [k.sb(f"cq_sb{i}", [128, 512], F32) for i in range(2)]
    sq_sb = [k.sb(f"sq_sb{i}", [128, 512], F32) for i in range(2)]
    ckv_sb = k.sb("ckv_sb", [128, 512], F32)
    sqkv = k.sb("sqkv", [128, 512], F32)
    rq = k.sb("rq", [128, 512], F32)
    rkv = k.sb("rkv", [128, 512], F32)
    cqn = [k.sb(f"cqn{i}", [128, 512], BF16) for i in range(2)]
    ckvn = k.sb("ckvn", [128, 512], BF16)
    t1 = k.sb("t1", [128, 512], F32)
    t2 = k.sb("t2", [128, 512], F32)
    nsq = k.sb("nsq", [96, 512], F32)
    mtmp = k.sb("mtmp", [128, 1], F32)

    def sincos(dst, src_ang):
        r6 = slice(64, 96)
        k.ts("dve", nf[r6, :], src_ang[r6, :], 1.0 / TWO_PI, None, ALU.mult)
        k.copy("dve", ni[r6, :], nf[r6, :])
        k.copy("dve", nf[r6, :], ni[r6, :])
        k.stt("dve", nf[r6, :], nf[r6, :], -TWO_PI, src_ang[r6, :], ALU.mult, ALU.add)
        k.ts("dve", nf[r6, :], nf[r6, :], 3.1415925, -3.1415925, ALU.min, ALU.max)
        k.act(dst[r6, :], nf[r6, :], AF.Sin)

    def rope_rows(dsts, bA, bB, cst, snt, col):
        r6 = slice(64, 96)
        k.stt("dve", t1[r6, :], bB[r6, :], sgn[r6, 0:1], snt[r6, :], ALU.mult, ALU.mult)
        k.tt("dve", t2[r6, :], bA[r6, :], cst[r6, :], ALU.mult)
        for i, d_ in enumerate(dsts):
            k.tt("pool", d_[r6, col], t1[r6, :], t2[r6, :], ALU.add)

    def normsq(src, col, slot):
        k.act(nsq[:, :], src[0:96, col], AF.Square)
        bk = nb()
        k.mm(bk[:, :], ones[0:96, :], nsq[:, :])
        k.reduce("dve", mtmp[:, :], bk[:, :], ALU.max)
        k.tt("dve", mx[:, slot:slot + 1], mx[:, slot:slot + 1], mtmp[:, :], ALU.max)

    for blk in range(NBLK):
        col = slice(blk * 512, (blk + 1) * 512)
        hb = hTb[blk % 2]
        pi_ = posi[blk % 2]
        cst, snt = cs[blk % 2], sn[blk % 2]
        for kc in range(8):
            k.dma("pool", hb[:, kc, :], hT_d[kc * 128:(kc + 1) * 128, col])
        k.dma("sp", pi_[:, :], pos_d.v(pos_d.h[:, col].partition_broadcast(128)))
        r6 = slice(64, 96)
        k.copy("dve", ang[r6, :], pi_[r6, :])
        k.ts("dve", ang[r6, :], ang[r6, :], frq[r6, 0:1], None, ALU.mult)
        k.ts("dve", ang2[r6, :], ang[r6, :], float(np.pi / 2), None, ALU.add)
        sincos(snt, ang)
        sincos(cst, ang2)
        for c in range(2):
            bk = nb()
            for kc in range(8):
                k.mm(bk[:, :], wlat[:, kc, c * 128:(c + 1) * 128], hb[:, kc, :], start=(kc == 0), stop=(kc == 7))
            k.copy("act", cq_sb[c][:, :], bk[:, :])
            k.act(sq_sb[c][:, :], bk[:, :], AF.Square)
        bk = nb()
        for kc in range(8):
            k.mm(bk[:, :], wlat[:, kc, 256:384], hb[:, kc, :], start=(kc == 0), stop=(kc == 7))
        k.copy("act", ckv_sb[:, :], bk[:, :])
        k.act(sqkv[:, :], bk[:, :], AF.Square)
        bA = nb()
        for kc in range(8):
            k.mm(bA[0:96, :], wlat[:, kc, 320:416], hb[:, kc, :], start=(kc == 0), stop=(kc == 7))
        bB = nb()
        for kc in range(8):
            k.mm(bB[0:96, :], wlat[:, kc, 352:448], hb[:, kc, :], start=(kc == 0), stop=(kc == 7))
        rope_rows([kT[0], kT[1]], bA, bB, cst, snt, col)
        bk = nb()
        k.mm(bk[:, :], ones[:, :], sq_sb[0][:, :], start=True, stop=False)
        k.mm(bk[:, :], ones[:, :], sq_sb[1][:, :], start=False, stop=True)
        k.rstd(rq[:, :], bk[:, :], 1.0 / 256, EPS)
        bk = nb()
        k.mm(bk[:, :], ones[:, :], sqkv[:, :])
        k.rstd(rkv[:, :], bk[:, :], 1.0 / 128, EPS)
        for c in range(2):
            k.tt("dve", cqn[c][:, :], cq_sb[c][:, :], rq[:, :], ALU.mult)
        k.tt("pool", ckvn[:, :], ckv_sb[:, :], rkv[:, :], ALU.mult)
        for hd in range(2):
            bk = nb()
            k.mm(bk[0:64, :], wuk[:, hd * 64:(hd + 1) * 64], ckvn[:, :])
            k.copy("act", kT[hd][0:64, col], bk[0:64, :])
        bk = nb()
        for tt_ in range(4):
            k.mm(bk[:, tt_ * 128:(tt_ + 1) * 128], ckvn[:, tt_ * 128:(tt_ + 1) * 128], wuv[:, :],
                 start=True, stop=True, inc=(tt_ == 3))
        k.copy("act", Vp[:, :, blk * 4:(blk + 1) * 4, 0:64],
               bk[:, :].f(lambda a: a.rearrange("p (t h d) -> p h t d", t=4, h=2)))
        for hd in range(2):
            bA = nb()
            for c in range(2):
                k.mm(bA[0:96, :], wuq[:, c, hd * 128:hd * 128 + 96], cqn[c][:, :], start=(c == 0), stop=(c == 1))
            bB = nb()
            for c in range(2):
                k.mm(bB[0:96, :], wuq[:, c, hd * 128 + 32:hd * 128 + 128], cqn[c][:, :], start=(c == 0), stop=(c == 1))
            k.copy("act", qT[hd][0:64, col], bA[0:64, :])
            rope_rows([qT[hd]], bA, bB, cst, snt, col)
        for hd in range(2):
            normsq(qT[hd], col, hd)
            normsq(kT[hd], col, 2 + hd)

    negc = k.sb("negc", [128, 2], F32)
    k.tt("dve", negc[:, :], mx[:, 0:2], mx[:, 2:4], ALU.mult)
    k.act(negc[:, :], negc[:, :], AF.Sqrt)
    k.ts("dve", negc[:, :], negc[:, :], -1.0, None, ALU.mult)

    acc_banks = BankRR(banks[0:2])
    s_banks = BankRR(banks[2:7])
    den_bank = banks[7]
    pT = [k.sb(f"pT{i}", [128, 512], BF16) for i in range(4)]
    osb = [k.sb(f"osb{i}", [128, 512], F32) for i in range(2)]
    ores = [k.sb(f"ores{i}", [64, 512], F32) for i in range(2)]
    npt = 0
    no = 0
    for hd in range(2):
        for qi in range(NBLK):
            oacc = acc_banks()
            nkb = 4 * qi + 4
            for kb in range(nkb):
                r = kb - 4 * qi
                c0 = 128 * r if r > 0 else 0
                qcol = slice(qi * 512 + c0, (qi + 1) * 512)
                sb_ = s_banks()
                pt = pT[npt % 4]
                npt += 1
                k.mm(sb_[:, c0:512], kT[hd][:, kb * 128:(kb + 1) * 128], qT[hd][:, qcol])
                k.act(pt[:, c0:512], sb_[:, c0:512], AF.Exp, bias=negc[:, hd:hd + 1], scale=1.0)
                if r >= 0:
                    k.tt("pool", pt[:, c0:c0 + 128], pt[:, c0:c0 + 128], tri[:, :], ALU.mult)
                k.mm(oacc[:, c0:512], Vp[:, hd, kb, :], pt[:, c0:512], start=(kb == 0), stop=(kb == nkb - 1))
            ob = osb[no % 2]
            orr = ores[no % 2]
            no += 1
            k.copy("act", ob[:, :], oacc[:, :])
            k.op("dve", lambda e: e.reciprocal(ob[64:128, :].ap, ob[64:128, :].ap), [ob], [ob])
            k.mm(den_bank[0:64, :], esel[:, :], ob[:, :])
            k.tt("dve", orr[:, :], ob[0:64, :], den_bank[0:64, :], ALU.mult)
            k.dma("sp", oT_d[hd * 64:(hd + 1) * 64, qi * 512:(qi + 1) * 512], orr[:, :])
    k.finish([oT_d])
    return nc


def mla_inputs(hT_b, pos_b, P, l, j, consts_only=False):
    inv_freq = (10000.0 ** (-np.arange(16, dtype=np.float32) / np.float32(16))).astype(np.float32)
    frq = np.zeros((128, 1), np.float32)
    frq[64:80, 0] = inv_freq
    frq[80:96, 0] = inv_freq
    sgn = np.zeros((128, 1), np.float32)
    sgn[64:80] = -1.0
    sgn[80:96] = 1.0
    tri = (np.arange(128)[:, None] <= np.arange(128)[None, :]).astype(np.float32)
    esel = np.zeros((128, 64), np.float32)
    esel[64, :] = 1.0
    if consts_only:
        return {"frq": frq, "sgn": sgn, "tri": tri, "esel": esel}
    w_in = P["w_in"][l]
    kr = w_in[:, 384:416]
    wlat = np.concatenate([w_in[:, 0:384], kr, kr[:, 16:32], kr[:, 0:16]], axis=1)
    wuq = P["mla_w_uq"][l].reshape(256, 8, 96)
    wukv = P["mla_w_ukv"][l].reshape(128, 8, 128)
    heads = (2 * j, 2 * j + 1)
    wuq_c = np.concatenate([np.concatenate([wuq[:, h, 0:64], wuq[:, h, 64:96], wuq[:, h, 80:96], wuq[:, h, 64:80]], axis=1)
                            for h in heads], axis=1)
    wuk_c = np.concatenate([wukv[:, h, 0:64] for h in heads], axis=1)
    wuv_c = np.concatenate([wukv[:, h, 64:128] for h in heads], axis=1)
    inv_freq = (10000.0 ** (-np.arange(16, dtype=np.float32) / np.float32(16))).astype(np.float32)
    frq = np.zeros((128, 1), np.float32)
    frq[64:80, 0] = inv_freq
    frq[80:96, 0] = inv_freq
    sgn = np.zeros((128, 1), np.float32)
    sgn[64:80] = -1.0
    sgn[80:96] = 1.0
    tri = (np.arange(128)[:, None] <= np.arange(128)[None, :]).astype(np.float32)
    esel = np.zeros((128, 64), np.float32)
    esel[64, :] = 1.0
    return {
        "hT": hT_b, "w_lat": np.ascontiguousarray(wlat),
        "g_q": np.ascontiguousarray(P["mla_q_norm"][l].reshape(2, 128).T),
        "g_kv": np.ascontiguousarray(P["mla_kv_norm"][l].reshape(128, 1)),
        "w_uq": np.ascontiguousarray(wuq_c), "w_uk": np.ascontiguousarray(wuk_c), "w_uv": np.ascontiguousarray(wuv_c),
        "pos": np.ascontiguousarray(pos_b.reshape(1, -1).astype(np.int32)),
        "frq": frq, "sgn": sgn, "tri": tri, "esel": esel,
    }


HC = 32


def hg_consts():
    t = np.arange(128)
    ch = t // HC
    same = ch[:, None] == ch[None, :]
    U = (same & (t[:, None] <= t[None, :])).astype(np.float32)
    mid = ch * HC + (HC // 2 - 1)
    Umid = (same & (t[:, None] <= mid[None, :])).astype(np.float32)
    W = (same & (t[:, None] > t[None, :])).astype(np.float32)
    cones = (ch[:, None] == np.arange(4)[None, :]).astype(np.float32)
    maskbd = (same & (t[:, None] <= t[None, :])).astype(np.float32)
    return {"cU": U, "cUrel": (U - Umid).astype(np.float32), "cW": W, "cones": cones, "maskbd": maskbd,
            "rowmask": cones.copy()}


def build_hg(T=S, layer=0):
    nc = bass.Bass("TRN2", target_bir_lowering=False)
    k = K(nc)
    NBLK = T // 512
    hT_d = k.dram("hT", [D, T], F32, "ExternalInput")
    w_d = k.dram("w_hg", [D, 512], F32, "ExternalInput")
    lbr_d = k.dram("lb_rows", [DEPTH, 128], F32, "ExternalInput")
    lbc_d = k.dram("lb_cols", [128, DEPTH], F32, "ExternalInput")
    on_d = k.dram("o_norm", [1, 128], F32, "ExternalInput")
    cU_d = k.dram("cU", [128, 128], F32, "ExternalInput")
    cUrel_d = k.dram("cUrel", [128, 128], F32, "ExternalInput")
    cW_d = k.dram("cW", [128, 128], F32, "ExternalInput")
    cones_d = k.dram("cones", [128, 4], F32, "ExternalInput")
    mbd_d = k.dram("maskbd", [128, 128], F32, "ExternalInput")
    rm_d = k.dram("rowmask", [128, 4], F32, "ExternalInput")
    o_d = k.dram("o", [T, 128], F32, "ExternalOutput")

    banks = [k.ps(f"bank{i}", [128, 512], F32) for i in range(8)]
    nb = BankRR(banks)

    whg = k.sb("whg", [128, 8, 512], BF16)
    for kc in range(8):
        k.dma("pool", whg[:, kc, :], w_d[kc * 128:(kc + 1) * 128, :])
    cU = k.sb("cU", [128, 128], F32)
    cUrel = k.sb("cUrel", [128, 128], F32)
    cW = k.sb("cW", [128, 128], F32)
    cones = k.sb("cones", [128, 4], F32)
    mbd = k.sb("mbd", [128, 128], F32)
    rowm = k.sb("rowm", [128, 4], F32)
    gb = k.sb("gb", [128, 128], F32)
    for t_, d_ in ((cU, cU_d), (cUrel, cUrel_d), (cW, cW_d), (cones, cones_d), (mbd, mbd_d), (rowm, rm_d)):
        k.dma("sp", t_[:, :], d_[:, :])
    k.dma("sp", gb[:, :], bcast_row(on_d))

    def lower_bound(x, n, name):
        m = k.sb(name + "_m", [128, n], F32)
        e = k.sb(name + "_e", [128, DEPTH, n], F32)
        ssum = k.sb(name + "_s", [128, n], F32)
        lb = k.sb(name + "_lb", [128, n], F32)
        oml = k.sb(name + "_oml", [128, n], F32)
        k.copy("dve", m[:, :], x[:, 0, :])
        for i in range(1, DEPTH):
            k.tt("dve", m[:, :], m[:, :], x[:, i, :], ALU.max)
        for i in range(DEPTH):
            k.tt("dve", e[:, i, :], x[:, i, :], m[:, :], ALU.subtract)
        k.act(e[:, :, :], e[:, :, :], AF.Exp)
        k.copy("dve", ssum[:, :], e[:, 0, :])
        for i in range(1, DEPTH):
            k.tt("dve", ssum[:, :], ssum[:, :], e[:, i, :], ALU.add)
        k.op("dve", lambda en: en.reciprocal(ssum[:, :].ap, ssum[:, :].ap), [ssum], [ssum])
        for i in range(DEPTH):
            k.tt("dve", e[:, i, :], e[:, i, :], ssum[:, :], ALU.mult)
        k.copy("dve", lb[:, :], e[:, 0, :])
        for i in range(1, layer + 1):
            k.tt("dve", lb[:, :], lb[:, :], e[:, i, :], ALU.add)
        k.tt("dve", lb[:, :], lb[:, :], e[:, 0, :], ALU.subtract)
        k.ts("dve", oml[:, :], lb[:, :], -1.0, 1.0, ALU.mult, ALU.add)
        return lb, oml

    xr = k.sb("xr", [128, DEPTH, 128], F32)
    for i in range(DEPTH):
        k.dma("sp", xr[:, i, :], lbr_d.v(lbr_d.h[i:i + 1, :].partition_broadcast(128)))
    lb_b, oml_b = lower_bound(xr, 128, "lbr")
    xc = k.sb("xc", [128, DEPTH, 1], F32)
    k.dma("sp", xc[:, :, 0], lbc_d[:, :])
    lb_c, oml_c = lower_bound(xc, 1, "lbc")
    noml_c = k.sb("noml_c", [128, 1], F32)
    k.ts("dve", noml_c[:, :], oml_c[:, :], -1.0, None, ALU.mult)

    NS = 8
    Sf = [k.sb(f"Sf{i}", [128, 128], F32) for i in range(2)]
    Sb = [k.sb(f"Sb{i}", [128, 128], BF16) for i in range(NS)]
    k.memset("dve", Sf[0][:, :], 0.0)
    k.memset("dve", Sb[0][:, :], 0.0)
    Z = [k.sb(f"Z{i}", [128, 4, 128], BF16) for i in range(2)]
    for z in Z:
        k.memset("pool", z[:, :, :], 0.0)
    si = 0

    hTb = [k.sb(f"hTb{i}", [128, 8, 512], BF16) for i in range(2)]
    qTs = [k.sb(f"qTs{i}", [128, 512], F32) for i in range(2)]
    kTs = [k.sb(f"kTs{i}", [128, 512], F32) for i in range(2)]

    def dbl(name, shape, dt, n=2):
        return [k.sb(f"{name}{i}", shape, dt) for i in range(n)]

    sg = dbl("sg", [128, 128], F32)
    uu = dbl("uu", [128, 128], F32)
    ff = dbl("ff", [128, 128], F32)
    ktm = dbl("ktm", [128, 128], F32)
    logf = dbl("logf", [128, 128], F32)
    vbf = dbl("vbf", [128, 128], BF16)
    sgate = dbl("sgate", [128, 128], F32)
    e1 = dbl("e1", [128, 128], F32)
    e2 = dbl("e2", [128, 128], F32)
    e3 = dbl("e3", [128, 128], F32)
    e4 = dbl("e4", [128, 128], F32)
    dl = dbl("dl", [128, 4], F32)
    qpT = dbl("qpT", [128, 128], BF16)
    kpT = dbl("kpT", [128, 128], BF16)
    kdp = dbl("kdp", [128, 4, 128], BF16)
    attm = dbl("attm", [128, 128], BF16)
    osq = dbl("osq", [128, 128], F32)
    ost = dbl("ost", [128, 2], F32)
    y1 = dbl("y1", [128, 128], F32)
    y2 = dbl("y2", [128, 128], F32)

    nt = 0
    for blk in range(NBLK):
        col = slice(blk * 512, (blk + 1) * 512)
        hb = hTb[blk % 2]
        for kc in range(8):
            k.dma("pool", hb[:, kc, :], hT_d[kc * 128:(kc + 1) * 128, col])
        qT_s, kT_s = qTs[blk % 2], kTs[blk % 2]
        bq = nb()
        for kc in range(8):
            k.mm(bq[:, :], whg[:, kc, 0:128], hb[:, kc, :], start=(kc == 0), stop=(kc == 7))
        k.act(qT_s[:, :], bq[:, :], AF.Silu)
        bz = nb()
        for kc in range(8):
            k.mm(bz[:, :], whg[:, kc, 128:256], hb[:, kc, :], start=(kc == 0), stop=(kc == 7))
        k.act(kT_s[:, :], bz[:, :], AF.Sigmoid)
        k.ts("dve", kT_s[:, :], kT_s[:, :], noml_c[:, 0:1], oml_c[:, 0:1], ALU.mult, ALU.add)
        for tt_ in range(4):
            p = nt % 2
            nt += 1
            tcol = slice(tt_ * 128, (tt_ + 1) * 128)
            tok = slice(blk * 512 + tt_ * 128, blk * 512 + (tt_ + 1) * 128)
            btm = nb()
            for kc in range(8):
                k.mm(btm[:, 0:384], hb[:, kc, tcol], whg[:, kc, 128:512], start=(kc == 0), stop=(kc == 7))
            k.act(sg[p][:, :], btm[:, 0:128], AF.Sigmoid)
            k.copy("act", vbf[p][:, :], btm[:, 128:256])
            k.act(sgate[p][:, :], btm[:, 256:384], AF.Silu)
            k.tt("dve", uu[p][:, :], sg[p][:, :], oml_b[:, :], ALU.mult)
            k.tt("pool", ff[p][:, :], uu[p][:, :], lb_b[:, :], ALU.add)
            k.tt("pool", ktm[p][:, :], oml_b[:, :], uu[p][:, :], ALU.subtract)
            k.ts("dve", ff[p][:, :], ff[p][:, :], 1e-30, None, ALU.max)
            k.act(logf[p][:, :], ff[p][:, :], AF.Ln)
            bc = nb()
            k.mm(bc[:, 0:128], logf[p][:, :], cU[:, :], inc=False)
            k.mm(bc[:, 128:256], logf[p][:, :], cUrel[:, :], inc=False)
            k.mm(bc[:, 256:384], cW[:, :], logf[p][:, :], inc=False)
            k.mm(bc[:, 384:388], logf[p][:, :], cones[:, :], inc=True)
            k.act(e1[p][:, :], bc[:, 0:128], AF.Exp)
            k.act(e2[p][:, :], bc[:, 128:256], AF.Exp)
            k.act(e3[p][:, :], bc[:, 128:256], AF.Exp, scale=-1.0)
            k.act(e4[p][:, :], bc[:, 256:384], AF.Exp)
            k.act(dl[p][:, :], bc[:, 384:388], AF.Exp)
            z = Z[p]
            zdiag = z.v(bass.AP(z.h, 0, [[512, 128], [160, 4], [1, 32]]))
            k.tt("dve", zdiag, qT_s[:, tcol].f(lambda a: a.rearrange("p (c x) -> p c x", c=4)),
                 e1[p][:, :].f(lambda a: a.rearrange("p (c x) -> p c x", c=4)), ALU.mult)
            k.tt("pool", qpT[p][:, :], qT_s[:, tcol], e2[p][:, :], ALU.mult)
            k.tt("pool", kpT[p][:, :], kT_s[:, tcol], e3[p][:, :], ALU.mult)
            for c in range(4):
                k.stt("dve" if c % 2 == 0 else "pool", kdp[p][:, c, :], ktm[p][:, :], rowm[:, c:c + 1], e4[p][:, :],
                      ALU.mult, ALU.mult) if c % 2 == 0 else None
            for c in range(4):
                if c % 2 == 1:
                    k.stt("dve", kdp[p][:, c, :], ktm[p][:, :], rowm[:, c:c + 1], e4[p][:, :], ALU.mult, ALU.mult)
            ba = nb()
            k.mm(ba[:, 0:128], kpT[p][:, :], qpT[p][:, :])
            k.tt("dve", attm[p][:, :], ba[:, 0:128], mbd[:, :], ALU.mult)
            bs = nb()
            for c in range(4):
                k.mm(bs[:, c * 128:(c + 1) * 128], kdp[p][:, c, :], vbf[p][:, :], inc=(c == 3))
            bo = nb()
            for c in range(4):
                k.mm(bo[:, 0:128], z[:, c, :], Sb[(si + c) % NS][:, :], start=(c == 0), stop=False, inc=False)
                s_old = Sf[(si + c) % 2]
                s_new = Sf[(si + c + 1) % 2]
                k.stt("dve", s_new[:, :], s_old[:, :], dl[p][:, c:c + 1], bs[:, c * 128:(c + 1) * 128],
                      ALU.mult, ALU.add)
                k.copy("act", Sb[(si + c + 1) % NS][:, :], s_new[:, :])
            k.mm(bo[:, 0:128], attm[p][:, :], vbf[p][:, :], start=False, stop=True, inc=True)
            si += 4
            k.act(osq[p][:, :], bo[:, 0:128], AF.Square, accum_out=ost[p][:, 0:1])
            k.rstd(ost[p][:, 1:2], ost[p][:, 0:1], 1.0 / 128, EPS)
            k.stt("dve", y1[p][:, :], bo[:, 0:128], ost[p][:, 1:2], gb[:, :], ALU.mult, ALU.mult)
            k.tt("pool", y2[p][:, :], y1[p][:, :], sgate[p][:, :], ALU.mult)
            k.dma("sp", o_d[tok, :], y2[p][:, :])
    k.finish([o_d])
    return nc


def hg_inputs(hT_b, P, l, j):
    w_in = P["w_in"][l]
    cols = [2472 + j * 128, 2984 + j * 128, 3496 + j * 128, 4008 + j * 128]
    w = np.concatenate([w_in[:, c:c + 128] for c in cols], axis=1)
    lb = P["hg_lower_bounds"][:, j * 128:(j + 1) * 128]
    m = {"hT": hT_b, "w_hg": np.ascontiguousarray(w), "lb_rows": np.ascontiguousarray(lb),
         "lb_cols": np.ascontiguousarray(lb.T), "o_norm": P["hg_o_norm"][l].reshape(1, 128)}
    m.update(hg_consts())
    return m


MASKV = 30000.0


def dn_consts():
    t = np.arange(128)
    uinc = (t[:, None] <= t[None, :]).astype(np.float32)
    lpos_s = np.where(t[None, :] < t[:, None], 0.0, MASKV).astype(np.float32)
    uneg = np.where(t[:, None] <= t[None, :], 0.0, -MASKV).astype(np.float32)
    return {"uinc": uinc, "lpos_s": lpos_s, "uneg": uneg, "ident": np.eye(128, dtype=np.float32)}


def build_dn(T=S):
    nc = bass.Bass("TRN2", target_bir_lowering=False)
    k = K(nc)
    NBLK = T // 512
    hT_d = k.dram("hT", [D, T], F32, "ExternalInput")
    w_d = k.dram("w_dn", [D, 384 + 130], F32, "ExternalInput")
    cw_d = k.dram("conv_w", [128, 3, 4], F32, "ExternalInput")
    alog_d = k.dram("a_log", [1, 1], F32, "ExternalInput")
    dtb_d = k.dram("dt_bias", [1, 1], F32, "ExternalInput")
    on_d = k.dram("o_norm", [1, 128], F32, "ExternalInput")
    uinc_d = k.dram("uinc", [128, 128], F32, "ExternalInput")
    lpos_d = k.dram("lpos_s", [128, 128], F32, "ExternalInput")
    uneg_d = k.dram("uneg", [128, 128], F32, "ExternalInput")
    ident_d = k.dram("ident", [128, 128], F32, "ExternalInput")
    o_d = k.dram("o", [T, 128], F32, "ExternalOutput")

    banks = [k.ps(f"bank{i}", [128, 512], F32) for i in range(8)]
    nb = BankRR(banks)

    wdn = k.sb("wdn", [128, 8, 514], BF16)
    for kc in range(8):
        k.dma("pool", wdn[:, kc, :], w_d[kc * 128:(kc + 1) * 128, :])
    cw = k.sb("cw", [128, 3, 4], F32)
    k.dma("sp", cw[:, :, :], cw_d[:, :, :])
    uinc = k.sb("uinc", [128, 128], F32)
    lpos = k.sb("lpos", [128, 128], F32)
    uneg = k.sb("uneg", [128, 128], F32)
    ident = k.sb("ident", [128, 128], F32)
    gb = k.sb("gb", [128, 128], F32)
    for t_, d_ in ((uinc, uinc_d), (lpos, lpos_d), (uneg, uneg_d), (ident, ident_d)):
        k.dma("sp", t_[:, :], d_[:, :])
    k.dma("sp", gb[:, :], bcast_row(on_d))
    sc = k.sb("sc", [128, 4], F32)
    k.dma("sp", sc[:, 0:1], bcast_row(alog_d))
    k.dma("sp", sc[:, 1:2], bcast_row(dtb_d))
    k.act(sc[:, 2:3], sc[:, 0:1], AF.Exp)
    k.ts("dve", sc[:, 2:3], sc[:, 2:3], -1.0, None, ALU.mult)
    ones = k.sb("ones", [128, 128], F32)
    k.memset("dve", ones[:, :], 1.0)

    Sf = [k.sb(f"S{i}", [128, 128], F32) for i in range(2)]
    k.memset("dve", Sf[0][:, :], 0.0)
    si = 0

    hTb = [k.sb(f"hTb{i}", [128, 8, 512], BF16) for i in range(2)]
    xh = [[k.sb(f"xh{w}_{i}", [128, 515], F32) for i in range(2)] for w in range(3)]
    for w in range(3):
        k.memset("dve", xh[w][1][:, 512:515], 0.0)
    cv = [k.sb(f"cv{w}", [128, 512], F32) for w in range(3)]
    sq = [k.sb(f"sq{w}", [128, 512], F32) for w in range(2)]
    rs = [k.sb(f"rs{w}", [128, 512], F32) for w in range(2)]

    def per_tile(name, shape, dt=F32):
        return [k.sb(f"{name}{i}", shape, dt) for i in range(4)]

    tmc = per_tile("tmc", [128, 8])
    sgate = per_tile("sgate", [128, 128])
    ktm = per_tile("ktm", [128, 128])
    vb = per_tile("vb", [128, 128])
    gbc = per_tile("gbc", [128, 128])
    xm = per_tile("xm", [128, 128])
    ym = per_tile("ym", [128, 128])
    dec_s = per_tile("dec_s", [128, 128])
    decT = per_tile("decT", [128, 128])
    egb = per_tile("egb", [128, 128])
    Mt = per_tile("M", [128, 128])
    Nt = per_tile("N", [128, 128])
    Pa = per_tile("Pa", [128, 128])
    Pat = per_tile("Pat", [128, 128])
    Pb = per_tile("Pb", [128, 128])
    Pbt = per_tile("Pbt", [128, 128])
    Rr = per_tile("R", [128, 128])
    Rt = per_tile("Rt", [128, 128])
    qkT = per_tile("qkT", [128, 128])
    qdT = per_tile("qdT", [128, 128])
    kbg = per_tile("kbg", [128, 128])
    kdec = per_tile("kdec", [128, 128])
    u_sb = per_tile("u", [128, 128])
    wT_sb = per_tile("wT", [128, 128])
    vnew = per_tile("vnew", [128, 128])
    osq = per_tile("osq", [128, 128])
    ost = per_tile("ost", [128, 2])
    y1 = per_tile("y1", [128, 128])
    y2 = per_tile("y2", [128, 128])

    for blk in range(NBLK):
        col = slice(blk * 512, (blk + 1) * 512)
        hb = hTb[blk % 2]
        for kc in range(8):
            k.dma("pool", hb[:, kc, :], hT_d[kc * 128:(kc + 1) * 128, col])
        for w in range(3):
            bk = nb()
            for kc in range(8):
                k.mm(bk[:, :], wdn[:, kc, w * 128:(w + 1) * 128], hb[:, kc, :], start=(kc == 0), stop=(kc == 7))
            cur, prv = xh[w][blk % 2], xh[w][(blk + 1) % 2]
            k.copy("act", cur[:, 3:515], bk[:, :])
            k.copy("act", cur[:, 0:3], prv[:, 512:515])
            y = cv[w]
            k.ts("dve", y[:, :], cur[:, 0:512], cw[:, w, 0:1], None, ALU.mult)
            for m in range(1, 4):
                k.stt("dve" if m != 2 else "dve", y[:, :], cur[:, m:m + 512], cw[:, w, m:m + 1], y[:, :], ALU.mult, ALU.add)
            k.act(y[:, :], y[:, :], AF.Silu)
        for w in range(2):
            k.act(sq[w][:, :], cv[w][:, :], AF.Square)
            bk = nb()
            k.mm(bk[:, :], ones[:, :], sq[w][:, :])
            k.rstd(rs[w][:, :], bk[:, :], 1.0, EPS)
            if w == 0:
                k.stt("pool", cv[w][:, :], cv[w][:, :], 1.0, rs[w][:, :], ALU.mult, ALU.mult) if False else None
        k.tt("pool", cv[0][:, :], cv[0][:, :], rs[0][:, :], ALU.mult)
        k.tt("pool", cv[1][:, :], cv[1][:, :], rs[1][:, :], ALU.mult)
        k.act(cv[0][:, :], cv[0][:, :], AF.Copy, scale=float(128 ** -0.5))
        qT_, kT_, vT_ = cv

        for t in range(4):
            tc_ = slice(t * 128, (t + 1) * 128)
            c = tmc[t]
            bk = nb()
            for kc in range(8):
                k.mm(bk[:, 0:130], hb[:, kc, tc_], wdn[:, kc, 384:514], start=(kc == 0), stop=(kc == 7))
            k.act(c[:, 0:1], bk[:, 0:1], AF.Sigmoid)
            k.act(c[:, 1:2], bk[:, 1:2], AF.Exp, bias=sc[:, 1:2], scale=1.0)
            k.act(sgate[t][:, :], bk[:, 2:130], AF.Silu)
            k.act(c[:, 1:2], c[:, 1:2], AF.Ln, bias=1.0, scale=1.0)
            k.tt("dve", c[:, 2:3], c[:, 1:2], sc[:, 2:3], ALU.mult)
            bk = nb()
            k.transpose(bk[:, 0:128], kT_[:, tc_], ident[:, :], inc=False)
            k.transpose(bk[:, 128:256], vT_[:, tc_], ident[:, :], inc=True)
            k.copy("act", ktm[t][:, :], bk[:, 0:128])
            k.act(vb[t][:, :], bk[:, 128:256], AF.Copy, scale=c[:, 0:1])
            k.ts("dve", gbc[t][:, :], ones[:, :], c[:, 2:3], None, ALU.mult)
            bk = nb()
            k.mm(bk[:, 0:128], gbc[t][:, :], uinc[:, :], inc=False)
            k.mm(bk[:, 128:129], uinc[:, :], c[:, 2:3], inc=True)
            k.copy("dve", c[:, 3:4], bk[:, 128:129])
            k.copy("dve", c[:, 7:8], bk[:, 127:128])
            k.stt("dve", xm[t][:, :], bk[:, 0:128], c[:, 3:4], lpos[:, :], ALU.subtract, ALU.max)
            k.stt("dve", ym[t][:, :], bk[:, 0:128], c[:, 3:4], uneg[:, :], ALU.subtract, ALU.min)
            k.act(egb[t][:, :], bk[:, 0:128], AF.Exp)
            k.act(dec_s[t][:, :], xm[t][:, :], AF.Exp, scale=-1.0)
            k.act(decT[t][:, :], ym[t][:, :], AF.Exp)
            k.act(c[:, 4:5], c[:, 3:4], AF.Exp)
            k.tt("dve", c[:, 4:5], c[:, 4:5], c[:, 0:1], ALU.mult)
            k.act(c[:, 5:6], c[:, 3:4], AF.Exp, bias=c[:, 7:8], scale=-1.0)
            k.act(c[:, 6:7], c[:, 7:8], AF.Exp)
            k.ts("pool", kbg[t][:, :], ktm[t][:, :], c[:, 4:5], None, ALU.mult) if False else None
            k.ts("dve", kbg[t][:, :], ktm[t][:, :], c[:, 4:5], None, ALU.mult)
            k.ts("dve", kdec[t][:, :], ktm[t][:, :], c[:, 5:6], None, ALU.mult)
            k.tt("pool", qdT[t][:, :], qT_[:, tc_], egb[t][:, :], ALU.mult)
            bk = nb()
            k.mm(bk[:, 0:128], kT_[:, tc_], kT_[:, tc_], inc=False)
            k.mm(bk[:, 128:256], kT_[:, tc_], qT_[:, tc_], inc=True)
            k.stt("dve", Mt[t][:, :], bk[:, 0:128], c[:, 0:1], dec_s[t][:, :], ALU.mult, ALU.mult)
            k.tt("dve", qkT[t][:, :], bk[:, 128:256], decT[t][:, :], ALU.mult)
            bk = nb()
            k.transpose(bk[:, 0:128], Mt[t][:, :], ident[:, :])
            k.copy("act", Nt[t][:, :], bk[:, 0:128])
            k.tt("pool", Rr[t][:, :], ident[:, :], Nt[t][:, :], ALU.subtract)
            k.tt("pool", Rt[t][:, :], ident[:, :], Mt[t][:, :], ALU.subtract)

        P = [Nt[t] for t in range(4)]
        Ptr = [Mt[t] for t in range(4)]
        for lvl in range(6):
            last = lvl == 5
            newP = Pa if lvl % 2 == 0 else Pb
            newPt = Pat if lvl % 2 == 0 else Pbt
            for t in range(4):
                bk = nb()
                k.mm(bk[:, 0:128], Ptr[t][:, :], P[t][:, :], inc=last)
                if not last:
                    k.mm(bk[:, 128:256], P[t][:, :], Ptr[t][:, :], inc=True)
                k.copy("act", newP[t][:, :], bk[:, 0:128])
                if not last:
                    k.copy("act", newPt[t][:, :], bk[:, 128:256])
            for t in range(4):
                bk = nb()
                k.mm(bk[:, 0:128], Rt[t][:, :], newP[t][:, :], inc=last)
                if not last:
                    k.mm(bk[:, 128:256], newP[t][:, :], Rt[t][:, :], inc=True)
                k.tt("dve", Rr[t][:, :], Rr[t][:, :], bk[:, 0:128], ALU.add)
                if not last:
                    k.tt("dve", Rt[t][:, :], Rt[t][:, :], bk[:, 128:256], ALU.add)
            P = [newP[t] for t in range(4)]
            Ptr = [newPt[t] for t in range(4)]

        for t in range(4):
            bk = nb()
            k.mm(bk[:, 0:128], Rr[t][:, :], vb[t][:, :], inc=False)
            k.mm(bk[:, 128:256], kbg[t][:, :], Rr[t][:, :], inc=True)
            k.copy("act", u_sb[t][:, :], bk[:, 0:128])
            k.copy("act", wT_sb[t][:, :], bk[:, 128:256])

        for t in range(4):
            tok = slice(blk * 512 + t * 128, blk * 512 + (t + 1) * 128)
            c = tmc[t]
            s_old = Sf[si % 2]
            s_new = Sf[(si + 1) % 2]
            si += 1
            bk = nb()
            k.mm(bk[:, 0:128], wT_sb[t][:, :], s_old[:, :])
            k.tt("dve", vnew[t][:, :], u_sb[t][:, :], bk[:, 0:128], ALU.subtract)
            bo = nb()
            k.mm(bo[:, 0:128], qdT[t][:, :], s_old[:, :], start=True, stop=False, inc=False)
            k.mm(bo[:, 0:128], qkT[t][:, :], vnew[t][:, :], start=False, stop=True, inc=True)
            bs = nb()
            k.mm(bs[:, 0:128], kdec[t][:, :], vnew[t][:, :])
            k.stt("dve", s_new[:, :], s_old[:, :], c[:, 6:7], bs[:, 0:128], ALU.mult, ALU.add)
            k.act(osq[t][:, :], bo[:, 0:128], AF.Square, accum_out=ost[t][:, 0:1])
            k.rstd(ost[t][:, 1:2], ost[t][:, 0:1], 1.0 / 128, EPS)
            k.stt("dve", y1[t][:, :], bo[:, 0:128], ost[t][:, 1:2], gb[:, :], ALU.mult, ALU.mult)
            k.tt("pool", y2[t][:, :], y1[t][:, :], sgate[t][:, :], ALU.mult)
            k.dma("sp", o_d[tok, :], y2[t][:, :])
    k.finish([o_d])
    return nc


def dn_inputs(hT_b, P, l, j):
    w_in = P["w_in"][l]
    cq, ck, cvv = 416 + j * 128, 416 + 512 + j * 128, 416 + 1024 + j * 128
    w = np.concatenate([w_in[:, cq:cq + 128], w_in[:, ck:ck + 128], w_in[:, cvv:cvv + 128],
                        w_in[:, 1952 + j:1953 + j], w_in[:, 1956 + j:1957 + j],
                        w_in[:, 1960 + j * 128:1960 + (j + 1) * 128]], axis=1)
    conv = P["dn_conv"][l]
    cwm = np.stack([conv[:, cq - 416:cq - 416 + 128], conv[:, ck - 416:ck - 416 + 128],
                    conv[:, cvv - 416:cvv - 416 + 128]], axis=0)
    m = {"hT": hT_b, "w_dn": np.ascontiguousarray(w),
         "conv_w": np.ascontiguousarray(cwm.transpose(2, 0, 1)),
         "a_log": P["dn_a_log"][l][j].reshape(1, 1), "dt_bias": P["dn_dt_bias"][l][j].reshape(1, 1),
         "o_norm": P["dn_o_norm"][l].reshape(1, 128)}
    m.update(dn_consts())
    return m


class Rec:
    _PASS = ("sb", "ps", "dram")

    def __init__(self, k):
        self._k = k
        self.segs = [[]]

    def sb(self, *a, **kw):
        return self._k.sb(*a, **kw)

    def push(self):
        pass

    def pop(self):
        pass

    def mark(self):
        self.segs.append([])

    def __getattr__(self, name):
        def f(*a, **kw):
            self.segs[-1].append((name, a, kw))
        return f


SEM_LAT = 1.2
_GHZ = {"pe": 1.9, "act": 1.2, "dve": 0.96, "pool": 0.6}
_FIX = {"pe": 0.06, "act": 0.2, "dve": 0.1, "pool": 0.25}


def _op_cost(k, rec):
    name, ar, kw = rec
    k.dry = []
    getattr(k, name)(*ar, **kw)
    infos, k.dry = k.dry, None
    n = 128
    out = ar[1] if name in ("tt", "ts", "stt", "copy", "memset", "reduce", "dma") else (ar[0] if ar else None)
    if name == "op":
        out = None
    if isinstance(out, V):
        try:
            n = out.ap.free_size()
        except Exception:
            n = 128
    res = []
    for eng, rd, wr in infos:
        if eng == "dma":
            d = 2.5
        else:
            passes = 1
            if name in ("mm", "transpose") and isinstance(ar[1], V) and ar[1].ap.dtype == F32:
                passes = 4
            d = _FIX[eng] + passes * n / (_GHZ[eng] * 1000.0)
        res.append((eng, rd, wr, d))
    return res


def replay_merged(k, *lists):
    lists = [l for l in lists if l]
    if not lists:
        return
    if len(lists) == 1:
        for name, ar, kw in lists[0]:
            getattr(k, name)(*ar, **kw)
        return
    free = {}
    ready = {}
    rdone = {}
    idx = [0] * len(lists)
    costs = [[None] * len(l) for l in lists]
    total = sum(len(l) for l in lists)

    def start_time(info):
        eng, rd, wr, d = info
        t = free.get(eng, 0.0)
        for r in rd:
            if r in ready:
                tr, pe_ = ready[r]
                t = max(t, tr + (SEM_LAT if pe_ != eng else 0.0))
        for w in wr:
            if w in ready:
                tr, pe_ = ready[w]
                t = max(t, tr + (SEM_LAT if pe_ != eng else 0.0))
            if w in rdone:
                t = max(t, rdone[w] + SEM_LAT)
        return t

    for _ in range(total):
        best, bt = None, None
        for n, l in enumerate(lists):
            if idx[n] < len(l):
                if costs[n][idx[n]] is None:
                    costs[n][idx[n]] = _op_cost(k, l[idx[n]])
                c = costs[n][idx[n]]
                t = start_time(c[0]) if c else 0.0
                key = (t, idx[n] / len(l))
                if bt is None or key < bt:
                    best, bt = n, key
        c = costs[best][idx[best]]
        for info in c:
            eng, rd, wr, d = info
            t = start_time(info)
            free[eng] = t + d
            for r in rd:
                rdone[r] = max(rdone.get(r, 0.0), t + d)
            for w in wr:
                ready[w] = (t + d, eng)
                rdone.pop(w, None)
        name, ar, kw = lists[best][idx[best]]
        idx[best] += 1
        getattr(k, name)(*ar, **kw)


def emit_dn_hg(k, banks, C, W, G, layer):
    NBLK = S // 512
    k.push()
    hTb = [k.sb(f"hTbS{i}", [128, 8, 512], BF16) for i in range(3)]
    ra, rb = Rec(k), Rec(k)
    emit_dn(ra, banks[0:DN_BANKS], C, W, G, hTb=hTb)
    emit_hg(rb, banks[DN_BANKS:8], C, W, G, layer, hTb=hTb)
    assert len(ra.segs) == 2 * NBLK + 1 and len(rb.segs) == NBLK + 1
    front = lambda i: ra.segs[1 + 2 * i] if i < NBLK else []
    back = lambda i: ra.segs[2 + 2 * i]

    def load(blk):
        if blk < NBLK:
            for kc in range(8):
                k.dma("sp", hTb[blk % 3][:, kc, :], G["hT_blk"](blk, kc))

    load(0)
    load(1)
    replay_merged(k, ra.segs[0], rb.segs[0])
    replay_merged(k, front(0))
    for blk in range(NBLK):
        load(blk + 2)
        replay_merged(k, front(blk + 1), back(blk), rb.segs[1 + blk])
        if blk % 4 == 3:
            q = blk // 4
            for br in (1, 2):
                k.collective("AllGather", [G["osrc"][br][q][:, :]], [G["odst"][br][q][:, :]], GROUPS)
    k.pop()


DN_BANKS = 6
DN_BACK_BANKS = 2

def emit_publish_tile(k, banks, ident, y, hTsb, t):
    tok = slice(t * 128, (t + 1) * 128)
    for q4 in range(2):
        bk = banks[4 + q4 + 2 * (t % 2)]
        for j in range(4):
            kc = q4 * 4 + j
            k.transpose(bk[:, j * 128:(j + 1) * 128], y[:, kc * 128:(kc + 1) * 128], ident[:, :], inc=(j == 3))
        k.copy("act", hTsb[:, q4 * 4:(q4 + 1) * 4, tok], bk[:, :].f(lambda a: a.rearrange("p (j t) -> p j t", j=4)))


def emit_allgather_h(k, hTsb, G):
    for kc in range(8):
        k.dma("sp", G["hsrc"][kc // 2][(kc % 2) * 128:(kc % 2 + 1) * 128, :], hTsb[:, kc, :])
    for q in range(4):
        k.collective("AllGather", [G["hsrc"][q][:, :]], [G["hdst"][q][:, :]], GROUPS)


def emit_allgather_o(k, G, br):
    for q in range(4):
        k.collective("AllGather", [G["osrc"][br][q][:, :]], [G["odst"][br][q][:, :]], GROUPS)


def emit_ln0(k, banks, C, G):
    ntok = S * B // NCORES
    k.push()
    x = G["x"]
    g_b = k.sb("g_b", [128, D], F32)
    b_b = k.sb("b_b", [128, D], F32)
    k.dma("sp", g_b[:, :], bcast_row(G["ln_in_g"]))
    k.dma("sp", b_b[:, :], bcast_row(G["ln_in_b"]))
    hTsb = k.sb("hTsb", [128, 8, ntok], BF16)
    xs = [k.sb(f"x{i}", [128, D], F32) for i in range(2)]
    ys = [k.sb(f"y{i}", [128, D], F32) for i in range(2)]
    tmps = [k.sb(f"t{i}", [128, D], F32) for i in range(2)]
    sts = [k.sb(f"s{i}", [128, 4], F32) for i in range(2)]
    NT = ntok // 128
    recs = [Rec(k), Rec(k)]

    def ld(i):
        recs[i % 2].dma("sp", xs[i % 2][:, :], x[i * 128:(i + 1) * 128, :])

    ld(0)
    ld(1)
    for i in range(NT):
        kr = recs[i % 2]
        xt, yt, tt_, st = xs[i % 2], ys[i % 2], tmps[i % 2], sts[i % 2]
        layer_norm_tile(kr, xt[:, :], yt[:, :], g_b[:, :], b_b[:, :], tt_[:, :], st[:, :], eng_g="dve")
        if i + 2 < NT:
            ld(i + 2)
        kr.dma("sp", G["h_cur"][i * 128:(i + 1) * 128, :], yt[:, :])
        emit_publish_tile(kr, banks, C["ident"], yt, hTsb, i)
    replay_merged(k, recs[0].segs[0], recs[1].segs[0])
    emit_allgather_h(k, hTsb, G)
    k.pop()


GROUPS = [[0, 1, 2, 3], [4, 5, 6, 7]]

def emit_mla(k0, banks, C, W, G):
    T = S
    NBLK = T // 512
    wlat_d, gq_d, gkv_d, wuq_d, wuk_d, wuv_d = W["w_lat"], W["g_q"], W["g_kv"], W["w_uq"], W["w_uk"], W["w_uv"]
    pos_d = G["pos"]
    frq, sgn, esel, tri = C["frq"], C["sgn"], C["esel"], C["tri"]
    k = k0
    k.push()

    wlat = k.sb("wlat", [128, 8, 448], BF16)
    for kc in range(8):
        k.dma("pool", wlat[:, kc, :], wlat_d[kc * 128:(kc + 1) * 128, :])
    gq = k.sb("gq", [128, 2], F32)
    gkv = k.sb("gkv", [128, 1], F32)
    k.dma("sp", gq[:, :], gq_d[:, :])
    k.dma("sp", gkv[:, :], gkv_d[:, :])
    wtmp = k.sb("wtmp", [128, 2, 256], F32)
    wuq = k.sb("wuq", [128, 2, 256], BF16)
    for c in range(2):
        k.dma("sp", wtmp[:, c, :], wuq_d[c * 128:(c + 1) * 128, :])
    for c in range(2):
        k.ts("dve", wuq[:, c, :], wtmp[:, c, :], gq[:, c:c + 1], QK_SCALE, ALU.mult, ALU.mult)
    wtmp2 = k.sb("wtmp2", [128, 2, 128], F32)
    wuk = k.sb("wuk", [128, 128], BF16)
    wuv = k.sb("wuv", [128, 128], BF16)
    k.dma("sp", wtmp2[:, 0, :], wuk_d[:, :])
    k.dma("sp", wtmp2[:, 1, :], wuv_d[:, :])
    k.ts("dve", wuk[:, :], wtmp2[:, 0, :], gkv[:, 0:1], None, ALU.mult)
    k.ts("dve", wuv[:, :], wtmp2[:, 1, :], gkv[:, 0:1], None, ALU.mult)
    ones = k.sb("ones", [128, 128], F32)
    k.memset("dve", ones[:, :], 1.0)

    kT = [k.sb(f"kT{h}", [96, T], BF16) for h in range(2)]
    qT = [k.sb(f"qT{h}", [96, T], BF16) for h in range(2)]
    Vp = k.sb("Vp", [128, 2, T // 128, 128], BF16)
    kTv = [[V(kT[h].h[:, b * 512:(b + 1) * 512], Res(f"kT{h}_{b}")) for b in range(NBLK)] for h in range(2)]
    qTv = [[V(qT[h].h[:, b * 512:(b + 1) * 512], Res(f"qT{h}_{b}")) for b in range(NBLK)] for h in range(2)]
    Vpv = [V(Vp.h[:, :, b * 4:(b + 1) * 4, :], Res(f"Vp_{b}")) for b in range(NBLK)]
    for b in range(NBLK):
        k.memset("pool", Vpv[b], 1.0)
    mx = k.sb("mx", [128, 4], F32)
    k.memset("dve", mx[:, :], 0.0)
    negcb = [k.sb(f"negc{b}", [128, 2], F32) for b in range(NBLK)]

    hTb = [k.sb(f"hTb{i}", [128, 8, 512], BF16) for i in range(2)]
    posi = [k.sb(f"posi{i}", [128, 512], I32) for i in range(2)]
    ang = k.sb("ang", [128, 1024], F32)
    ni = k.sb("ni", [128, 1024], I32)
    nf = k.sb("nf", [128, 1024], F32)
    scs = [k.sb(f"scs{i}", [128, 1024], F32) for i in range(2)]
    cq_sb = [k.sb(f"cq_sb{i}", [128, 512], F32) for i in range(2)]
    sq_sb = [k.sb(f"sq_sb{i}", [128, 512], F32) for i in range(2)]
    ckv_sb = k.sb("ckv_sb", [128, 512], F32)
    sqkv = k.sb("sqkv", [128, 512], F32)
    rq = k.sb("rq", [128, 512], F32)
    rkv = k.sb("rkv", [128, 512], F32)
    cqn = [k.sb(f"cqn{i}", [128, 512], BF16) for i in range(2)]
    ckvn = k.sb("ckvn", [128, 512], BF16)
    t1 = k.sb("t1", [128, 512], F32)
    t2 = k.sb("t2", [128, 512], F32)
    nsq = k.sb("nsq", [96, 512], F32)
    mtmp = k.sb("mtmp", [128, 1], F32)
    osb = [k.sb(f"osb{i}", [128, 512], F32) for i in range(2)]
    ores = [k.sb(f"ores{i}", [64, 512], BF16) for i in range(2)]
    pT = [k.sb(f"pTx{i}", [128, 512], BF16) for i in range(7)]

    def load(blk):
        if blk < NBLK:
            col = slice(blk * 512, (blk + 1) * 512)
            for kc in range(8):
                k0.dma("sp", hTb[blk % 2][:, kc, :], G["hT_blk"](blk, kc))
            k0.dma("sp", posi[blk % 2][:, :], pos_d.v(pos_d.h[:, col].partition_broadcast(128)))

    rp = Rec(k0)
    k = rp
    nb = BankRR(banks[6:8])
    r6 = slice(64, 96)

    def rope_rows(dsts, bA, bB, cst, snt):
        k.stt("dve", t1[r6, :], bB[r6, :], sgn[r6, 0:1], snt, ALU.mult, ALU.mult)
        k.tt("dve", t2[r6, :], bA[r6, :], cst, ALU.mult)
        for d_ in dsts:
            k.tt("pool", d_[r6, :], t1[r6, :], t2[r6, :], ALU.add)

    def normsq(src, slot, running):
        k.act(nsq[:, :], src[0:96, :], AF.Square)
        bk = nb()
        k.mm(bk[:, :], ones[0:96, :], nsq[:, :])
        k.reduce("dve", mtmp[:, :], bk[:, :], ALU.max)
        if running:
            k.tt("dve", mx[:, slot:slot + 1], mx[:, slot:slot + 1], mtmp[:, :], ALU.max)
        else:
            k.copy("dve", mx[:, slot:slot + 1], mtmp[:, :])

    for blk in range(NBLK):
        k.mark()
        hb = hTb[blk % 2]
        pi_ = posi[blk % 2]
        sc_ = scs[blk % 2]
        snt, cst = sc_[r6, 0:512], sc_[r6, 512:1024]
        k.copy("dve", ang[r6, 0:512], pi_[r6, :])
        k.ts("dve", ang[r6, 0:512], ang[r6, 0:512], frq[r6, 0:1], None, ALU.mult)
        k.ts("dve", ang[r6, 512:1024], ang[r6, 0:512], float(np.pi / 2), None, ALU.add)
        k.ts("dve", nf[r6, :], ang[r6, :], 1.0 / TWO_PI, None, ALU.mult)
        k.copy("dve", ni[r6, :], nf[r6, :])
        k.copy("dve", nf[r6, :], ni[r6, :])
        k.stt("dve", nf[r6, :], nf[r6, :], -TWO_PI, ang[r6, :], ALU.mult, ALU.add)
        k.ts("dve", nf[r6, :], nf[r6, :], 3.1415925, -3.1415925, ALU.min, ALU.max)
        k.act(sc_[r6, :], nf[r6, :], AF.Sin)
        for c in range(2):
            bk = nb()
            for kc in range(8):
                k.mm(bk[:, :], wlat[:, kc, c * 128:(c + 1) * 128], hb[:, kc, :], start=(kc == 0), stop=(kc == 7))
            k.copy("act", cq_sb[c][:, :], bk[:, :])
            k.act(sq_sb[c][:, :], bk[:, :], AF.Square)
        bk = nb()
        for kc in range(8):
            k.mm(bk[:, :], wlat[:, kc, 256:384], hb[:, kc, :], start=(kc == 0), stop=(kc == 7))
        k.copy("act", ckv_sb[:, :], bk[:, :])
        k.act(sqkv[:, :], bk[:, :], AF.Square)
        bA = nb()
        for kc in range(8):
            k.mm(bA[0:96, :], wlat[:, kc, 320:416], hb[:, kc, :], start=(kc == 0), stop=(kc == 7))
        bB = nb()
        for kc in range(8):
            k.mm(bB[0:96, :], wlat[:, kc, 352:448], hb[:, kc, :], start=(kc == 0), stop=(kc == 7))
        rope_rows([kTv[0][blk], kTv[1][blk]], bA, bB, cst, snt)
        bk = nb()
        k.mm(bk[:, :], ones[:, :], sq_sb[0][:, :], start=True, stop=False)
        k.mm(bk[:, :], ones[:, :], sq_sb[1][:, :], start=False, stop=True)
        k.rstd_ln(rq[:, :], bk[:, :], 1.0 / 256, EPS)
        bk = nb()
        k.mm(bk[:, :], ones[:, :], sqkv[:, :])
        k.rstd_ln(rkv[:, :], bk[:, :], 1.0 / 128, EPS)
        for c in range(2):
            k.tt("dve", cqn[c][:, :], cq_sb[c][:, :], rq[:, :], ALU.mult)
        k.tt("pool", ckvn[:, :], ckv_sb[:, :], rkv[:, :], ALU.mult)
        for hd in range(2):
            bk = nb()
            k.mm(bk[0:64, :], wuk[:, hd * 64:(hd + 1) * 64], ckvn[:, :])
            k.copy("act", kTv[hd][blk][0:64, :], bk[0:64, :])
        bk = nb()
        for tt_ in range(4):
            k.mm(bk[:, tt_ * 128:(tt_ + 1) * 128], ckvn[:, tt_ * 128:(tt_ + 1) * 128], wuv[:, :],
                 start=True, stop=True, inc=(tt_ == 3))
        k.copy("act", Vpv[blk][:, :, :, 0:64],
               bk[:, :].f(lambda a: a.rearrange("p (t h d) -> p h t d", t=4, h=2)))
        for hd in range(2):
            bA = nb()
            for c in range(2):
                k.mm(bA[0:96, :], wuq[:, c, hd * 128:hd * 128 + 96], cqn[c][:, :], start=(c == 0), stop=(c == 1))
            bB = nb()
            for c in range(2):
                k.mm(bB[0:96, :], wuq[:, c, hd * 128 + 32:hd * 128 + 128], cqn[c][:, :], start=(c == 0), stop=(c == 1))
            k.copy("act", qTv[hd][blk][0:64, :], bA[0:64, :])
            rope_rows([qTv[hd][blk]], bA, bB, cst, snt)
        for hd in range(2):
            normsq(qTv[hd][blk], hd, False)
            normsq(kTv[hd][blk], 2 + hd, True)
        ng = negcb[blk]
        k.tt("dve", ng[:, :], mx[:, 0:2], mx[:, 2:4], ALU.mult)
        k.act(ng[:, :], ng[:, :], AF.Ln)
        k.act(ng[:, :], ng[:, :], AF.Exp, scale=0.5)
        k.ts("dve", ng[:, :], ng[:, :], -1.0, None, ALU.mult)

    ra = Rec(k0)
    k = ra
    s_banks = BankRR(banks[2:5])
    den_bank = banks[5]
    blocks = [(hd, qi, kb) for qi in range(NBLK) for hd in range(2) for kb in range(4 * qi + 4)]
    LOOKAHEAD = 4

    def stage1(i):
        hd, qi, kb = blocks[i]
        r = kb - 4 * qi
        c0 = 128 * r if r > 0 else 0
        sb_ = s_banks()
        pt = pT[i % 7]
        kblk, ko = kb // 4, (kb % 4) * 128
        k.mm(sb_[:, c0:512], kTv[hd][kblk][:, ko:ko + 128], qTv[hd][qi][:, c0:512])
        k.act(pt[:, c0:512], sb_[:, c0:512], AF.Exp, bias=negcb[qi][:, hd:hd + 1], scale=1.0)
        if r >= 0:
            k.tt("pool", pt[:, c0:c0 + 128], pt[:, c0:c0 + 128], tri[:, :], ALU.mult)
        return pt, c0

    def stage2(i, pt, c0):
        hd, qi, kb = blocks[i]
        nkb = 4 * qi + 4
        g = qi * 2 + hd
        oacc = banks[g % 2]
        k.mm(oacc[:, c0:512], Vpv[kb // 4][:, hd, kb % 4, :], pt[:, c0:512], start=(kb == 0), stop=(kb == nkb - 1))
        if kb == nkb - 1:
            ob = osb[g % 2]
            orr = ores[g % 2]
            k.copy("act", ob[:, :], oacc[:, :])
            k.op("dve", lambda e, ob=ob: e.reciprocal(ob[64:128, :].ap, ob[64:128, :].ap), [ob], [ob])
            k.mm(den_bank[0:64, :], esel[:, :], ob[:, :])
            k.tt("dve", orr[:, :], ob[0:64, :], den_bank[0:64, :], ALU.mult)
            k.dma("sp", G["osrc"][0][qi // 4][hd * 64:(hd + 1) * 64, (qi % 4) * 512:(qi % 4 + 1) * 512], orr[:, :])

    pend = []
    cur_qi = -1
    for i in range(len(blocks)):
        if blocks[i][1] != cur_qi:
            cur_qi = blocks[i][1]
            k.mark()
        pend.append((i,) + stage1(i))
        if len(pend) > LOOKAHEAD:
            stage2(*pend.pop(0))
    while pend:
        stage2(*pend.pop(0))

    k = k0
    assert len(rp.segs) == NBLK + 1 and len(ra.segs) == NBLK + 1 and not rp.segs[0] and not ra.segs[0]
    load(0)
    load(1)
    replay_merged(k, rp.segs[1])
    for qi in range(NBLK):
        load(qi + 2)
        replay_merged(k, ra.segs[1 + qi], rp.segs[2 + qi] if qi + 1 < NBLK else [])
    k.pop()


def emit_hg(k, banks, C, W, G, layer, hTb=None):
    T = S
    NBLK = T // 512
    w_d, lbr_d, lbc_d, on_d = W["w_hg"], G["lb_rows"], G["lb_cols"], W["hg_o_norm"]
    cU, cUrel, cW, cones, mbd, rowm, ident = C["cU"], C["cUrel"], C["cW"], C["cones"], C["maskbd"], C["rowmask"], C["ident"]
    nb = BankRR(banks)
    k.push()

    whg = k.sb("whg", [128, 8, 512], BF16)
    for kc in range(8):
        k.dma("pool", whg[:, kc, :], w_d[kc * 128:(kc + 1) * 128, :])
    gb = k.sb("gb", [128, 128], F32)
    k.dma("sp", gb[:, :], bcast_row(on_d))
    oTb = [k.sb(f"oTb{i}", [128, 512], BF16) for i in range(2)]

    def lower_bound(x, n, name):
        m = k.sb(name + "_m", [128, n], F32)
        e = k.sb(name + "_e", [128, DEPTH, n], F32)
        ssum = k.sb(name + "_s", [128, n], F32)
        lb = k.sb(name + "_lb", [128, n], F32)
        oml = k.sb(name + "_oml", [128, n], F32)
        k.copy("dve", m[:, :], x[:, 0, :])
        for i in range(1, DEPTH):
            k.tt("dve", m[:, :], m[:, :], x[:, i, :], ALU.max)
        for i in range(DEPTH):
            k.tt("dve", e[:, i, :], x[:, i, :], m[:, :], ALU.subtract)
        k.act(e[:, :, :], e[:, :, :], AF.Exp)
        k.copy("dve", ssum[:, :], e[:, 0, :])
        for i in range(1, DEPTH):
            k.tt("dve", ssum[:, :], ssum[:, :], e[:, i, :], ALU.add)
        k.op("dve", lambda en: en.reciprocal(ssum[:, :].ap, ssum[:, :].ap), [ssum], [ssum])
        for i in range(DEPTH):
            k.tt("dve", e[:, i, :], e[:, i, :], ssum[:, :], ALU.mult)
        k.copy("dve", lb[:, :], e[:, 0, :])
        for i in range(1, layer + 1):
            k.tt("dve", lb[:, :], lb[:, :], e[:, i, :], ALU.add)
        k.tt("dve", lb[:, :], lb[:, :], e[:, 0, :], ALU.subtract)
        k.ts("dve", oml[:, :], lb[:, :], -1.0, 1.0, ALU.mult, ALU.add)
        return lb, oml

    xr = k.sb("xr", [128, DEPTH, 128], F32)
    for i in range(DEPTH):
        k.dma("sp", xr[:, i, :], lbr_d.v(lbr_d.h[i:i + 1, :].partition_broadcast(128)))
    lb_b, oml_b = lower_bound(xr, 128, "lbr")
    xc = k.sb("xc", [128, DEPTH, 1], F32)
    k.dma("sp", xc[:, :, 0], lbc_d[:, :])
    lb_c, oml_c = lower_bound(xc, 1, "lbc")

    NS = 8
    Sf = [k.sb(f"Sf{i}", [128, 128], F32) for i in range(2)]
    Sb = [k.sb(f"Sb{i}", [128, 128], BF16) for i in range(NS)]
    k.memset("dve", Sf[0][:, :], 0.0)
    k.memset("dve", Sb[0][:, :], 0.0)
    Z = [k.sb(f"Z{i}", [128, 4, 128], BF16) for i in range(2)]
    for z in Z:
        k.memset("pool", z[:, :, :], 0.0)
    si = 0

    shared = hTb is not None
    if not shared:
        hTb = [k.sb(f"hTb{i}", [128, 8, 512], BF16) for i in range(2)]
    qTs = [k.sb(f"qTs{i}", [128, 512], F32) for i in range(2)]
    kTs = [k.sb(f"kTs{i}", [128, 512], F32) for i in range(2)]

    def dbl(name, shape, dt, n=2):
        return [k.sb(f"{name}{i}", shape, dt) for i in range(n)]

    sgx = dbl("sgx", [128, 384], F32)
    sg = [s_[:, 0:128] for s_ in sgx]
    uu = dbl("uu", [128, 128], F32)
    ff = dbl("ff", [128, 128], F32)
    ktm = dbl("ktm", [128, 128], F32)
    logf = dbl("logf", [128, 128], F32)
    vbf = dbl("vbf", [128, 128], BF16)
    sgate = dbl("sgate", [128, 128], F32)
    e1 = dbl("e1", [128, 128], F32)
    e2 = dbl("e2", [128, 128], F32)
    e3 = dbl("e3", [128, 128], F32)
    e4 = dbl("e4", [128, 128], F32)
    dl = dbl("dl", [128, 4], F32)
    qpT = dbl("qpT", [128, 128], BF16)
    kpT = dbl("kpT", [128, 128], BF16)
    kdp = dbl("kdp", [128, 4, 128], BF16)
    attm = dbl("attm", [128, 128], BF16)
    osq = dbl("osq", [128, 128], F32)
    ost = dbl("ost", [128, 2], F32)
    y1 = dbl("y1", [128, 128], F32)
    y2 = dbl("y2", [128, 128], F32)

    nt = 0
    for blk in range(NBLK):
        col = slice(blk * 512, (blk + 1) * 512)
        hb = hTb[blk % len(hTb)]
        if shared:
            k.mark()
        else:
            for kc in range(8):
                k.dma("sp", hb[:, kc, :], G["hT_blk"](blk, kc))
        qT_s, kT_s = qTs[blk % 2], kTs[blk % 2]
        bq = nb()
        for kc in range(8):
            k.mm(bq[:, :], whg[:, kc, 0:128], hb[:, kc, :], start=(kc == 0), stop=(kc == 7))
        k.act(qT_s[:, :], bq[:, :], AF.Exp, scale=-1.0)
        k.act(qT_s[:, :], qT_s[:, :], AF.Ln, bias=1.0, scale=1.0)
        k.act(qT_s[:, :], qT_s[:, :], AF.Exp, scale=-1.0)
        k.tt("dve", qT_s[:, :], bq[:, :], qT_s[:, :], ALU.mult)
        bz = nb()
        for kc in range(8):
            k.mm(bz[:, :], whg[:, kc, 128:256], hb[:, kc, :], start=(kc == 0), stop=(kc == 7))
        k.act(kT_s[:, :], bz[:, :], AF.Exp)
        k.act(kT_s[:, :], kT_s[:, :], AF.Ln, bias=1.0, scale=1.0)
        k.act(kT_s[:, :], kT_s[:, :], AF.Exp, scale=-1.0)
        k.ts("dve", kT_s[:, :], kT_s[:, :], oml_c[:, 0:1], None, ALU.mult)
        for tt_ in range(4):
            p = nt % 2
            nt += 1
            tcol = slice(tt_ * 128, (tt_ + 1) * 128)
            tok = slice(blk * 512 + tt_ * 128, blk * 512 + (tt_ + 1) * 128)
            btm = nb()
            for kc in range(8):
                k.mm(btm[:, 0:384], hb[:, kc, tcol], whg[:, kc, 128:512], start=(kc == 0), stop=(kc == 7))
            k.act(sgx[p][:, :], btm[:, 0:384], AF.Exp, scale=-1.0)
            k.copy("act", vbf[p][:, :], btm[:, 128:256])
            k.act(sgx[p][:, :], sgx[p][:, :], AF.Ln, bias=1.0, scale=1.0)
            k.act(sgx[p][:, :], sgx[p][:, :], AF.Exp, scale=-1.0)
            k.tt("dve", sgate[p][:, :], btm[:, 256:384], sgx[p][:, 256:384], ALU.mult)
            k.tt("dve", uu[p][:, :], sg[p][:, :], oml_b[:, :], ALU.mult)
            k.tt("pool", ff[p][:, :], uu[p][:, :], lb_b[:, :], ALU.add)
            k.tt("pool", ktm[p][:, :], oml_b[:, :], uu[p][:, :], ALU.subtract)
            k.ts("dve", ff[p][:, :], ff[p][:, :], 1e-30, None, ALU.max)
            k.act(logf[p][:, :], ff[p][:, :], AF.Ln)
            bc = nb()
            k.mm(bc[:, 0:128], logf[p][:, :], cU[:, :], inc=False)
            k.mm(bc[:, 128:256], logf[p][:, :], cUrel[:, :], inc=False)
            k.mm(bc[:, 256:384], cW[:, :], logf[p][:, :], inc=False)
            k.mm(bc[:, 384:388], logf[p][:, :], cones[:, :], inc=True)
            k.act(e1[p][:, :], bc[:, 0:128], AF.Exp)
            k.act(e2[p][:, :], bc[:, 128:256], AF.Exp)
            k.act(e3[p][:, :], bc[:, 128:256], AF.Exp, scale=-1.0)
            k.act(e4[p][:, :], bc[:, 256:384], AF.Exp)
            k.act(dl[p][:, :], bc[:, 384:388], AF.Exp)
            z = Z[p]
            zdiag = z.v(bass.AP(z.h, 0, [[512, 128], [160, 4], [1, 32]]))
            k.tt("dve", zdiag, qT_s[:, tcol].f(lambda a: a.rearrange("p (c x) -> p c x", c=4)),
                 e1[p][:, :].f(lambda a: a.rearrange("p (c x) -> p c x", c=4)), ALU.mult)
            k.tt("pool", qpT[p][:, :], qT_s[:, tcol], e2[p][:, :], ALU.mult)
            k.tt("pool", kpT[p][:, :], kT_s[:, tcol], e3[p][:, :], ALU.mult)
            for c in range(4):
                k.stt("dve" if c % 2 == 0 else "pool", kdp[p][:, c, :], ktm[p][:, :], rowm[:, c:c + 1], e4[p][:, :],
                      ALU.mult, ALU.mult) if c % 2 == 0 else None
            for c in range(4):
                if c % 2 == 1:
                    k.stt("dve", kdp[p][:, c, :], ktm[p][:, :], rowm[:, c:c + 1], e4[p][:, :], ALU.mult, ALU.mult)
            ba = nb()
            k.mm(ba[:, 0:128], kpT[p][:, :], qpT[p][:, :])
            k.tt("dve", attm[p][:, :], ba[:, 0:128], mbd[:, :], ALU.mult)
            bs = nb()
            for c in range(4):
                k.mm(bs[:, c * 128:(c + 1) * 128], kdp[p][:, c, :], vbf[p][:, :], inc=(c == 3))
            bo = nb()
            for c in range(4):
                k.mm(bo[:, 0:128], z[:, c, :], Sb[(si + c) % NS][:, :], start=(c == 0), stop=False, inc=False)
                s_old = Sf[(si + c) % 2]
                s_new = Sf[(si + c + 1) % 2]
                k.stt("dve", s_new[:, :], s_old[:, :], dl[p][:, c:c + 1], bs[:, c * 128:(c + 1) * 128],
                      ALU.mult, ALU.add)
                k.copy("pool", Sb[(si + c + 1) % NS][:, :], s_new[:, :])
            k.mm(bo[:, 0:128], attm[p][:, :], vbf[p][:, :], start=False, stop=True, inc=True)
            si += 4
            k.act(osq[p][:, :], bo[:, 0:128], AF.Square, accum_out=ost[p][:, 0:1])
            k.rstd_ln(ost[p][:, 1:2], ost[p][:, 0:1], 1.0 / 128, EPS)
            k.stt("dve", y1[p][:, :], bo[:, 0:128], ost[p][:, 1:2], gb[:, :], ALU.mult, ALU.mult)
            k.tt("pool", y2[p][:, :], y1[p][:, :], sgate[p][:, :], ALU.mult)
            bt = nb()
            k.transpose(bt[:, 0:128], y2[p][:, :], ident[:, :])
            k.copy("act", oTb[blk % 2][:, tcol], bt[:, 0:128])
        k.dma("sp", G["osrc"][2][blk // 4][:, (blk % 4) * 512:(blk % 4 + 1) * 512], oTb[blk % 2][:, :])
    k.pop()


def emit_dn(k, banks, C, W, G, hTb=None):
    T = S
    NBLK = T // 512
    w_d, cw_d, alog_d, dtb_d, on_d = W["w_dn"], W["conv_w"], W["a_log"], W["dt_bias"], W["dn_o_norm"]
    uinc, lpos, uneg, ident = C["uinc"], C["lpos_s"], C["uneg"], C["ident"]
    if hTb is not None:
        nb = BankRR(banks[:-DN_BACK_BANKS])
        nbb = BankRR(banks[-DN_BACK_BANKS:])
    else:
        nb = nbb = BankRR(banks)
    k.push()

    wdn = k.sb("wdn", [128, 8, 514], BF16)
    for kc in range(8):
        k.dma("pool", wdn[:, kc, :], w_d[kc * 128:(kc + 1) * 128, :])
    cw = k.sb("cw", [128, 3, 4], F32)
    k.dma("sp", cw[:, :, :], cw_d[:, :, :])
    gb = k.sb("gb", [128, 128], F32)
    k.dma("sp", gb[:, :], bcast_row(on_d))
    oTb = [k.sb(f"oTb{i}", [128, 512], BF16) for i in range(2)]
    sc = k.sb("sc", [128, 4], F32)
    k.dma("sp", sc[:, 0:1], bcast_row(alog_d))
    k.dma("sp", sc[:, 1:2], bcast_row(dtb_d))
    k.act(sc[:, 2:3], sc[:, 0:1], AF.Exp)
    k.ts("dve", sc[:, 2:3], sc[:, 2:3], -1.0, None, ALU.mult)
    ones = k.sb("ones", [128, 128], F32)
    k.memset("dve", ones[:, :], 1.0)
    ey = k.sb("ey", [128, 512], F32)

    Sf = [k.sb(f"S{i}", [128, 128], F32) for i in range(2)]
    k.memset("dve", Sf[0][:, :], 0.0)
    si = 0

    shared = hTb is not None
    if not shared:
        hTb = [k.sb(f"hTb{i}", [128, 8, 512], BF16) for i in range(2)]
    xh = [[k.sb(f"xh{w}_{i}", [128, 515], F32) for i in range(2)] for w in range(3)]
    for w in range(3):
        k.memset("dve", xh[w][1][:, 512:515], 0.0)
    cv = [k.sb(f"cv{w}", [128, 512], F32) for w in range(3)]
    sq = [k.sb(f"sq{w}", [128, 512], F32) for w in range(2)]
    rs = [k.sb(f"rs{w}", [128, 512], F32) for w in range(2)]

    def per_tile(name, shape, dt=F32):
        return [k.sb(f"{name}{i}", shape, dt) for i in range(4)]

    def per_tile2(name, shape, dt=F32):
        return [k.sb(f"{name}{i}", shape, dt) for i in range(8 if shared else 4)]

    tmcA = per_tile2("tmc", [128, 8])
    sgateA = per_tile2("sgate", [128, 128])
    r130 = per_tile("r130", [128, 130])
    ktm = per_tile("ktm", [128, 128])
    vb = per_tile("vb", [128, 128])
    gbc = per_tile("gbc", [128, 128])
    xm = per_tile("xm", [128, 128])
    ym = per_tile("ym", [128, 128])
    dec_s = per_tile("dec_s", [128, 128])
    decT = per_tile("decT", [128, 128])
    egb = per_tile("egb", [128, 128])
    Mt = per_tile("M", [128, 128])
    Nt = per_tile("N", [128, 128])
    Pa = per_tile("Pa", [128, 128])
    Pat = per_tile("Pat", [128, 128])
    Pb = per_tile("Pb", [128, 128])
    Pbt = per_tile("Pbt", [128, 128])
    Rr = per_tile("R", [128, 128])
    Rt = per_tile("Rt", [128, 128])
    qkTA = per_tile2("qkT", [128, 128])
    qdTA = per_tile2("qdT", [128, 128])
    kbg = per_tile("kbg", [128, 128])
    kdecA = per_tile2("kdec", [128, 128])
    u_sbA = per_tile2("u", [128, 128])
    wT_sbA = per_tile2("wT", [128, 128])
    vnew = per_tile("vnew", [128, 128])
    osq = per_tile("osq", [128, 128])
    ost = per_tile("ost", [128, 2])
    y1 = per_tile("y1", [128, 128])
    y2 = per_tile("y2", [128, 128])

    for blk in range(NBLK):
        col = slice(blk * 512, (blk + 1) * 512)
        hb = hTb[blk % len(hTb)]
        if shared:
            k.mark()
        else:
            for kc in range(8):
                k.dma("sp", hb[:, kc, :], G["hT_blk"](blk, kc))
        pb = (blk % 2) * 4 if shared else 0
        tmc, sgate, qkT, qdT, kdec, u_sb, wT_sb = (x[pb:pb + 4] for x in (tmcA, sgateA, qkTA, qdTA, kdecA, u_sbA, wT_sbA))
        for w in range(3):
            bk = nb()
            for kc in range(8):
                k.mm(bk[:, :], wdn[:, kc, w * 128:(w + 1) * 128], hb[:, kc, :], start=(kc == 0), stop=(kc == 7))
            cur, prv = xh[w][blk % 2], xh[w][(blk + 1) % 2]
            k.copy("act", cur[:, 3:515], bk[:, :])
            k.copy("act", cur[:, 0:3], prv[:, 512:515])
            y = cv[w]
            k.ts("dve", y[:, :], cur[:, 0:512], cw[:, w, 0:1], None, ALU.mult)
            for m in range(1, 4):
                k.stt("dve" if m != 2 else "dve", y[:, :], cur[:, m:m + 512], cw[:, w, m:m + 1], y[:, :], ALU.mult, ALU.add)
            k.act(ey[:, :], y[:, :], AF.Exp, scale=-1.0)
            k.act(ey[:, :], ey[:, :], AF.Ln, bias=1.0, scale=1.0)
            k.act(ey[:, :], ey[:, :], AF.Exp, scale=-1.0)
            k.tt("dve", y[:, :], y[:, :], ey[:, :], ALU.mult)
        for w in range(2):
            k.act(sq[w][:, :], cv[w][:, :], AF.Square)
            bk = nb()
            k.mm(bk[:, :], ones[:, :], sq[w][:, :])
            k.rstd_ln(rs[w][:, :], bk[:, :], 1.0, EPS)
            if w == 0:
                k.stt("pool", cv[w][:, :], cv[w][:, :], 1.0, rs[w][:, :], ALU.mult, ALU.mult) if False else None
        k.tt("dve", cv[0][:, :], cv[0][:, :], rs[0][:, :], ALU.mult)
        k.tt("dve", cv[1][:, :], cv[1][:, :], rs[1][:, :], ALU.mult)
        k.act(cv[0][:, :], cv[0][:, :], AF.Copy, scale=float(128 ** -0.5))
        qT_, kT_, vT_ = cv

        for t in range(4):
            tc_ = slice(t * 128, (t + 1) * 128)
            c = tmc[t]
            bk = nb()
            for kc in range(8):
                k.mm(bk[:, 0:130], hb[:, kc, tc_], wdn[:, kc, 384:514], start=(kc == 0), stop=(kc == 7))
            k.act(r130[t][:, :], bk[:, 0:130], AF.Exp, scale=-1.0)
            k.act(c[:, 1:2], bk[:, 1:2], AF.Exp, bias=sc[:, 1:2], scale=1.0)
            k.act(r130[t][:, :], r130[t][:, :], AF.Ln, bias=1.0, scale=1.0)
            k.act(r130[t][:, :], r130[t][:, :], AF.Exp, scale=-1.0)
            k.copy("dve", c[:, 0:1], r130[t][:, 0:1])
            k.tt("dve", sgate[t][:, :], bk[:, 2:130], r130[t][:, 2:130], ALU.mult)
            k.act(c[:, 1:2], c[:, 1:2], AF.Ln, bias=1.0, scale=1.0)
            k.tt("dve", c[:, 2:3], c[:, 1:2], sc[:, 2:3], ALU.mult)
            bk = nb()
            k.transpose(bk[:, 0:128], kT_[:, tc_], ident[:, :], inc=False)
            k.transpose(bk[:, 128:256], vT_[:, tc_], ident[:, :], inc=True)
            k.copy("act", ktm[t][:, :], bk[:, 0:128])
            k.act(vb[t][:, :], bk[:, 128:256], AF.Copy, scale=c[:, 0:1])
            k.ts("dve", gbc[t][:, :], ones[:, :], c[:, 2:3], None, ALU.mult)
            bk = nb()
            k.mm(bk[:, 0:128], gbc[t][:, :], uinc[:, :], inc=False)
            k.mm(bk[:, 128:129], uinc[:, :], c[:, 2:3], inc=True)
            k.copy("dve", c[:, 3:4], bk[:, 128:129])
            k.copy("dve", c[:, 7:8], bk[:, 127:128])
            k.stt("dve", xm[t][:, :], bk[:, 0:128], c[:, 3:4], lpos[:, :], ALU.subtract, ALU.max)
            k.stt("dve", ym[t][:, :], bk[:, 0:128], c[:, 3:4], uneg[:, :], ALU.subtract, ALU.min)
            k.act(egb[t][:, :], bk[:, 0:128], AF.Exp)
            k.act(dec_s[t][:, :], xm[t][:, :], AF.Exp, scale=-1.0)
            k.act(decT[t][:, :], ym[t][:, :], AF.Exp)
            k.act(c[:, 4:5], c[:, 3:4], AF.Exp)
            k.tt("dve", c[:, 4:5], c[:, 4:5], c[:, 0:1], ALU.mult)
            k.act(c[:, 5:6], c[:, 3:4], AF.Exp, bias=c[:, 7:8], scale=-1.0)
            k.act(c[:, 6:7], c[:, 7:8], AF.Exp)
            k.ts("pool", kbg[t][:, :], ktm[t][:, :], c[:, 4:5], None, ALU.mult) if False else None
            k.ts("dve", kbg[t][:, :], ktm[t][:, :], c[:, 4:5], None, ALU.mult)
            k.ts("dve", kdec[t][:, :], ktm[t][:, :], c[:, 5:6], None, ALU.mult)
            k.tt("pool", qdT[t][:, :], qT_[:, tc_], egb[t][:, :], ALU.mult)
            bk = nb()
            k.mm(bk[:, 0:128], kT_[:, tc_], kT_[:, tc_], inc=False)
            k.mm(bk[:, 128:256], kT_[:, tc_], qT_[:, tc_], inc=True)
            k.stt("dve", Mt[t][:, :], bk[:, 0:128], c[:, 0:1], dec_s[t][:, :], ALU.mult, ALU.mult)
            k.tt("dve", qkT[t][:, :], bk[:, 128:256], decT[t][:, :], ALU.mult)
            bk = nb()
            k.transpose(bk[:, 0:128], Mt[t][:, :], ident[:, :])
            k.copy("act", Nt[t][:, :], bk[:, 0:128])
            k.tt("pool", Rr[t][:, :], ident[:, :], Nt[t][:, :], ALU.subtract)
            k.tt("pool", Rt[t][:, :], ident[:, :], Mt[t][:, :], ALU.subtract)

        P = [Nt[t] for t in range(4)]
        Ptr = [Mt[t] for t in range(4)]
        for lvl in range(6):
            last = lvl == 5
            newP = Pa if lvl % 2 == 0 else Pb
            newPt = Pat if lvl % 2 == 0 else Pbt
            for t in range(4):
                bk = nb()
                k.mm(bk[:, 0:128], Ptr[t][:, :], P[t][:, :], inc=last)
                if not last:
                    k.mm(bk[:, 128:256], P[t][:, :], Ptr[t][:, :], inc=True)
                k.copy("act", newP[t][:, :], bk[:, 0:128])
                if not last:
                    k.copy("dve", newPt[t][:, :], bk[:, 128:256])
            for t in range(4):
                bk = nb()
                k.mm(bk[:, 0:128], Rt[t][:, :], newP[t][:, :], inc=last)
                if not last:
                    k.mm(bk[:, 128:256], newP[t][:, :], Rt[t][:, :], inc=True)
                k.tt("dve", Rr[t][:, :], Rr[t][:, :], bk[:, 0:128], ALU.add)
                if not last:
                    k.tt("dve", Rt[t][:, :], Rt[t][:, :], bk[:, 128:256], ALU.add)
            P = [newP[t] for t in range(4)]
            Ptr = [newPt[t] for t in range(4)]

        for t in range(4):
            bk = nb()
            k.mm(bk[:, 0:128], Rr[t][:, :], vb[t][:, :], inc=False)
            k.mm(bk[:, 128:256], kbg[t][:, :], Rr[t][:, :], inc=True)
            k.copy("act", u_sb[t][:, :], bk[:, 0:128])
            k.copy("dve", wT_sb[t][:, :], bk[:, 128:256])

        if shared:
            k.mark()
        for t in range(4):
            tok = slice(blk * 512 + t * 128, blk * 512 + (t + 1) * 128)
            c = tmc[t]
            s_old = Sf[si % 2]
            s_new = Sf[(si + 1) % 2]
            si += 1
            bk = nbb()
            k.mm(bk[:, 0:128], wT_sb[t][:, :], s_old[:, :])
            k.tt("dve", vnew[t][:, :], u_sb[t][:, :], bk[:, 0:128], ALU.subtract)
            bo = nbb()
            k.mm(bo[:, 0:128], qdT[t][:, :], s_old[:, :], start=True, stop=False, inc=False)
            k.mm(bo[:, 0:128], qkT[t][:, :], vnew[t][:, :], start=False, stop=True, inc=True)
            bs = nbb()
            k.mm(bs[:, 0:128], kdec[t][:, :], vnew[t][:, :])
            k.stt("dve", s_new[:, :], s_old[:, :], c[:, 6:7], bs[:, 0:128], ALU.mult, ALU.add)
            k.act(osq[t][:, :], bo[:, 0:128], AF.Square, accum_out=ost[t][:, 0:1])
            k.rstd_ln(ost[t][:, 1:2], ost[t][:, 0:1], 1.0 / 128, EPS)
            k.stt("dve", y1[t][:, :], bo[:, 0:128], ost[t][:, 1:2], gb[:, :], ALU.mult, ALU.mult)
            k.tt("pool", y2[t][:, :], y1[t][:, :], sgate[t][:, :], ALU.mult)
            bt = nbb()
            k.transpose(bt[:, 0:128], y2[t][:, :], ident[:, :])
            k.copy("act", oTb[blk % 2][:, t * 128:(t + 1) * 128], bt[:, 0:128])
        k.dma("sp", G["osrc"][1][blk // 4][:, (blk % 4) * 512:(blk % 4 + 1) * 512], oTb[blk % 2][:, :])
    k.pop()


def emit_stage_c(k, banks, C, W, G, last):
    ntok = S * B // NCORES
    NT = ntok // 128
    NB = ntok // 512
    upto = 9
    h_d = G["h_cur"]
    wg_d, wout_d = W["w_gates"], W["w_out"]
    wbr_d = [W["w_br_a"], W["w_br_b"], W["w_br_c"]]
    ln1g_d, ln1b_d, ln2g_d, ln2b_d = W["ln1_g"], W["ln1_b"], W["ln2_g"], W["ln2_b"]
    wr_d, br_d = W["w_router"], W["b_router"]
    ewg_d, ewu_d, ewd_d = W["exp_w_gate"], W["exp_w_up"], W["exp_w_down"]
    out_d = G["out"]
    ident = C["ident"]
    k.push()

    comb_all = k.sb("comb_all", [128, NT, 32], F32)
    mixT = k.sb("mixT", [128, 8, ntok], BF16)

    k.push()
    wg = k.sb("wg", [128, 8, 3 * D], BF16)
    wbr = k.sb("wbr", [128, 12, D], BF16)
    for kc in range(8):
        k.dma("pool", wg[:, kc, :], wg_d[kc * 128:(kc + 1) * 128, :])
    for br in range(3):
        for kc in range(4):
            k.dma("pool", wbr[:, br * 4 + kc, :], wbr_d[br][kc * 128:(kc + 1) * 128, :])
    hTb = [k.sb(f"hTb{i}", [128, 8, 512], BF16) for i in range(2)]
    oTb = [k.sb(f"oTb{i}", [128, 12, 512], BF16) for i in range(2)]
    sg = [k.sb(f"sg{i}", [128, 512], BF16) for i in range(2)]
    tmx = [k.sb(f"tmx{i}", [128, 512], F32) for i in range(2)]
    mixf = [k.sb(f"mixf{i}", [128, 512], F32) for i in range(2)]
    nb = 0
    for tb in range(NB):
        tsl = slice(tb * 512, (tb + 1) * 512)
        hb, ob = hTb[tb % 2], oTb[tb % 2]
        for kc in range(8):
            k.dma("sp", hb[:, kc, :], G["hsrc"][kc // 2][(kc % 2) * 128:(kc % 2 + 1) * 128, tsl])
        for br in range(3):
            for kc in range(4):
                k.dma("sp", ob[:, br * 4 + kc, :], G["o_own"](br, kc, tsl), extra_reads=G["o_own_res"](br))
        for r in range(8):
            mf = mixf[r % 2]
            for br in range(3):
                bg = banks[nb % 2]
                by = banks[2 + nb % 2]
                sgt = sg[nb % 2]
                tm = tmx[nb % 2]
                nb += 1
                col = br * D + r * 128
                for kc in range(8):
                    k.mm(bg[:, :], wg[:, kc, col:col + 128], hb[:, kc, :], start=(kc == 0), stop=(kc == 7))
                k.act(sgt[:, :], bg[:, :], AF.Sigmoid)
                for kc in range(4):
                    k.mm(by[:, :], wbr[:, br * 4 + kc, r * 128:(r + 1) * 128], ob[:, br * 4 + kc, :],
                         start=(kc == 0), stop=(kc == 3))
                if br == 0:
                    k.tt("dve", mf[:, :], by[:, :], sgt[:, :], ALU.mult)
                elif br == 1:
                    k.tt("dve", tm[:, :], by[:, :], sgt[:, :], ALU.mult)
                    k.tt("pool", mf[:, :], mf[:, :], tm[:, :], ALU.add)
                else:
                    k.tt("dve", tm[:, :], by[:, :], sgt[:, :], ALU.mult)
                    k.tt("pool", mixT[:, r, tsl], mf[:, :], tm[:, :], ALU.add)
    k.pop()

    acc = [k.sb(f"acc{i}", [128, D], F32) for i in range(NT)]
    h1T = k.sb("h1T", [128, 8, ntok], BF16)
    k.push()
    wout = k.sb("wout", [128, 8, D], BF16)
    for kc in range(8):
        k.dma("pool", wout[:, kc, :], wout_d[kc * 128:(kc + 1) * 128, :])
    wr = k.sb("wr", [128, 8, 36], F32)
    for kc in range(8):
        k.dma("sp", wr[:, kc, :], wr_d[kc * 128:(kc + 1) * 128, :])
    brb = k.sb("brb", [128, 36], F32)
    k.dma("sp", brb[:, :], bcast_row(br_d))
    g1 = k.sb("g1", [128, D], F32)
    b1 = k.sb("b1", [128, D], F32)
    k.dma("sp", g1[:, :], bcast_row(ln1g_d))
    k.dma("sp", b1[:, :], bcast_row(ln1b_d))
    hts = [k.sb(f"ht{i}", [128, D], F32) for i in range(2)]
    x1s = [k.sb(f"x1{i}", [128, D], F32) for i in range(2)]
    h1s = [k.sb(f"h1{i}", [128, D], F32) for i in range(2)]
    tmps = [k.sb(f"lt{i}", [128, D], F32) for i in range(2)]
    sts = [k.sb(f"ls{i}", [128, 4], F32) for i in range(2)]
    hTf = [k.sb(f"hTf{i}", [128, 8, 128], F32) for i in range(2)]
    rl = [k.sb(f"rl{i}", [128, 36], F32) for i in range(2)]
    rs = [k.sb(f"rs{i}", [128, 16], F32) for i in range(2)]
    elm = [k.sb(f"elm{i}", [128, 32], F32) for i in range(2)]
    elm2 = [k.sb(f"elm2{i}", [128, 32], F32) for i in range(2)]
    oh1 = [k.sb(f"oh1{i}", [128, 32], F32) for i in range(2)]
    oh2 = [k.sb(f"oh2{i}", [128, 32], F32) for i in range(2)]
    k_main = k
    recs = [Rec(k_main), Rec(k_main)]
    for t in range(NT):
        p = t % 2
        k = recs[p]
        tok = slice(t * 128, (t + 1) * 128)
        ht, x1, h1t, tmp, st = hts[p], x1s[p], h1s[p], tmps[p], sts[p]
        k.dma("sp", ht[:, :], h_d[tok, :])
        for half in range(2):
            bk = banks[half + 2 * p]
            for kc in range(8):
                k.mm(bk[:, :], mixT[:, kc, tok], wout[:, kc, half * 512:(half + 1) * 512],
                     start=(kc == 0), stop=(kc == 7))
            k.stt("dve", x1[:, half * 512:(half + 1) * 512], ht[:, half * 512:(half + 1) * 512], ALPHA,
                  bk[:, :], ALU.mult, ALU.add)
        layer_norm_tile(k, x1[:, :], h1t[:, :], g1[:, :], b1[:, :], tmp[:, :], st[:, :])
        k.act(acc[t][:, :], h1t[:, :], AF.Copy, scale=ALPHA)
        hf = hTf[p]
        for q4 in range(2):
            bk = banks[4 + q4 + 2 * p]
            for j in range(4):
                kc = q4 * 4 + j
                k.transpose(bk[:, j * 128:(j + 1) * 128], h1t[:, kc * 128:(kc + 1) * 128], ident[:, :],
                            inc=(j == 3))
            k.copy("dve", hf[:, q4 * 4:(q4 + 1) * 4, :],
                   bk[:, :].f(lambda a: a.rearrange("p (j t) -> p j t", j=4)))
        k.copy("act", h1T[:, :, tok], hf[:, :, :])
        bk = banks[2 * p]
        for kc in range(8):
            k.mm(bk[:, 0:36], hf[:, kc, :], wr[:, kc, :], start=(kc == 0), stop=(kc == 7))
        l, s_, em, em2, o1, o2 = rl[p], rs[p], elm[p], elm2[p], oh1[p], oh2[p]
        cb = comb_all[:, t, :]
        k.tt("dve", l[:, :], bk[:, 0:36], brb[:, :], ALU.add)
        k.reduce("dve", s_[:, 0:1], l[:, 0:4], ALU.max)
        k.ts("dve", s_[:, 1:2], s_[:, 0:1], -1.0, None, ALU.mult)
        k.act(s_[:, 8:12], l[:, 0:4], AF.Exp, bias=s_[:, 1:2], scale=1.0, accum_out=s_[:, 2:3])
        k.op("dve", lambda e, s_=s_: e.reciprocal(s_[:, 3:4].ap, s_[:, 2:3].ap), [s_], [s_])
        k.ts("dve", s_[:, 12:16], l[:, 0:4], s_[:, 0:1], None, ALU.is_equal)
        k.ts("dve", s_[:, 12:16], s_[:, 12:16], BIG, -BIG, ALU.mult, ALU.add)
        k.tt("dve", em[:, :].f(lambda a: a.rearrange("p (g e) -> p g e", g=4)),
             l[:, 4:36].f(lambda a: a.rearrange("p (g e) -> p g e", g=4)),
             s_[:, 12:16].f(lambda a: a.unsqueeze(2).broadcast_to([128, 4, 8])), ALU.add)
        k.reduce("dve", s_[:, 4:5], em[:, :], ALU.max)
        k.ts("dve", o1[:, :], em[:, :], s_[:, 4:5], None, ALU.is_equal)
        k.stt("dve", em2[:, :], o1[:, :], -BIG, em[:, :], ALU.mult, ALU.add)
        k.reduce("dve", s_[:, 5:6], em2[:, :], ALU.max)
        k.ts("dve", o2[:, :], em2[:, :], s_[:, 5:6], None, ALU.is_equal)
        k.tt("dve", s_[:, 6:7], s_[:, 5:6], s_[:, 4:5], ALU.subtract)
        k.act(s_[:, 6:7], s_[:, 6:7], AF.Exp)
        k.ts("dve", s_[:, 7:8], s_[:, 6:7], 1.0, None, ALU.add)
        k.op("dve", lambda e, s_=s_: e.reciprocal(s_[:, 7:8].ap, s_[:, 7:8].ap), [s_], [s_])
        k.tt("dve", s_[:, 7:8], s_[:, 7:8], s_[:, 3:4], ALU.mult)
        k.tt("dve", s_[:, 6:7], s_[:, 6:7], s_[:, 7:8], ALU.mult)
        k.ts("dve", cb, o1[:, :], s_[:, 7:8], None, ALU.mult)
        k.stt("dve", cb, o2[:, :], s_[:, 6:7], cb, ALU.mult, ALU.add)
    k = k_main
    replay_merged(k, recs[0].segs[0], recs[1].segs[0])
    k.pop()

    k.push()
    NW = 3
    ewg = [k.sb(f"ewg{i}", [128, 8, 256], BF16) for i in range(NW)]
    ewu = [k.sb(f"ewu{i}", [128, 8, 256], BF16) for i in range(NW)]
    ewd = [k.sb(f"ewd{i}", [128, 2, D], BF16) for i in range(NW)]
    sgs = [k.sb(f"sG{i}", [128, 512], BF16) for i in range(2)]
    hcs = [k.sb(f"Hc{i}", [128, 2, 512], BF16) for i in range(2)]
    n1 = 0
    n2 = [0]
    pending = None

    def down(e, tb, hc, wdt):
        for tt_ in range(4):
            t = tb * 4 + tt_
            for half in range(2):
                bO = banks[5 + n2[0] % 3]
                n2[0] += 1
                for fc in range(2):
                    k.mm(bO[:, :], hc[:, fc, tt_ * 128:(tt_ + 1) * 128], wdt[:, fc, half * 512:(half + 1) * 512],
                         start=(fc == 0), stop=(fc == 1))
                k.stt("dve", acc[t][:, half * 512:(half + 1) * 512], bO[:, :], comb_all[:, t, e:e + 1],
                      acc[t][:, half * 512:(half + 1) * 512], ALU.mult, ALU.add)

    for e in range(NEXP):
        wgt, wut, wdt = ewg[e % NW], ewu[e % NW], ewd[e % NW]
        k.dma("pool", wgt[:, :, :], ewg_d.v(ewg_d.h[e].rearrange("(kc p) f -> p kc f", p=128)))
        k.dma("pool", wut[:, :, :], ewu_d.v(ewu_d.h[e].rearrange("(kc p) f -> p kc f", p=128)))
        k.dma("pool", wdt[:, :, :], ewd_d.v(ewd_d.h[e].rearrange("(fc p) d -> p fc d", p=128)))
        for tb in range(NB):
            tsl = slice(tb * 512, (tb + 1) * 512)
            hc = hcs[tb % 2]
            for fc in range(2):
                bG = banks[1 + n1 % 2]
                bU = banks[3 + n1 % 2]
                sgt = sgs[n1 % 2]
                n1 += 1
                for kc in range(8):
                    k.mm(bG[:, :], wgt[:, kc, fc * 128:(fc + 1) * 128], h1T[:, kc, tsl], start=(kc == 0), stop=(kc == 7))
                for kc in range(8):
                    k.mm(bU[:, :], wut[:, kc, fc * 128:(fc + 1) * 128], h1T[:, kc, tsl], start=(kc == 0), stop=(kc == 7))
                k.act(sgt[:, :], bG[:, :], AF.Silu)
                k.tt("dve", hc[:, fc, :], bU[:, :], sgt[:, :], ALU.mult)
            if pending is not None:
                down(*pending)
            pending = (e, tb, hc, wdt)
    down(*pending)
    k.pop()

    k.push()
    g2 = k.sb("g2", [128, D], F32)
    b2 = k.sb("b2", [128, D], F32)
    k.dma("sp", g2[:, :], bcast_row(ln2g_d))
    k.dma("sp", b2[:, :], bcast_row(ln2b_d))
    ys = [k.sb(f"y{i}", [128, D], F32) for i in range(2)]
    tmps = [k.sb(f"lt{i}", [128, D], F32) for i in range(2)]
    sts = [k.sb(f"ls{i}", [128, 4], F32) for i in range(2)]
    if not last:
        hTsb = k.sb("hTsb", [128, 8, ntok], BF16)
    recs = [Rec(k), Rec(k)]
    for t in range(NT):
        p = t % 2
        kr = recs[p]
        layer_norm_tile(kr, acc[t][:, :], ys[p][:, :], g2[:, :], b2[:, :], tmps[p][:, :], sts[p][:, :], eng_g="dve")
        if last:
            kr.dma("sp", out_d[t * 128:(t + 1) * 128, :], ys[p][:, :])
        else:
            kr.dma("sp", h_d[t * 128:(t + 1) * 128, :], ys[p][:, :])
            emit_publish_tile(kr, banks, ident, ys[p], hTsb, t)
    replay_merged(k, recs[0].segs[0], recs[1].segs[0])
    if not last:
        emit_allgather_h(k, hTsb, G)
    k.pop()
    k.pop()


CONST_SHAPES = {"ident": [128, 128], "esel": [128, 64], "frq": [128, 1], "sgn": [128, 1],
                "cU": [128, 128], "cUrel": [128, 128], "cW": [128, 128], "cones": [128, 4], "maskbd": [128, 128],
                "rowmask": [128, 4], "uinc": [128, 128], "lpos_s": [128, 128], "uneg": [128, 128]}

LAYER_SHAPES = {
    "w_lat": [D, 448], "g_q": [128, 2], "g_kv": [128, 1], "w_uq": [256, 256], "w_uk": [128, 128], "w_uv": [128, 128],
    "w_hg": [D, 512], "hg_o_norm": [1, 128],
    "w_dn": [D, 514], "conv_w": [128, 3, 4], "a_log": [1, 1], "dt_bias": [1, 1], "dn_o_norm": [1, 128],
    "w_gates": [D, 3 * D], "w_br_a": [512, D], "w_br_b": [512, D], "w_br_c": [512, D], "w_out": [D, D],
    "ln1_g": [1, D], "ln1_b": [1, D], "ln2_g": [1, D], "ln2_b": [1, D],
    "w_router": [D, 36], "b_router": [1, 36],
    "exp_w_gate": [NEXP, D, 256], "exp_w_up": [NEXP, D, 256], "exp_w_down": [NEXP, 256, D],
}


def build_fused():
    nc = bass.Bass("TRN2", target_bir_lowering=False)
    k = K(nc)
    ntok = S * B // NCORES
    G = {}
    G["x"] = k.dram("x", [ntok, D], F32, "ExternalInput")
    G["ln_in_g"] = k.dram("ln_in_g", [1, D], F32, "ExternalInput")
    G["ln_in_b"] = k.dram("ln_in_b", [1, D], F32, "ExternalInput")
    G["pos"] = k.dram("pos", [1, S], I32, "ExternalInput")
    G["lb_rows"] = k.dram("lb_rows", [DEPTH, 128], F32, "ExternalInput")
    G["lb_cols"] = k.dram("lb_cols", [128, DEPTH], F32, "ExternalInput")
    rank_d = k.dram("rank", [1, 1], I32, "ExternalInput")
    tri_d = k.dram("tri", [128, 128], F32, "ExternalInput")
    G["out"] = k.dram("out", [ntok, D], F32, "ExternalOutput")
    cdram = {n: k.dram("c_" + n, shp, F32, "ExternalInput") for n, shp in CONST_SHAPES.items()}
    Ws = [{n: k.dram(f"L{l}_{n}", shp, F32, "ExternalInput") for n, shp in LAYER_SHAPES.items()} for l in range(DEPTH)]

    G["h_cur"] = k.dram("h_cur", [ntok, D], F32, "Internal")
    G["hsrc"] = [k.dram(f"hsrc{q}", [256, ntok], BF16, "Internal") for q in range(4)]
    G["hdst"] = [k.dram(f"hdst{q}", [4 * 256, ntok], BF16, "Internal") for q in range(4)]
    G["osrc"] = [[k.dram(f"osrc{br}_{q}", [128, ntok], BF16, "Internal") for q in range(4)] for br in range(3)]
    odst_full = [nc.dram_tensor(f"odst{br}", [4, 512, ntok], BF16, kind="Internal").ap() for br in range(3)]
    G["odst"] = [[T(odst_full[br][q], f"odst{br}_{q}") for q in range(4)] for br in range(3)]

    def hT_blk(blk, kc):
        r, t0, q = blk // 4, (blk % 4) * 512, kc // 2
        row0 = r * 256 + (kc % 2) * 128
        return G["hdst"][q][row0:row0 + 128, t0:t0 + 512]

    G["hT_blk"] = hT_blk

    reg = nc.sync.alloc_register("rank")
    nc.sync.reg_load(reg, rank_d.h[0:1, 0:1])
    rank_off = nc.sync.snap(reg, min_val=0, max_val=3)

    def o_own(br, kc, tsl):
        ap = odst_full[br][bass.ds(rank_off, 1), kc * 128:(kc + 1) * 128, tsl].rearrange("o p c -> (o p) c")
        return V(ap, G["odst"][br][0].res)

    G["o_own"] = o_own
    G["o_own_res"] = lambda br: [G["odst"][br][q].res for q in range(1, 4)]

    banks = [k.ps(f"bank{i}", [128, 512], F32) for i in range(8)]

    cres = Res("consts")
    C = {}
    for n, shp in CONST_SHAPES.items():
        C[n] = k.sb("c_" + n, shp, F32, res=cres)
        k.dma("sp", C[n][tuple(slice(None) for _ in shp)], cdram[n][tuple(slice(None) for _ in shp)])
    C["tri"] = k.sb("c_tri", [128, 128], BF16)
    k.dma("pool", C["tri"][:, :], tri_d[:, :])

    emit_ln0(k, banks, C, G)
    for l in range(DEPTH):
        W = Ws[l]
        emit_mla(k, banks, C, W, G)
        emit_allgather_o(k, G, 0)
        emit_dn_hg(k, banks, C, W, G, l)
        emit_stage_c(k, banks, C, W, G, last=(l == DEPTH - 1))
    k.finish([G["out"]])
    assert 5 + k.ndsem + k.ncoll <= 100, (k.ndsem, k.ncoll)
    build_fused.stats = (k.ndsem, k.ncoll, dict(k.tok))
    return nc


def layer_inputs(P, l, j):
    m = {}
    a = mla_inputs(None, np.zeros(1, np.int32), P, l, j)
    for n in ("w_lat", "g_q", "g_kv", "w_uq", "w_uk", "w_uv"):
        m[n] = a[n]
    hgi = hg_inputs(None, P, l, j)
    m["w_hg"] = hgi["w_hg"]
    m["hg_o_norm"] = hgi["o_norm"]
    dni = dn_inputs(None, P, l, j)
    for n in ("w_dn", "conv_w", "a_log", "dt_bias"):
        m[n] = dni[n]
    m["dn_o_norm"] = dni["o_norm"]
    w_in = P["w_in"][l]
    m.update({
        "w_gates": np.ascontiguousarray(w_in[:, 4520:]),
        "w_br_a": P["w_br_a"][l], "w_br_b": P["w_br_b"][l], "w_br_c": P["w_br_c"][l], "w_out": P["w_out"][l],
        "ln1_g": P["ln1_g"][l].reshape(1, D), "ln1_b": P["ln1_b"][l].reshape(1, D),
        "ln2_g": P["ln2_g"][l].reshape(1, D), "ln2_b": P["ln2_b"][l].reshape(1, D),
        "w_router": np.ascontiguousarray(np.concatenate([P["router_group_w"][l], P["router_expert_w"][l]], axis=1)),
        "b_router": np.concatenate([P["router_group_b"][l], P["router_expert_b"][l]]).reshape(1, 36),
        "exp_w_gate": P["exp_w_gate"][l], "exp_w_up": P["exp_w_up"][l], "exp_w_down": P["exp_w_down"][l],
    })
    return {f"L{l}_{n}": np.ascontiguousarray(v, dtype=np.float32) for n, v in m.items()}


def const_inputs():
    c = {}
    c.update(hg_consts())
    c.update(dn_consts())
    a = mla_inputs(None, np.zeros(1, np.int32), None, 0, 0, consts_only=True)
    c.update({n: a[n] for n in ("frq", "sgn", "esel")})
    out = {"c_" + n: np.ascontiguousarray(c[n], dtype=np.float32) for n in CONST_SHAPES}
    out["tri"] = a["tri"]
    return out


def kernel(**inputs):
    P = {k_: np.asarray(v) for k_, v in inputs.items()}
    x = np.asarray(P["x"], dtype=np.float32).reshape(B * S, D)
    pos = P["positions"]
    per = B * S // NCORES
    nc = build_fused()
    consts = const_inputs()
    in_maps = []
    for c in range(NCORES):
        b, j = c // 4, c % 4
        m = dict(consts)
        m["x"] = np.ascontiguousarray(x[c * per:(c + 1) * per])
        m["ln_in_g"] = P["ln_in_g"].reshape(1, D).astype(np.float32)
        m["ln_in_b"] = P["ln_in_b"].reshape(1, D).astype(np.float32)
        m["pos"] = np.ascontiguousarray(pos[b].reshape(1, S).astype(np.int32))
        lb = P["hg_lower_bounds"][:, j * 128:(j + 1) * 128].astype(np.float32)
        m["lb_rows"] = np.ascontiguousarray(lb)
        m["lb_cols"] = np.ascontiguousarray(lb.T)
        m["rank"] = np.array([[j]], np.int32)
        for l in range(DEPTH):
            m.update(layer_inputs(P, l, j))
        in_maps.append(m)
    res = run_bass_kernel_spmd(nc, in_maps, core_ids=list(range(NCORES)))
    out = np.concatenate([r["out"] for r in res.results], axis=0)
    return np.ascontiguousarray(out.reshape(B, S, D).astype(np.float32))
```

```python
from contextlib import ExitStack
import numpy as np
import concourse.bass as bass
import concourse.mybir as mybir
from concourse.bass_utils import run_bass_kernel_spmd

F32 = mybir.dt.float32
BF16 = mybir.dt.bfloat16
I32 = mybir.dt.int32
AF = mybir.ActivationFunctionType
ALU = mybir.AluOpType
AX = mybir.AxisListType

NCORES = 8
D = 1024
B = 2
S = 8192
DEPTH = 2
ALPHA = (2 * DEPTH) ** 0.25
EPS = 1e-6
IN_COLS = 7592
NEXP = 32


class Res:
    __slots__ = ("name", "w", "r", "dsem", "dkey", "dcount", "wdma", "excl")

    def __init__(self, name=""):
        self.name = name
        self.w = None
        self.r = {}
        self.dsem = None
        self.dkey = None
        self.dcount = 0
        self.wdma = False
        self.excl = False


class V:
    __slots__ = ("ap", "res")

    def __init__(self, ap, res):
        self.ap = ap
        self.res = res

    def __getitem__(self, key):
        return V(self.ap[key], self.res)

    def f(self, fn):
        return V(fn(self.ap), self.res)

    def bitcast(self, dt):
        return V(self.ap.bitcast(dt), self.res)


class T:
    def __init__(self, handle, name, res=None):
        self.h = handle
        self.res = res if res is not None else Res(name)

    def __getitem__(self, key):
        return V(self.h[key], self.res)

    def v(self, ap):
        return V(ap, self.res)


class _Dummy:
    def then_inc(self, *a, **kw):
        return self


_DUMMY = _Dummy()


class K:
    def __init__(self, nc):
        self.nc = nc
        self.E = {"pe": nc.tensor, "dve": nc.vector, "act": nc.scalar, "pool": nc.gpsimd, "sp": nc.sync}
        self.semobj = {}
        self.tok = {}
        self.seen = {n: {} for n in self.E}
        for n in self.E:
            self.semobj["s_" + n] = nc.alloc_semaphore("s_" + n)
            self.tok[n] = 0
        self.ndsem = 0
        self.dres = []
        self.nuniq = 0
        self.stacks = []
        self.phase_res = []
        self.free_dsems = []
        self.ncoll = 0
        self.coll_tokens = []
        self.dry = None

    def sb(self, name, shape, dt, res=None):
        self.nuniq += 1
        if self.stacks:
            h = self.stacks[-1].enter_context(self.nc.sbuf_tensor(f"{name}_{self.nuniq}", list(shape), dt))
        else:
            h = self.nc.alloc_sbuf_tensor(f"{name}_{self.nuniq}", list(shape), dt)
        t = T(h, name, res=res)
        if self.stacks and res is None:
            self.phase_res[-1].append(t.res)
        return t

    def push(self):
        self.stacks.append(ExitStack())
        self.phase_res.append([])

    def pop(self):
        self.barrier()
        self.stacks.pop().close()
        for r in self.phase_res.pop():
            if r.dsem is not None:
                self.free_dsems.append((r.dsem, r.dkey, r.dcount))
                self.dres.remove(r)
                r.dsem = None

    def ps(self, name, shape, dt=F32):
        self.nuniq += 1
        t = T(self.nc.alloc_psum_tensor(f"{name}_{self.nuniq}", list(shape), dt), name)
        t.res.excl = True
        return t

    def dram(self, name, shape, dt, kind):
        h = self.nc.dram_tensor(name, list(shape), dt, kind=kind)
        return T(h.ap(), name)

    def _gather(self, eng, reads, writes):
        own = "s_" + eng
        deps = {}

        def add(t, raw):
            if t is None:
                return
            k, v = t
            if k == own and (eng == "pe" or not raw):
                return
            if deps.get(k, 0) < v:
                deps[k] = v

        for r in reads:
            add(r.w, True)
            if r.excl:
                for k, v in r.r.items():
                    add((k, v), False)
        for w in writes:
            add(w.w, False)
            for k, v in w.r.items():
                add((k, v), False)
        return deps

    def _emit_waits(self, eng, deps):
        e = self.E[eng]
        seen = self.seen[eng]
        for k, v in deps.items():
            if k.startswith("s_"):
                assert v <= self.tok[k[2:]], f"wait on unrealised token {k} {v} > {self.tok[k[2:]]}"
            if seen.get(k, 0) >= v:
                continue
            e.wait_ge(self.semobj[k], v)
            seen[k] = v

    def op(self, eng, fn, reads, writes, inc=True):
        reads = [r.res if isinstance(r, (V, T)) else r for r in reads if r is not None]
        writes = [w.res if isinstance(w, (V, T)) else w for w in writes if w is not None]
        if self.dry is not None:
            self.dry.append((eng, reads, writes))
            return _DUMMY
        deps = self._gather(eng, reads, writes)
        self._emit_waits(eng, deps)
        ins = fn(self.E[eng])
        key = "s_" + eng
        if inc:
            ins.then_inc(self.semobj[key], 1)
            self.tok[eng] += 1
            t = (key, self.tok[eng])
        else:
            t = (key, self.tok[eng] + 1)
        for w in writes:
            w.w = t
            w.r = {}
            w.wdma = False
        for r in reads:
            if r in writes:
                continue
            if r.r.get(key, 0) < t[1]:
                r.r[key] = t[1]
        return ins

    def collective(self, kind, ins, outs, groups):
        deps = {}

        def add(t):
            if t is None:
                return
            k_, v = t
            if deps.get(k_, 0) < v:
                deps[k_] = v

        for i in ins:
            add(i.res.w)
        for o in outs:
            add(o.res.w)
            for k_, v in o.res.r.items():
                add((k_, v))
        self._emit_waits("pool", deps)
        self.ncoll += 1
        key = f"cc{self.ncoll}"
        sem = self.nc.alloc_semaphore(key)
        self.semobj[key] = sem
        self.E["pool"].collective_compute(kind, ALU.bypass, replica_groups=groups,
                                          ins=[i.ap.opt() for i in ins], outs=[o.ap.opt() for o in outs]).then_inc(sem, 1)
        self.coll_tokens.append((key, 1))
        for o in outs:
            o.res.w = (key, 1)
            o.res.r = {}
            o.res.wdma = False
        for i in ins:
            i.res.r[key] = 1

    def dma(self, q, out, in_, extra_reads=(), **kw):
        w = out.res
        rd = in_.res
        if self.dry is not None:
            self.dry.append(("dma", [rd] + list(extra_reads), [w]))
            return
        own = "s_" + q
        deps = {}

        def add(t):
            if t is None:
                return
            k, v = t
            if deps.get(k, 0) < v:
                deps[k] = v

        add(rd.w)
        for xr in extra_reads:
            add(xr.w)
        if not (w.wdma and not w.r):
            add(w.w)
        for k, v in w.r.items():
            add((k, v))
        self._emit_waits(q, deps)
        if w.dsem is None:
            if self.free_dsems:
                w.dsem, w.dkey, w.dcount = self.free_dsems.pop()
            else:
                self.ndsem += 1
                w.dkey = f"d{self.ndsem}"
                w.dsem = self.nc.alloc_semaphore(w.dkey)
                self.semobj[w.dkey] = w.dsem
                w.dcount = 0
            self.dres.append(w)
        self.E[q].dma_start(out=out.ap, in_=in_.ap, **kw).then_inc(w.dsem, 16)
        w.dcount += 16
        t = (w.dkey, w.dcount)
        w.w = t
        w.r = {}
        w.wdma = True
        if rd.r.get(t[0], 0) < t[1]:
            rd.r[t[0]] = t[1]
        for xr in extra_reads:
            if xr.r.get(t[0], 0) < t[1]:
                xr.r[t[0]] = t[1]

    def barrier(self):
        for eng in self.E:
            deps = {}
            for x in self.E:
                if x != eng and self.tok[x] > 0:
                    deps["s_" + x] = self.tok[x]
            for r in self.dres:
                deps[r.dkey] = max(deps.get(r.dkey, 0), r.dcount)
            for key, v in self.coll_tokens:
                deps[key] = v
            self._emit_waits(eng, deps)

    def finish(self, outs):
        deps = {}
        for o in outs:
            r = o.res if isinstance(o, (V, T)) else o
            deps[r.w[0]] = r.w[1]
        self._emit_waits("sp", deps)

    def mm(self, out, lhsT, rhs, start=True, stop=True, inc=None, extra_reads=()):
        if inc is None:
            inc = stop
        return self.op("pe", lambda e: e.matmul(out.ap, lhsT.ap, rhs.ap, start=start, stop=stop),
                       [lhsT, rhs] + list(extra_reads), [out], inc=inc)

    def transpose(self, out, in_, ident, inc=True):
        return self.op("pe", lambda e: e.transpose(out.ap, in_.ap, ident.ap), [in_, ident], [out], inc=inc)

    def act(self, out, in_, func, bias=None, scale=None, accum_out=None, eng="act"):
        kw = {}
        rd = [in_]
        if bias is not None:
            if isinstance(bias, V):
                kw["bias"] = bias.ap
                rd.append(bias)
            else:
                kw["bias"] = bias
        if scale is not None:
            if isinstance(scale, V):
                kw["scale"] = scale.ap
                rd.append(scale)
            else:
                kw["scale"] = scale
        wr = [out]
        if accum_out is not None:
            kw["accum_out"] = accum_out.ap
            wr.append(accum_out)
        return self.op("act", lambda e: e.activation(out.ap, in_.ap, func, **kw), rd, wr)

    def tt(self, eng, out, in0, in1, op):
        return self.op(eng, lambda e: e.tensor_tensor(out.ap, in0.ap, in1.ap, op), [in0, in1], [out])

    def ts(self, eng, out, in0, s1, s2, op0, op1=None, accum_out=None):
        rd = [in0]
        a1 = s1
        a2 = s2
        if isinstance(s1, V):
            rd.append(s1)
            a1 = s1.ap
        if isinstance(s2, V):
            rd.append(s2)
            a2 = s2.ap
        wr = [out]
        kw = {}
        if op1 is not None:
            kw["op1"] = op1
        if accum_out is not None:
            kw["accum_out"] = accum_out.ap
            wr.append(accum_out)
        return self.op(eng, lambda e: e.tensor_scalar(out.ap, in0.ap, a1, a2, op0, **kw), rd, wr)

    def stt(self, eng, out, in0, scalar, in1, op0, op1):
        rd = [in0, in1]
        a = scalar
        if isinstance(scalar, V):
            rd.append(scalar)
            a = scalar.ap
        return self.op(eng, lambda e: e.scalar_tensor_tensor(out.ap, in0.ap, a, in1.ap, op0, op1), rd, [out])

    def rstd(self, out, in_, scale, eps):
        self.act(out, in_, AF.Sqrt, bias=eps, scale=scale)
        self.op("dve", lambda e: e.reciprocal(out.ap, out.ap), [out], [out])

    def rstd_ln(self, out, in_, scale, eps):
        self.act(out, in_, AF.Ln, bias=eps, scale=scale)
        self.act(out, out, AF.Exp, scale=-0.5)

    def copy(self, eng, out, in_):
        if eng == "act":
            return self.op("act", lambda e: e.copy(out.ap, in_.ap), [in_], [out])
        return self.op(eng, lambda e: e.tensor_copy(out.ap, in_.ap), [in_], [out])

    def memset(self, eng, out, val):
        return self.op(eng, lambda e: e.memset(out.ap, val), [], [out])

    def reduce(self, eng, out, in_, op, axis=AX.X):
        return self.op(eng, lambda e: e.tensor_reduce(out.ap, in_.ap, axis, op), [in_], [out])


def layer_norm_tile(k, x, out, g_b, b_b, tmp, st, pre_scale_res=None, eng_g="pool"):
    k.reduce("dve", st[:, 0:1], x, ALU.add)
    k.ts("dve", st[:, 1:2], st[:, 0:1], -1.0 / D, None, ALU.mult)
    k.act(tmp, x, AF.Square, bias=st[:, 1:2], scale=1.0, accum_out=st[:, 2:3])
    k.rstd_ln(st[:, 3:4], st[:, 2:3], 1.0 / D, EPS)
    k.ts("dve", tmp, x, st[:, 1:2], st[:, 3:4], ALU.add, ALU.mult)
    k.tt(eng_g, tmp, tmp, g_b, ALU.mult)
    k.tt("pool", out, tmp, b_b, ALU.add)


def build_ln0(ntok):
    nc = bass.Bass("TRN2", target_bir_lowering=False)
    k = K(nc)
    x = k.dram("x", [ntok, D], F32, "ExternalInput")
    g = k.dram("g", [1, D], F32, "ExternalInput")
    b = k.dram("b", [1, D], F32, "ExternalInput")
    y = k.dram("y", [ntok, D], F32, "ExternalOutput")
    g_b = k.sb("g_b", [128, D], F32)
    b_b = k.sb("b_b", [128, D], F32)
    k.dma("sp", g_b[:, :], g.v(g.h.partition_broadcast(128)))
    k.dma("sp", b_b[:, :], b.v(b.h.partition_broadcast(128)))
    nt = ntok // 128
    xs = [k.sb(f"x{i}", [128, D], F32) for i in range(2)]
    ys = [k.sb(f"y{i}", [128, D], F32) for i in range(2)]
    tmps = [k.sb(f"t{i}", [128, D], F32) for i in range(2)]
    sts = [k.sb(f"s{i}", [128, 4], F32) for i in range(2)]
    for i in range(nt):
        xt, yt, tt_, st = xs[i % 2], ys[i % 2], tmps[i % 2], sts[i % 2]
        k.dma("sp", xt[:, :], x[i * 128:(i + 1) * 128, :])
        layer_norm_tile(k, xt[:, :], yt[:, :], g_b[:, :], b_b[:, :], tt_[:, :], st[:, :])
        k.dma("sp", y[i * 128:(i + 1) * 128, :], yt[:, :])
    k.finish([y])
    return nc


def run_ln0(x, g, b):
    T_ = x.shape[0] * x.shape[1]
    xs = x.reshape(T_, D)
    per = T_ // NCORES
    nc = build_ln0(per)
    in_maps = [{"x": np.ascontiguousarray(xs[c * per:(c + 1) * per]), "g": g.reshape(1, D), "b": b.reshape(1, D)}
               for c in range(NCORES)]
    res = run_bass_kernel_spmd(nc, in_maps, core_ids=list(range(NCORES)))
    return np.concatenate([r["y"] for r in res.results], axis=0)


BIG = 1.0e30


def bcast_row(t, n=128):
    return t.v(t.h.partition_broadcast(n))


def build_stage_c(ntok, upto=9):
    nc = bass.Bass("TRN2", target_bir_lowering=False)
    k = K(nc)
    NT = ntok // 128
    NB = ntok // 512
    h_d = k.dram("h", [ntok, D], F32, "ExternalInput")
    hT_d = k.dram("hT", [D, ntok], F32, "ExternalInput")
    oT_d = [k.dram(n, [512, ntok], F32, "ExternalInput") for n in ("oaT", "obT", "ocT")]
    wg_d = k.dram("w_gates", [D, 3 * D], F32, "ExternalInput")
    wbr_d = [k.dram(n, [512, D], F32, "ExternalInput") for n in ("w_br_a", "w_br_b", "w_br_c")]
    wout_d = k.dram("w_out", [D, D], F32, "ExternalInput")
    ln1g_d = k.dram("ln1_g", [1, D], F32, "ExternalInput")
    ln1b_d = k.dram("ln1_b", [1, D], F32, "ExternalInput")
    ln2g_d = k.dram("ln2_g", [1, D], F32, "ExternalInput")
    ln2b_d = k.dram("ln2_b", [1, D], F32, "ExternalInput")
    wr_d = k.dram("w_router", [D, 36], F32, "ExternalInput")
    br_d = k.dram("b_router", [1, 36], F32, "ExternalInput")
    ewg_d = k.dram("exp_w_gate", [NEXP, D, 256], F32, "ExternalInput")
    ewu_d = k.dram("exp_w_up", [NEXP, D, 256], F32, "ExternalInput")
    ewd_d = k.dram("exp_w_down", [NEXP, 256, D], F32, "ExternalInput")
    ident_d = k.dram("ident", [128, 128], F32, "ExternalInput")
    out_d = k.dram("out", [ntok, D], F32, "ExternalOutput")

    banks = [k.ps(f"bank{i}", [128, 512], F32) for i in range(8)]

    ident = k.sb("ident", [128, 128], F32)
    k.dma("sp", ident[:, :], ident_d[:, :])
    comb_all = k.sb("comb_all", [128, NT, 32], F32)
    mixT = k.sb("mixT", [128, 8, ntok], BF16)

    k.push()
    wg = k.sb("wg", [128, 8, 3 * D], BF16)
    wbr = k.sb("wbr", [128, 12, D], BF16)
    for kc in range(8):
        k.dma("pool", wg[:, kc, :], wg_d[kc * 128:(kc + 1) * 128, :])
    for br in range(3):
        for kc in range(4):
            k.dma("pool", wbr[:, br * 4 + kc, :], wbr_d[br][kc * 128:(kc + 1) * 128, :])
    hTb = [k.sb(f"hTb{i}", [128, 8, 512], BF16) for i in range(2)]
    oTb = [k.sb(f"oTb{i}", [128, 12, 512], BF16) for i in range(2)]
    sg = [k.sb(f"sg{i}", [128, 512], BF16) for i in range(2)]
    tmx = [k.sb(f"tmx{i}", [128, 512], F32) for i in range(2)]
    mixf = [k.sb(f"mixf{i}", [128, 512], F32) for i in range(2)]
    nb = 0
    for tb in range(NB):
        tsl = slice(tb * 512, (tb + 1) * 512)
        hb, ob = hTb[tb % 2], oTb[tb % 2]
        for kc in range(8):
            k.dma("pool", hb[:, kc, :], hT_d[kc * 128:(kc + 1) * 128, tsl])
        for br in range(3):
            for kc in range(4):
                k.dma("pool", ob[:, br * 4 + kc, :], oT_d[br][kc * 128:(kc + 1) * 128, tsl])
        for r in range(8):
            mf = mixf[r % 2]
            for br in range(3):
                bg = banks[nb % 2]
                by = banks[2 + nb % 2]
                sgt = sg[nb % 2]
                tm = tmx[nb % 2]
                nb += 1
                col = br * D + r * 128
                for kc in range(8):
                    k.mm(bg[:, :], wg[:, kc, col:col + 128], hb[:, kc, :], start=(kc == 0), stop=(kc == 7))
                k.act(sgt[:, :], bg[:, :], AF.Sigmoid)
                for kc in range(4):
                    k.mm(by[:, :], wbr[:, br * 4 + kc, r * 128:(r + 1) * 128], ob[:, br * 4 + kc, :],
                         start=(kc == 0), stop=(kc == 3))
                if br == 0:
                    k.tt("dve", mf[:, :], by[:, :], sgt[:, :], ALU.mult)
                elif br == 1:
                    k.tt("dve", tm[:, :], by[:, :], sgt[:, :], ALU.mult)
                    k.tt("pool", mf[:, :], mf[:, :], tm[:, :], ALU.add)
                else:
                    k.tt("dve", tm[:, :], by[:, :], sgt[:, :], ALU.mult)
                    k.tt("pool", mixT[:, r, tsl], mf[:, :], tm[:, :], ALU.add)
    k.pop()
    if upto == 0:
        dbg = k.dram("dbg", [128, 8 * ntok], BF16, "ExternalOutput")
        k.dma("sp", dbg[:, :], mixT[:, :, :].f(lambda a: a.rearrange("p a b -> p (a b)")))
        k.finish([dbg])
        return nc

    acc = [k.sb(f"acc{i}", [128, D], F32) for i in range(NT)]
    h1T = k.sb("h1T", [128, 8, ntok], BF16)
    k.push()
    wout = k.sb("wout", [128, 8, D], BF16)
    for kc in range(8):
        k.dma("pool", wout[:, kc, :], wout_d[kc * 128:(kc + 1) * 128, :])
    wr = k.sb("wr", [128, 8, 36], F32)
    for kc in range(8):
        k.dma("sp", wr[:, kc, :], wr_d[kc * 128:(kc + 1) * 128, :])
    brb = k.sb("brb", [128, 36], F32)
    k.dma("sp", brb[:, :], bcast_row(br_d))
    g1 = k.sb("g1", [128, D], F32)
    b1 = k.sb("b1", [128, D], F32)
    k.dma("sp", g1[:, :], bcast_row(ln1g_d))
    k.dma("sp", b1[:, :], bcast_row(ln1b_d))
    hts = [k.sb(f"ht{i}", [128, D], F32) for i in range(2)]
    x1s = [k.sb(f"x1{i}", [128, D], F32) for i in range(2)]
    h1s = [k.sb(f"h1{i}", [128, D], F32) for i in range(2)]
    tmps = [k.sb(f"lt{i}", [128, D], F32) for i in range(2)]
    sts = [k.sb(f"ls{i}", [128, 4], F32) for i in range(2)]
    hTf = [k.sb(f"hTf{i}", [128, 8, 128], F32) for i in range(2)]
    rl = [k.sb(f"rl{i}", [128, 36], F32) for i in range(2)]
    rs = [k.sb(f"rs{i}", [128, 16], F32) for i in range(2)]
    elm = [k.sb(f"elm{i}", [128, 32], F32) for i in range(2)]
    elm2 = [k.sb(f"elm2{i}", [128, 32], F32) for i in range(2)]
    oh1 = [k.sb(f"oh1{i}", [128, 32], F32) for i in range(2)]
    oh2 = [k.sb(f"oh2{i}", [128, 32], F32) for i in range(2)]
    for t in range(NT):
        p = t % 2
        tok = slice(t * 128, (t + 1) * 128)
        ht, x1, h1t, tmp, st = hts[p], x1s[p], h1s[p], tmps[p], sts[p]
        k.dma("sp", ht[:, :], h_d[tok, :])
        for half in range(2):
            bk = banks[half + 2 * p]
            for kc in range(8):
                k.mm(bk[:, :], mixT[:, kc, tok], wout[:, kc, half * 512:(half + 1) * 512],
                     start=(kc == 0), stop=(kc == 7))
            k.stt("dve", x1[:, half * 512:(half + 1) * 512], ht[:, half * 512:(half + 1) * 512], ALPHA,
                  bk[:, :], ALU.mult, ALU.add)
        layer_norm_tile(k, x1[:, :], h1t[:, :], g1[:, :], b1[:, :], tmp[:, :], st[:, :])
        k.act(acc[t][:, :], h1t[:, :], AF.Copy, scale=ALPHA)
        hf = hTf[p]
        for q4 in range(2):
            bk = banks[4 + q4 + 2 * p]
            for j in range(4):
                kc = q4 * 4 + j
                k.transpose(bk[:, j * 128:(j + 1) * 128], h1t[:, kc * 128:(kc + 1) * 128], ident[:, :],
                            inc=(j == 3))
            k.copy("dve", hf[:, q4 * 4:(q4 + 1) * 4, :],
                   bk[:, :].f(lambda a: a.rearrange("p (j t) -> p j t", j=4)))
        k.copy("act", h1T[:, :, tok], hf[:, :, :])
        bk = banks[p]
        for kc in range(8):
            k.mm(bk[:, 0:36], hf[:, kc, :], wr[:, kc, :], start=(kc == 0), stop=(kc == 7))
        l, s_, em, em2, o1, o2 = rl[p], rs[p], elm[p], elm2[p], oh1[p], oh2[p]
        cb = comb_all[:, t, :]
        k.tt("dve", l[:, :], bk[:, 0:36], brb[:, :], ALU.add)
        k.reduce("dve", s_[:, 0:1], l[:, 0:4], ALU.max)
        k.ts("dve", s_[:, 1:2], s_[:, 0:1], -1.0, None, ALU.mult)
        k.act(s_[:, 8:12], l[:, 0:4], AF.Exp, bias=s_[:, 1:2], scale=1.0, accum_out=s_[:, 2:3])
        k.op("dve", lambda e: e.reciprocal(s_[:, 3:4].ap, s_[:, 2:3].ap), [s_], [s_])
        k.ts("dve", s_[:, 12:16], l[:, 0:4], s_[:, 0:1], None, ALU.is_equal)
        k.ts("dve", s_[:, 12:16], s_[:, 12:16], BIG, -BIG, ALU.mult, ALU.add)
        k.tt("dve", em[:, :].f(lambda a: a.rearrange("p (g e) -> p g e", g=4)),
             l[:, 4:36].f(lambda a: a.rearrange("p (g e) -> p g e", g=4)),
             s_[:, 12:16].f(lambda a: a.unsqueeze(2).broadcast_to([128, 4, 8])), ALU.add)
        k.reduce("dve", s_[:, 4:5], em[:, :], ALU.max)
        k.ts("dve", o1[:, :], em[:, :], s_[:, 4:5], None, ALU.is_equal)
        k.stt("dve", em2[:, :], o1[:, :], -BIG, em[:, :], ALU.mult, ALU.add)
        k.reduce("dve", s_[:, 5:6], em2[:, :], ALU.max)
        k.ts("dve", o2[:, :], em2[:, :], s_[:, 5:6], None, ALU.is_equal)
        k.tt("dve", s_[:, 6:7], s_[:, 5:6], s_[:, 4:5], ALU.subtract)
        k.act(s_[:, 6:7], s_[:, 6:7], AF.Exp)
        k.ts("dve", s_[:, 7:8], s_[:, 6:7], 1.0, None, ALU.add)
        k.op("dve", lambda e: e.reciprocal(s_[:, 7:8].ap, s_[:, 7:8].ap), [s_], [s_])
        k.tt("dve", s_[:, 7:8], s_[:, 7:8], s_[:, 3:4], ALU.mult)
        k.tt("dve", s_[:, 6:7], s_[:, 6:7], s_[:, 7:8], ALU.mult)
        k.ts("dve", cb, o1[:, :], s_[:, 7:8], None, ALU.mult)
        k.stt("dve", cb, o2[:, :], s_[:, 6:7], cb, ALU.mult, ALU.add)
    k.pop()
    if upto == 1:
        dbg = k.dram("dbg", [128, NT * 32], F32, "ExternalOutput")
        k.dma("sp", dbg[:, :], comb_all[:, :, :].f(lambda a: a.rearrange("p a b -> p (a b)")))
        dbg2 = k.dram("dbg2", [ntok, D], F32, "ExternalOutput")
        for t in range(NT):
            k.dma("sp", dbg2[t * 128:(t + 1) * 128, :], acc[t][:, :])
        k.finish([dbg, dbg2])
        return nc

    k.push()
    NW = 3
    ewg = [k.sb(f"ewg{i}", [128, 8, 256], BF16) for i in range(NW)]
    ewu = [k.sb(f"ewu{i}", [128, 8, 256], BF16) for i in range(NW)]
    ewd = [k.sb(f"ewd{i}", [128, 2, D], BF16) for i in range(NW)]
    sgs = [k.sb(f"sG{i}", [128, 512], BF16) for i in range(2)]
    hcs = [k.sb(f"Hc{i}", [128, 2, 512], BF16) for i in range(2)]
    n1 = 0
    n2 = 0
    for e in range(NEXP):
        wgt, wut, wdt = ewg[e % NW], ewu[e % NW], ewd[e % NW]
        k.dma("pool", wgt[:, :, :], ewg_d.v(ewg_d.h[e].rearrange("(kc p) f -> p kc f", p=128)))
        k.dma("pool", wut[:, :, :], ewu_d.v(ewu_d.h[e].rearrange("(kc p) f -> p kc f", p=128)))
        k.dma("pool", wdt[:, :, :], ewd_d.v(ewd_d.h[e].rearrange("(fc p) d -> p fc d", p=128)))
        for tb in range(NB):
            tsl = slice(tb * 512, (tb + 1) * 512)
            hc = hcs[tb % 2]
            for fc in range(2):
                bG = banks[1 + n1 % 2]
                bU = banks[3 + n1 % 2]
                sgt = sgs[n1 % 2]
                n1 += 1
                for kc in range(8):
                    k.mm(bG[:, :], wgt[:, kc, fc * 128:(fc + 1) * 128], h1T[:, kc, tsl], start=(kc == 0), stop=(kc == 7))
                for kc in range(8):
                    k.mm(bU[:, :], wut[:, kc, fc * 128:(fc + 1) * 128], h1T[:, kc, tsl], start=(kc == 0), stop=(kc == 7))
                k.act(sgt[:, :], bG[:, :], AF.Silu)
                k.tt("dve", hc[:, fc, :], bU[:, :], sgt[:, :], ALU.mult)
            for tt_ in range(4):
                t = tb * 4 + tt_
                for half in range(2):
                    bO = banks[5 + n2 % 3]
                    n2 += 1
                    for fc in range(2):
                        k.mm(bO[:, :], hc[:, fc, tt_ * 128:(tt_ + 1) * 128], wdt[:, fc, half * 512:(half + 1) * 512],
                             start=(fc == 0), stop=(fc == 1))
                    k.stt("dve", acc[t][:, half * 512:(half + 1) * 512], bO[:, :], comb_all[:, t, e:e + 1],
                          acc[t][:, half * 512:(half + 1) * 512], ALU.mult, ALU.add)
    k.pop()

    k.push()
    g2 = k.sb("g2", [128, D], F32)
    b2 = k.sb("b2", [128, D], F32)
    k.dma("sp", g2[:, :], bcast_row(ln2g_d))
    k.dma("sp", b2[:, :], bcast_row(ln2b_d))
    ys = [k.sb(f"y{i}", [128, D], F32) for i in range(2)]
    tmps = [k.sb(f"lt{i}", [128, D], F32) for i in range(2)]
    sts = [k.sb(f"ls{i}", [128, 4], F32) for i in range(2)]
    for t in range(NT):
        p = t % 2
        layer_norm_tile(k, acc[t][:, :], ys[p][:, :], g2[:, :], b2[:, :], tmps[p][:, :], sts[p][:, :])
        k.dma("sp", out_d[t * 128:(t + 1) * 128, :], ys[p][:, :])
    k.finish([out_d])
    k.pop()
    return nc


def stage_c_consts():
    return np.eye(128, dtype=np.float32)


def run_stage_c(h, oa, ob, oc, P, l, upto=9):
    T_ = h.shape[0]
    per = T_ // NCORES
    nc = build_stage_c(per, upto)
    ident = stage_c_consts()
    w_in = P["w_in"][l]
    common = {
        "w_gates": np.ascontiguousarray(w_in[:, 4520:]),
        "w_br_a": P["w_br_a"][l], "w_br_b": P["w_br_b"][l], "w_br_c": P["w_br_c"][l],
        "w_out": P["w_out"][l],
        "ln1_g": P["ln1_g"][l].reshape(1, D), "ln1_b": P["ln1_b"][l].reshape(1, D),
        "ln2_g": P["ln2_g"][l].reshape(1, D), "ln2_b": P["ln2_b"][l].reshape(1, D),
        "w_router": np.ascontiguousarray(np.concatenate([P["router_group_w"][l], P["router_expert_w"][l]], axis=1)),
        "b_router": np.concatenate([P["router_group_b"][l], P["router_expert_b"][l]]).reshape(1, 36),
        "exp_w_gate": P["exp_w_gate"][l], "exp_w_up": P["exp_w_up"][l], "exp_w_down": P["exp_w_down"][l],
        "ident": ident,
    }
    in_maps = []
    for c in range(NCORES):
        sl = slice(c * per, (c + 1) * per)
        m = dict(common)
        m["h"] = np.ascontiguousarray(h[sl])
        m["hT"] = np.ascontiguousarray(h[sl].T)
        m["oaT"] = np.ascontiguousarray(oa[sl].T)
        m["obT"] = np.ascontiguousarray(ob[sl].T)
        m["ocT"] = np.ascontiguousarray(oc[sl].T)
        in_maps.append(m)
    res = run_bass_kernel_spmd(nc, in_maps, core_ids=list(range(NCORES)))
    if upto < 9:
        return res.results
    return np.concatenate([r["out"] for r in res.results], axis=0)


QK_SCALE = 96 ** -0.5
TWO_PI = 2.0 * np.pi


class BankRR:
    def __init__(self, banks):
        self.banks = banks
        self.i = 0

    def __call__(self):
        b = self.banks[self.i % len(self.banks)]
        self.i += 1
        return b


def build_mla(T=S, nblk=None):
    nc = bass.Bass("TRN2", target_bir_lowering=False)
    k = K(nc)
    NBLK = T // 512 if nblk is None else nblk
    hT_d = k.dram("hT", [D, T], F32, "ExternalInput")
    wlat_d = k.dram("w_lat", [D, 448], F32, "ExternalInput")
    gq_d = k.dram("g_q", [128, 2], F32, "ExternalInput")
    gkv_d = k.dram("g_kv", [128, 1], F32, "ExternalInput")
    wuq_d = k.dram("w_uq", [256, 256], F32, "ExternalInput")
    wuk_d = k.dram("w_uk", [128, 128], F32, "ExternalInput")
    wuv_d = k.dram("w_uv", [128, 128], F32, "ExternalInput")
    pos_d = k.dram("pos", [1, T], I32, "ExternalInput")
    frq_d = k.dram("frq", [128, 1], F32, "ExternalInput")
    sgn_d = k.dram("sgn", [128, 1], F32, "ExternalInput")
    tri_d = k.dram("tri", [128, 128], F32, "ExternalInput")
    esel_d = k.dram("esel", [128, 64], F32, "ExternalInput")
    oT_d = k.dram("oT", [128, T], F32, "ExternalOutput")

    banks = [k.ps(f"bank{i}", [128, 512], F32) for i in range(8)]
    nb = BankRR(banks)

    wlat = k.sb("wlat", [128, 8, 448], BF16)
    for kc in range(8):
        k.dma("pool", wlat[:, kc, :], wlat_d[kc * 128:(kc + 1) * 128, :])
    gq = k.sb("gq", [128, 2], F32)
    gkv = k.sb("gkv", [128, 1], F32)
    frq = k.sb("frq", [128, 1], F32)
    sgn = k.sb("sgn", [128, 1], F32)
    esel = k.sb("esel", [128, 64], F32)
    tri = k.sb("tri", [128, 128], BF16)
    k.dma("sp", gq[:, :], gq_d[:, :])
    k.dma("sp", gkv[:, :], gkv_d[:, :])
    k.dma("sp", frq[:, :], frq_d[:, :])
    k.dma("sp", sgn[:, :], sgn_d[:, :])
    k.dma("sp", esel[:, :], esel_d[:, :])
    k.dma("pool", tri[:, :], tri_d[:, :])
    wtmp = k.sb("wtmp", [128, 2, 256], F32)
    wuq = k.sb("wuq", [128, 2, 256], BF16)
    for c in range(2):
        k.dma("sp", wtmp[:, c, :], wuq_d[c * 128:(c + 1) * 128, :])
    for c in range(2):
        k.ts("dve", wuq[:, c, :], wtmp[:, c, :], gq[:, c:c + 1], QK_SCALE, ALU.mult, ALU.mult)
    wtmp2 = k.sb("wtmp2", [128, 2, 128], F32)
    wuk = k.sb("wuk", [128, 128], BF16)
    wuv = k.sb("wuv", [128, 128], BF16)
    k.dma("sp", wtmp2[:, 0, :], wuk_d[:, :])
    k.dma("sp", wtmp2[:, 1, :], wuv_d[:, :])
    k.ts("dve", wuk[:, :], wtmp2[:, 0, :], gkv[:, 0:1], None, ALU.mult)
    k.ts("dve", wuv[:, :], wtmp2[:, 1, :], gkv[:, 0:1], None, ALU.mult)
    ones = k.sb("ones", [128, 128], F32)
    k.memset("dve", ones[:, :], 1.0)

    kT = [k.sb(f"kT{h}", [96, T], BF16) for h in range(2)]
    qT = [k.sb(f"qT{h}", [96, T], BF16) for h in range(2)]
    Vp = k.sb("Vp", [128, 2, T // 128, 128], BF16)
    k.memset("pool", Vp[:, :, :, :], 1.0)
    mx = k.sb("mx", [128, 4], F32)
    k.memset("dve", mx[:, :], 0.0)

    hTb = [k.sb(f"hTb{i}", [128, 8, 512], BF16) for i in range(2)]
    posi = [k.sb(f"posi{i}", [128, 512], I32) for i in range(2)]
    ang = k.sb("ang", [128, 512], F32)
    ang2 = k.sb("ang2", [128, 512], F32)
    ni = k.sb("ni", [128, 512], I32)
    nf = k.sb("nf", [128, 512], F32)
    cs = [k.sb(f"cs{i}", [128, 512], F32) for i in range(2)]
    sn = [k.sb(f"sn{i}", [128, 512], F32) for i in range(2)]
    cq_sb = [k.sb(f"cq_sb{i}", [128, 512], F32) for i in range(2)]
    sq_sb = [k.sb(f"sq_sb{i}", [128, 512], F32) for i in range(2)]
    ckv_sb = k.sb("ckv_sb", [128, 512], F32)
    sqkv = k.sb("sqkv", [128, 512], F32)
    rq = k.sb("rq", [128, 512], F32)
    rkv = k.sb("rkv", [128, 512], F32)
    cqn = [k.sb(f"cqn{i}", [128, 512], BF16) for i in range(2)]
    ckvn = k.sb("ckvn", [128, 512], BF16)
    t1 = k.sb("t1", [128, 512], F32)
    t2 = k.sb("t2", [128, 512], F32)
    nsq = k.sb("nsq", [96, 512], F32)
    mtmp = k.sb("mtmp", [128, 1], F32)

    def sincos(dst, src_ang):
        r6 = slice(64, 96)
        k.ts("dve", nf[r6, :], src_ang[r6, :], 1.0 / TWO_PI, None, ALU.mult)
        k.copy("dve", ni[r6, :], nf[r6, :])
        k.copy("dve", nf[r6, :], ni[r6, :])
        k.stt("dve", nf[r6, :], nf[r6, :], -TWO_PI, src_ang[r6, :], ALU.mult, ALU.add)
        k.ts("dve", nf[r6, :], nf[r6, :], 3.1415925, -3.1415925, ALU.min, ALU.max)
        k.act(dst[r6, :], nf[r6, :], AF.Sin)

    def rope_rows(dsts, bA, bB, cst, snt, col):
        r6 = slice(64, 96)
        k.stt("dve", t1[r6, :], bB[r6, :], sgn[r6, 0:1], snt[r6, :], ALU.mult, ALU.mult)
        k.tt("dve", t2[r6, :], bA[r6, :], cst[r6, :], ALU.mult)
        for i, d_ in enumerate(dsts):
            k.tt("pool", d_[r6, col], t1[r6, :], t2[r6, :], ALU.add)

    def normsq(src, col, slot):
        k.act(nsq[:, :], src[0:96, col], AF.Square)
        bk = nb()
        k.mm(bk[:, :], ones[0:96, :], nsq[:, :])
        k.reduce("dve", mtmp[:, :], bk[:, :], ALU.max)
        k.tt("dve", mx[:, slot:slot + 1], mx[:, slot:slot + 1], mtmp[:, :], ALU.max)

    for blk in range(NBLK):
        col = slice(blk * 512, (blk + 1) * 512)
        hb = hTb[blk % 2]
        pi_ = posi[blk % 2]
        cst, snt = cs[blk % 2], sn[blk % 2]
        for kc in range(8):
            k.dma("pool", hb[:, kc, :], hT_d[kc * 128:(kc + 1) * 128, col])
        k.dma("sp", pi_[:, :], pos_d.v(pos_d.h[:, col].partition_broadcast(128)))
        r6 = slice(64, 96)
        k.copy("dve", ang[r6, :], pi_[r6, :])
        k.ts("dve", ang[r6, :], ang[r6, :], frq[r6, 0:1], None, ALU.mult)
        k.ts("dve", ang2[r6, :], ang[r6, :], float(np.pi / 2), None, ALU.add)
        sincos(snt, ang)
        sincos(cst, ang2)
        for c in range(2):
            bk = nb()
            for kc in range(8):
                k.mm(bk[:, :], wlat[:, kc, c * 128:(c + 1) * 128], hb[:, kc, :], start=(kc == 0), stop=(kc == 7))
            k.copy("act", cq_sb[c][:, :], bk[:, :])
            k.act(sq_sb[c][:, :], bk[:, :], AF.Square)
        bk = nb()
        for kc in range(8):
            k.mm(bk[:, :], wlat[:, kc, 256:384], hb[:, kc, :], start=(kc == 0), stop=(kc == 7))
        k.copy("act", ckv_sb[:, :], bk[:, :])
        k.act(sqkv[:, :], bk[:, :], AF.Square)
        bA = nb()
        for kc in range(8):
            k.mm(bA[0:96, :], wlat[:, kc, 320:416], hb[:, kc, :], start=(kc == 0), stop=(kc == 7))
        bB = nb()
        for kc in range(8):
            k.mm(bB[0:96, :], wlat[:, kc, 352:448], hb[:, kc, :], start=(kc == 0), stop=(kc == 7))
        rope_rows([kT[0], kT[1]], bA, bB, cst, snt, col)
        bk = nb()
        k.mm(bk[:, :], ones[:, :], sq_sb[0][:, :], start=True, stop=False)
        k.mm(bk[:, :], ones[:, :], sq_sb[1][:, :], start=False, stop=True)
        k.rstd(rq[:, :], bk[:, :], 1.0 / 256, EPS)
        bk = nb()
        k.mm(bk[:, :], ones[:, :], sqkv[:, :])
        k.rstd(rkv[:, :], bk[:, :], 1.0 / 128, EPS)
        for c in range(2):
            k.tt("dve", cqn[c][:, :], cq_sb[c][:, :], rq[:, :], ALU.mult)
        k.tt("pool", ckvn[:, :], ckv_sb[:, :], rkv[:, :], ALU.mult)
        for hd in range(2):
            bk = nb()
            k.mm(bk[0:64, :], wuk[:, hd * 64:(hd + 1) * 64], ckvn[:, :])
            k.copy("act", kT[hd][0:64, col], bk[0:64, :])
        bk = nb()
        for tt_ in range(4):
            k.mm(bk[:, tt_ * 128:(tt_ + 1) * 128], ckvn[:, tt_ * 128:(tt_ + 1) * 128], wuv[:, :],
                 start=True, stop=True, inc=(tt_ == 3))
        k.copy("act", Vp[:, :, blk * 4:(blk + 1) * 4, 0:64],
               bk[:, :].f(lambda a: a.rearrange("p (t h d) -> p h t d", t=4, h=2)))
        for hd in range(2):
            bA = nb()
            for c in range(2):
                k.mm(bA[0:96, :], wuq[:, c, hd * 128:hd * 128 + 96], cqn[c][:, :], start=(c == 0), stop=(c == 1))
            bB = nb()
            for c in range(2):
                k.mm(bB[0:96, :], wuq[:, c, hd * 128 + 32:hd * 128 + 128], cqn[c][:, :], start=(c == 0), stop=(c == 1))
            k.copy("act", qT[hd][0:64, col], bA[0:64, :])
            rope_rows([qT[hd]], bA, bB, cst, snt, col)
        for hd in range(2):
            normsq(qT[hd], col, hd)
            normsq(kT[hd], col, 2 + hd)

    negc = k.sb("negc", [128, 2], F32)
    k.tt("dve", negc[:, :], mx[:, 0:2], mx[:, 2:4], ALU.mult)
    k.act(negc[:, :], negc[:, :], AF.Sqrt)
    k.ts("dve", negc[:, :], negc[:, :], -1.0, None, ALU.mult)

    acc_banks = BankRR(banks[0:2])
    s_banks = BankRR(banks[2:7])
    den_bank = banks[7]
    pT = [k.sb(f"pT{i}", [128, 512], BF16) for i in range(4)]
    osb = [k.sb(f"osb{i}", [128, 512], F32) for i in range(2)]
    ores = [k.sb(f"ores{i}", [64, 512], F32) for i in range(2)]
    npt = 0
    no = 0
    for hd in range(2):
        for qi in range(NBLK):
            oacc = acc_banks()
            nkb = 4 * qi + 4
            for kb in range(nkb):
                r = kb - 4 * qi
                c0 = 128 * r if r > 0 else 0
                qcol = slice(qi * 512 + c0, (qi + 1) * 512)
                sb_ = s_banks()
                pt = pT[npt % 4]
                npt += 1
                k.mm(sb_[:, c0:512], kT[hd][:, kb * 128:(kb + 1) * 128], qT[hd][:, qcol])
                k.act(pt[:, c0:512], sb_[:, c0:512], AF.Exp, bias=negc[:, hd:hd + 1], scale=1.0)
                if r >= 0:
                    k.tt("pool", pt[:, c0:c0 + 128], pt[:, c0:c0 + 128], tri[:, :], ALU.mult)
                k.mm(oacc[:, c0:512], Vp[:, hd, kb, :], pt[:, c0:512], start=(kb == 0), stop=(kb == nkb - 1))
            ob = osb[no % 2]
            orr = ores[no % 2]
            no += 1
            k.copy("act", ob[:, :], oacc[:, :])
            k.op("dve", lambda e: e.reciprocal(ob[64:128, :].ap, ob[64:128, :].ap), [ob], [ob])
            k.mm(den_bank[0:64, :], esel[:, :], ob[:, :])
            k.tt("dve", orr[:, :], ob[0:64, :], den_bank[0:64, :], ALU.mult)
            k.dma("sp", oT_d[hd * 64:(hd + 1) * 64, qi * 512:(qi + 1) * 512], orr[:, :])
    k.finish([oT_d])
    return nc


def mla_inputs(hT_b, pos_b, P, l, j, consts_only=False):
    inv_freq = (10000.0 ** (-np.arange(16, dtype=np.float32) / np.float32(16))).astype(np.float32)
    frq = np.zeros((128, 1), np.float32)
    frq[64:80, 0] = inv_freq
    frq[80:96, 0] = inv_freq
    sgn = np.zeros((128, 1), np.float32)
    sgn[64:80] = -1.0
    sgn[80:96] = 1.0
    tri = (np.arange(128)[:, None] <= np.arange(128)[None, :]).astype(np.float32)
    esel = np.zeros((128, 64), np.float32)
    esel[64, :] = 1.0
    if consts_only:
        return {"frq": frq, "sgn": sgn, "tri": tri, "esel": esel}
    w_in = P["w_in"][l]
    kr = w_in[:, 384:416]
    wlat = np.concatenate([w_in[:, 0:384], kr, kr[:, 16:32], kr[:, 0:16]], axis=1)
    wuq = P["mla_w_uq"][l].reshape(256, 8, 96)
    wukv = P["mla_w_ukv"][l].reshape(128, 8, 128)
    heads = (2 * j, 2 * j + 1)
    wuq_c = np.concatenate([np.concatenate([wuq[:, h, 0:64], wuq[:, h, 64:96], wuq[:, h, 80:96], wuq[:, h, 64:80]], axis=1)
                            for h in heads], axis=1)
    wuk_c = np.concatenate([wukv[:, h, 0:64] for h in heads], axis=1)
    wuv_c = np.concatenate([wukv[:, h, 64:128] for h in heads], axis=1)
    inv_freq = (10000.0 ** (-np.arange(16, dtype=np.float32) / np.float32(16))).astype(np.float32)
    frq = np.zeros((128, 1), np.float32)
    frq[64:80, 0] = inv_freq
    frq[80:96, 0] = inv_freq
    sgn = np.zeros((128, 1), np.float32)
    sgn[64:80] = -1.0
    sgn[80:96] = 1.0
    tri = (np.arange(128)[:, None] <= np.arange(128)[None, :]).astype(np.float32)
    esel = np.zeros((128, 64), np.float32)
    esel[64, :] = 1.0
    return {
        "hT": hT_b, "w_lat": np.ascontiguousarray(wlat),
        "g_q": np.ascontiguousarray(P["mla_q_norm"][l].reshape(2, 128).T),
        "g_kv": np.ascontiguousarray(P["mla_kv_norm"][l].reshape(128, 1)),
        "w_uq": np.ascontiguousarray(wuq_c), "w_uk": np.ascontiguousarray(wuk_c), "w_uv": np.ascontiguousarray(wuv_c),
        "pos": np.ascontiguousarray(pos_b.reshape(1, -1).astype(np.int32)),
        "frq": frq, "sgn": sgn, "tri": tri, "esel": esel,
    }


HC = 32


def hg_consts():
    t = np.arange(128)
    ch = t // HC
    same = ch[:, None] == ch[None, :]
    U = (same & (t[:, None] <= t[None, :])).astype(np.float32)
    mid = ch * HC + (HC // 2 - 1)
    Umid = (same & (t[:, None] <= mid[None, :])).astype(np.float32)
    W = (same & (t[:, None] > t[None, :])).astype(np.float32)
    cones = (ch[:, None] == np.arange(4)[None, :]).astype(np.float32)
    maskbd = (same & (t[:, None] <= t[None, :])).astype(np.float32)
    return {"cU": U, "cUrel": (U - Umid).astype(np.float32), "cW": W, "cones": cones, "maskbd": maskbd,
            "rowmask": cones.copy()}


def build_hg(T=S, layer=0):
    nc = bass.Bass("TRN2", target_bir_lowering=False)
    k = K(nc)
    NBLK = T // 512
    hT_d = k.dram("hT", [D, T], F32, "ExternalInput")
    w_d = k.dram("w_hg", [D, 512], F32, "ExternalInput")
    lbr_d = k.dram("lb_rows", [DEPTH, 128], F32, "ExternalInput")
    lbc_d = k.dram("lb_cols", [128, DEPTH], F32, "ExternalInput")
    on_d = k.dram("o_norm", [1, 128], F32, "ExternalInput")
    cU_d = k.dram("cU", [128, 128], F32, "ExternalInput")
    cUrel_d = k.dram("cUrel", [128, 128], F32, "ExternalInput")
    cW_d = k.dram("cW", [128, 128], F32, "ExternalInput")
    cones_d = k.dram("cones", [128, 4], F32, "ExternalInput")
    mbd_d = k.dram("maskbd", [128, 128], F32, "ExternalInput")
    rm_d = k.dram("rowmask", [128, 4], F32, "ExternalInput")
    o_d = k.dram("o", [T, 128], F32, "ExternalOutput")

    banks = [k.ps(f"bank{i}", [128, 512], F32) for i in range(8)]
    nb = BankRR(banks)

    whg = k.sb("whg", [128, 8, 512], BF16)
    for kc in range(8):
        k.dma("pool", whg[:, kc, :], w_d[kc * 128:(kc + 1) * 128, :])
    cU = k.sb("cU", [128, 128], F32)
    cUrel = k.sb("cUrel", [128, 128], F32)
    cW = k.sb("cW", [128, 128], F32)
    cones = k.sb("cones", [128, 4], F32)
    mbd = k.sb("mbd", [128, 128], F32)
    rowm = k.sb("rowm", [128, 4], F32)
    gb = k.sb("gb", [128, 128], F32)
    for t_, d_ in ((cU, cU_d), (cUrel, cUrel_d), (cW, cW_d), (cones, cones_d), (mbd, mbd_d), (rowm, rm_d)):
        k.dma("sp", t_[:, :], d_[:, :])
    k.dma("sp", gb[:, :], bcast_row(on_d))

    def lower_bound(x, n, name):
        m = k.sb(name + "_m", [128, n], F32)
        e = k.sb(name + "_e", [128, DEPTH, n], F32)
        ssum = k.sb(name + "_s", [128, n], F32)
        lb = k.sb(name + "_lb", [128, n], F32)
        oml = k.sb(name + "_oml", [128, n], F32)
        k.copy("dve", m[:, :], x[:, 0, :])
        for i in range(1, DEPTH):
            k.tt("dve", m[:, :], m[:, :], x[:, i, :], ALU.max)
        for i in range(DEPTH):
            k.tt("dve", e[:, i, :], x[:, i, :], m[:, :], ALU.subtract)
        k.act(e[:, :, :], e[:, :, :], AF.Exp)
        k.copy("dve", ssum[:, :], e[:, 0, :])
        for i in range(1, DEPTH):
            k.tt("dve", ssum[:, :], ssum[:, :], e[:, i, :], ALU.add)
        k.op("dve", lambda en: en.reciprocal(ssum[:, :].ap, ssum[:, :].ap), [ssum], [ssum])
        for i in range(DEPTH):
            k.tt("dve", e[:, i, :], e[:, i, :], ssum[:, :], ALU.mult)
        k.copy("dve", lb[:, :], e[:, 0, :])
        for i in range(1, layer + 1):
            k.tt("dve", lb[:, :], lb[:, :], e[:, i, :], ALU.add)
        k.tt("dve", lb[:, :], lb[:, :], e[:, 0, :], ALU.subtract)
        k.ts("dve", oml[:, :], lb[:, :], -1.0, 1.0, ALU.mult, ALU.add)
        return lb, oml

    xr = k.sb("xr", [128, DEPTH, 128], F32)
    for i in range(DEPTH):
        k.dma("sp", xr[:, i, :], lbr_d.v(lbr_d.h[i:i + 1, :].partition_broadcast(128)))
    lb_b, oml_b = lower_bound(xr, 128, "lbr")
    xc = k.sb("xc", [128, DEPTH, 1], F32)
    k.dma("sp", xc[:, :, 0], lbc_d[:, :])
    lb_c, oml_c = lower_bound(xc, 1, "lbc")
    noml_c = k.sb("noml_c", [128, 1], F32)
    k.ts("dve", noml_c[:, :], oml_c[:, :], -1.0, None, ALU.mult)

    NS = 8
    Sf = [k.sb(f"Sf{i}", [128, 128], F32) for i in range(2)]
    Sb = [k.sb(f"Sb{i}", [128, 128], BF16) for i in range(NS)]
    k.memset("dve", Sf[0][:, :], 0.0)
    k.memset("dve", Sb[0][:, :], 0.0)
    Z = [k.sb(f"Z{i}", [128, 4, 128], BF16) for i in range(2)]
    for z in Z:
        k.memset("pool", z[:, :, :], 0.0)
    si = 0

    hTb = [k.sb(f"hTb{i}", [128, 8, 512], BF16) for i in range(2)]
    qTs = [k.sb(f"qTs{i}", [128, 512], F32) for i in range(2)]
    kTs = [k.sb(f"kTs{i}", [128, 512], F32) for i in range(2)]

    def dbl(name, shape, dt, n=2):
        return [k.sb(f"{name}{i}", shape, dt) for i in range(n)]

    sg = dbl("sg", [128, 128], F32)
    uu = dbl("uu", [128, 128], F32)
    ff = dbl("ff", [128, 128], F32)
    ktm = dbl("ktm", [128, 128], F32)
    logf = dbl("logf", [128, 128], F32)
    vbf = dbl("vbf", [128, 128], BF16)
    sgate = dbl("sgate", [128, 128], F32)
    e1 = dbl("e1", [128, 128], F32)
    e2 = dbl("e2", [128, 128], F32)
    e3 = dbl("e3", [128, 128], F32)
    e4 = dbl("e4", [128, 128], F32)
    dl = dbl("dl", [128, 4], F32)
    qpT = dbl("qpT", [128, 128], BF16)
    kpT = dbl("kpT", [128, 128], BF16)
    kdp = dbl("kdp", [128, 4, 128], BF16)
    attm = dbl("attm", [128, 128], BF16)
    osq = dbl("osq", [128, 128], F32)
    ost = dbl("ost", [128, 2], F32)
    y1 = dbl("y1", [128, 128], F32)
    y2 = dbl("y2", [128, 128], F32)

    nt = 0
    for blk in range(NBLK):
        col = slice(blk * 512, (blk + 1) * 512)
        hb = hTb[blk % 2]
        for kc in range(8):
            k.dma("pool", hb[:, kc, :], hT_d[kc * 128:(kc + 1) * 128, col])
        qT_s, kT_s = qTs[blk % 2], kTs[blk % 2]
        bq = nb()
        for kc in range(8):
            k.mm(bq[:, :], whg[:, kc, 0:128], hb[:, kc, :], start=(kc == 0), stop=(kc == 7))
        k.act(qT_s[:, :], bq[:, :], AF.Silu)
        bz = nb()
        for kc in range(8):
            k.mm(bz[:, :], whg[:, kc, 128:256], hb[:, kc, :], start=(kc == 0), stop=(kc == 7))
        k.act(kT_s[:, :], bz[:, :], AF.Sigmoid)
        k.ts("dve", kT_s[:, :], kT_s[:, :], noml_c[:, 0:1], oml_c[:, 0:1], ALU.mult, ALU.add)
        for tt_ in range(4):
            p = nt % 2
            nt += 1
            tcol = slice(tt_ * 128, (tt_ + 1) * 128)
            tok = slice(blk * 512 + tt_ * 128, blk * 512 + (tt_ + 1) * 128)
            btm = nb()
            for kc in range(8):
                k.mm(btm[:, 0:384], hb[:, kc, tcol], whg[:, kc, 128:512], start=(kc == 0), stop=(kc == 7))
            k.act(sg[p][:, :], btm[:, 0:128], AF.Sigmoid)
            k.copy("act", vbf[p][:, :], btm[:, 128:256])
            k.act(sgate[p][:, :], btm[:, 256:384], AF.Silu)
            k.tt("dve", uu[p][:, :], sg[p][:, :], oml_b[:, :], ALU.mult)
            k.tt("pool", ff[p][:, :], uu[p][:, :], lb_b[:, :], ALU.add)
            k.tt("pool", ktm[p][:, :], oml_b[:, :], uu[p][:, :], ALU.subtract)
            k.ts("dve", ff[p][:, :], ff[p][:, :], 1e-30, None, ALU.max)
            k.act(logf[p][:, :], ff[p][:, :], AF.Ln)
            bc = nb()
            k.mm(bc[:, 0:128], logf[p][:, :], cU[:, :], inc=False)
            k.mm(bc[:, 128:256], logf[p][:, :], cUrel[:, :], inc=False)
            k.mm(bc[:, 256:384], cW[:, :], logf[p][:, :], inc=False)
            k.mm(bc[:, 384:388], logf[p][:, :], cones[:, :], inc=True)
            k.act(e1[p][:, :], bc[:, 0:128], AF.Exp)
            k.act(e2[p][:, :], bc[:, 128:256], AF.Exp)
            k.act(e3[p][:, :], bc[:, 128:256], AF.Exp, scale=-1.0)
            k.act(e4[p][:, :], bc[:, 256:384], AF.Exp)
            k.act(dl[p][:, :], bc[:, 384:388], AF.Exp)
            z = Z[p]
            zdiag = z.v(bass.AP(z.h, 0, [[512, 128], [160, 4], [1, 32]]))
            k.tt("dve", zdiag, qT_s[:, tcol].f(lambda a: a.rearrange("p (c x) -> p c x", c=4)),
                 e1[p][:, :].f(lambda a: a.rearrange("p (c x) -> p c x", c=4)), ALU.mult)
            k.tt("pool", qpT[p][:, :], qT_s[:, tcol], e2[p][:, :], ALU.mult)
            k.tt("pool", kpT[p][:, :], kT_s[:, tcol], e3[p][:, :], ALU.mult)
            for c in range(4):
                k.stt("dve" if c % 2 == 0 else "pool", kdp[p][:, c, :], ktm[p][:, :], rowm[:, c:c + 1], e4[p][:, :],
                      ALU.mult, ALU.mult) if c % 2 == 0 else None
            for c in range(4):
                if c % 2 == 1:
                    k.stt("dve", kdp[p][:, c, :], ktm[p][:, :], rowm[:, c:c + 1], e4[p][:, :], ALU.mult, ALU.mult)
            ba = nb()
            k.mm(ba[:, 0:128], kpT[p][:, :], qpT[p][:, :])
            k.tt("dve", attm[p][:, :], ba[:, 0:128], mbd[:, :], ALU.mult)
            bs = nb()
            for c in range(4):
                k.mm(bs[:, c * 128:(c + 1) * 128], kdp[p][:, c, :], vbf[p][:, :], inc=(c == 3))
            bo = nb()
            for c in range(4):
                k.mm(bo[:, 0:128], z[:, c, :], Sb[(si + c) % NS][:, :], start=(c == 0), stop=False, inc=False)
                s_old = Sf[(si + c) % 2]
                s_new = Sf[(si + c + 1) % 2]
                k.stt("dve", s_new[:, :], s_old[:, :], dl[p][:, c:c + 1], bs[:, c * 128:(c + 1) * 128],
                      ALU.mult, ALU.add)
                k.copy("act", Sb[(si + c + 1) % NS][:, :], s_new[:, :])
            k.mm(bo[:, 0:128], attm[p][:, :], vbf[p][:, :], start=False, stop=True, inc=True)
            si += 4
            k.act(osq[p][:, :], bo[:, 0:128], AF.Square, accum_out=ost[p][:, 0:1])
            k.rstd(ost[p][:, 1:2], ost[p][:, 0:1], 1.0 / 128, EPS)
            k.stt("dve", y1[p][:, :], bo[:, 0:128], ost[p][:, 1:2], gb[:, :], ALU.mult, ALU.mult)
            k.tt("pool", y2[p][:, :], y1[p][:, :], sgate[p][:, :], ALU.mult)
            k.dma("sp", o_d[tok, :], y2[p][:, :])
    k.finish([o_d])
    return nc


def hg_inputs(hT_b, P, l, j):
    w_in = P["w_in"][l]
    cols = [2472 + j * 128, 2984 + j * 128, 3496 + j * 128, 4008 + j * 128]
    w = np.concatenate([w_in[:, c:c + 128] for c in cols], axis=1)
    lb = P["hg_lower_bounds"][:, j * 128:(j + 1) * 128]
    m = {"hT": hT_b, "w_hg": np.ascontiguousarray(w), "lb_rows": np.ascontiguousarray(lb),
         "lb_cols": np.ascontiguousarray(lb.T), "o_norm": P["hg_o_norm"][l].reshape(1, 128)}
    m.update(hg_consts())
    return m


MASKV = 30000.0


def dn_consts():
    t = np.arange(128)
    uinc = (t[:, None] <= t[None, :]).astype(np.float32)
    lpos_s = np.where(t[None, :] < t[:, None], 0.0, MASKV).astype(np.float32)
    uneg = np.where(t[:, None] <= t[None, :], 0.0, -MASKV).astype(np.float32)
    return {"uinc": uinc, "lpos_s": lpos_s, "uneg": uneg, "ident": np.eye(128, dtype=np.float32)}


def build_dn(T=S):
    nc = bass.Bass("TRN2", target_bir_lowering=False)
    k = K(nc)
    NBLK = T // 512
    hT_d = k.dram("hT", [D, T], F32, "ExternalInput")
    w_d = k.dram("w_dn", [D, 384 + 130], F32, "ExternalInput")
    cw_d = k.dram("conv_w", [128, 3, 4], F32, "ExternalInput")
    alog_d = k.dram("a_log", [1, 1], F32, "ExternalInput")
    dtb_d = k.dram("dt_bias", [1, 1], F32, "ExternalInput")
    on_d = k.dram("o_norm", [1, 128], F32, "ExternalInput")
    uinc_d = k.dram("uinc", [128, 128], F32, "ExternalInput")
    lpos_d = k.dram("lpos_s", [128, 128], F32, "ExternalInput")
    uneg_d = k.dram("uneg", [128, 128], F32, "ExternalInput")
    ident_d = k.dram("ident", [128, 128], F32, "ExternalInput")
    o_d = k.dram("o", [T, 128], F32, "ExternalOutput")

    banks = [k.ps(f"bank{i}", [128, 512], F32) for i in range(8)]
    nb = BankRR(banks)

    wdn = k.sb("wdn", [128, 8, 514], BF16)
    for kc in range(8):
        k.dma("pool", wdn[:, kc, :], w_d[kc * 128:(kc + 1) * 128, :])
    cw = k.sb("cw", [128, 3, 4], F32)
    k.dma("sp", cw[:, :, :], cw_d[:, :, :])
    uinc = k.sb("uinc", [128, 128], F32)
    lpos = k.sb("lpos", [128, 128], F32)
    uneg = k.sb("uneg", [128, 128], F32)
    ident = k.sb("ident", [128, 128], F32)
    gb = k.sb("gb", [128, 128], F32)
    for t_, d_ in ((uinc, uinc_d), (lpos, lpos_d), (uneg, uneg_d), (ident, ident_d)):
        k.dma("sp", t_[:, :], d_[:, :])
    k.dma("sp", gb[:, :], bcast_row(on_d))
    sc = k.sb("sc", [128, 4], F32)
    k.dma("sp", sc[:, 0:1], bcast_row(alog_d))
    k.dma("sp", sc[:, 1:2], bcast_row(dtb_d))
    k.act(sc[:, 2:3], sc[:, 0:1], AF.Exp)
    k.ts("dve", sc[:, 2:3], sc[:, 2:3], -1.0, None, ALU.mult)
    ones = k.sb("ones", [128, 128], F32)
    k.memset("dve", ones[:, :], 1.0)

    Sf = [k.sb(f"S{i}", [128, 128], F32) for i in range(2)]
    k.memset("dve", Sf[0][:, :], 0.0)
    si = 0

    hTb = [k.sb(f"hTb{i}", [128, 8, 512], BF16) for i in range(2)]
    xh = [[k.sb(f"xh{w}_{i}", [128, 515], F32) for i in range(2)] for w in range(3)]
    for w in range(3):
        k.memset("dve", xh[w][1][:, 512:515], 0.0)
    cv = [k.sb(f"cv{w}", [128, 512], F32) for w in range(3)]
    sq = [k.sb(f"sq{w}", [128, 512], F32) for w in range(2)]
    rs = [k.sb(f"rs{w}", [128, 512], F32) for w in range(2)]

    def per_tile(name, shape, dt=F32):
        return [k.sb(f"{name}{i}", shape, dt) for i in range(4)]

    tmc = per_tile("tmc", [128, 8])
    sgate = per_tile("sgate", [128, 128])
    ktm = per_tile("ktm", [128, 128])
    vb = per_tile("vb", [128, 128])
    gbc = per_tile("gbc", [128, 128])
    xm = per_tile("xm", [128, 128])
    ym = per_tile("ym", [128, 128])
    dec_s = per_tile("dec_s", [128, 128])
    decT = per_tile("decT", [128, 128])
    egb = per_tile("egb", [128, 128])
    Mt = per_tile("M", [128, 128])
    Nt = per_tile("N", [128, 128])
    Pa = per_tile("Pa", [128, 128])
    Pat = per_tile("Pat", [128, 128])
    Pb = per_tile("Pb", [128, 128])
    Pbt = per_tile("Pbt", [128, 128])
    Rr = per_tile("R", [128, 128])
    Rt = per_tile("Rt", [128, 128])
    qkT = per_tile("qkT", [128, 128])
    qdT = per_tile("qdT", [128, 128])
    kbg = per_tile("kbg", [128, 128])
    kdec = per_tile("kdec", [128, 128])
    u_sb = per_tile("u", [128, 128])
    wT_sb = per_tile("wT", [128, 128])
    vnew = per_tile("vnew", [128, 128])
    osq = per_tile("osq", [128, 128])
    ost = per_tile("ost", [128, 2])
    y1 = per_tile("y1", [128, 128])
    y2 = per_tile("y2", [128, 128])

    for blk in range(NBLK):
        col = slice(blk * 512, (blk + 1) * 512)
        hb = hTb[blk % 2]
        for kc in range(8):
            k.dma("pool", hb[:, kc, :], hT_d[kc * 128:(kc + 1) * 128, col])
        for w in range(3):
            bk = nb()
            for kc in range(8):
                k.mm(bk[:, :], wdn[:, kc, w * 128:(w + 1) * 128], hb[:, kc, :], start=(kc == 0), stop=(kc == 7))
            cur, prv = xh[w][blk % 2], xh[w][(blk + 1) % 2]
            k.copy("act", cur[:, 3:515], bk[:, :])
            k.copy("act", cur[:, 0:3], prv[:, 512:515])
            y = cv[w]
            k.ts("dve", y[:, :], cur[:, 0:512], cw[:, w, 0:1], None, ALU.mult)
            for m in range(1, 4):
                k.stt("dve" if m != 2 else "dve", y[:, :], cur[:, m:m + 512], cw[:, w, m:m + 1], y[:, :], ALU.mult, ALU.add)
            k.act(y[:, :], y[:, :], AF.Silu)
        for w in range(2):
            k.act(sq[w][:, :], cv[w][:, :], AF.Square)
            bk = nb()
            k.mm(bk[:, :], ones[:, :], sq[w][:, :])
            k.rstd(rs[w][:, :], bk[:, :], 1.0, EPS)
            if w == 0:
                k.stt("pool", cv[w][:, :], cv[w][:, :], 1.0, rs[w][:, :], ALU.mult, ALU.mult) if False else None
        k.tt("pool", cv[0][:, :], cv[0][:, :], rs[0][:, :], ALU.mult)
        k.tt("pool", cv[1][:, :], cv[1][:, :], rs[1][:, :], ALU.mult)
        k.act(cv[0][:, :], cv[0][:, :], AF.Copy, scale=float(128 ** -0.5))
        qT_, kT_, vT_ = cv

        for t in range(4):
            tc_ = slice(t * 128, (t + 1) * 128)
            c = tmc[t]
            bk = nb()
            for kc in range(8):
                k.mm(bk[:, 0:130], hb[:, kc, tc_], wdn[:, kc, 384:514], start=(kc == 0), stop=(kc == 7))
            k.act(c[:, 0:1], bk[:, 0:1], AF.Sigmoid)
            k.act(c[:, 1:2], bk[:, 1:2], AF.Exp, bias=sc[:, 1:2], scale=1.0)
            k.act(sgate[t][:, :], bk[:, 2:130], AF.Silu)
            k.act(c[:, 1:2], c[:, 1:2], AF.Ln, bias=1.0, scale=1.0)
            k.tt("dve", c[:, 2:3], c[:, 1:2], sc[:, 2:3], ALU.mult)
            bk = nb()
            k.transpose(bk[:, 0:128], kT_[:, tc_], ident[:, :], inc=False)
            k.transpose(bk[:, 128:256], vT_[:, tc_], ident[:, :], inc=True)
            k.copy("act", ktm[t][:, :], bk[:, 0:128])
            k.act(vb[t][:, :], bk[:, 128:256], AF.Copy, scale=c[:, 0:1])
            k.ts("dve", gbc[t][:, :], ones[:, :], c[:, 2:3], None, ALU.mult)
            bk = nb()
            k.mm(bk[:, 0:128], gbc[t][:, :], uinc[:, :], inc=False)
            k.mm(bk[:, 128:129], uinc[:, :], c[:, 2:3], inc=True)
            k.copy("dve", c[:, 3:4], bk[:, 128:129])
            k.copy("dve", c[:, 7:8], bk[:, 127:128])
            k.stt("dve", xm[t][:, :], bk[:, 0:128], c[:, 3:4], lpos[:, :], ALU.subtract, ALU.max)
            k.stt("dve", ym[t][:, :], bk[:, 0:128], c[:, 3:4], uneg[:, :], ALU.subtract, ALU.min)
            k.act(egb[t][:, :], bk[:, 0:128], AF.Exp)
            k.act(dec_s[t][:, :], xm[t][:, :], AF.Exp, scale=-1.0)
            k.act(decT[t][:, :], ym[t][:, :], AF.Exp)
            k.act(c[:, 4:5], c[:, 3:4], AF.Exp)
            k.tt("dve", c[:, 4:5], c[:, 4:5], c[:, 0:1], ALU.mult)
            k.act(c[:, 5:6], c[:, 3:4], AF.Exp, bias=c[:, 7:8], scale=-1.0)
            k.act(c[:, 6:7], c[:, 7:8], AF.Exp)
            k.ts("pool", kbg[t][:, :], ktm[t][:, :], c[:, 4:5], None, ALU.mult) if False else None
            k.ts("dve", kbg[t][:, :], ktm[t][:, :], c[:, 4:5], None, ALU.mult)
            k.ts("dve", kdec[t][:, :], ktm[t][:, :], c[:, 5:6], None, ALU.mult)
            k.tt("pool", qdT[t][:, :], qT_[:, tc_], egb[t][:, :], ALU.mult)
            bk = nb()
            k.mm(bk[:, 0:128], kT_[:, tc_], kT_[:, tc_], inc=False)
            k.mm(bk[:, 128:256], kT_[:, tc_], qT_[:, tc_], inc=True)
            k.stt("dve", Mt[t][:, :], bk[:, 0:128], c[:, 0:1], dec_s[t][:, :], ALU.mult, ALU.mult)
            k.tt("dve", qkT[t][:, :], bk[:, 128:256], decT[t][:, :], ALU.mult)
            bk = nb()
            k.transpose(bk[:, 0:128], Mt[t][:, :], ident[:, :])
            k.copy("act", Nt[t][:, :], bk[:, 0:128])
            k.tt("pool", Rr[t][:, :], ident[:, :], Nt[t][:, :], ALU.subtract)
            k.tt("pool", Rt[t][:, :], ident[:, :], Mt[t][:, :], ALU.subtract)

        P = [Nt[t] for t in range(4)]
        Ptr = [Mt[t] for t in range(4)]
        for lvl in range(6):
            last = lvl == 5
            newP = Pa if lvl % 2 == 0 else Pb
            newPt = Pat if lvl % 2 == 0 else Pbt
            for t in range(4):
                bk = nb()
                k.mm(bk[:, 0:128], Ptr[t][:, :], P[t][:, :], inc=last)
                if not last:
                    k.mm(bk[:, 128:256], P[t][:, :], Ptr[t][:, :], inc=True)
                k.copy("act", newP[t][:, :], bk[:, 0:128])
                if not last:
                    k.copy("act", newPt[t][:, :], bk[:, 128:256])
            for t in range(4):
                bk = nb()
                k.mm(bk[:, 0:128], Rt[t][:, :], newP[t][:, :], inc=last)
                if not last:
                    k.mm(bk[:, 128:256], newP[t][:, :], Rt[t][:, :], inc=True)
                k.tt("dve", Rr[t][:, :], Rr[t][:, :], bk[:, 0:128], ALU.add)
                if not last:
                    k.tt("dve", Rt[t][:, :], Rt[t][:, :], bk[:, 128:256], ALU.add)
            P = [newP[t] for t in range(4)]
            Ptr = [newPt[t] for t in range(4)]

        for t in range(4):
            bk = nb()
            k.mm(bk[:, 0:128], Rr[t][:, :], vb[t][:, :], inc=False)
            k.mm(bk[:, 128:256], kbg[t][:, :], Rr[t][:, :], inc=True)
            k.copy("act", u_sb[t][:, :], bk[:, 0:128])
            k.copy("act", wT_sb[t][:, :], bk[:, 128:256])

        for t in range(4):
            tok = slice(blk * 512 + t * 128, blk * 512 + (t + 1) * 128)
            c = tmc[t]
            s_old = Sf[si % 2]
            s_new = Sf[(si + 1) % 2]
            si += 1
            bk = nb()
            k.mm(bk[:, 0:128], wT_sb[t][:, :], s_old[:, :])
            k.tt("dve", vnew[t][:, :], u_sb[t][:, :], bk[:, 0:128], ALU.subtract)
            bo = nb()
            k.mm(bo[:, 0:128], qdT[t][:, :], s_old[:, :], start=True, stop=False, inc=False)
            k.mm(bo[:, 0:128], qkT[t][:, :], vnew[t][:, :], start=False, stop=True, inc=True)
            bs = nb()
            k.mm(bs[:, 0:128], kdec[t][:, :], vnew[t][:, :])
            k.stt("dve", s_new[:, :], s_old[:, :], c[:, 6:7], bs[:, 0:128], ALU.mult, ALU.add)
            k.act(osq[t][:, :], bo[:, 0:128], AF.Square, accum_out=ost[t][:, 0:1])
            k.rstd(ost[t][:, 1:2], ost[t][:, 0:1], 1.0 / 128, EPS)
            k.stt("dve", y1[t][:, :], bo[:, 0:128], ost[t][:, 1:2], gb[:, :], ALU.mult, ALU.mult)
            k.tt("pool", y2[t][:, :], y1[t][:, :], sgate[t][:, :], ALU.mult)
            k.dma("sp", o_d[tok, :], y2[t][:, :])
    k.finish([o_d])
    return nc


def dn_inputs(hT_b, P, l, j):
    w_in = P["w_in"][l]
    cq, ck, cvv = 416 + j * 128, 416 + 512 + j * 128, 416 + 1024 + j * 128
    w = np.concatenate([w_in[:, cq:cq + 128], w_in[:, ck:ck + 128], w_in[:, cvv:cvv + 128],
                        w_in[:, 1952 + j:1953 + j], w_in[:, 1956 + j:1957 + j],
                        w_in[:, 1960 + j * 128:1960 + (j + 1) * 128]], axis=1)
    conv = P["dn_conv"][l]
    cwm = np.stack([conv[:, cq - 416:cq - 416 + 128], conv[:, ck - 416:ck - 416 + 128],
                    conv[:, cvv - 416:cvv - 416 + 128]], axis=0)
    m = {"hT": hT_b, "w_dn": np.ascontiguousarray(w),
         "conv_w": np.ascontiguousarray(cwm.transpose(2, 0, 1)),
         "a_log": P["dn_a_log"][l][j].reshape(1, 1), "dt_bias": P["dn_dt_bias"][l][j].reshape(1, 1),
         "o_norm": P["dn_o_norm"][l].reshape(1, 128)}
    m.update(dn_consts())
    return m


class Rec:
    _PASS = ("sb", "ps", "dram")

    def __init__(self, k):
        self._k = k
        self.segs = [[]]

    def sb(self, *a, **kw):
        return self._k.sb(*a, **kw)

    def push(self):
        pass

    def pop(self):
        pass

    def mark(self):
        self.segs.append([])

    def __getattr__(self, name):
        def f(*a, **kw):
            self.segs[-1].append((name, a, kw))
        return f


SEM_LAT = 0.8
_GHZ = {"pe": 1.9, "act": 1.2, "dve": 0.96, "pool": 0.6}
_FIX = {"pe": 0.06, "act": 0.2, "dve": 0.1, "pool": 0.25}


def _op_cost(k, rec):
    name, ar, kw = rec
    k.dry = []
    getattr(k, name)(*ar, **kw)
    infos, k.dry = k.dry, None
    n = 128
    out = ar[1] if name in ("tt", "ts", "stt", "copy", "memset", "reduce", "dma") else (ar[0] if ar else None)
    if name == "op":
        out = None
    if isinstance(out, V):
        try:
            n = out.ap.free_size()
        except Exception:
            n = 128
    res = []
    for eng, rd, wr in infos:
        if eng == "dma":
            d = 2.5
        else:
            passes = 1
            if name in ("mm", "transpose") and isinstance(ar[1], V) and ar[1].ap.dtype == F32:
                passes = 4
            d = _FIX[eng] + passes * n / (_GHZ[eng] * 1000.0)
        res.append((eng, rd, wr, d))
    return res


def replay_merged(k, *lists):
    lists = [l for l in lists if l]
    if not lists:
        return
    if len(lists) == 1:
        for name, ar, kw in lists[0]:
            getattr(k, name)(*ar, **kw)
        return
    free = {}
    ready = {}
    rdone = {}
    idx = [0] * len(lists)
    costs = [[None] * len(l) for l in lists]
    total = sum(len(l) for l in lists)

    def start_time(info):
        eng, rd, wr, d = info
        t = free.get(eng, 0.0)
        for r in rd:
            if r in ready:
                tr, pe_ = ready[r]
                t = max(t, tr + (SEM_LAT if pe_ != eng else 0.0))
        for w in wr:
            if w in ready:
                tr, pe_ = ready[w]
                t = max(t, tr + (SEM_LAT if pe_ != eng else 0.0))
            if w in rdone:
                t = max(t, rdone[w] + SEM_LAT)
        return t

    for _ in range(total):
        best, bt = None, None
        for n, l in enumerate(lists):
            if idx[n] < len(l):
                if costs[n][idx[n]] is None:
                    costs[n][idx[n]] = _op_cost(k, l[idx[n]])
                c = costs[n][idx[n]]
                t = start_time(c[0]) if c else 0.0
                key = (t, idx[n] / len(l))
                if bt is None or key < bt:
                    best, bt = n, key
        c = costs[best][idx[best]]
        for info in c:
            eng, rd, wr, d = info
            t = start_time(info)
            free[eng] = t + d
            for r in rd:
                rdone[r] = max(rdone.get(r, 0.0), t + d)
            for w in wr:
                ready[w] = (t + d, eng)
                rdone.pop(w, None)
        name, ar, kw = lists[best][idx[best]]
        idx[best] += 1
        getattr(k, name)(*ar, **kw)


def emit_dn_hg(k, banks, C, W, G, layer):
    NBLK = S // 512
    k.push()
    hTb = [k.sb(f"hTbS{i}", [128, 8, 512], BF16) for i in range(3)]
    ra, rb = Rec(k), Rec(k)
    emit_dn(ra, banks[0:DN_BANKS], C, W, G, hTb=hTb)
    emit_hg(rb, banks[DN_BANKS:8], C, W, G, layer, hTb=hTb)
    assert len(ra.segs) == 2 * NBLK + 1 and len(rb.segs) == NBLK + 1
    front = lambda i: ra.segs[1 + 2 * i] if i < NBLK else []
    back = lambda i: ra.segs[2 + 2 * i]

    def load(blk):
        if blk < NBLK:
            for kc in range(8):
                k.dma("sp", hTb[blk % 3][:, kc, :], G["hT_blk"](blk, kc))

    load(0)
    load(1)
    replay_merged(k, ra.segs[0], rb.segs[0])
    replay_merged(k, front(0))
    for blk in range(NBLK):
        load(blk + 2)
        replay_merged(k, front(blk + 1), back(blk), rb.segs[1 + blk])
        if blk % 4 == 3:
            q = blk // 4
            for br in (1, 2):
                k.collective("AllGather", [G["osrc"][br][q][:, :]], [G["odst"][br][q][:, :]], GROUPS)
    k.pop()


DN_BANKS = 6
DN_BACK_BANKS = 2

def emit_publish_tile(k, banks, ident, y, hTsb, t):
    tok = slice(t * 128, (t + 1) * 128)
    for q4 in range(2):
        bk = banks[4 + q4 + 2 * (t % 2)]
        for j in range(4):
            kc = q4 * 4 + j
            k.transpose(bk[:, j * 128:(j + 1) * 128], y[:, kc * 128:(kc + 1) * 128], ident[:, :], inc=(j == 3))
        k.copy("act", hTsb[:, q4 * 4:(q4 + 1) * 4, tok], bk[:, :].f(lambda a: a.rearrange("p (j t) -> p j t", j=4)))


def emit_allgather_h(k, hTsb, G):
    for kc in range(8):
        k.dma("sp", G["hsrc"][kc // 2][(kc % 2) * 128:(kc % 2 + 1) * 128, :], hTsb[:, kc, :])
    for q in range(4):
        k.collective("AllGather", [G["hsrc"][q][:, :]], [G["hdst"][q][:, :]], GROUPS)


def emit_allgather_o(k, G, br):
    for q in range(4):
        k.collective("AllGather", [G["osrc"][br][q][:, :]], [G["odst"][br][q][:, :]], GROUPS)


def emit_ln0(k, banks, C, G):
    ntok = S * B // NCORES
    k.push()
    x = G["x"]
    g_b = k.sb("g_b", [128, D], F32)
    b_b = k.sb("b_b", [128, D], F32)
    k.dma("sp", g_b[:, :], bcast_row(G["ln_in_g"]))
    k.dma("sp", b_b[:, :], bcast_row(G["ln_in_b"]))
    hTsb = k.sb("hTsb", [128, 8, ntok], BF16)
    xs = [k.sb(f"x{i}", [128, D], F32) for i in range(2)]
    ys = [k.sb(f"y{i}", [128, D], F32) for i in range(2)]
    tmps = [k.sb(f"t{i}", [128, D], F32) for i in range(2)]
    sts = [k.sb(f"s{i}", [128, 4], F32) for i in range(2)]
    NT = ntok // 128
    recs = [Rec(k), Rec(k)]

    def ld(i):
        recs[i % 2].dma("sp", xs[i % 2][:, :], x[i * 128:(i + 1) * 128, :])

    ld(0)
    ld(1)
    for i in range(NT):
        kr = recs[i % 2]
        xt, yt, tt_, st = xs[i % 2], ys[i % 2], tmps[i % 2], sts[i % 2]
        layer_norm_tile(kr, xt[:, :], yt[:, :], g_b[:, :], b_b[:, :], tt_[:, :], st[:, :], eng_g="dve")
        if i + 2 < NT:
            ld(i + 2)
        kr.dma("sp", G["h_cur"][i * 128:(i + 1) * 128, :], yt[:, :])
        emit_publish_tile(kr, banks, C["ident"], yt, hTsb, i)
    replay_merged(k, recs[0].segs[0], recs[1].segs[0])
    emit_allgather_h(k, hTsb, G)
    k.pop()


GROUPS = [[0, 1, 2, 3], [4, 5, 6, 7]]

def emit_mla(k0, banks, C, W, G):
    T = S
    NBLK = T // 512
    wlat_d, gq_d, gkv_d, wuq_d, wuk_d, wuv_d = W["w_lat"], W["g_q"], W["g_kv"], W["w_uq"], W["w_uk"], W["w_uv"]
    pos_d = G["pos"]
    frq, sgn, esel, tri = C["frq"], C["sgn"], C["esel"], C["tri"]
    k = k0
    k.push()

    wlat = k.sb("wlat", [128, 8, 448], BF16)
    for kc in range(8):
        k.dma("pool", wlat[:, kc, :], wlat_d[kc * 128:(kc + 1) * 128, :])
    gq = k.sb("gq", [128, 2], F32)
    gkv = k.sb("gkv", [128, 1], F32)
    k.dma("sp", gq[:, :], gq_d[:, :])
    k.dma("sp", gkv[:, :], gkv_d[:, :])
    wtmp = k.sb("wtmp", [128, 2, 256], F32)
    wuq = k.sb("wuq", [128, 2, 256], BF16)
    for c in range(2):
        k.dma("sp", wtmp[:, c, :], wuq_d[c * 128:(c + 1) * 128, :])
    for c in range(2):
        k.ts("dve", wuq[:, c, :], wtmp[:, c, :], gq[:, c:c + 1], QK_SCALE, ALU.mult, ALU.mult)
    wtmp2 = k.sb("wtmp2", [128, 2, 128], F32)
    wuk = k.sb("wuk", [128, 128], BF16)
    wuv = k.sb("wuv", [128, 128], BF16)
    k.dma("sp", wtmp2[:, 0, :], wuk_d[:, :])
    k.dma("sp", wtmp2[:, 1, :], wuv_d[:, :])
    k.ts("dve", wuk[:, :], wtmp2[:, 0, :], gkv[:, 0:1], None, ALU.mult)
    k.ts("dve", wuv[:, :], wtmp2[:, 1, :], gkv[:, 0:1], None, ALU.mult)
    ones = k.sb("ones", [128, 128], F32)
    k.memset("dve", ones[:, :], 1.0)

    kT = [k.sb(f"kT{h}", [96, T], BF16) for h in range(2)]
    qT = [k.sb(f"qT{h}", [96, T], BF16) for h in range(2)]
    Vp = k.sb("Vp", [128, 2, T // 128, 128], BF16)
    kTv = [[V(kT[h].h[:, b * 512:(b + 1) * 512], Res(f"kT{h}_{b}")) for b in range(NBLK)] for h in range(2)]
    qTv = [[V(qT[h].h[:, b * 512:(b + 1) * 512], Res(f"qT{h}_{b}")) for b in range(NBLK)] for h in range(2)]
    Vpv = [V(Vp.h[:, :, b * 4:(b + 1) * 4, :], Res(f"Vp_{b}")) for b in range(NBLK)]
    for b in range(NBLK):
        k.memset("pool", Vpv[b], 1.0)
    mx = k.sb("mx", [128, 4], F32)
    k.memset("dve", mx[:, :], 0.0)
    negcb = [k.sb(f"negc{b}", [128, 2], F32) for b in range(NBLK)]

    hTb = [k.sb(f"hTb{i}", [128, 8, 512], BF16) for i in range(2)]
    posi = [k.sb(f"posi{i}", [128, 512], I32) for i in range(2)]
    ang = k.sb("ang", [128, 1024], F32)
    ni = k.sb("ni", [128, 1024], I32)
    nf = k.sb("nf", [128, 1024], F32)
    scs = [k.sb(f"scs{i}", [128, 1024], F32) for i in range(2)]
    cq_sb = [k.sb(f"cq_sb{i}", [128, 512], F32) for i in range(2)]
    sq_sb = [k.sb(f"sq_sb{i}", [128, 512], F32) for i in range(2)]
    ckv_sb = k.sb("ckv_sb", [128, 512], F32)
    sqkv = k.sb("sqkv", [128, 512], F32)
    rq = k.sb("rq", [128, 512], F32)
    rkv = k.sb("rkv", [128, 512], F32)
    cqn = [k.sb(f"cqn{i}", [128, 512], BF16) for i in range(2)]
    ckvn = k.sb("ckvn", [128, 512], BF16)
    t1 = k.sb("t1", [128, 512], F32)
    t2 = k.sb("t2", [128, 512], F32)
    nsq = k.sb("nsq", [96, 512], F32)
    mtmp = k.sb("mtmp", [128, 1], F32)
    osb = [k.sb(f"osb{i}", [128, 512], F32) for i in range(2)]
    ores = [k.sb(f"ores{i}", [64, 512], BF16) for i in range(2)]
    pT = [k.sb(f"pTx{i}", [128, 512], BF16) for i in range(7)]

    def load(blk):
        if blk < NBLK:
            col = slice(blk * 512, (blk + 1) * 512)
            for kc in range(8):
                k0.dma("sp", hTb[blk % 2][:, kc, :], G["hT_blk"](blk, kc))
            k0.dma("sp", posi[blk % 2][:, :], pos_d.v(pos_d.h[:, col].partition_broadcast(128)))

    rp = Rec(k0)
    k = rp
    nb = BankRR(banks[6:8])
    r6 = slice(64, 96)

    def rope_rows(dsts, bA, bB, cst, snt):
        k.stt("dve", t1[r6, :], bB[r6, :], sgn[r6, 0:1], snt, ALU.mult, ALU.mult)
        k.tt("dve", t2[r6, :], bA[r6, :], cst, ALU.mult)
        for d_ in dsts:
            k.tt("pool", d_[r6, :], t1[r6, :], t2[r6, :], ALU.add)

    def normsq(src, slot, running):
        k.act(nsq[:, :], src[0:96, :], AF.Square)
        bk = nb()
        k.mm(bk[:, :], ones[0:96, :], nsq[:, :])
        k.reduce("dve", mtmp[:, :], bk[:, :], ALU.max)
        if running:
            k.tt("dve", mx[:, slot:slot + 1], mx[:, slot:slot + 1], mtmp[:, :], ALU.max)
        else:
            k.copy("dve", mx[:, slot:slot + 1], mtmp[:, :])

    for blk in range(NBLK):
        k.mark()
        hb = hTb[blk % 2]
        pi_ = posi[blk % 2]
        sc_ = scs[blk % 2]
        snt, cst = sc_[r6, 0:512], sc_[r6, 512:1024]
        k.copy("dve", ang[r6, 0:512], pi_[r6, :])
        k.ts("dve", ang[r6, 0:512], ang[r6, 0:512], frq[r6, 0:1], None, ALU.mult)
        k.ts("dve", ang[r6, 512:1024], ang[r6, 0:512], float(np.pi / 2), None, ALU.add)
        k.ts("dve", nf[r6, :], ang[r6, :], 1.0 / TWO_PI, None, ALU.mult)
        k.copy("dve", ni[r6, :], nf[r6, :])
        k.copy("dve", nf[r6, :], ni[r6, :])
        k.stt("dve", nf[r6, :], nf[r6, :], -TWO_PI, ang[r6, :], ALU.mult, ALU.add)
        k.ts("dve", nf[r6, :], nf[r6, :], 3.1415925, -3.1415925, ALU.min, ALU.max)
        k.act(sc_[r6, :], nf[r6, :], AF.Sin)
        for c in range(2):
            bk = nb()
            for kc in range(8):
                k.mm(bk[:, :], wlat[:, kc, c * 128:(c + 1) * 128], hb[:, kc, :], start=(kc == 0), stop=(kc == 7))
            k.copy("act", cq_sb[c][:, :], bk[:, :])
            k.act(sq_sb[c][:, :], bk[:, :], AF.Square)
        bk = nb()
        for kc in range(8):
            k.mm(bk[:, :], wlat[:, kc, 256:384], hb[:, kc, :], start=(kc == 0), stop=(kc == 7))
        k.copy("act", ckv_sb[:, :], bk[:, :])
        k.act(sqkv[:, :], bk[:, :], AF.Square)
        bA = nb()
        for kc in range(8):
            k.mm(bA[0:96, :], wlat[:, kc, 320:416], hb[:, kc, :], start=(kc == 0), stop=(kc == 7))
        bB = nb()
        for kc in range(8):
            k.mm(bB[0:96, :], wlat[:, kc, 352:448], hb[:, kc, :], start=(kc == 0), stop=(kc == 7))
        rope_rows([kTv[0][blk], kTv[1][blk]], bA, bB, cst, snt)
        bk = nb()
        k.mm(bk[:, :], ones[:, :], sq_sb[0][:, :], start=True, stop=False)
        k.mm(bk[:, :], ones[:, :], sq_sb[1][:, :], start=False, stop=True)
        k.rstd_ln(rq[:, :], bk[:, :], 1.0 / 256, EPS)
        bk = nb()
        k.mm(bk[:, :], ones[:, :], sqkv[:, :])
        k.rstd_ln(rkv[:, :], bk[:, :], 1.0 / 128, EPS)
        for c in range(2):
            k.tt("dve", cqn[c][:, :], cq_sb[c][:, :], rq[:, :], ALU.mult)
        k.tt("pool", ckvn[:, :], ckv_sb[:, :], rkv[:, :], ALU.mult)
        for hd in range(2):
            bk = nb()
            k.mm(bk[0:64, :], wuk[:, hd * 64:(hd + 1) * 64], ckvn[:, :])
            k.copy("act", kTv[hd][blk][0:64, :], bk[0:64, :])
        bk = nb()
        for tt_ in range(4):
            k.mm(bk[:, tt_ * 128:(tt_ + 1) * 128], ckvn[:, tt_ * 128:(tt_ + 1) * 128], wuv[:, :],
                 start=True, stop=True, inc=(tt_ == 3))
        k.copy("act", Vpv[blk][:, :, :, 0:64],
               bk[:, :].f(lambda a: a.rearrange("p (t h d) -> p h t d", t=4, h=2)))
        for hd in range(2):
            bA = nb()
            for c in range(2):
                k.mm(bA[0:96, :], wuq[:, c, hd * 128:hd * 128 + 96], cqn[c][:, :], start=(c == 0), stop=(c == 1))
            bB = nb()
            for c in range(2):
                k.mm(bB[0:96, :], wuq[:, c, hd * 128 + 32:hd * 128 + 128], cqn[c][:, :], start=(c == 0), stop=(c == 1))
            k.copy("act", qTv[hd][blk][0:64, :], bA[0:64, :])
            rope_rows([qTv[hd][blk]], bA, bB, cst, snt)
        for hd in range(2):
            normsq(qTv[hd][blk], hd, False)
            normsq(kTv[hd][blk], 2 + hd, True)
        ng = negcb[blk]
        k.tt("dve", ng[:, :], mx[:, 0:2], mx[:, 2:4], ALU.mult)
        k.act(ng[:, :], ng[:, :], AF.Ln)
        k.act(ng[:, :], ng[:, :], AF.Exp, scale=0.5)
        k.ts("dve", ng[:, :], ng[:, :], -1.0, None, ALU.mult)

    ra = Rec(k0)
    k = ra
    s_banks = BankRR(banks[2:5])
    den_bank = banks[5]
    blocks = [(hd, qi, kb) for qi in range(NBLK) for hd in range(2) for kb in range(4 * qi + 4)]
    LOOKAHEAD = 4

    def stage1(i):
        hd, qi, kb = blocks[i]
        r = kb - 4 * qi
        c0 = 128 * r if r > 0 else 0
        sb_ = s_banks()
        pt = pT[i % 7]
        kblk, ko = kb // 4, (kb % 4) * 128
        k.mm(sb_[:, c0:512], kTv[hd][kblk][:, ko:ko + 128], qTv[hd][qi][:, c0:512])
        k.act(pt[:, c0:512], sb_[:, c0:512], AF.Exp, bias=negcb[qi][:, hd:hd + 1], scale=1.0)
        if r >= 0:
            k.tt("pool", pt[:, c0:c0 + 128], pt[:, c0:c0 + 128], tri[:, :], ALU.mult)
        return pt, c0

    def stage2(i, pt, c0):
        hd, qi, kb = blocks[i]
        nkb = 4 * qi + 4
        g = qi * 2 + hd
        oacc = banks[g % 2]
        k.mm(oacc[:, c0:512], Vpv[kb // 4][:, hd, kb % 4, :], pt[:, c0:512], start=(kb == 0), stop=(kb == nkb - 1))
        if kb == nkb - 1:
            ob = osb[g % 2]
            orr = ores[g % 2]
            k.copy("act", ob[:, :], oacc[:, :])
            k.op("dve", lambda e, ob=ob: e.reciprocal(ob[64:128, :].ap, ob[64:128, :].ap), [ob], [ob])
            k.mm(den_bank[0:64, :], esel[:, :], ob[:, :])
            k.tt("dve", orr[:, :], ob[0:64, :], den_bank[0:64, :], ALU.mult)
            k.dma("sp", G["osrc"][0][qi // 4][hd * 64:(hd + 1) * 64, (qi % 4) * 512:(qi % 4 + 1) * 512], orr[:, :])

    pend = []
    cur_qi = -1
    for i in range(len(blocks)):
        if blocks[i][1] != cur_qi:
            cur_qi = blocks[i][1]
            k.mark()
        pend.append((i,) + stage1(i))
        if len(pend) > LOOKAHEAD:
            stage2(*pend.pop(0))
    while pend:
        stage2(*pend.pop(0))

    k = k0
    assert len(rp.segs) == NBLK + 1 and len(ra.segs) == NBLK + 1 and not rp.segs[0] and not ra.segs[0]
    load(0)
    load(1)
    replay_merged(k, rp.segs[1])
    for qi in range(NBLK):
        load(qi + 2)
        replay_merged(k, ra.segs[1 + qi], rp.segs[2 + qi] if qi + 1 < NBLK else [])
    k.pop()


def emit_hg(k, banks, C, W, G, layer, hTb=None):
    T = S
    NBLK = T // 512
    w_d, lbr_d, lbc_d, on_d = W["w_hg"], G["lb_rows"], G["lb_cols"], W["hg_o_norm"]
    cU, cUrel, cW, cones, mbd, rowm, ident = C["cU"], C["cUrel"], C["cW"], C["cones"], C["maskbd"], C["rowmask"], C["ident"]
    nb = BankRR(banks)
    k.push()

    whg = k.sb("whg", [128, 8, 512], BF16)
    for kc in range(8):
        k.dma("pool", whg[:, kc, :], w_d[kc * 128:(kc + 1) * 128, :])
    gb = k.sb("gb", [128, 128], F32)
    k.dma("sp", gb[:, :], bcast_row(on_d))
    oTb = [k.sb(f"oTb{i}", [128, 512], BF16) for i in range(2)]

    def lower_bound(x, n, name):
        m = k.sb(name + "_m", [128, n], F32)
        e = k.sb(name + "_e", [128, DEPTH, n], F32)
        ssum = k.sb(name + "_s", [128, n], F32)
        lb = k.sb(name + "_lb", [128, n], F32)
        oml = k.sb(name + "_oml", [128, n], F32)
        k.copy("dve", m[:, :], x[:, 0, :])
        for i in range(1, DEPTH):
            k.tt("dve", m[:, :], m[:, :], x[:, i, :], ALU.max)
        for i in range(DEPTH):
            k.tt("dve", e[:, i, :], x[:, i, :], m[:, :], ALU.subtract)
        k.act(e[:, :, :], e[:, :, :], AF.Exp)
        k.copy("dve", ssum[:, :], e[:, 0, :])
        for i in range(1, DEPTH):
            k.tt("dve", ssum[:, :], ssum[:, :], e[:, i, :], ALU.add)
        k.op("dve", lambda en: en.reciprocal(ssum[:, :].ap, ssum[:, :].ap), [ssum], [ssum])
        for i in range(DEPTH):
            k.tt("dve", e[:, i, :], e[:, i, :], ssum[:, :], ALU.mult)
        k.copy("dve", lb[:, :], e[:, 0, :])
        for i in range(1, layer + 1):
            k.tt("dve", lb[:, :], lb[:, :], e[:, i, :], ALU.add)
        k.tt("dve", lb[:, :], lb[:, :], e[:, 0, :], ALU.subtract)
        k.ts("dve", oml[:, :], lb[:, :], -1.0, 1.0, ALU.mult, ALU.add)
        return lb, oml

    xr = k.sb("xr", [128, DEPTH, 128], F32)
    for i in range(DEPTH):
        k.dma("sp", xr[:, i, :], lbr_d.v(lbr_d.h[i:i + 1, :].partition_broadcast(128)))
    lb_b, oml_b = lower_bound(xr, 128, "lbr")
    xc = k.sb("xc", [128, DEPTH, 1], F32)
    k.dma("sp", xc[:, :, 0], lbc_d[:, :])
    lb_c, oml_c = lower_bound(xc, 1, "lbc")

    NS = 8
    Sf = [k.sb(f"Sf{i}", [128, 128], F32) for i in range(2)]
    Sb = [k.sb(f"Sb{i}", [128, 128], BF16) for i in range(NS)]
    k.memset("dve", Sf[0][:, :], 0.0)
    k.memset("dve", Sb[0][:, :], 0.0)
    Z = [k.sb(f"Z{i}", [128, 4, 128], BF16) for i in range(2)]
    for z in Z:
        k.memset("pool", z[:, :, :], 0.0)
    si = 0

    shared = hTb is not None
    if not shared:
        hTb = [k.sb(f"hTb{i}", [128, 8, 512], BF16) for i in range(2)]
    qTs = [k.sb(f"qTs{i}", [128, 512], F32) for i in range(2)]
    kTs = [k.sb(f"kTs{i}", [128, 512], F32) for i in range(2)]

    def dbl(name, shape, dt, n=2):
        return [k.sb(f"{name}{i}", shape, dt) for i in range(n)]

    sgx = dbl("sgx", [128, 384], F32)
    sg = [s_[:, 0:128] for s_ in sgx]
    uu = dbl("uu", [128, 128], F32)
    ff = dbl("ff", [128, 128], F32)
    ktm = dbl("ktm", [128, 128], F32)
    logf = dbl("logf", [128, 128], F32)
    vbf = dbl("vbf", [128, 128], BF16)
    sgate = dbl("sgate", [128, 128], F32)
    e1 = dbl("e1", [128, 128], F32)
    e2 = dbl("e2", [128, 128], F32)
    e3 = dbl("e3", [128, 128], F32)
    e4 = dbl("e4", [128, 128], F32)
    dl = dbl("dl", [128, 4], F32)
    qpT = dbl("qpT", [128, 128], BF16)
    kpT = dbl("kpT", [128, 128], BF16)
    kdp = dbl("kdp", [128, 4, 128], BF16)
    attm = dbl("attm", [128, 128], BF16)
    osq = dbl("osq", [128, 128], F32)
    ost = dbl("ost", [128, 2], F32)
    y1 = dbl("y1", [128, 128], F32)
    y2 = dbl("y2", [128, 128], F32)

    nt = 0
    for blk in range(NBLK):
        col = slice(blk * 512, (blk + 1) * 512)
        hb = hTb[blk % len(hTb)]
        if shared:
            k.mark()
        else:
            for kc in range(8):
                k.dma("sp", hb[:, kc, :], G["hT_blk"](blk, kc))
        qT_s, kT_s = qTs[blk % 2], kTs[blk % 2]
        bq = nb()
        for kc in range(8):
            k.mm(bq[:, :], whg[:, kc, 0:128], hb[:, kc, :], start=(kc == 0), stop=(kc == 7))
        k.act(qT_s[:, :], bq[:, :], AF.Exp, scale=-1.0)
        k.act(qT_s[:, :], qT_s[:, :], AF.Ln, bias=1.0, scale=1.0)
        k.act(qT_s[:, :], qT_s[:, :], AF.Exp, scale=-1.0)
        k.tt("dve", qT_s[:, :], bq[:, :], qT_s[:, :], ALU.mult)
        bz = nb()
        for kc in range(8):
            k.mm(bz[:, :], whg[:, kc, 128:256], hb[:, kc, :], start=(kc == 0), stop=(kc == 7))
        k.act(kT_s[:, :], bz[:, :], AF.Exp)
        k.act(kT_s[:, :], kT_s[:, :], AF.Ln, bias=1.0, scale=1.0)
        k.act(kT_s[:, :], kT_s[:, :], AF.Exp, scale=-1.0)
        k.ts("dve", kT_s[:, :], kT_s[:, :], oml_c[:, 0:1], None, ALU.mult)
        for tt_ in range(4):
            p = nt % 2
            nt += 1
            tcol = slice(tt_ * 128, (tt_ + 1) * 128)
            tok = slice(blk * 512 + tt_ * 128, blk * 512 + (tt_ + 1) * 128)
            btm = nb()
            for kc in range(8):
                k.mm(btm[:, 0:384], hb[:, kc, tcol], whg[:, kc, 128:512], start=(kc == 0), stop=(kc == 7))
            k.act(sgx[p][:, :], btm[:, 0:384], AF.Exp, scale=-1.0)
            k.copy("act", vbf[p][:, :], btm[:, 128:256])
            k.act(sgx[p][:, :], sgx[p][:, :], AF.Ln, bias=1.0, scale=1.0)
            k.act(sgx[p][:, :], sgx[p][:, :], AF.Exp, scale=-1.0)
            k.tt("dve", sgate[p][:, :], btm[:, 256:384], sgx[p][:, 256:384], ALU.mult)
            k.tt("dve", uu[p][:, :], sg[p][:, :], oml_b[:, :], ALU.mult)
            k.tt("pool", ff[p][:, :], uu[p][:, :], lb_b[:, :], ALU.add)
            k.tt("pool", ktm[p][:, :], oml_b[:, :], uu[p][:, :], ALU.subtract)
            k.ts("dve", ff[p][:, :], ff[p][:, :], 1e-30, None, ALU.max)
            k.act(logf[p][:, :], ff[p][:, :], AF.Ln)
            bc = nb()
            k.mm(bc[:, 0:128], logf[p][:, :], cU[:, :], inc=False)
            k.mm(bc[:, 128:256], logf[p][:, :], cUrel[:, :], inc=False)
            k.mm(bc[:, 256:384], cW[:, :], logf[p][:, :], inc=False)
            k.mm(bc[:, 384:388], logf[p][:, :], cones[:, :], inc=True)
            k.act(e1[p][:, :], bc[:, 0:128], AF.Exp)
            k.act(e2[p][:, :], bc[:, 128:256], AF.Exp)
            k.act(e3[p][:, :], bc[:, 128:256], AF.Exp, scale=-1.0)
            k.act(e4[p][:, :], bc[:, 256:384], AF.Exp)
            k.act(dl[p][:, :], bc[:, 384:388], AF.Exp)
            z = Z[p]
            zdiag = z.v(bass.AP(z.h, 0, [[512, 128], [160, 4], [1, 32]]))
            k.tt("dve", zdiag, qT_s[:, tcol].f(lambda a: a.rearrange("p (c x) -> p c x", c=4)),
                 e1[p][:, :].f(lambda a: a.rearrange("p (c x) -> p c x", c=4)), ALU.mult)
            k.tt("pool", qpT[p][:, :], qT_s[:, tcol], e2[p][:, :], ALU.mult)
            k.tt("pool", kpT[p][:, :], kT_s[:, tcol], e3[p][:, :], ALU.mult)
            for c in range(4):
                k.stt("dve" if c % 2 == 0 else "pool", kdp[p][:, c, :], ktm[p][:, :], rowm[:, c:c + 1], e4[p][:, :],
                      ALU.mult, ALU.mult) if c % 2 == 0 else None
            for c in range(4):
                if c % 2 == 1:
                    k.stt("dve", kdp[p][:, c, :], ktm[p][:, :], rowm[:, c:c + 1], e4[p][:, :], ALU.mult, ALU.mult)
            ba = nb()
            k.mm(ba[:, 0:128], kpT[p][:, :], qpT[p][:, :])
            k.tt("dve", attm[p][:, :], ba[:, 0:128], mbd[:, :], ALU.mult)
            bs = nb()
            for c in range(4):
                k.mm(bs[:, c * 128:(c + 1) * 128], kdp[p][:, c, :], vbf[p][:, :], inc=(c == 3))
            bo = nb()
            for c in range(4):
                k.mm(bo[:, 0:128], z[:, c, :], Sb[(si + c) % NS][:, :], start=(c == 0), stop=False, inc=False)
                s_old = Sf[(si + c) % 2]
                s_new = Sf[(si + c + 1) % 2]
                k.stt("dve", s_new[:, :], s_old[:, :], dl[p][:, c:c + 1], bs[:, c * 128:(c + 1) * 128],
                      ALU.mult, ALU.add)
                k.copy("pool", Sb[(si + c + 1) % NS][:, :], s_new[:, :])
            k.mm(bo[:, 0:128], attm[p][:, :], vbf[p][:, :], start=False, stop=True, inc=True)
            si += 4
            k.act(osq[p][:, :], bo[:, 0:128], AF.Square, accum_out=ost[p][:, 0:1])
            k.rstd_ln(ost[p][:, 1:2], ost[p][:, 0:1], 1.0 / 128, EPS)
            k.stt("dve", y1[p][:, :], bo[:, 0:128], ost[p][:, 1:2], gb[:, :], ALU.mult, ALU.mult)
            k.tt("pool", y2[p][:, :], y1[p][:, :], sgate[p][:, :], ALU.mult)
            bt = nb()
            k.transpose(bt[:, 0:128], y2[p][:, :], ident[:, :])
            k.copy("act", oTb[blk % 2][:, tcol], bt[:, 0:128])
        k.dma("sp", G["osrc"][2][blk // 4][:, (blk % 4) * 512:(blk % 4 + 1) * 512], oTb[blk % 2][:, :])
    k.pop()


def emit_dn(k, banks, C, W, G, hTb=None):
    T = S
    NBLK = T // 512
    w_d, cw_d, alog_d, dtb_d, on_d = W["w_dn"], W["conv_w"], W["a_log"], W["dt_bias"], W["dn_o_norm"]
    uinc, lpos, uneg, ident = C["uinc"], C["lpos_s"], C["uneg"], C["ident"]
    if hTb is not None:
        nb = BankRR(banks[:-DN_BACK_BANKS])
        nbb = BankRR(banks[-DN_BACK_BANKS:])
    else:
        nb = nbb = BankRR(banks)
    k.push()

    wdn = k.sb("wdn", [128, 8, 514], BF16)
    for kc in range(8):
        k.dma("pool", wdn[:, kc, :], w_d[kc * 128:(kc + 1) * 128, :])
    cw = k.sb("cw", [128, 3, 4], F32)
    k.dma("sp", cw[:, :, :], cw_d[:, :, :])
    gb = k.sb("gb", [128, 128], F32)
    k.dma("sp", gb[:, :], bcast_row(on_d))
    oTb = [k.sb(f"oTb{i}", [128, 512], BF16) for i in range(2)]
    sc = k.sb("sc", [128, 4], F32)
    k.dma("sp", sc[:, 0:1], bcast_row(alog_d))
    k.dma("sp", sc[:, 1:2], bcast_row(dtb_d))
    k.act(sc[:, 2:3], sc[:, 0:1], AF.Exp)
    k.ts("dve", sc[:, 2:3], sc[:, 2:3], -1.0, None, ALU.mult)
    ones = k.sb("ones", [128, 128], F32)
    k.memset("dve", ones[:, :], 1.0)
    ey = k.sb("ey", [128, 512], F32)

    Sf = [k.sb(f"S{i}", [128, 128], F32) for i in range(2)]
    k.memset("dve", Sf[0][:, :], 0.0)
    si = 0

    shared = hTb is not None
    if not shared:
        hTb = [k.sb(f"hTb{i}", [128, 8, 512], BF16) for i in range(2)]
    xh = [[k.sb(f"xh{w}_{i}", [128, 515], F32) for i in range(2)] for w in range(3)]
    for w in range(3):
        k.memset("dve", xh[w][1][:, 512:515], 0.0)
    cv = [k.sb(f"cv{w}", [128, 512], F32) for w in range(3)]
    sq = [k.sb(f"sq{w}", [128, 512], F32) for w in range(2)]
    rs = [k.sb(f"rs{w}", [128, 512], F32) for w in range(2)]

    def per_tile(name, shape, dt=F32):
        return [k.sb(f"{name}{i}", shape, dt) for i in range(4)]

    def per_tile2(name, shape, dt=F32):
        return [k.sb(f"{name}{i}", shape, dt) for i in range(8 if shared else 4)]

    tmcA = per_tile2("tmc", [128, 8])
    sgateA = per_tile2("sgate", [128, 128])
    r130 = per_tile("r130", [128, 130])
    ktm = per_tile("ktm", [128, 128])
    vb = per_tile("vb", [128, 128])
    gbc = per_tile("gbc", [128, 128])
    xm = per_tile("xm", [128, 128])
    ym = per_tile("ym", [128, 128])
    dec_s = per_tile("dec_s", [128, 128])
    decT = per_tile("decT", [128, 128])
    egb = per_tile("egb", [128, 128])
    Mt = per_tile("M", [128, 128])
    Nt = per_tile("N", [128, 128])
    Pa = per_tile("Pa", [128, 128])
    Pat = per_tile("Pat", [128, 128])
    Pb = per_tile("Pb", [128, 128])
    Pbt = per_tile("Pbt", [128, 128])
    Rr = per_tile("R", [128, 128])
    Rt = per_tile("Rt", [128, 128])
    qkTA = per_tile2("qkT", [128, 128])
    qdTA = per_tile2("qdT", [128, 128])
    kbg = per_tile("kbg", [128, 128])
    kdecA = per_tile2("kdec", [128, 128])
    u_sbA = per_tile2("u", [128, 128])
    wT_sbA = per_tile2("wT", [128, 128])
    vnew = per_tile("vnew", [128, 128])
    osq = per_tile("osq", [128, 128])
    ost = per_tile("ost", [128, 2])
    y1 = per_tile("y1", [128, 128])
    y2 = per_tile("y2", [128, 128])

    for blk in range(NBLK):
        col = slice(blk * 512, (blk + 1) * 512)
        hb = hTb[blk % len(hTb)]
        if shared:
            k.mark()
        else:
            for kc in range(8):
                k.dma("sp", hb[:, kc, :], G["hT_blk"](blk, kc))
        pb = (blk % 2) * 4 if shared else 0
        tmc, sgate, qkT, qdT, kdec, u_sb, wT_sb = (x[pb:pb + 4] for x in (tmcA, sgateA, qkTA, qdTA, kdecA, u_sbA, wT_sbA))
        for w in range(3):
            bk = nb()
            for kc in range(8):
                k.mm(bk[:, :], wdn[:, kc, w * 128:(w + 1) * 128], hb[:, kc, :], start=(kc == 0), stop=(kc == 7))
            cur, prv = xh[w][blk % 2], xh[w][(blk + 1) % 2]
            k.copy("act", cur[:, 3:515], bk[:, :])
            k.copy("act", cur[:, 0:3], prv[:, 512:515])
            y = cv[w]
            k.ts("dve", y[:, :], cur[:, 0:512], cw[:, w, 0:1], None, ALU.mult)
            for m in range(1, 4):
                k.stt("dve" if m != 2 else "dve", y[:, :], cur[:, m:m + 512], cw[:, w, m:m + 1], y[:, :], ALU.mult, ALU.add)
            k.act(ey[:, :], y[:, :], AF.Exp, scale=-1.0)
            k.act(ey[:, :], ey[:, :], AF.Ln, bias=1.0, scale=1.0)
            k.act(ey[:, :], ey[:, :], AF.Exp, scale=-1.0)
            k.tt("pool", y[:, :], y[:, :], ey[:, :], ALU.mult)
        for w in range(2):
            k.act(sq[w][:, :], cv[w][:, :], AF.Square)
            bk = nb()
            k.mm(bk[:, :], ones[:, :], sq[w][:, :])
            k.rstd_ln(rs[w][:, :], bk[:, :], 1.0, EPS)
            if w == 0:
                k.stt("pool", cv[w][:, :], cv[w][:, :], 1.0, rs[w][:, :], ALU.mult, ALU.mult) if False else None
        k.tt("pool", cv[0][:, :], cv[0][:, :], rs[0][:, :], ALU.mult)
        k.tt("pool", cv[1][:, :], cv[1][:, :], rs[1][:, :], ALU.mult)
        k.act(cv[0][:, :], cv[0][:, :], AF.Copy, scale=float(128 ** -0.5))
        qT_, kT_, vT_ = cv

        for t in range(4):
            tc_ = slice(t * 128, (t + 1) * 128)
            c = tmc[t]
            bk = nb()
            for kc in range(8):
                k.mm(bk[:, 0:130], hb[:, kc, tc_], wdn[:, kc, 384:514], start=(kc == 0), stop=(kc == 7))
            k.act(r130[t][:, :], bk[:, 0:130], AF.Exp, scale=-1.0)
            k.act(c[:, 1:2], bk[:, 1:2], AF.Exp, bias=sc[:, 1:2], scale=1.0)
            k.act(r130[t][:, :], r130[t][:, :], AF.Ln, bias=1.0, scale=1.0)
            k.act(r130[t][:, :], r130[t][:, :], AF.Exp, scale=-1.0)
            k.copy("dve", c[:, 0:1], r130[t][:, 0:1])
            k.tt("dve", sgate[t][:, :], bk[:, 2:130], r130[t][:, 2:130], ALU.mult)
            k.act(c[:, 1:2], c[:, 1:2], AF.Ln, bias=1.0, scale=1.0)
            k.tt("dve", c[:, 2:3], c[:, 1:2], sc[:, 2:3], ALU.mult)
            bk = nb()
            k.transpose(bk[:, 0:128], kT_[:, tc_], ident[:, :], inc=False)
            k.transpose(bk[:, 128:256], vT_[:, tc_], ident[:, :], inc=True)
            k.copy("act", ktm[t][:, :], bk[:, 0:128])
            k.act(vb[t][:, :], bk[:, 128:256], AF.Copy, scale=c[:, 0:1])
            k.ts("dve", gbc[t][:, :], ones[:, :], c[:, 2:3], None, ALU.mult)
            bk = nb()
            k.mm(bk[:, 0:128], gbc[t][:, :], uinc[:, :], inc=False)
            k.mm(bk[:, 128:129], uinc[:, :], c[:, 2:3], inc=True)
            k.copy("dve", c[:, 3:4], bk[:, 128:129])
            k.copy("dve", c[:, 7:8], bk[:, 127:128])
            k.stt("dve", xm[t][:, :], bk[:, 0:128], c[:, 3:4], lpos[:, :], ALU.subtract, ALU.max)
            k.stt("dve", ym[t][:, :], bk[:, 0:128], c[:, 3:4], uneg[:, :], ALU.subtract, ALU.min)
            k.act(egb[t][:, :], bk[:, 0:128], AF.Exp)
            k.act(dec_s[t][:, :], xm[t][:, :], AF.Exp, scale=-1.0)
            k.act(decT[t][:, :], ym[t][:, :], AF.Exp)
            k.act(c[:, 4:5], c[:, 3:4], AF.Exp)
            k.tt("dve", c[:, 4:5], c[:, 4:5], c[:, 0:1], ALU.mult)
            k.act(c[:, 5:6], c[:, 3:4], AF.Exp, bias=c[:, 7:8], scale=-1.0)
            k.act(c[:, 6:7], c[:, 7:8], AF.Exp)
            k.ts("pool", kbg[t][:, :], ktm[t][:, :], c[:, 4:5], None, ALU.mult) if False else None
            k.ts("dve", kbg[t][:, :], ktm[t][:, :], c[:, 4:5], None, ALU.mult)
            k.ts("dve", kdec[t][:, :], ktm[t][:, :], c[:, 5:6], None, ALU.mult)
            k.tt("pool", qdT[t][:, :], qT_[:, tc_], egb[t][:, :], ALU.mult)
            bk = nb()
            k.mm(bk[:, 0:128], kT_[:, tc_], kT_[:, tc_], inc=False)
            k.mm(bk[:, 128:256], kT_[:, tc_], qT_[:, tc_], inc=True)
            k.stt("dve", Mt[t][:, :], bk[:, 0:128], c[:, 0:1], dec_s[t][:, :], ALU.mult, ALU.mult)
            k.tt("dve", qkT[t][:, :], bk[:, 128:256], decT[t][:, :], ALU.mult)
            bk = nb()
            k.transpose(bk[:, 0:128], Mt[t][:, :], ident[:, :])
            k.copy("act", Nt[t][:, :], bk[:, 0:128])
            k.tt("pool", Rr[t][:, :], ident[:, :], Nt[t][:, :], ALU.subtract)
            k.tt("pool", Rt[t][:, :], ident[:, :], Mt[t][:, :], ALU.subtract)

        P = [Nt[t] for t in range(4)]
        Ptr = [Mt[t] for t in range(4)]
        for lvl in range(6):
            last = lvl == 5
            newP = Pa if lvl % 2 == 0 else Pb
            newPt = Pat if lvl % 2 == 0 else Pbt
            for t in range(4):
                bk = nb()
                k.mm(bk[:, 0:128], Ptr[t][:, :], P[t][:, :], inc=last)
                if not last:
                    k.mm(bk[:, 128:256], P[t][:, :], Ptr[t][:, :], inc=True)
                k.copy("act", newP[t][:, :], bk[:, 0:128])
                if not last:
                    k.copy("dve", newPt[t][:, :], bk[:, 128:256])
            for t in range(4):
                bk = nb()
                k.mm(bk[:, 0:128], Rt[t][:, :], newP[t][:, :], inc=last)
                if not last:
                    k.mm(bk[:, 128:256], newP[t][:, :], Rt[t][:, :], inc=True)
                k.tt("dve", Rr[t][:, :], Rr[t][:, :], bk[:, 0:128], ALU.add)
                if not last:
                    k.tt("dve", Rt[t][:, :], Rt[t][:, :], bk[:, 128:256], ALU.add)
            P = [newP[t] for t in range(4)]
            Ptr = [newPt[t] for t in range(4)]

        for t in range(4):
            bk = nb()
            k.mm(bk[:, 0:128], Rr[t][:, :], vb[t][:, :], inc=False)
            k.mm(bk[:, 128:256], kbg[t][:, :], Rr[t][:, :], inc=True)
            k.copy("act", u_sb[t][:, :], bk[:, 0:128])
            k.copy("dve", wT_sb[t][:, :], bk[:, 128:256])

        if shared:
            k.mark()
        for t in range(4):
            tok = slice(blk * 512 + t * 128, blk * 512 + (t + 1) * 128)
            c = tmc[t]
            s_old = Sf[si % 2]
            s_new = Sf[(si + 1) % 2]
            si += 1
            bk = nbb()
            k.mm(bk[:, 0:128], wT_sb[t][:, :], s_old[:, :])
            k.tt("dve", vnew[t][:, :], u_sb[t][:, :], bk[:, 0:128], ALU.subtract)
            bo = nbb()
            k.mm(bo[:, 0:128], qdT[t][:, :], s_old[:, :], start=True, stop=False, inc=False)
            k.mm(bo[:, 0:128], qkT[t][:, :], vnew[t][:, :], start=False, stop=True, inc=True)
            bs = nbb()
            k.mm(bs[:, 0:128], kdec[t][:, :], vnew[t][:, :])
            k.stt("dve", s_new[:, :], s_old[:, :], c[:, 6:7], bs[:, 0:128], ALU.mult, ALU.add)
            k.act(osq[t][:, :], bo[:, 0:128], AF.Square, accum_out=ost[t][:, 0:1])
            k.rstd_ln(ost[t][:, 1:2], ost[t][:, 0:1], 1.0 / 128, EPS)
            k.stt("dve", y1[t][:, :], bo[:, 0:128], ost[t][:, 1:2], gb[:, :], ALU.mult, ALU.mult)
            k.tt("pool", y2[t][:, :], y1[t][:, :], sgate[t][:, :], ALU.mult)
            bt = nbb()
            k.transpose(bt[:, 0:128], y2[t][:, :], ident[:, :])
            k.copy("act", oTb[blk % 2][:, t * 128:(t + 1) * 128], bt[:, 0:128])
        k.dma("sp", G["osrc"][1][blk // 4][:, (blk % 4) * 512:(blk % 4 + 1) * 512], oTb[blk % 2][:, :])
    k.pop()


def emit_stage_c(k, banks, C, W, G, last):
    ntok = S * B // NCORES
    NT = ntok // 128
    NB = ntok // 512
    upto = 9
    h_d = G["h_cur"]
    wg_d, wout_d = W["w_gates"], W["w_out"]
    wbr_d = [W["w_br_a"], W["w_br_b"], W["w_br_c"]]
    ln1g_d, ln1b_d, ln2g_d, ln2b_d = W["ln1_g"], W["ln1_b"], W["ln2_g"], W["ln2_b"]
    wr_d, br_d = W["w_router"], W["b_router"]
    ewg_d, ewu_d, ewd_d = W["exp_w_gate"], W["exp_w_up"], W["exp_w_down"]
    out_d = G["out"]
    ident = C["ident"]
    k.push()

    comb_all = k.sb("comb_all", [128, NT, 32], F32)
    mixT = k.sb("mixT", [128, 8, ntok], BF16)

    k.push()
    wg = k.sb("wg", [128, 8, 3 * D], BF16)
    wbr = k.sb("wbr", [128, 12, D], BF16)
    for kc in range(8):
        k.dma("pool", wg[:, kc, :], wg_d[kc * 128:(kc + 1) * 128, :])
    for br in range(3):
        for kc in range(4):
            k.dma("pool", wbr[:, br * 4 + kc, :], wbr_d[br][kc * 128:(kc + 1) * 128, :])
    hTb = [k.sb(f"hTb{i}", [128, 8, 512], BF16) for i in range(2)]
    oTb = [k.sb(f"oTb{i}", [128, 12, 512], BF16) for i in range(2)]
    sg = [k.sb(f"sg{i}", [128, 512], BF16) for i in range(2)]
    tmx = [k.sb(f"tmx{i}", [128, 512], F32) for i in range(2)]
    mixf = [k.sb(f"mixf{i}", [128, 512], F32) for i in range(2)]
    nb = 0
    for tb in range(NB):
        tsl = slice(tb * 512, (tb + 1) * 512)
        hb, ob = hTb[tb % 2], oTb[tb % 2]
        for kc in range(8):
            k.dma("sp", hb[:, kc, :], G["hsrc"][kc // 2][(kc % 2) * 128:(kc % 2 + 1) * 128, tsl])
        for br in range(3):
            for kc in range(4):
                k.dma("sp", ob[:, br * 4 + kc, :], G["o_own"](br, kc, tsl), extra_reads=G["o_own_res"](br))
        for r in range(8):
            mf = mixf[r % 2]
            for br in range(3):
                bg = banks[nb % 2]
                by = banks[2 + nb % 2]
                sgt = sg[nb % 2]
                tm = tmx[nb % 2]
                nb += 1
                col = br * D + r * 128
                for kc in range(8):
                    k.mm(bg[:, :], wg[:, kc, col:col + 128], hb[:, kc, :], start=(kc == 0), stop=(kc == 7))
                k.act(sgt[:, :], bg[:, :], AF.Sigmoid)
                for kc in range(4):
                    k.mm(by[:, :], wbr[:, br * 4 + kc, r * 128:(r + 1) * 128], ob[:, br * 4 + kc, :],
                         start=(kc == 0), stop=(kc == 3))
                if br == 0:
                    k.tt("dve", mf[:, :], by[:, :], sgt[:, :], ALU.mult)
                elif br == 1:
                    k.tt("dve", tm[:, :], by[:, :], sgt[:, :], ALU.mult)
                    k.tt("pool", mf[:, :], mf[:, :], tm[:, :], ALU.add)
                else:
                    k.tt("dve", tm[:, :], by[:, :], sgt[:, :], ALU.mult)
                    k.tt("pool", mixT[:, r, tsl], mf[:, :], tm[:, :], ALU.add)
    k.pop()

    acc = [k.sb(f"acc{i}", [128, D], F32) for i in range(NT)]
    h1T = k.sb("h1T", [128, 8, ntok], BF16)
    k.push()
    wout = k.sb("wout", [128, 8, D], BF16)
    for kc in range(8):
        k.dma("pool", wout[:, kc, :], wout_d[kc * 128:(kc + 1) * 128, :])
    wr = k.sb("wr", [128, 8, 36], F32)
    for kc in range(8):
        k.dma("sp", wr[:, kc, :], wr_d[kc * 128:(kc + 1) * 128, :])
    brb = k.sb("brb", [128, 36], F32)
    k.dma("sp", brb[:, :], bcast_row(br_d))
    g1 = k.sb("g1", [128, D], F32)
    b1 = k.sb("b1", [128, D], F32)
    k.dma("sp", g1[:, :], bcast_row(ln1g_d))
    k.dma("sp", b1[:, :], bcast_row(ln1b_d))
    hts = [k.sb(f"ht{i}", [128, D], F32) for i in range(2)]
    x1s = [k.sb(f"x1{i}", [128, D], F32) for i in range(2)]
    h1s = [k.sb(f"h1{i}", [128, D], F32) for i in range(2)]
    tmps = [k.sb(f"lt{i}", [128, D], F32) for i in range(2)]
    sts = [k.sb(f"ls{i}", [128, 4], F32) for i in range(2)]
    hTf = [k.sb(f"hTf{i}", [128, 8, 128], F32) for i in range(2)]
    rl = [k.sb(f"rl{i}", [128, 36], F32) for i in range(2)]
    rs = [k.sb(f"rs{i}", [128, 16], F32) for i in range(2)]
    elm = [k.sb(f"elm{i}", [128, 32], F32) for i in range(2)]
    elm2 = [k.sb(f"elm2{i}", [128, 32], F32) for i in range(2)]
    oh1 = [k.sb(f"oh1{i}", [128, 32], F32) for i in range(2)]
    oh2 = [k.sb(f"oh2{i}", [128, 32], F32) for i in range(2)]
    k_main = k
    recs = [Rec(k_main), Rec(k_main)]
    for t in range(NT):
        p = t % 2
        k = recs[p]
        tok = slice(t * 128, (t + 1) * 128)
        ht, x1, h1t, tmp, st = hts[p], x1s[p], h1s[p], tmps[p], sts[p]
        k.dma("sp", ht[:, :], h_d[tok, :])
        for half in range(2):
            bk = banks[half + 2 * p]
            for kc in range(8):
                k.mm(bk[:, :], mixT[:, kc, tok], wout[:, kc, half * 512:(half + 1) * 512],
                     start=(kc == 0), stop=(kc == 7))
            k.stt("dve", x1[:, half * 512:(half + 1) * 512], ht[:, half * 512:(half + 1) * 512], ALPHA,
                  bk[:, :], ALU.mult, ALU.add)
        layer_norm_tile(k, x1[:, :], h1t[:, :], g1[:, :], b1[:, :], tmp[:, :], st[:, :])
        k.act(acc[t][:, :], h1t[:, :], AF.Copy, scale=ALPHA)
        hf = hTf[p]
        for q4 in range(2):
            bk = banks[4 + q4 + 2 * p]
            for j in range(4):
                kc = q4 * 4 + j
                k.transpose(bk[:, j * 128:(j + 1) * 128], h1t[:, kc * 128:(kc + 1) * 128], ident[:, :],
                            inc=(j == 3))
            k.copy("dve", hf[:, q4 * 4:(q4 + 1) * 4, :],
                   bk[:, :].f(lambda a: a.rearrange("p (j t) -> p j t", j=4)))
        k.copy("act", h1T[:, :, tok], hf[:, :, :])
        bk = banks[2 * p]
        for kc in range(8):
            k.mm(bk[:, 0:36], hf[:, kc, :], wr[:, kc, :], start=(kc == 0), stop=(kc == 7))
        l, s_, em, em2, o1, o2 = rl[p], rs[p], elm[p], elm2[p], oh1[p], oh2[p]
        cb = comb_all[:, t, :]
        k.tt("dve", l[:, :], bk[:, 0:36], brb[:, :], ALU.add)
        k.reduce("dve", s_[:, 0:1], l[:, 0:4], ALU.max)
        k.ts("dve", s_[:, 1:2], s_[:, 0:1], -1.0, None, ALU.mult)
        k.act(s_[:, 8:12], l[:, 0:4], AF.Exp, bias=s_[:, 1:2], scale=1.0, accum_out=s_[:, 2:3])
        k.op("dve", lambda e, s_=s_: e.reciprocal(s_[:, 3:4].ap, s_[:, 2:3].ap), [s_], [s_])
        k.ts("dve", s_[:, 12:16], l[:, 0:4], s_[:, 0:1], None, ALU.is_equal)
        k.ts("dve", s_[:, 12:16], s_[:, 12:16], BIG, -BIG, ALU.mult, ALU.add)
        k.tt("dve", em[:, :].f(lambda a: a.rearrange("p (g e) -> p g e", g=4)),
             l[:, 4:36].f(lambda a: a.rearrange("p (g e) -> p g e", g=4)),
             s_[:, 12:16].f(lambda a: a.unsqueeze(2).broadcast_to([128, 4, 8])), ALU.add)
        k.reduce("dve", s_[:, 4:5], em[:, :], ALU.max)
        k.ts("dve", o1[:, :], em[:, :], s_[:, 4:5], None, ALU.is_equal)
        k.stt("dve", em2[:, :], o1[:, :], -BIG, em[:, :], ALU.mult, ALU.add)
        k.reduce("dve", s_[:, 5:6], em2[:, :], ALU.max)
        k.ts("dve", o2[:, :], em2[:, :], s_[:, 5:6], None, ALU.is_equal)
        k.tt("dve", s_[:, 6:7], s_[:, 5:6], s_[:, 4:5], ALU.subtract)
        k.act(s_[:, 6:7], s_[:, 6:7], AF.Exp)
        k.ts("dve", s_[:, 7:8], s_[:, 6:7], 1.0, None, ALU.add)
        k.op("dve", lambda e, s_=s_: e.reciprocal(s_[:, 7:8].ap, s_[:, 7:8].ap), [s_], [s_])
        k.tt("dve", s_[:, 7:8], s_[:, 7:8], s_[:, 3:4], ALU.mult)
        k.tt("dve", s_[:, 6:7], s_[:, 6:7], s_[:, 7:8], ALU.mult)
        k.ts("dve", cb, o1[:, :], s_[:, 7:8], None, ALU.mult)
        k.stt("dve", cb, o2[:, :], s_[:, 6:7], cb, ALU.mult, ALU.add)
    k = k_main
    replay_merged(k, recs[0].segs[0], recs[1].segs[0])
    k.pop()

    k.push()
    NW = 3
    ewg = [k.sb(f"ewg{i}", [128, 8, 256], BF16) for i in range(NW)]
    ewu = [k.sb(f"ewu{i}", [128, 8, 256], BF16) for i in range(NW)]
    ewd = [k.sb(f"ewd{i}", [128, 2, D], BF16) for i in range(NW)]
    sgs = [k.sb(f"sG{i}", [128, 512], BF16) for i in range(2)]
    hcs = [k.sb(f"Hc{i}", [128, 2, 512], BF16) for i in range(2)]
    n1 = 0
    n2 = [0]
    pending = None

    def down(e, tb, hc, wdt):
        for tt_ in range(4):
            t = tb * 4 + tt_
            for half in range(2):
                bO = banks[5 + n2[0] % 3]
                n2[0] += 1
                for fc in range(2):
                    k.mm(bO[:, :], hc[:, fc, tt_ * 128:(tt_ + 1) * 128], wdt[:, fc, half * 512:(half + 1) * 512],
                         start=(fc == 0), stop=(fc == 1))
                k.stt("dve", acc[t][:, half * 512:(half + 1) * 512], bO[:, :], comb_all[:, t, e:e + 1],
                      acc[t][:, half * 512:(half + 1) * 512], ALU.mult, ALU.add)

    for e in range(NEXP):
        wgt, wut, wdt = ewg[e % NW], ewu[e % NW], ewd[e % NW]
        k.dma("pool", wgt[:, :, :], ewg_d.v(ewg_d.h[e].rearrange("(kc p) f -> p kc f", p=128)))
        k.dma("pool", wut[:, :, :], ewu_d.v(ewu_d.h[e].rearrange("(kc p) f -> p kc f", p=128)))
        k.dma("pool", wdt[:, :, :], ewd_d.v(ewd_d.h[e].rearrange("(fc p) d -> p fc d", p=128)))
        for tb in range(NB):
            tsl = slice(tb * 512, (tb + 1) * 512)
            hc = hcs[tb % 2]
            for fc in range(2):
                bG = banks[1 + n1 % 2]
                bU = banks[3 + n1 % 2]
                sgt = sgs[n1 % 2]
                n1 += 1
                for kc in range(8):
                    k.mm(bG[:, :], wgt[:, kc, fc * 128:(fc + 1) * 128], h1T[:, kc, tsl], start=(kc == 0), stop=(kc == 7))
                for kc in range(8):
                    k.mm(bU[:, :], wut[:, kc, fc * 128:(fc + 1) * 128], h1T[:, kc, tsl], start=(kc == 0), stop=(kc == 7))
                k.act(sgt[:, :], bG[:, :], AF.Silu)
                k.tt("dve", hc[:, fc, :], bU[:, :], sgt[:, :], ALU.mult)
            if pending is not None:
                down(*pending)
            pending = (e, tb, hc, wdt)
    down(*pending)
    k.pop()

    k.push()
    g2 = k.sb("g2", [128, D], F32)
    b2 = k.sb("b2", [128, D], F32)
    k.dma("sp", g2[:, :], bcast_row(ln2g_d))
    k.dma("sp", b2[:, :], bcast_row(ln2b_d))
    ys = [k.sb(f"y{i}", [128, D], F32) for i in range(2)]
    tmps = [k.sb(f"lt{i}", [128, D], F32) for i in range(2)]
    sts = [k.sb(f"ls{i}", [128, 4], F32) for i in range(2)]
    if not last:
        hTsb = k.sb("hTsb", [128, 8, ntok], BF16)
    recs = [Rec(k), Rec(k)]
    for t in range(NT):
        p = t % 2
        kr = recs[p]
        layer_norm_tile(kr, acc[t][:, :], ys[p][:, :], g2[:, :], b2[:, :], tmps[p][:, :], sts[p][:, :], eng_g="dve")
        if last:
            kr.dma("sp", out_d[t * 128:(t + 1) * 128, :], ys[p][:, :])
        else:
            kr.dma("sp", h_d[t * 128:(t + 1) * 128, :], ys[p][:, :])
            emit_publish_tile(kr, banks, ident, ys[p], hTsb, t)
    replay_merged(k, recs[0].segs[0], recs[1].segs[0])
    if not last:
        emit_allgather_h(k, hTsb, G)
    k.pop()
    k.pop()


CONST_SHAPES = {"ident": [128, 128], "esel": [128, 64], "frq": [128, 1], "sgn": [128, 1],
                "cU": [128, 128], "cUrel": [128, 128], "cW": [128, 128], "cones": [128, 4], "maskbd": [128, 128],
                "rowmask": [128, 4], "uinc": [128, 128], "lpos_s": [128, 128], "uneg": [128, 128]}

LAYER_SHAPES = {
    "w_lat": [D, 448], "g_q": [128, 2], "g_kv": [128, 1], "w_uq": [256, 256], "w_uk": [128, 128], "w_uv": [128, 128],
    "w_hg": [D, 512], "hg_o_norm": [1, 128],
    "w_dn": [D, 514], "conv_w": [128, 3, 4], "a_log": [1, 1], "dt_bias": [1, 1], "dn_o_norm": [1, 128],
    "w_gates": [D, 3 * D], "w_br_a": [512, D], "w_br_b": [512, D], "w_br_c": [512, D], "w_out": [D, D],
    "ln1_g": [1, D], "ln1_b": [1, D], "ln2_g": [1, D], "ln2_b": [1, D],
    "w_router": [D, 36], "b_router": [1, 36],
    "exp_w_gate": [NEXP, D, 256], "exp_w_up": [NEXP, D, 256], "exp_w_down": [NEXP, 256, D],
}


def build_fused():
    nc = bass.Bass("TRN2", target_bir_lowering=False)
    k = K(nc)
    ntok = S * B // NCORES
    G = {}
    G["x"] = k.dram("x", [ntok, D], F32, "ExternalInput")
    G["ln_in_g"] = k.dram("ln_in_g", [1, D], F32, "ExternalInput")
    G["ln_in_b"] = k.dram("ln_in_b", [1, D], F32, "ExternalInput")
    G["pos"] = k.dram("pos", [1, S], I32, "ExternalInput")
    G["lb_rows"] = k.dram("lb_rows", [DEPTH, 128], F32, "ExternalInput")
    G["lb_cols"] = k.dram("lb_cols", [128, DEPTH], F32, "ExternalInput")
    rank_d = k.dram("rank", [1, 1], I32, "ExternalInput")
    tri_d = k.dram("tri", [128, 128], F32, "ExternalInput")
    G["out"] = k.dram("out", [ntok, D], F32, "ExternalOutput")
    cdram = {n: k.dram("c_" + n, shp, F32, "ExternalInput") for n, shp in CONST_SHAPES.items()}
    Ws = [{n: k.dram(f"L{l}_{n}", shp, F32, "ExternalInput") for n, shp in LAYER_SHAPES.items()} for l in range(DEPTH)]

    G["h_cur"] = k.dram("h_cur", [ntok, D], F32, "Internal")
    G["hsrc"] = [k.dram(f"hsrc{q}", [256, ntok], BF16, "Internal") for q in range(4)]
    G["hdst"] = [k.dram(f"hdst{q}", [4 * 256, ntok], BF16, "Internal") for q in range(4)]
    G["osrc"] = [[k.dram(f"osrc{br}_{q}", [128, ntok], BF16, "Internal") for q in range(4)] for br in range(3)]
    odst_full = [nc.dram_tensor(f"odst{br}", [4, 512, ntok], BF16, kind="Internal").ap() for br in range(3)]
    G["odst"] = [[T(odst_full[br][q], f"odst{br}_{q}") for q in range(4)] for br in range(3)]

    def hT_blk(blk, kc):
        r, t0, q = blk // 4, (blk % 4) * 512, kc // 2
        row0 = r * 256 + (kc % 2) * 128
        return G["hdst"][q][row0:row0 + 128, t0:t0 + 512]

    G["hT_blk"] = hT_blk

    reg = nc.sync.alloc_register("rank")
    nc.sync.reg_load(reg, rank_d.h[0:1, 0:1])
    rank_off = nc.sync.snap(reg, min_val=0, max_val=3)

    def o_own(br, kc, tsl):
        ap = odst_full[br][bass.ds(rank_off, 1), kc * 128:(kc + 1) * 128, tsl].rearrange("o p c -> (o p) c")
        return V(ap, G["odst"][br][0].res)

    G["o_own"] = o_own
    G["o_own_res"] = lambda br: [G["odst"][br][q].res for q in range(1, 4)]

    banks = [k.ps(f"bank{i}", [128, 512], F32) for i in range(8)]

    cres = Res("consts")
    C = {}
    for n, shp in CONST_SHAPES.items():
        C[n] = k.sb("c_" + n, shp, F32, res=cres)
        k.dma("sp", C[n][tuple(slice(None) for _ in shp)], cdram[n][tuple(slice(None) for _ in shp)])
    C["tri"] = k.sb("c_tri", [128, 128], BF16)
    k.dma("pool", C["tri"][:, :], tri_d[:, :])

    emit_ln0(k, banks, C, G)
    for l in range(DEPTH):
        W = Ws[l]
        emit_mla(k, banks, C, W, G)
        emit_allgather_o(k, G, 0)
        emit_dn_hg(k, banks, C, W, G, l)
        emit_stage_c(k, banks, C, W, G, last=(l == DEPTH - 1))
    k.finish([G["out"]])
    assert 5 + k.ndsem + k.ncoll <= 100, (k.ndsem, k.ncoll)
    build_fused.stats = (k.ndsem, k.ncoll, dict(k.tok))
    return nc


def layer_inputs(P, l, j):
    m = {}
    a = mla_inputs(None, np.zeros(1, np.int32), P, l, j)
    for n in ("w_lat", "g_q", "g_kv", "w_uq", "w_uk", "w_uv"):
        m[n] = a[n]
    hgi = hg_inputs(None, P, l, j)
    m["w_hg"] = hgi["w_hg"]
    m["hg_o_norm"] = hgi["o_norm"]
    dni = dn_inputs(None, P, l, j)
    for n in ("w_dn", "conv_w", "a_log", "dt_bias"):
        m[n] = dni[n]
    m["dn_o_norm"] = dni["o_norm"]
    w_in = P["w_in"][l]
    m.update({
        "w_gates": np.ascontiguousarray(w_in[:, 4520:]),
        "w_br_a": P["w_br_a"][l], "w_br_b": P["w_br_b"][l], "w_br_c": P["w_br_c"][l], "w_out": P["w_out"][l],
        "ln1_g": P["ln1_g"][l].reshape(1, D), "ln1_b": P["ln1_b"][l].reshape(1, D),
        "ln2_g": P["ln2_g"][l].reshape(1, D), "ln2_b": P["ln2_b"][l].reshape(1, D),
        "w_router": np.ascontiguousarray(np.concatenate([P["router_group_w"][l], P["router_expert_w"][l]], axis=1)),
        "b_router": np.concatenate([P["router_group_b"][l], P["router_expert_b"][l]]).reshape(1, 36),
        "exp_w_gate": P["exp_w_gate"][l], "exp_w_up": P["exp_w_up"][l], "exp_w_down": P["exp_w_down"][l],
    })
    return {f"L{l}_{n}": np.ascontiguousarray(v, dtype=np.float32) for n, v in m.items()}


def const_inputs():
    c = {}
    c.update(hg_consts())
    c.update(dn_consts())
    a = mla_inputs(None, np.zeros(1, np.int32), None, 0, 0, consts_only=True)
    c.update({n: a[n] for n in ("frq", "sgn", "esel")})
    out = {"c_" + n: np.ascontiguousarray(c[n], dtype=np.float32) for n in CONST_SHAPES}
    out["tri"] = a["tri"]
    return out


def kernel(**inputs):
    P = {k_: np.asarray(v) for k_, v in inputs.items()}
    x = np.asarray(P["x"], dtype=np.float32).reshape(B * S, D)
    pos = P["positions"]
    per = B * S // NCORES
    nc = build_fused()
    consts = const_inputs()
    in_maps = []
    for c in range(NCORES):
        b, j = c // 4, c % 4
        m = dict(consts)
        m["x"] = np.ascontiguousarray(x[c * per:(c + 1) * per])
        m["ln_in_g"] = P["ln_in_g"].reshape(1, D).astype(np.float32)
        m["ln_in_b"] = P["ln_in_b"].reshape(1, D).astype(np.float32)
        m["pos"] = np.ascontiguousarray(pos[b].reshape(1, S).astype(np.int32))
        lb = P["hg_lower_bounds"][:, j * 128:(j + 1) * 128].astype(np.float32)
        m["lb_rows"] = np.ascontiguousarray(lb)
        m["lb_cols"] = np.ascontiguousarray(lb.T)
        m["rank"] = np.array([[j]], np.int32)
        for l in range(DEPTH):
            m.update(layer_inputs(P, l, j))
        in_maps.append(m)
    res = run_bass_kernel_spmd(nc, in_maps, core_ids=list(range(NCORES)))
    out = np.concatenate([r["out"] for r in res.results], axis=0)
    return np.ascontiguousarray(out.reshape(B, S, D).astype(np.float32))
```

```python
from contextlib import ExitStack
import numpy as np
import concourse.bass as bass
import concourse.mybir as mybir
from concourse.bass_utils import run_bass_kernel_spmd

F32 = mybir.dt.float32
BF16 = mybir.dt.bfloat16
I32 = mybir.dt.int32
AF = mybir.ActivationFunctionType
ALU = mybir.AluOpType
AX = mybir.AxisListType

NCORES = 8
D = 1024
B = 2
S = 8192
DEPTH = 2
ALPHA = (2 * DEPTH) ** 0.25
EPS = 1e-6
IN_COLS = 7592
NEXP = 32


class Res:
    __slots__ = ("name", "w", "r", "dsem", "dkey", "dcount", "wdma", "excl")

    def __init__(self, name=""):
        self.name = name
        self.w = None
        self.r = {}
        self.dsem = None
        self.dkey = None
        self.dcount = 0
        self.wdma = False
        self.excl = False


class V:
    __slots__ = ("ap", "res")

    def __init__(self, ap, res):
        self.ap = ap
        self.res = res

    def __getitem__(self, key):
        return V(self.ap[key], self.res)

    def f(self, fn):
        return V(fn(self.ap), self.res)

    def bitcast(self, dt):
        return V(self.ap.bitcast(dt), self.res)


class T:
    def __init__(self, handle, name, res=None):
        self.h = handle
        self.res = res if res is not None else Res(name)

    def __getitem__(self, key):
        return V(self.h[key], self.res)

    def v(self, ap):
        return V(ap, self.res)


class _Dummy:
    def then_inc(self, *a, **kw):
        return self


_DUMMY = _Dummy()


class K:
    def __init__(self, nc):
        self.nc = nc
        self.E = {"pe": nc.tensor, "dve": nc.vector, "act": nc.scalar, "pool": nc.gpsimd, "sp": nc.sync}
        self.semobj = {}
        self.tok = {}
        self.seen = {n: {} for n in self.E}
        for n in self.E:
            self.semobj["s_" + n] = nc.alloc_semaphore("s_" + n)
            self.tok[n] = 0
        self.ndsem = 0
        self.dres = []
        self.nuniq = 0
        self.stacks = []
        self.phase_res = []
        self.free_dsems = []
        self.ncoll = 0
        self.coll_tokens = []
        self.dry = None

    def sb(self, name, shape, dt, res=None):
        self.nuniq += 1
        if self.stacks:
            h = self.stacks[-1].enter_context(self.nc.sbuf_tensor(f"{name}_{self.nuniq}", list(shape), dt))
        else:
            h = self.nc.alloc_sbuf_tensor(f"{name}_{self.nuniq}", list(shape), dt)
        t = T(h, name, res=res)
        if self.stacks and res is None:
            self.phase_res[-1].append(t.res)
        return t

    def push(self):
        self.stacks.append(ExitStack())
        self.phase_res.append([])

    def pop(self):
        self.barrier()
        self.stacks.pop().close()
        for r in self.phase_res.pop():
            if r.dsem is not None:
                self.free_dsems.append((r.dsem, r.dkey, r.dcount))
                self.dres.remove(r)
                r.dsem = None

    def ps(self, name, shape, dt=F32):
        self.nuniq += 1
        t = T(self.nc.alloc_psum_tensor(f"{name}_{self.nuniq}", list(shape), dt), name)
        t.res.excl = True
        return t

    def dram(self, name, shape, dt, kind):
        h = self.nc.dram_tensor(name, list(shape), dt, kind=kind)
        return T(h.ap(), name)

    def _gather(self, eng, reads, writes):
        own = "s_" + eng
        deps = {}

        def add(t, raw):
            if t is None:
                return
            k, v = t
            if k == own and (eng == "pe" or not raw):
                return
            if deps.get(k, 0) < v:
                deps[k] = v

        for r in reads:
            add(r.w, True)
            if r.excl:
                for k, v in r.r.items():
                    add((k, v), False)
        for w in writes:
            add(w.w, False)
            for k, v in w.r.items():
                add((k, v), False)
        return deps

    def _emit_waits(self, eng, deps):
        e = self.E[eng]
        seen = self.seen[eng]
        for k, v in deps.items():
            if k.startswith("s_"):
                assert v <= self.tok[k[2:]], f"wait on unrealised token {k} {v} > {self.tok[k[2:]]}"
            if seen.get(k, 0) >= v:
                continue
            e.wait_ge(self.semobj[k], v)
            seen[k] = v

    def op(self, eng, fn, reads, writes, inc=True):
        reads = [r.res if isinstance(r, (V, T)) else r for r in reads if r is not None]
        writes = [w.res if isinstance(w, (V, T)) else w for w in writes if w is not None]
        if self.dry is not None:
            self.dry.append((eng, reads, writes))
            return _DUMMY
        deps = self._gather(eng, reads, writes)
        self._emit_waits(eng, deps)
        ins = fn(self.E[eng])
        key = "s_" + eng
        if inc:
            ins.then_inc(self.semobj[key], 1)
            self.tok[eng] += 1
            t = (key, self.tok[eng])
        else:
            t = (key, self.tok[eng] + 1)
        for w in writes:
            w.w = t
            w.r = {}
            w.wdma = False
        for r in reads:
            if r in writes:
                continue
            if r.r.get(key, 0) < t[1]:
                r.r[key] = t[1]
        return ins

    def collective(self, kind, ins, outs, groups):
        deps = {}

        def add(t):
            if t is None:
                return
            k_, v = t
            if deps.get(k_, 0) < v:
                deps[k_] = v

        for i in ins:
            add(i.res.w)
        for o in outs:
            add(o.res.w)
            for k_, v in o.res.r.items():
                add((k_, v))
        self._emit_waits("pool", deps)
        self.ncoll += 1
        key = f"cc{self.ncoll}"
        sem = self.nc.alloc_semaphore(key)
        self.semobj[key] = sem
        self.E["pool"].collective_compute(kind, ALU.bypass, replica_groups=groups,
                                          ins=[i.ap.opt() for i in ins], outs=[o.ap.opt() for o in outs]).then_inc(sem, 1)
        self.coll_tokens.append((key, 1))
        for o in outs:
            o.res.w = (key, 1)
            o.res.r = {}
            o.res.wdma = False
        for i in ins:
            i.res.r[key] = 1

    def dma(self, q, out, in_, extra_reads=(), **kw):
        w = out.res
        rd = in_.res
        if self.dry is not None:
            self.dry.append(("dma", [rd] + list(extra_reads), [w]))
            return
        own = "s_" + q
        deps = {}

        def add(t):
            if t is None:
                return
            k, v = t
            if deps.get(k, 0) < v:
                deps[k] = v

        add(rd.w)
        for xr in extra_reads:
            add(xr.w)
        if not (w.wdma and not w.r):
            add(w.w)
        for k, v in w.r.items():
            add((k, v))
        self._emit_waits(q, deps)
        if w.dsem is None:
            if self.free_dsems:
                w.dsem, w.dkey, w.dcount = self.free_dsems.pop()
            else:
                self.ndsem += 1
                w.dkey = f"d{self.ndsem}"
                w.dsem = self.nc.alloc_semaphore(w.dkey)
                self.semobj[w.dkey] = w.dsem
                w.dcount = 0
            self.dres.append(w)
        self.E[q].dma_start(out=out.ap, in_=in_.ap, **kw).then_inc(w.dsem, 16)
        w.dcount += 16
        t = (w.dkey, w.dcount)
        w.w = t
        w.r = {}
        w.wdma = True
        if rd.r.get(t[0], 0) < t[1]:
            rd.r[t[0]] = t[1]
        for xr in extra_reads:
            if xr.r.get(t[0], 0) < t[1]:
                xr.r[t[0]] = t[1]

    def barrier(self):
        for eng in self.E:
            deps = {}
            for x in self.E:
                if x != eng and self.tok[x] > 0:
                    deps["s_" + x] = self.tok[x]
            for r in self.dres:
                deps[r.dkey] = max(deps.get(r.dkey, 0), r.dcount)
            for key, v in self.coll_tokens:
                deps[key] = v
            self._emit_waits(eng, deps)

    def finish(self, outs):
        deps = {}
        for o in outs:
            r = o.res if isinstance(o, (V, T)) else o
            deps[r.w[0]] = r.w[1]
        self._emit_waits("sp", deps)

    def mm(self, out, lhsT, rhs, start=True, stop=True, inc=None, extra_reads=()):
        if inc is None:
            inc = stop
        return self.op("pe", lambda e: e.matmul(out.ap, lhsT.ap, rhs.ap, start=start, stop=stop),
                       [lhsT, rhs] + list(extra_reads), [out], inc=inc)

    def transpose(self, out, in_, ident, inc=True):
        return self.op("pe", lambda e: e.transpose(out.ap, in_.ap, ident.ap), [in_, ident], [out], inc=inc)

    def act(self, out, in_, func, bias=None, scale=None, accum_out=None, eng="act"):
        kw = {}
        rd = [in_]
        if bias is not None:
            if isinstance(bias, V):
                kw["bias"] = bias.ap
                rd.append(bias)
            else:
                kw["bias"] = bias
        if scale is not None:
            if isinstance(scale, V):
                kw["scale"] = scale.ap
                rd.append(scale)
            else:
                kw["scale"] = scale
        wr = [out]
        if accum_out is not None:
            kw["accum_out"] = accum_out.ap
            wr.append(accum_out)
        return self.op("act", lambda e: e.activation(out.ap, in_.ap, func, **kw), rd, wr)

    def tt(self, eng, out, in0, in1, op):
        return self.op(eng, lambda e: e.tensor_tensor(out.ap, in0.ap, in1.ap, op), [in0, in1], [out])

    def ts(self, eng, out, in0, s1, s2, op0, op1=None, accum_out=None):
        rd = [in0]
        a1 = s1
        a2 = s2
        if isinstance(s1, V):
            rd.append(s1)
            a1 = s1.ap
        if isinstance(s2, V):
            rd.append(s2)
            a2 = s2.ap
        wr = [out]
        kw = {}
        if op1 is not None:
            kw["op1"] = op1
        if accum_out is not None:
            kw["accum_out"] = accum_out.ap
            wr.append(accum_out)
        return self.op(eng, lambda e: e.tensor_scalar(out.ap, in0.ap, a1, a2, op0, **kw), rd, wr)

    def stt(self, eng, out, in0, scalar, in1, op0, op1):
        rd = [in0, in1]
        a = scalar
        if isinstance(scalar, V):
            rd.append(scalar)
            a = scalar.ap
        return self.op(eng, lambda e: e.scalar_tensor_tensor(out.ap, in0.ap, a, in1.ap, op0, op1), rd, [out])

    def rstd(self, out, in_, scale, eps):
        self.act(out, in_, AF.Sqrt, bias=eps, scale=scale)
        self.op("dve", lambda e: e.reciprocal(out.ap, out.ap), [out], [out])

    def rstd_ln(self, out, in_, scale, eps):
        self.act(out, in_, AF.Ln, bias=eps, scale=scale)
        self.act(out, out, AF.Exp, scale=-0.5)

    def copy(self, eng, out, in_):
        if eng == "act":
            return self.op("act", lambda e: e.copy(out.ap, in_.ap), [in_], [out])
        return self.op(eng, lambda e: e.tensor_copy(out.ap, in_.ap), [in_], [out])

    def memset(self, eng, out, val):
        return self.op(eng, lambda e: e.memset(out.ap, val), [], [out])

    def reduce(self, eng, out, in_, op, axis=AX.X):
        return self.op(eng, lambda e: e.tensor_reduce(out.ap, in_.ap, axis, op), [in_], [out])


def layer_norm_tile(k, x, out, g_b, b_b, tmp, st, pre_scale_res=None, eng_g="pool"):
    k.reduce("dve", st[:, 0:1], x, ALU.add)
    k.ts("dve", st[:, 1:2], st[:, 0:1], -1.0 / D, None, ALU.mult)
    k.act(tmp, x, AF.Square, bias=st[:, 1:2], scale=1.0, accum_out=st[:, 2:3])
    k.rstd_ln(st[:, 3:4], st[:, 2:3], 1.0 / D, EPS)
    k.ts("dve", tmp, x, st[:, 1:2], st[:, 3:4], ALU.add, ALU.mult)
    k.tt(eng_g, tmp, tmp, g_b, ALU.mult)
    k.tt("pool", out, tmp, b_b, ALU.add)


def build_ln0(ntok):
    nc = bass.Bass("TRN2", target_bir_lowering=False)
    k = K(nc)
    x = k.dram("x", [ntok, D], F32, "ExternalInput")
    g = k.dram("g", [1, D], F32, "ExternalInput")
    b = k.dram("b", [1, D], F32, "ExternalInput")
    y = k.dram("y", [ntok, D], F32, "ExternalOutput")
    g_b = k.sb("g_b", [128, D], F32)
    b_b = k.sb("b_b", [128, D], F32)
    k.dma("sp", g_b[:, :], g.v(g.h.partition_broadcast(128)))
    k.dma("sp", b_b[:, :], b.v(b.h.partition_broadcast(128)))
    nt = ntok // 128
    xs = [k.sb(f"x{i}", [128, D], F32) for i in range(2)]
    ys = [k.sb(f"y{i}", [128, D], F32) for i in range(2)]
    tmps = [k.sb(f"t{i}", [128, D], F32) for i in range(2)]
    sts = [k.sb(f"s{i}", [128, 4], F32) for i in range(2)]
    for i in range(nt):
        xt, yt, tt_, st = xs[i % 2], ys[i % 2], tmps[i % 2], sts[i % 2]
        k.dma("sp", xt[:, :], x[i * 128:(i + 1) * 128, :])
        layer_norm_tile(k, xt[:, :], yt[:, :], g_b[:, :], b_b[:, :], tt_[:, :], st[:, :])
        k.dma("sp", y[i * 128:(i + 1) * 128, :], yt[:, :])
    k.finish([y])
    return nc


def run_ln0(x, g, b):
    T_ = x.shape[0] * x.shape[1]
    xs = x.reshape(T_, D)
    per = T_ // NCORES
    nc = build_ln0(per)
    in_maps = [{"x": np.ascontiguousarray(xs[c * per:(c + 1) * per]), "g": g.reshape(1, D), "b": b.reshape(1, D)}
               for c in range(NCORES)]
    res = run_bass_kernel_spmd(nc, in_maps, core_ids=list(range(NCORES)))
    return np.concatenate([r["y"] for r in res.results], axis=0)


BIG = 1.0e30


def bcast_row(t, n=128):
    return t.v(t.h.partition_broadcast(n))


def build_stage_c(ntok, upto=9):
    nc = bass.Bass("TRN2", target_bir_lowering=False)
    k = K(nc)
    NT = ntok // 128
    NB = ntok // 512
    h_d = k.dram("h", [ntok, D], F32, "ExternalInput")
    hT_d = k.dram("hT", [D, ntok], F32, "ExternalInput")
    oT_d = [k.dram(n, [512, ntok], F32, "ExternalInput") for n in ("oaT", "obT", "ocT")]
    wg_d = k.dram("w_gates", [D, 3 * D], F32, "ExternalInput")
    wbr_d = [k.dram(n, [512, D], F32, "ExternalInput") for n in ("w_br_a", "w_br_b", "w_br_c")]
    wout_d = k.dram("w_out", [D, D], F32, "ExternalInput")
    ln1g_d = k.dram("ln1_g", [1, D], F32, "ExternalInput")
    ln1b_d = k.dram("ln1_b", [1, D], F32, "ExternalInput")
    ln2g_d = k.dram("ln2_g", [1, D], F32, "ExternalInput")
    ln2b_d = k.dram("ln2_b", [1, D], F32, "ExternalInput")
    wr_d = k.dram("w_router", [D, 36], F32, "ExternalInput")
    br_d = k.dram("b_router", [1, 36], F32, "ExternalInput")
    ewg_d = k.dram("exp_w_gate", [NEXP, D, 256], F32, "ExternalInput")
    ewu_d = k.dram("exp_w_up", [NEXP, D, 256], F32, "ExternalInput")
    ewd_d = k.dram("exp_w_down", [NEXP, 256, D], F32, "ExternalInput")
    ident_d = k.dram("ident", [128, 128], F32, "ExternalInput")
    out_d = k.dram("out", [ntok, D], F32, "ExternalOutput")

    banks = [k.ps(f"bank{i}", [128, 512], F32) for i in range(8)]

    ident = k.sb("ident", [128, 128], F32)
    k.dma("sp", ident[:, :], ident_d[:, :])
    comb_all = k.sb("comb_all", [128, NT, 32], F32)
    mixT = k.sb("mixT", [128, 8, ntok], BF16)

    k.push()
    wg = k.sb("wg", [128, 8, 3 * D], BF16)
    wbr = k.sb("wbr", [128, 12, D], BF16)
    for kc in range(8):
        k.dma("pool", wg[:, kc, :], wg_d[kc * 128:(kc + 1) * 128, :])
    for br in range(3):
        for kc in range(4):
            k.dma("pool", wbr[:, br * 4 + kc, :], wbr_d[br][kc * 128:(kc + 1) * 128, :])
    hTb = [k.sb(f"hTb{i}", [128, 8, 512], BF16) for i in range(2)]
    oTb = [k.sb(f"oTb{i}", [128, 12, 512], BF16) for i in range(2)]
    sg = [k.sb(f"sg{i}", [128, 512], BF16) for i in range(2)]
    tmx = [k.sb(f"tmx{i}", [128, 512], F32) for i in range(2)]
    mixf = [k.sb(f"mixf{i}", [128, 512], F32) for i in range(2)]
    nb = 0
    for tb in range(NB):
        tsl = slice(tb * 512, (tb + 1) * 512)
        hb, ob = hTb[tb % 2], oTb[tb % 2]
        for kc in range(8):
            k.dma("pool", hb[:, kc, :], hT_d[kc * 128:(kc + 1) * 128, tsl])
        for br in range(3):
            for kc in range(4):
                k.dma("pool", ob[:, br * 4 + kc, :], oT_d[br][kc * 128:(kc + 1) * 128, tsl])
        for r in range(8):
            mf = mixf[r % 2]
            for br in range(3):
                bg = banks[nb % 2]
                by = banks[2 + nb % 2]
                sgt = sg[nb % 2]
                tm = tmx[nb % 2]
                nb += 1
                col = br * D + r * 128
                for kc in range(8):
                    k.mm(bg[:, :], wg[:, kc, col:col + 128], hb[:, kc, :], start=(kc == 0), stop=(kc == 7))
                k.act(sgt[:, :], bg[:, :], AF.Sigmoid)
                for kc in range(4):
                    k.mm(by[:, :], wbr[:, br * 4 + kc, r * 128:(r + 1) * 128], ob[:, br * 4 + kc, :],
                         start=(kc == 0), stop=(kc == 3))
                if br == 0:
                    k.tt("dve", mf[:, :], by[:, :], sgt[:, :], ALU.mult)
                elif br == 1:
                    k.tt("dve", tm[:, :], by[:, :], sgt[:, :], ALU.mult)
                    k.tt("pool", mf[:, :], mf[:, :], tm[:, :], ALU.add)
                else:
                    k.tt("dve", tm[:, :], by[:, :], sgt[:, :], ALU.mult)
                    k.tt("pool", mixT[:, r, tsl], mf[:, :], tm[:, :], ALU.add)
    k.pop()
    if upto == 0:
        dbg = k.dram("dbg", [128, 8 * ntok], BF16, "ExternalOutput")
        k.dma("sp", dbg[:, :], mixT[:, :, :].f(lambda a: a.rearrange("p a b -> p (a b)")))
        k.finish([dbg])
        return nc

    acc = [k.sb(f"acc{i}", [128, D], F32) for i in range(NT)]
    h1T = k.sb("h1T", [128, 8, ntok], BF16)
    k.push()
    wout = k.sb("wout", [128, 8, D], BF16)
    for kc in range(8):
        k.dma("pool", wout[:, kc, :], wout_d[kc * 128:(kc + 1) * 128, :])
    wr = k.sb("wr", [128, 8, 36], F32)
    for kc in range(8):
        k.dma("sp", wr[:, kc, :], wr_d[kc * 128:(kc + 1) * 128, :])
    brb = k.sb("brb", [128, 36], F32)
    k.dma("sp", brb[:, :], bcast_row(br_d))
    g1 = k.sb("g1", [128, D], F32)
    b1 = k.sb("b1", [128, D], F32)
    k.dma("sp", g1[:, :], bcast_row(ln1g_d))
    k.dma("sp", b1[:, :], bcast_row(ln1b_d))
    hts = [k.sb(f"ht{i}", [128, D], F32) for i in range(2)]
    x1s = [k.sb(f"x1{i}", [128, D], F32) for i in range(2)]
    h1s = [k.sb(f"h1{i}", [128, D], F32) for i in range(2)]
    tmps = [k.sb(f"lt{i}", [128, D], F32) for i in range(2)]
    sts = [k.sb(f"ls{i}", [128, 4], F32) for i in range(2)]
    hTf = [k.sb(f"hTf{i}", [128, 8, 128], F32) for i in range(2)]
    rl = [k.sb(f"rl{i}", [128, 36], F32) for i in range(2)]
    rs = [k.sb(f"rs{i}", [128, 16], F32) for i in range(2)]
    elm = [k.sb(f"elm{i}", [128, 32], F32) for i in range(2)]
    elm2 = [k.sb(f"elm2{i}", [128, 32], F32) for i in range(2)]
    oh1 = [k.sb(f"oh1{i}", [128, 32], F32) for i in range(2)]
    oh2 = [k.sb(f"oh2{i}", [128, 32], F32) for i in range(2)]
    for t in range(NT):
        p = t % 2
        tok = slice(t * 128, (t + 1) * 128)
        ht, x1, h1t, tmp, st = hts[p], x1s[p], h1s[p], tmps[p], sts[p]
        k.dma("sp", ht[:, :], h_d[tok, :])
        for half in range(2):
            bk = banks[half + 2 * p]
            for kc in range(8):
                k.mm(bk[:, :], mixT[:, kc, tok], wout[:, kc, half * 512:(half + 1) * 512],
                     start=(kc == 0), stop=(kc == 7))
            k.stt("dve", x1[:, half * 512:(half + 1) * 512], ht[:, half * 512:(half + 1) * 512], ALPHA,
                  bk[:, :], ALU.mult, ALU.add)
        layer_norm_tile(k, x1[:, :], h1t[:, :], g1[:, :], b1[:, :], tmp[:, :], st[:, :])
        k.act(acc[t][:, :], h1t[:, :], AF.Copy, scale=ALPHA)
        hf = hTf[p]
        for q4 in range(2):
            bk = banks[4 + q4 + 2 * p]
            for j in range(4):
                kc = q4 * 4 + j
                k.transpose(bk[:, j * 128:(j + 1) * 128], h1t[:, kc * 128:(kc + 1) * 128], ident[:, :],
                            inc=(j == 3))
            k.copy("dve", hf[:, q4 * 4:(q4 + 1) * 4, :],
                   bk[:, :].f(lambda a: a.rearrange("p (j t) -> p j t", j=4)))
        k.copy("act", h1T[:, :, tok], hf[:, :, :])
        bk = banks[p]
        for kc in range(8):
            k.mm(bk[:, 0:36], hf[:, kc, :], wr[:, kc, :], start=(kc == 0), stop=(kc == 7))
        l, s_, em, em2, o1, o2 = rl[p], rs[p], elm[p], elm2[p], oh1[p], oh2[p]
        cb = comb_all[:, t, :]
        k.tt("dve", l[:, :], bk[:, 0:36], brb[:, :], ALU.add)
        k.reduce("dve", s_[:, 0:1], l[:, 0:4], ALU.max)
        k.ts("dve", s_[:, 1:2], s_[:, 0:1], -1.0, None, ALU.mult)
        k.act(s_[:, 8:12], l[:, 0:4], AF.Exp, bias=s_[:, 1:2], scale=1.0, accum_out=s_[:, 2:3])
        k.op("dve", lambda e: e.reciprocal(s_[:, 3:4].ap, s_[:, 2:3].ap), [s_], [s_])
        k.ts("dve", s_[:, 12:16], l[:, 0:4], s_[:, 0:1], None, ALU.is_equal)
        k.ts("dve", s_[:, 12:16], s_[:, 12:16], BIG, -BIG, ALU.mult, ALU.add)
        k.tt("dve", em[:, :].f(lambda a: a.rearrange("p (g e) -> p g e", g=4)),
             l[:, 4:36].f(lambda a: a.rearrange("p (g e) -> p g e", g=4)),
             s_[:, 12:16].f(lambda a: a.unsqueeze(2).broadcast_to([128, 4, 8])), ALU.add)
        k.reduce("dve", s_[:, 4:5], em[:, :], ALU.max)
        k.ts("dve", o1[:, :], em[:, :], s_[:, 4:5], None, ALU.is_equal)
        k.stt("dve", em2[:, :], o1[:, :], -BIG, em[:, :], ALU.mult, ALU.add)
        k.reduce("dve", s_[:, 5:6], em2[:, :], ALU.max)
        k.ts("dve", o2[:, :], em2[:, :], s_[:, 5:6], None, ALU.is_equal)
        k.tt("dve", s_[:, 6:7], s_[:, 5:6], s_[:, 4:5], ALU.subtract)
        k.act(s_[:, 6:7], s_[:, 6:7], AF.Exp)
        k.ts("dve", s_[:, 7:8], s_[:, 6:7], 1.0, None, ALU.add)
        k.op("dve", lambda e: e.reciprocal(s_[:, 7:8].ap, s_[:, 7:8].ap), [s_], [s_])
        k.tt("dve", s_[:, 7:8], s_[:, 7:8], s_[:, 3:4], ALU.mult)
        k.tt("dve", s_[:, 6:7], s_[:, 6:7], s_[:, 7:8], ALU.mult)
        k.ts("dve", cb, o1[:, :], s_[:, 7:8], None, ALU.mult)
        k.stt("dve", cb, o2[:, :], s_[:, 6:7], cb, ALU.mult, ALU.add)
    k.pop()
    if upto == 1:
        dbg = k.dram("dbg", [128, NT * 32], F32, "ExternalOutput")
        k.dma("sp", dbg[:, :], comb_all[:, :, :].f(lambda a: a.rearrange("p a b -> p (a b)")))
        dbg2 = k.dram("dbg2", [ntok, D], F32, "ExternalOutput")
        for t in range(NT):
            k.dma("sp", dbg2[t * 128:(t + 1) * 128, :], acc[t][:, :])
        k.finish([dbg, dbg2])
        return nc

    k.push()
    NW = 3
    ewg = [k.sb(f"ewg{i}", [128, 8, 256], BF16) for i in range(NW)]
    ewu = [k.sb(f"ewu{i}", [128, 8, 256], BF16) for i in range(NW)]
    ewd = [k.sb(f"ewd{i}", [128, 2, D], BF16) for i in range(NW)]
    sgs = [k.sb(f"sG{i}", [128, 512], BF16) for i in range(2)]
    hcs = [k.sb(f"Hc{i}", [128, 2, 512], BF16) for i in range(2)]
    n1 = 0
    n2 = 0
    for e in range(NEXP):
        wgt, wut, wdt = ewg[e % NW], ewu[e % NW], ewd[e % NW]
        k.dma("pool", wgt[:, :, :], ewg_d.v(ewg_d.h[e].rearrange("(kc p) f -> p kc f", p=128)))
        k.dma("pool", wut[:, :, :], ewu_d.v(ewu_d.h[e].rearrange("(kc p) f -> p kc f", p=128)))
        k.dma("pool", wdt[:, :, :], ewd_d.v(ewd_d.h[e].rearrange("(fc p) d -> p fc d", p=128)))
        for tb in range(NB):
            tsl = slice(tb * 512, (tb + 1) * 512)
            hc = hcs[tb % 2]
            for fc in range(2):
                bG = banks[1 + n1 % 2]
                bU = banks[3 + n1 % 2]
                sgt = sgs[n1 % 2]
                n1 += 1
                for kc in range(8):
                    k.mm(bG[:, :], wgt[:, kc, fc * 128:(fc + 1) * 128], h1T[:, kc, tsl], start=(kc == 0), stop=(kc == 7))
                for kc in range(8):
                    k.mm(bU[:, :], wut[:, kc, fc * 128:(fc + 1) * 128], h1T[:, kc, tsl], start=(kc == 0), stop=(kc == 7))
                k.act(sgt[:, :], bG[:, :], AF.Silu)
                k.tt("dve", hc[:, fc, :], bU[:, :], sgt[:, :], ALU.mult)
            for tt_ in range(4):
                t = tb * 4 + tt_
                for half in range(2):
                    bO = banks[5 + n2 % 3]
                    n2 += 1
                    for fc in range(2):
                        k.mm(bO[:, :], hc[:, fc, tt_ * 128:(tt_ + 1) * 128], wdt[:, fc, half * 512:(half + 1) * 512],
                             start=(fc == 0), stop=(fc == 1))
                    k.stt("dve", acc[t][:, half * 512:(half + 1) * 512], bO[:, :], comb_all[:, t, e:e + 1],
                          acc[t][:, half * 512:(half + 1) * 512], ALU.mult, ALU.add)
    k.pop()

    k.push()
    g2 = k.sb("g2", [128, D], F32)
    b2 = k.sb("b2", [128, D], F32)
    k.dma("sp", g2[:, :], bcast_row(ln2g_d))
    k.dma("sp", b2[:, :], bcast_row(ln2b_d))
    ys = [k.sb(f"y{i}", [128, D], F32) for i in range(2)]
    tmps = [k.sb(f"lt{i}", [128, D], F32) for i in range(2)]
    sts = [k.sb(f"ls{i}", [128, 4], F32) for i in range(2)]
    for t in range(NT):
        p = t % 2
        layer_norm_tile(k, acc[t][:, :], ys[p][:, :], g2[:, :], b2[:, :], tmps[p][:, :], sts[p][:, :])
        k.dma("sp", out_d[t * 128:(t + 1) * 128, :], ys[p][:, :])
    k.finish([out_d])
    k.pop()
    return nc


def stage_c_consts():
    return np.eye(128, dtype=np.float32)


def run_stage_c(h, oa, ob, oc, P, l, upto=9):
    T_ = h.shape[0]
    per = T_ // NCORES
    nc = build_stage_c(per, upto)
    ident = stage_c_consts()
    w_in = P["w_in"][l]
    common = {
        "w_gates": np.ascontiguousarray(w_in[:, 4520:]),
        "w_br_a": P["w_br_a"][l], "w_br_b": P["w_br_b"][l], "w_br_c": P["w_br_c"][l],
        "w_out": P["w_out"][l],
        "ln1_g": P["ln1_g"][l].reshape(1, D), "ln1_b": P["ln1_b"][l].reshape(1, D),
        "ln2_g": P["ln2_g"][l].reshape(1, D), "ln2_b": P["ln2_b"][l].reshape(1, D),
        "w_router": np.ascontiguousarray(np.concatenate([P["router_group_w"][l], P["router_expert_w"][l]], axis=1)),
        "b_router": np.concatenate([P["router_group_b"][l], P["router_expert_b"][l]]).reshape(1, 36),
        "exp_w_gate": P["exp_w_gate"][l], "exp_w_up": P["exp_w_up"][l], "exp_w_down": P["exp_w_down"][l],
        "ident": ident,
    }
    in_maps = []
    for c in range(NCORES):
        sl = slice(c * per, (c + 1) * per)
        m = dict(common)
        m["h"] = np.ascontiguousarray(h[sl])
        m["hT"] = np.ascontiguousarray(h[sl].T)
        m["oaT"] = np.ascontiguousarray(oa[sl].T)
        m["obT"] = np.ascontiguousarray(ob[sl].T)
        m["ocT"] = np.ascontiguousarray(oc[sl].T)
        in_maps.append(m)
    res = run_bass_kernel_spmd(nc, in_maps, core_ids=list(range(NCORES)))
    if upto < 9:
        return res.results
    return np.concatenate([r["out"] for r in res.results], axis=0)


QK_SCALE = 96 ** -0.5
TWO_PI = 2.0 * np.pi


class BankRR:
    def __init__(self, banks):
        self.banks = banks
        self.i = 0

    def __call__(self):
        b = self.banks[self.i % len(self.banks)]
        self.i += 1
        return b


def build_mla(T=S, nblk=None):
    nc = bass.Bass("TRN2", target_bir_lowering=False)
    k = K(nc)
    NBLK = T // 512 if nblk is None else nblk
    hT_d = k.dram("hT", [D, T], F32, "ExternalInput")
    wlat_d = k.dram("w_lat", [D, 448], F32, "ExternalInput")
    gq_d = k.dram("g_q", [128, 2], F32, "ExternalInput")
    gkv_d = k.dram("g_kv", [128, 1], F32, "ExternalInput")
    wuq_d = k.dram("w_uq", [256, 256], F32, "ExternalInput")
    wuk_d = k.dram("w_uk", [128, 128], F32, "ExternalInput")
    wuv_d = k.dram("w_uv", [128, 128], F32, "ExternalInput")
    pos_d = k.dram("pos", [1, T], I32, "ExternalInput")
    frq_d = k.dram("frq", [128, 1], F32, "ExternalInput")
    sgn_d = k.dram("sgn", [128, 1], F32, "ExternalInput")
    tri_d = k.dram("tri", [128, 128], F32, "ExternalInput")
    esel_d = k.dram("esel", [128, 64], F32, "ExternalInput")
    oT_d = k.dram("oT", [128, T], F32, "ExternalOutput")

    banks = [k.ps(f"bank{i}", [128, 512], F32) for i in range(8)]
    nb = BankRR(banks)

    wlat = k.sb("wlat", [128, 8, 448], BF16)
    for kc in range(8):
        k.dma("pool", wlat[:, kc, :], wlat_d[kc * 128:(kc + 1) * 128, :])
    gq = k.sb("gq", [128, 2], F32)
    gkv = k.sb("gkv", [128, 1], F32)
    frq = k.sb("frq", [128, 1], F32)
    sgn = k.sb("sgn", [128, 1], F32)
    esel = k.sb("esel", [128, 64], F32)
    tri = k.sb("tri", [128, 128], BF16)
    k.dma("sp", gq[:, :], gq_d[:, :])
    k.dma("sp", gkv[:, :], gkv_d[:, :])
    k.dma("sp", frq[:, :], frq_d[:, :])
    k.dma("sp", sgn[:, :], sgn_d[:, :])
    k.dma("sp", esel[:, :], esel_d[:, :])
    k.dma("pool", tri[:, :], tri_d[:, :])
    wtmp = k.sb("wtmp", [128, 2, 256], F32)
    wuq = k.sb("wuq", [128, 2, 256], BF16)
    for c in range(2):
        k.dma("sp", wtmp[:, c, :], wuq_d[c * 128:(c + 1) * 128, :])
    for c in range(2):
        k.ts("dve", wuq[:, c, :], wtmp[:, c, :], gq[:, c:c + 1], QK_SCALE, ALU.mult, ALU.mult)
    wtmp2 = k.sb("wtmp2", [128, 2, 128], F32)
    wuk = k.sb("wuk", [128, 128], BF16)
    wuv = k.sb("wuv", [128, 128], BF16)
    k.dma("sp", wtmp2[:, 0, :], wuk_d[:, :])
    k.dma("sp", wtmp2[:, 1, :], wuv_d[:, :])
    k.ts("dve", wuk[:, :], wtmp2[:, 0, :], gkv[:, 0:1], None, ALU.mult)
    k.ts("dve", wuv[:, :], wtmp2[:, 1, :], gkv[:, 0:1], None, ALU.mult)
    ones = k.sb("ones", [128, 128], F32)
    k.memset("dve", ones[:, :], 1.0)

    kT = [k.sb(f"kT{h}", [96, T], BF16) for h in range(2)]
    qT = [k.sb(f"qT{h}", [96, T], BF16) for h in range(2)]
    Vp = k.sb("Vp", [128, 2, T // 128, 128], BF16)
    k.memset("pool", Vp[:, :, :, :], 1.0)
    mx = k.sb("mx", [128, 4], F32)
    k.memset("dve", mx[:, :], 0.0)

    hTb = [k.sb(f"hTb{i}", [128, 8, 512], BF16) for i in range(2)]
    posi = [k.sb(f"posi{i}", [128, 512], I32) for i in range(2)]
    ang = k.sb("ang", [128, 512], F32)
    ang2 = k.sb("ang2", [128, 512], F32)
    ni = k.sb("ni", [128, 512], I32)
    nf = k.sb("nf", [128, 512], F32)
    cs = [k.sb(f"cs{i}", [128, 512], F32) for i in range(2)]
    sn = [k.sb(f"sn{i}", [128, 512], F32) for i in range(2)]
    cq_sb = [k.sb(f"cq_sb{i}", [128, 512], F32) for i in range(2)]
    sq_sb = [k.sb(f"sq_sb{i}", [128, 512], F32) for i in range(2)]
    ckv_sb = k.sb("ckv_sb", [128, 512], F32)
    sqkv = k.sb("sqkv", [128, 512], F32)
    rq = k.sb("rq", [128, 512], F32)
    rkv = k.sb("rkv", [128, 512], F32)
    cqn = [k.sb(f"cqn{i}", [128, 512], BF16) for i in range(2)]
    ckvn = k.sb("ckvn", [128, 512], BF16)
    t1 = k.sb("t1", [128, 512], F32)
    t2 = k.sb("t2", [128, 512], F32)
    nsq = k.sb("nsq", [96, 512], F32)
    mtmp = k.sb("mtmp", [128, 1], F32)

    def sincos(dst, src_ang):
        r6 = slice(64, 96)
        k.ts("dve", nf[r6, :], src_ang[r6, :], 1.0 / TWO_PI, None, ALU.mult)
        k.copy("dve", ni[r6, :], nf[r6, :])
        k.copy("dve", nf[r6, :], ni[r6, :])
        k.stt("dve", nf[r6, :], nf[r6, :], -TWO_PI, src_ang[r6, :], ALU.mult, ALU.add)
        k.ts("dve", nf[r6, :], nf[r6, :], 3.1415925, -3.1415925, ALU.min, ALU.max)
        k.act(dst[r6, :], nf[r6, :], AF.Sin)

    def rope_rows(dsts, bA, bB, cst, snt, col):
        r6 = slice(64, 96)
        k.stt("dve", t1[r6, :], bB[r6, :], sgn[r6, 0:1], snt[r6, :], ALU.mult, ALU.mult)
        k.tt("dve", t2[r6, :], bA[r6, :], cst[r6, :], ALU.mult)
        for i, d_ in enumerate(dsts):
            k.tt("pool", d_[r6, col], t1[r6, :], t2[r6, :], ALU.add)

    def normsq(src, col, slot):
        k.act(nsq[:, :], src[0:96, col], AF.Square)
        bk = nb()
        k.mm(bk[:, :], ones[0:96, :], nsq[:, :])
        k.reduce("dve", mtmp[:, :], bk[:, :], ALU.max)
        k.tt("dve", mx[:, slot:slot + 1], mx[:, slot:slot + 1], mtmp[:, :], ALU.max)

    for blk in range(NBLK):
        col = slice(blk * 512, (blk + 1) * 512)
        hb = hTb[blk % 2]
        pi_ = posi[blk % 2]
        cst, snt = cs[blk % 2], sn[blk % 2]
        for kc in range(8):
            k.dma("pool", hb[:, kc, :], hT_d[kc * 128:(kc + 1) * 128, col])
        k.dma("sp", pi_[:, :], pos_d.v(pos_d.h[:, col].partition_broadcast(128)))
        r6 = slice(64, 96)
        k.copy("dve", ang[r6, :], pi_[r6, :])
        k.ts("dve", ang[r6, :], ang[r6, :], frq[r6, 0:1], None, ALU.mult)
        k.ts("dve", ang2[r6, :], ang[r6, :], float(np.pi / 2), None, ALU.add)
        sincos(snt, ang)
        sincos(cst, ang2)
        for c in range(2):
            bk = nb()
            for kc in range(8):
                k.mm(bk[:, :], wlat[:, kc, c * 128:(c + 1) * 128], hb[:, kc, :], start=(kc == 0), stop=(kc == 7))
            k.copy("act", cq_sb[c][:, :], bk[:, :])
            k.act(sq_sb[c][:, :], bk[:, :], AF.Square)
        bk = nb()
        for kc in range(8):
            k.mm(bk[:, :], wlat[:, kc, 256:384], hb[:, kc, :], start=(kc == 0), stop=(kc == 7))
        k.copy("act", ckv_sb[:, :], bk[:, :])
        k.act(sqkv[:, :], bk[:, :], AF.Square)
        bA = nb()
        for kc in range(8):
            k.mm(bA[0:96, :], wlat[:, kc, 320:416], hb[:, kc, :], start=(kc == 0), stop=(kc == 7))
        bB = nb()
        for kc in range(8):
            k.mm(bB[0:96, :], wlat[:, kc, 352:448], hb[:, kc, :], start=(kc == 0), stop=(kc == 7))
        rope_rows([kT[0], kT[1]], bA, bB, cst, snt, col)
        bk = nb()
        k.mm(bk[:, :], ones[:, :], sq_sb[0][:, :], start=True, stop=False)
        k.mm(bk[:, :], ones[:, :], sq_sb[1][:, :], start=False, stop=True)
        k.rstd(rq[:, :], bk[:, :], 1.0 / 256, EPS)
        bk = nb()
        k.mm(bk[:, :], ones[:, :], sqkv[:, :])
        k.rstd(rkv[:, :], bk[:, :], 1.0 / 128, EPS)
        for c in range(2):
            k.tt("dve", cqn[c][:, :], cq_sb[c][:, :], rq[:, :], ALU.mult)
        k.tt("pool", ckvn[:, :], ckv_sb[:, :], rkv[:, :], ALU.mult)
        for hd in range(2):
            bk = nb()
            k.mm(bk[0:64, :], wuk[:, hd * 64:(hd + 1) * 64], ckvn[:, :])
            k.copy("act", kT[hd][0:64, col], bk[0:64, :])
        bk = nb()
        for tt_ in range(4):
            k.mm(bk[:, tt_ * 128:(tt_ + 1) * 128], ckvn[:, tt_ * 128:(tt_ + 1) * 128], wuv[:, :],
                 start=True, stop=True, inc=(tt_ == 3))
        k.copy("act", Vp[:, :, blk * 4:(blk + 1) * 4, 0:64],
               bk[:, :].f(lambda a: a.rearrange("p (t h d) -> p h t d", t=4, h=2)))
        for hd in range(2):
            bA = nb()
            for c in range(2):
                k.mm(bA[0:96, :], wuq[:, c, hd * 128:hd * 128 + 96], cqn[c][:, :], start=(c == 0), stop=(c == 1))
            bB = nb()
            for c in range(2):
                k.mm(bB[0:96, :], wuq[:, c, hd * 128 + 32:hd * 128 + 128], cqn[c][:, :], start=(c == 0), stop=(c == 1))
            k.copy("act", qT[hd][0:64, col], bA[0:64, :])
            rope_rows([qT[hd]], bA, bB, cst, snt, col)
        for hd in range(2):
            normsq(qT[hd], col, hd)
            normsq(kT[hd], col, 2 + hd)

    negc = k.sb("negc", [128, 2], F32)
    k.tt("dve", negc[:, :], mx[:, 0:2], mx[:, 2:4], ALU.mult)
    k.act(negc[:, :], negc[:, :], AF.Sqrt)
    k.ts("dve", negc[:, :], negc[:, :], -1.0, None, ALU.mult)

    acc_banks = BankRR(banks[0:2])
    s_banks = BankRR(banks[2:7])
    den_bank = banks[7]
    pT = [k.sb(f"pT{i}", [128, 512], BF16) for i in range(4)]
    osb = [k.sb(f"osb{i}", [128, 512], F32) for i in range(2)]
    ores = [k.sb(f"ores{i}", [64, 512], F32) for i in range(2)]
    npt = 0
    no = 0
    for hd in range(2):
        for qi in range(NBLK):
            oacc = acc_banks()
            nkb = 4 * qi + 4
            for kb in range(nkb):
                r = kb - 4 * qi
                c0 = 128 * r if r > 0 else 0
                qcol = slice(qi * 512 + c0, (qi + 1) * 512)
                sb_ = s_banks()
                pt = pT[npt % 4]
                npt += 1
                k.mm(sb_[:, c0:512], kT[hd][:, kb * 128:(kb + 1) * 128], qT[hd][:, qcol])
                k.act(pt[:, c0:512], sb_[:, c0:512], AF.Exp, bias=negc[:, hd:hd + 1], scale=1.0)
                if r >= 0:
                    k.tt("pool", pt[:, c0:c0 + 128], pt[:, c0:c0 + 128], tri[:, :], ALU.mult)
                k.mm(oacc[:, c0:512], Vp[:, hd, kb, :], pt[:, c0:512], start=(kb == 0), stop=(kb == nkb - 1))
            ob = osb[no % 2]
            orr = ores[no % 2]
            no += 1
            k.copy("act", ob[:, :], oacc[:, :])
            k.op("dve", lambda e: e.reciprocal(ob[64:128, :].ap, ob[64:128, :].ap), [ob], [ob])
            k.mm(den_bank[0:64, :], esel[:, :], ob[:, :])
            k.tt("dve", orr[:, :], ob[0:64, :], den_bank[0:64, :], ALU.mult)
            k.dma("sp", oT_d[hd * 64:(hd + 1) * 64, qi * 512:(qi + 1) * 512], orr[:, :])
    k.finish([oT_d])
    return nc


def mla_inputs(hT_b, pos_b, P, l, j, consts_only=False):
    inv_freq = (10000.0 ** (-np.arange(16, dtype=np.float32) / np.float32(16))).astype(np.float32)
    frq = np.zeros((128, 1), np.float32)
    frq[64:80, 0] = inv_freq
    frq[80:96, 0] = inv_freq
    sgn = np.zeros((128, 1), np.float32)
    sgn[64:80] = -1.0
    sgn[80:96] = 1.0
    tri = (np.arange(128)[:, None] <= np.arange(128)[None, :]).astype(np.float32)
    esel = np.zeros((128, 64), np.float32)
    esel[64, :] = 1.0
    if consts_only:
        return {"frq": frq, "sgn": sgn, "tri": tri, "esel": esel}
    w_in = P["w_in"][l]
    kr = w_in[:, 384:416]
    wlat = np.concatenate([w_in[:, 0:384], kr, kr[:, 16:32], kr[:, 0:16]], axis=1)
    wuq = P["mla_w_uq"][l].reshape(256, 8, 96)
    wukv = P["mla_w_ukv"][l].reshape(128, 8, 128)
    heads = (2 * j, 2 * j + 1)
    wuq_c = np.concatenate([np.concatenate([wuq[:, h, 0:64], wuq[:, h, 64:96], wuq[:, h, 80:96], wuq[:, h, 64:80]], axis=1)
                            for h in heads], axis=1)
    wuk_c = np.concatenate([wukv[:, h, 0:64] for h in heads], axis=1)
    wuv_c = np.concatenate([wukv[:, h, 64:128] for h in heads], axis=1)
    inv_freq = (10000.0 ** (-np.arange(16, dtype=np.float32) / np.float32(16))).astype(np.float32)
    frq = np.zeros((128, 1), np.float32)
    frq[64:80, 0] = inv_freq
    frq[80:96, 0] = inv_freq
    sgn = np.zeros((128, 1), np.float32)
    sgn[64:80] = -1.0
    sgn[80:96] = 1.0
    tri = (np.arange(128)[:, None] <= np.arange(128)[None, :]).astype(np.float32)
    esel = np.zeros((128, 64), np.float32)
    esel[64, :] = 1.0
    return {
        "hT": hT_b, "w_lat": np.ascontiguousarray(wlat),
        "g_q": np.ascontiguousarray(P["mla_q_norm"][l].reshape(2, 128).T),
        "g_kv": np.ascontiguousarray(P["mla_kv_norm"][l].reshape(128, 1)),
        "w_uq": np.ascontiguousarray(wuq_c), "w_uk": np.ascontiguousarray(wuk_c), "w_uv": np.ascontiguousarray(wuv_c),
        "pos": np.ascontiguousarray(pos_b.reshape(1, -1).astype(np.int32)),
        "frq": frq, "sgn": sgn, "tri": tri, "esel": esel,
    }


HC = 32


def hg_consts():
    t = np.arange(128)
    ch = t // HC
    same = ch[:, None] == ch[None, :]
    U = (same & (t[:, None] <= t[None, :])).astype(np.float32)
    mid = ch * HC + (HC // 2 - 1)
    Umid = (same & (t[:, None] <= mid[None, :])).astype(np.float32)
    W = (same & (t[:, None] > t[None, :])).astype(np.float32)
    cones = (ch[:, None] == np.arange(4)[None, :]).astype(np.float32)
    maskbd = (same & (t[:, None] <= t[None, :])).astype(np.float32)
    return {"cU": U, "cUrel": (U - Umid).astype(np.float32), "cW": W, "cones": cones, "maskbd": maskbd,
            "rowmask": cones.copy()}


def build_hg(T=S, layer=0):
    nc = bass.Bass("TRN2", target_bir_lowering=False)
    k = K(nc)
    NBLK = T // 512
    hT_d = k.dram("hT", [D, T], F32, "ExternalInput")
    w_d = k.dram("w_hg", [D, 512], F32, "ExternalInput")
    lbr_d = k.dram("lb_rows", [DEPTH, 128], F32, "ExternalInput")
    lbc_d = k.dram("lb_cols", [128, DEPTH], F32, "ExternalInput")
    on_d = k.dram("o_norm", [1, 128], F32, "ExternalInput")
    cU_d = k.dram("cU", [128, 128], F32, "ExternalInput")
    cUrel_d = k.dram("cUrel", [128, 128], F32, "ExternalInput")
    cW_d = k.dram("cW", [128, 128], F32, "ExternalInput")
    cones_d = k.dram("cones", [128, 4], F32, "ExternalInput")
    mbd_d = k.dram("maskbd", [128, 128], F32, "ExternalInput")
    rm_d = k.dram("rowmask", [128, 4], F32, "ExternalInput")
    o_d = k.dram("o", [T, 128], F32, "ExternalOutput")

    banks = [k.ps(f"bank{i}", [128, 512], F32) for i in range(8)]
    nb = BankRR(banks)

    whg = k.sb("whg", [128, 8, 512], BF16)
    for kc in range(8):
        k.dma("pool", whg[:, kc, :], w_d[kc * 128:(kc + 1) * 128, :])
    cU = k.sb("cU", [128, 128], F32)
    cUrel = k.sb("cUrel", [128, 128], F32)
    cW = k.sb("cW", [128, 128], F32)
    cones = k.sb("cones", [128, 4], F32)
    mbd = k.sb("mbd", [128, 128], F32)
    rowm = k.sb("rowm", [128, 4], F32)
    gb = k.sb("gb", [128, 128], F32)
    for t_, d_ in ((cU, cU_d), (cUrel, cUrel_d), (cW, cW_d), (cones, cones_d), (mbd, mbd_d), (rowm, rm_d)):
        k.dma("sp", t_[:, :], d_[:, :])
    k.dma("sp", gb[:, :], bcast_row(on_d))

    def lower_bound(x, n, name):
        m = k.sb(name + "_m", [128, n], F32)
        e = k.sb(name + "_e", [128, DEPTH, n], F32)
        ssum = k.sb(name + "_s", [128, n], F32)
        lb = k.sb(name + "_lb", [128, n], F32)
        oml = k.sb(name + "_oml", [128, n], F32)
        k.copy("dve", m[:, :], x[:, 0, :])
        for i in range(1, DEPTH):
            k.tt("dve", m[:, :], m[:, :], x[:, i, :], ALU.max)
        for i in range(DEPTH):
            k.tt("dve", e[:, i, :], x[:, i, :], m[:, :], ALU.subtract)
        k.act(e[:, :, :], e[:, :, :], AF.Exp)
        k.copy("dve", ssum[:, :], e[:, 0, :])
        for i in range(1, DEPTH):
            k.tt("dve", ssum[:, :], ssum[:, :], e[:, i, :], ALU.add)
        k.op("dve", lambda en: en.reciprocal(ssum[:, :].ap, ssum[:, :].ap), [ssum], [ssum])
        for i in range(DEPTH):
            k.tt("dve", e[:, i, :], e[:, i, :], ssum[:, :], ALU.mult)
        k.copy("dve", lb[:, :], e[:, 0, :])
        for i in range(1, layer + 1):
            k.tt("dve", lb[:, :], lb[:, :], e[:, i, :], ALU.add)
        k.tt("dve", lb[:, :], lb[:, :], e[:, 0, :], ALU.subtract)
        k.ts("dve", oml[:, :], lb[:, :], -1.0, 1.0, ALU.mult, ALU.add)
        return lb, oml

    xr = k.sb("xr", [128, DEPTH, 128], F32)
    for i in range(DEPTH):
        k.dma("sp", xr[:, i, :], lbr_d.v(lbr_d.h[i:i + 1, :].partition_broadcast(128)))
    lb_b, oml_b = lower_bound(xr, 128, "lbr")
    xc = k.sb("xc", [128, DEPTH, 1], F32)
    k.dma("sp", xc[:, :, 0], lbc_d[:, :])
    lb_c, oml_c = lower_bound(xc, 1, "lbc")
    noml_c = k.sb("noml_c", [128, 1], F32)
    k.ts("dve", noml_c[:, :], oml_c[:, :], -1.0, None, ALU.mult)

    NS = 8
    Sf = [k.sb(f"Sf{i}", [128, 128], F32) for i in range(2)]
    Sb = [k.sb(f"Sb{i}", [128, 128], BF16) for i in range(NS)]
    k.memset("dve", Sf[0][:, :], 0.0)
    k.memset("dve", Sb[0][:, :], 0.0)
    Z = [k.sb(f"Z{i}", [128, 4, 128], BF16) for i in range(2)]
    for z in Z:
        k.memset("pool", z[:, :, :], 0.0)
    si = 0

    hTb = [k.sb(f"hTb{i}", [128, 8, 512], BF16) for i in range(2)]
    qTs = [k.sb(f"qTs{i}", [128, 512], F32) for i in range(2)]
    kTs = [k.sb(f"kTs{i}", [128, 512], F32) for i in range(2)]

    def dbl(name, shape, dt, n=2):
        return [k.sb(f"{name}{i}", shape, dt) for i in range(n)]

    sg = dbl("sg", [128, 128], F32)
    uu = dbl("uu", [128, 128], F32)
    ff = dbl("ff", [128, 128], F32)
    ktm = dbl("ktm", [128, 128], F32)
    logf = dbl("logf", [128, 128], F32)
    vbf = dbl("vbf", [128, 128], BF16)
    sgate = dbl("sgate", [128, 128], F32)
    e1 = dbl("e1", [128, 128], F32)
    e2 = dbl("e2", [128, 128], F32)
    e3 = dbl("e3", [128, 128], F32)
    e4 = dbl("e4", [128, 128], F32)
    dl = dbl("dl", [128, 4], F32)
    qpT = dbl("qpT", [128, 128], BF16)
    kpT = dbl("kpT", [128, 128], BF16)
    kdp = dbl("kdp", [128, 4, 128], BF16)
    attm = dbl("attm", [128, 128], BF16)
    osq = dbl("osq", [128, 128], F32)
    ost = dbl("ost", [128, 2], F32)
    y1 = dbl("y1", [128, 128], F32)
    y2 = dbl("y2", [128, 128], F32)

    nt = 0
    for blk in range(NBLK):
        col = slice(blk * 512, (blk + 1) * 512)
        hb = hTb[blk % 2]
        for kc in range(8):
            k.dma("pool", hb[:, kc, :], hT_d[kc * 128:(kc + 1) * 128, col])
        qT_s, kT_s = qTs[blk % 2], kTs[blk % 2]
        bq = nb()
        for kc in range(8):
            k.mm(bq[:, :], whg[:, kc, 0:128], hb[:, kc, :], start=(kc == 0), stop=(kc == 7))
        k.act(qT_s[:, :], bq[:, :], AF.Silu)
        bz = nb()
        for kc in range(8):
            k.mm(bz[:, :], whg[:, kc, 128:256], hb[:, kc, :], start=(kc == 0), stop=(kc == 7))
        k.act(kT_s[:, :], bz[:, :], AF.Sigmoid)
        k.ts("dve", kT_s[:, :], kT_s[:, :], noml_c[:, 0:1], oml_c[:, 0:1], ALU.mult, ALU.add)
        for tt_ in range(4):
            p = nt % 2
            nt += 1
            tcol = slice(tt_ * 128, (tt_ + 1) * 128)
            tok = slice(blk * 512 + tt_ * 128, blk * 512 + (tt_ + 1) * 128)
            btm = nb()
            for kc in range(8):
                k.mm(btm[:, 0:384], hb[:, kc, tcol], whg[:, kc, 128:512], start=(kc == 0), stop=(kc == 7))
            k.act(sg[p][:, :], btm[:, 0:128], AF.Sigmoid)
            k.copy("act", vbf[p][:, :], btm[:, 128:256])
            k.act(sgate[p][:, :], btm[:, 256:384], AF.Silu)
            k.tt("dve", uu[p][:, :], sg[p][:, :], oml_b[:, :], ALU.mult)
            k.tt("pool", ff[p][:, :], uu[p][:, :], lb_b[:, :], ALU.add)
            k.tt("pool", ktm[p][:, :], oml_b[:, :], uu[p][:, :], ALU.subtract)
            k.ts("dve", ff[p][:, :], ff[p][:, :], 1e-30, None, ALU.max)
            k.act(logf[p][:, :], ff[p][:, :], AF.Ln)
            bc = nb()
            k.mm(bc[:, 0:128], logf[p][:, :], cU[:, :], inc=False)
            k.mm(bc[:, 128:256], logf[p][:, :], cUrel[:, :], inc=False)
            k.mm(bc[:, 256:384], cW[:, :], logf[p][:, :], inc=False)
            k.mm(bc[:, 384:388], logf[p][:, :], cones[:, :], inc=True)
            k.act(e1[p][:, :], bc[:, 0:128], AF.Exp)
            k.act(e2[p][:, :], bc[:, 128:256], AF.Exp)
            k.act(e3[p][:, :], bc[:, 128:256], AF.Exp, scale=-1.0)
            k.act(e4[p][:, :], bc[:, 256:384], AF.Exp)
            k.act(dl[p][:, :], bc[:, 384:388], AF.Exp)
            z = Z[p]
            zdiag = z.v(bass.AP(z.h, 0, [[512, 128], [160, 4], [1, 32]]))
            k.tt("dve", zdiag, qT_s[:, tcol].f(lambda a: a.rearrange("p (c x) -> p c x", c=4)),
                 e1[p][:, :].f(lambda a: a.rearrange("p (c x) -> p c x", c=4)), ALU.mult)
            k.tt("pool", qpT[p][:, :], qT_s[:, tcol], e2[p][:, :], ALU.mult)
            k.tt("pool", kpT[p][:, :], kT_s[:, tcol], e3[p][:, :], ALU.mult)
            for c in range(4):
                k.stt("dve" if c % 2 == 0 else "pool", kdp[p][:, c, :], ktm[p][:, :], rowm[:, c:c + 1], e4[p][:, :],
                      ALU.mult, ALU.mult) if c % 2 == 0 else None
            for c in range(4):
                if c % 2 == 1:
                    k.stt("dve", kdp[p][:, c, :], ktm[p][:, :], rowm[:, c:c + 1], e4[p][:, :], ALU.mult, ALU.mult)
            ba = nb()
            k.mm(ba[:, 0:128], kpT[p][:, :], qpT[p][:, :])
            k.tt("dve", attm[p][:, :], ba[:, 0:128], mbd[:, :], ALU.mult)
            bs = nb()
            for c in range(4):
                k.mm(bs[:, c * 128:(c + 1) * 128], kdp[p][:, c, :], vbf[p][:, :], inc=(c == 3))
            bo = nb()
            for c in range(4):
                k.mm(bo[:, 0:128], z[:, c, :], Sb[(si + c) % NS][:, :], start=(c == 0), stop=False, inc=False)
                s_old = Sf[(si + c) % 2]
                s_new = Sf[(si + c + 1) % 2]
                k.stt("dve", s_new[:, :], s_old[:, :], dl[p][:, c:c + 1], bs[:, c * 128:(c + 1) * 128],
                      ALU.mult, ALU.add)
                k.copy("act", Sb[(si + c + 1) % NS][:, :], s_new[:, :])
            k.mm(bo[:, 0:128], attm[p][:, :], vbf[p][:, :], start=False, stop=True, inc=True)
            si += 4
            k.act(osq[p][:, :], bo[:, 0:128], AF.Square, accum_out=ost[p][:, 0:1])
            k.rstd(ost[p][:, 1:2], ost[p][:, 0:1], 1.0 / 128, EPS)
            k.stt("dve", y1[p][:, :], bo[:, 0:128], ost[p][:, 1:2], gb[:, :], ALU.mult, ALU.mult)
            k.tt("pool", y2[p][:, :], y1[p][:, :], sgate[p][:, :], ALU.mult)
            k.dma("sp", o_d[tok, :], y2[p][:, :])
    k.finish([o_d])
    return nc


def hg_inputs(hT_b, P, l, j):
    w_in = P["w_in"][l]
    cols = [2472 + j * 128, 2984 + j * 128, 3496 + j * 128, 4008 + j * 128]
    w = np.concatenate([w_in[:, c:c + 128] for c in cols], axis=1)
    lb = P["hg_lower_bounds"][:, j * 128:(j + 1) * 128]
    m = {"hT": hT_b, "w_hg": np.ascontiguousarray(w), "lb_rows": np.ascontiguousarray(lb),
         "lb_cols": np.ascontiguousarray(lb.T), "o_norm": P["hg_o_norm"][l].reshape(1, 128)}
    m.update(hg_consts())
    return m


MASKV = 30000.0


def dn_consts():
    t = np.arange(128)
    uinc = (t[:, None] <= t[None, :]).astype(np.float32)
    lpos_s = np.where(t[None, :] < t[:, None], 0.0, MASKV).astype(np.float32)
    uneg = np.where(t[:, None] <= t[None, :], 0.0, -MASKV).astype(np.float32)
    return {"uinc": uinc, "lpos_s": lpos_s, "uneg": uneg, "ident": np.eye(128, dtype=np.float32)}


def build_dn(T=S):
    nc = bass.Bass("TRN2", target_bir_lowering=False)
    k = K(nc)
    NBLK = T // 512
    hT_d = k.dram("hT", [D, T], F32, "ExternalInput")
    w_d = k.dram("w_dn", [D, 384 + 130], F32, "ExternalInput")
    cw_d = k.dram("conv_w", [128, 3, 4], F32, "ExternalInput")
    alog_d = k.dram("a_log", [1, 1], F32, "ExternalInput")
    dtb_d = k.dram("dt_bias", [1, 1], F32, "ExternalInput")
    on_d = k.dram("o_norm", [1, 128], F32, "ExternalInput")
    uinc_d = k.dram("uinc", [128, 128], F32, "ExternalInput")
    lpos_d = k.dram("lpos_s", [128, 128], F32, "ExternalInput")
    uneg_d = k.dram("uneg", [128, 128], F32, "ExternalInput")
    ident_d = k.dram("ident", [128, 128], F32, "ExternalInput")
    o_d = k.dram("o", [T, 128], F32, "ExternalOutput")

    banks = [k.ps(f"bank{i}", [128, 512], F32) for i in range(8)]
    nb = BankRR(banks)

    wdn = k.sb("wdn", [128, 8, 514], BF16)
    for kc in range(8):
        k.dma("pool", wdn[:, kc, :], w_d[kc * 128:(kc + 1) * 128, :])
    cw = k.sb("cw", [128, 3, 4], F32)
    k.dma("sp", cw[:, :, :], cw_d[:, :, :])
    uinc = k.sb("uinc", [128, 128], F32)
    lpos = k.sb("lpos", [128, 128], F32)
    uneg = k.sb("uneg", [128, 128], F32)
    ident = k.sb("ident", [128, 128], F32)
    gb = k.sb("gb", [128, 128], F32)
    for t_, d_ in ((uinc, uinc_d), (lpos, lpos_d), (uneg, uneg_d), (ident, ident_d)):
        k.dma("sp", t_[:, :], d_[:, :])
    k.dma("sp", gb[:, :], bcast_row(on_d))
    sc = k.sb("sc", [128, 4], F32)
    k.dma("sp", sc[:, 0:1], bcast_row(alog_d))
    k.dma("sp", sc[:, 1:2], bcast_row(dtb_d))
    k.act(sc[:, 2:3], sc[:, 0:1], AF.Exp)
    k.ts("dve", sc[:, 2:3], sc[:, 2:3], -1.0, None, ALU.mult)
    ones = k.sb("ones", [128, 128], F32)
    k.memset("dve", ones[:, :], 1.0)

    Sf = [k.sb(f"S{i}", [128, 128], F32) for i in range(2)]
    k.memset("dve", Sf[0][:, :], 0.0)
    si = 0

    hTb = [k.sb(f"hTb{i}", [128, 8, 512], BF16) for i in range(2)]
    xh = [[k.sb(f"xh{w}_{i}", [128, 515], F32) for i in range(2)] for w in range(3)]
    for w in range(3):
        k.memset("dve", xh[w][1][:, 512:515], 0.0)
    cv = [k.sb(f"cv{w}", [128, 512], F32) for w in range(3)]
    sq = [k.sb(f"sq{w}", [128, 512], F32) for w in range(2)]
    rs = [k.sb(f"rs{w}", [128, 512], F32) for w in range(2)]

    def per_tile(name, shape, dt=F32):
        return [k.sb(f"{name}{i}", shape, dt) for i in range(4)]

    tmc = per_tile("tmc", [128, 8])
    sgate = per_tile("sgate", [128, 128])
    ktm = per_tile("ktm", [128, 128])
    vb = per_tile("vb", [128, 128])
    gbc = per_tile("gbc", [128, 128])
    xm = per_tile("xm", [128, 128])
    ym = per_tile("ym", [128, 128])
    dec_s = per_tile("dec_s", [128, 128])
    decT = per_tile("decT", [128, 128])
    egb = per_tile("egb", [128, 128])
    Mt = per_tile("M", [128, 128])
    Nt = per_tile("N", [128, 128])
    Pa = per_tile("Pa", [128, 128])
    Pat = per_tile("Pat", [128, 128])
    Pb = per_tile("Pb", [128, 128])
    Pbt = per_tile("Pbt", [128, 128])
    Rr = per_tile("R", [128, 128])
    Rt = per_tile("Rt", [128, 128])
    qkT = per_tile("qkT", [128, 128])
    qdT = per_tile("qdT", [128, 128])
    kbg = per_tile("kbg", [128, 128])
    kdec = per_tile("kdec", [128, 128])
    u_sb = per_tile("u", [128, 128])
    wT_sb = per_tile("wT", [128, 128])
    vnew = per_tile("vnew", [128, 128])
    osq = per_tile("osq", [128, 128])
    ost = per_tile("ost", [128, 2])
    y1 = per_tile("y1", [128, 128])
    y2 = per_tile("y2", [128, 128])

    for blk in range(NBLK):
        col = slice(blk * 512, (blk + 1) * 512)
        hb = hTb[blk % 2]
        for kc in range(8):
            k.dma("pool", hb[:, kc, :], hT_d[kc * 128:(kc + 1) * 128, col])
        for w in range(3):
            bk = nb()
            for kc in range(8):
                k.mm(bk[:, :], wdn[:, kc, w * 128:(w + 1) * 128], hb[:, kc, :], start=(kc == 0), stop=(kc == 7))
            cur, prv = xh[w][blk % 2], xh[w][(blk + 1) % 2]
            k.copy("act", cur[:, 3:515], bk[:, :])
            k.copy("act", cur[:, 0:3], prv[:, 512:515])
            y = cv[w]
            k.ts("dve", y[:, :], cur[:, 0:512], cw[:, w, 0:1], None, ALU.mult)
            for m in range(1, 4):
                k.stt("dve" if m != 2 else "dve", y[:, :], cur[:, m:m + 512], cw[:, w, m:m + 1], y[:, :], ALU.mult, ALU.add)
            k.act(y[:, :], y[:, :], AF.Silu)
        for w in range(2):
            k.act(sq[w][:, :], cv[w][:, :], AF.Square)
            bk = nb()
            k.mm(bk[:, :], ones[:, :], sq[w][:, :])
            k.rstd(rs[w][:, :], bk[:, :], 1.0, EPS)
            if w == 0:
                k.stt("pool", cv[w][:, :], cv[w][:, :], 1.0, rs[w][:, :], ALU.mult, ALU.mult) if False else None
        k.tt("pool", cv[0][:, :], cv[0][:, :], rs[0][:, :], ALU.mult)
        k.tt("pool", cv[1][:, :], cv[1][:, :], rs[1][:, :], ALU.mult)
        k.act(cv[0][:, :], cv[0][:, :], AF.Copy, scale=float(128 ** -0.5))
        qT_, kT_, vT_ = cv

        for t in range(4):
            tc_ = slice(t * 128, (t + 1) * 128)
            c = tmc[t]
            bk = nb()
            for kc in range(8):
                k.mm(bk[:, 0:130], hb[:, kc, tc_], wdn[:, kc, 384:514], start=(kc == 0), stop=(kc == 7))
            k.act(c[:, 0:1], bk[:, 0:1], AF.Sigmoid)
            k.act(c[:, 1:2], bk[:, 1:2], AF.Exp, bias=sc[:, 1:2], scale=1.0)
            k.act(sgate[t][:, :], bk[:, 2:130], AF.Silu)
            k.act(c[:, 1:2], c[:, 1:2], AF.Ln, bias=1.0, scale=1.0)
            k.tt("dve", c[:, 2:3], c[:, 1:2], sc[:, 2:3], ALU.mult)
            bk = nb()
            k.transpose(bk[:, 0:128], kT_[:, tc_], ident[:, :], inc=False)
            k.transpose(bk[:, 128:256], vT_[:, tc_], ident[:, :], inc=True)
            k.copy("act", ktm[t][:, :], bk[:, 0:128])
            k.act(vb[t][:, :], bk[:, 128:256], AF.Copy, scale=c[:, 0:1])
            k.ts("dve", gbc[t][:, :], ones[:, :], c[:, 2:3], None, ALU.mult)
            bk = nb()
            k.mm(bk[:, 0:128], gbc[t][:, :], uinc[:, :], inc=False)
            k.mm(bk[:, 128:129], uinc[:, :], c[:, 2:3], inc=True)
            k.copy("dve", c[:, 3:4], bk[:, 128:129])
            k.copy("dve", c[:, 7:8], bk[:, 127:128])
            k.stt("dve", xm[t][:, :], bk[:, 0:128], c[:, 3:4], lpos[:, :], ALU.subtract, ALU.max)
            k.stt("dve", ym[t][:, :], bk[:, 0:128], c[:, 3:4], uneg[:, :], ALU.subtract, ALU.min)
            k.act(egb[t][:, :], bk[:, 0:128], AF.Exp)
            k.act(dec_s[t][:, :], xm[t][:, :], AF.Exp, scale=-1.0)
            k.act(decT[t][:, :], ym[t][:, :], AF.Exp)
            k.act(c[:, 4:5], c[:, 3:4], AF.Exp)
            k.tt("dve", c[:, 4:5], c[:, 4:5], c[:, 0:1], ALU.mult)
            k.act(c[:, 5:6], c[:, 3:4], AF.Exp, bias=c[:, 7:8], scale=-1.0)
            k.act(c[:, 6:7], c[:, 7:8], AF.Exp)
            k.ts("pool", kbg[t][:, :], ktm[t][:, :], c[:, 4:5], None, ALU.mult) if False else None
            k.ts("dve", kbg[t][:, :], ktm[t][:, :], c[:, 4:5], None, ALU.mult)
            k.ts("dve", kdec[t][:, :], ktm[t][:, :], c[:, 5:6], None, ALU.mult)
            k.tt("pool", qdT[t][:, :], qT_[:, tc_], egb[t][:, :], ALU.mult)
            bk = nb()
            k.mm(bk[:, 0:128], kT_[:, tc_], kT_[:, tc_], inc=False)
            k.mm(bk[:, 128:256], kT_[:, tc_], qT_[:, tc_], inc=True)
            k.stt("dve", Mt[t][:, :], bk[:, 0:128], c[:, 0:1], dec_s[t][:, :], ALU.mult, ALU.mult)
            k.tt("dve", qkT[t][:, :], bk[:, 128:256], decT[t][:, :], ALU.mult)
            bk = nb()
            k.transpose(bk[:, 0:128], Mt[t][:, :], ident[:, :])
            k.copy("act", Nt[t][:, :], bk[:, 0:128])
            k.tt("pool", Rr[t][:, :], ident[:, :], Nt[t][:, :], ALU.subtract)
            k.tt("pool", Rt[t][:, :], ident[:, :], Mt[t][:, :], ALU.subtract)

        P = [Nt[t] for t in range(4)]
        Ptr = [Mt[t] for t in range(4)]
        for lvl in range(6):
            last = lvl == 5
            newP = Pa if lvl % 2 == 0 else Pb
            newPt = Pat if lvl % 2 == 0 else Pbt
            for t in range(4):
                bk = nb()
                k.mm(bk[:, 0:128], Ptr[t][:, :], P[t][:, :], inc=last)
                if not last:
                    k.mm(bk[:, 128:256], P[t][:, :], Ptr[t][:, :], inc=True)
                k.copy("act", newP[t][:, :], bk[:, 0:128])
                if not last:
                    k.copy("act", newPt[t][:, :], bk[:, 128:256])
            for t in range(4):
                bk = nb()
                k.mm(bk[:, 0:128], Rt[t][:, :], newP[t][:, :], inc=last)
                if not last:
                    k.mm(bk[:, 128:256], newP[t][:, :], Rt[t][:, :], inc=True)
                k.tt("dve", Rr[t][:, :], Rr[t][:, :], bk[:, 0:128], ALU.add)
                if not last:
                    k.tt("dve", Rt[t][:, :], Rt[t][:, :], bk[:, 128:256], ALU.add)
            P = [newP[t] for t in range(4)]
            Ptr = [newPt[t] for t in range(4)]

        for t in range(4):
            bk = nb()
            k.mm(bk[:, 0:128], Rr[t][:, :], vb[t][:, :], inc=False)
            k.mm(bk[:, 128:256], kbg[t][:, :], Rr[t][:, :], inc=True)
            k.copy("act", u_sb[t][:, :], bk[:, 0:128])
            k.copy("act", wT_sb[t][:, :], bk[:, 128:256])

        for t in range(4):
            tok = slice(blk * 512 + t * 128, blk * 512 + (t + 1) * 128)
            c = tmc[t]
            s_old = Sf[si % 2]
            s_new = Sf[(si + 1) % 2]
            si += 1
            bk = nb()
            k.mm(bk[:, 0:128], wT_sb[t][:, :], s_old[:, :])
            k.tt("dve", vnew[t][:, :], u_sb[t][:, :], bk[:, 0:128], ALU.subtract)
            bo = nb()
            k.mm(bo[:, 0:128], qdT[t][:, :], s_old[:, :], start=True, stop=False, inc=False)
            k.mm(bo[:, 0:128], qkT[t][:, :], vnew[t][:, :], start=False, stop=True, inc=True)
            bs = nb()
            k.mm(bs[:, 0:128], kdec[t][:, :], vnew[t][:, :])
            k.stt("dve", s_new[:, :], s_old[:, :], c[:, 6:7], bs[:, 0:128], ALU.mult, ALU.add)
            k.act(osq[t][:, :], bo[:, 0:128], AF.Square, accum_out=ost[t][:, 0:1])
            k.rstd(ost[t][:, 1:2], ost[t][:, 0:1], 1.0 / 128, EPS)
            k.stt("dve", y1[t][:, :], bo[:, 0:128], ost[t][:, 1:2], gb[:, :], ALU.mult, ALU.mult)
            k.tt("pool", y2[t][:, :], y1[t][:, :], sgate[t][:, :], ALU.mult)
            k.dma("sp", o_d[tok, :], y2[t][:, :])
    k.finish([o_d])
    return nc


def dn_inputs(hT_b, P, l, j):
    w_in = P["w_in"][l]
    cq, ck, cvv = 416 + j * 128, 416 + 512 + j * 128, 416 + 1024 + j * 128
    w = np.concatenate([w_in[:, cq:cq + 128], w_in[:, ck:ck + 128], w_in[:, cvv:cvv + 128],
                        w_in[:, 1952 + j:1953 + j], w_in[:, 1956 + j:1957 + j],
                        w_in[:, 1960 + j * 128:1960 + (j + 1) * 128]], axis=1)
    conv = P["dn_conv"][l]
    cwm = np.stack([conv[:, cq - 416:cq - 416 + 128], conv[:, ck - 416:ck - 416 + 128],
                    conv[:, cvv - 416:cvv - 416 + 128]], axis=0)
    m = {"hT": hT_b, "w_dn": np.ascontiguousarray(w),
         "conv_w": np.ascontiguousarray(cwm.transpose(2, 0, 1)),
         "a_log": P["dn_a_log"][l][j].reshape(1, 1), "dt_bias": P["dn_dt_bias"][l][j].reshape(1, 1),
         "o_norm": P["dn_o_norm"][l].reshape(1, 128)}
    m.update(dn_consts())
    return m


class Rec:
    _PASS = ("sb", "ps", "dram")

    def __init__(self, k):
        self._k = k
        self.segs = [[]]

    def sb(self, *a, **kw):
        return self._k.sb(*a, **kw)

    def push(self):
        pass

    def pop(self):
        pass

    def mark(self):
        self.segs.append([])

    def __getattr__(self, name):
        def f(*a, **kw):
            self.segs[-1].append((name, a, kw))
        return f


SEM_LAT = 1.2
_GHZ = {"pe": 1.9, "act": 1.2, "dve": 0.96, "pool": 0.6}
_FIX = {"pe": 0.06, "act": 0.2, "dve": 0.1, "pool": 0.25}


def _op_cost(k, rec):
    name, ar, kw = rec
    k.dry = []
    getattr(k, name)(*ar, **kw)
    infos, k.dry = k.dry, None
    n = 128
    out = ar[1] if name in ("tt", "ts", "stt", "copy", "memset", "reduce", "dma") else (ar[0] if ar else None)
    if name == "op":
        out = None
    if isinstance(out, V):
        try:
            n = out.ap.free_size()
        except Exception:
            n = 128
    res = []
    for eng, rd, wr in infos:
        if eng == "dma":
            d = 2.5
        else:
            passes = 1
            if name in ("mm", "transpose") and isinstance(ar[1], V) and ar[1].ap.dtype == F32:
                passes = 4
            d = _FIX[eng] + passes * n / (_GHZ[eng] * 1000.0)
        res.append((eng, rd, wr, d))
    return res


def replay_merged(k, *lists):
    lists = [l for l in lists if l]
    if not lists:
        return
    if len(lists) == 1:
        for name, ar, kw in lists[0]:
            getattr(k, name)(*ar, **kw)
        return
    free = {}
    ready = {}
    rdone = {}
    idx = [0] * len(lists)
    costs = [[None] * len(l) for l in lists]
    total = sum(len(l) for l in lists)

    def start_time(info):
        eng, rd, wr, d = info
        t = free.get(eng, 0.0)
        for r in rd:
            if r in ready:
                tr, pe_ = ready[r]
                t = max(t, tr + (SEM_LAT if pe_ != eng else 0.0))
        for w in wr:
            if w in ready:
                tr, pe_ = ready[w]
                t = max(t, tr + (SEM_LAT if pe_ != eng else 0.0))
            if w in rdone:
                t = max(t, rdone[w] + SEM_LAT)
        return t

    for _ in range(total):
        best, bt = None, None
        for n, l in enumerate(lists):
            if idx[n] < len(l):
                if costs[n][idx[n]] is None:
                    costs[n][idx[n]] = _op_cost(k, l[idx[n]])
                c = costs[n][idx[n]]
                t = start_time(c[0]) if c else 0.0
                key = (t, idx[n] / len(l))
                if bt is None or key < bt:
                    best, bt = n, key
        c = costs[best][idx[best]]
        for info in c:
            eng, rd, wr, d = info
            t = start_time(info)
            free[eng] = t + d
            for r in rd:
                rdone[r] = max(rdone.get(r, 0.0), t + d)
            for w in wr:
                ready[w] = (t + d, eng)
                rdone.pop(w, None)
        name, ar, kw = lists[best][idx[best]]
        idx[best] += 1
        getattr(k, name)(*ar, **kw)


def emit_dn_hg(k, banks, C, W, G, layer):
    NBLK = S // 512
    k.push()
    hTb = [k.sb(f"hTbS{i}", [128, 8, 512], BF16) for i in range(3)]
    ra, rb = Rec(k), Rec(k)
    emit_dn(ra, banks[0:DN_BANKS], C, W, G, hTb=hTb)
    emit_hg(rb, banks[DN_BANKS:8], C, W, G, layer, hTb=hTb)
    assert len(ra.segs) == 2 * NBLK + 1 and len(rb.segs) == NBLK + 1
    front = lambda i: ra.segs[1 + 2 * i] if i < NBLK else []
    back = lambda i: ra.segs[2 + 2 * i]

    def load(blk):
        if blk < NBLK:
            for kc in range(8):
                k.dma("sp", hTb[blk % 3][:, kc, :], G["hT_blk"](blk, kc))

    load(0)
    load(1)
    replay_merged(k, ra.segs[0], rb.segs[0])
    replay_merged(k, front(0))
    for blk in range(NBLK):
        load(blk + 2)
        replay_merged(k, front(blk + 1), back(blk), rb.segs[1 + blk])
        if blk % 4 == 3:
            q = blk // 4
            for br in (1, 2):
                k.collective("AllGather", [G["osrc"][br][q][:, :]], [G["odst"][br][q][:, :]], GROUPS)
    k.pop()


DN_BANKS = 6
DN_BACK_BANKS = 2

def emit_publish_tile(k, banks, ident, y, hTsb, t):
    tok = slice(t * 128, (t + 1) * 128)
    for q4 in range(2):
        bk = banks[4 + q4 + 2 * (t % 2)]
        for j in range(4):
            kc = q4 * 4 + j
            k.transpose(bk[:, j * 128:(j + 1) * 128], y[:, kc * 128:(kc + 1) * 128], ident[:, :], inc=(j == 3))
        k.copy("act", hTsb[:, q4 * 4:(q4 + 1) * 4, tok], bk[:, :].f(lambda a: a.rearrange("p (j t) -> p j t", j=4)))


def emit_allgather_h(k, hTsb, G):
    for kc in range(8):
        k.dma("sp", G["hsrc"][kc // 2][(kc % 2) * 128:(kc % 2 + 1) * 128, :], hTsb[:, kc, :])
    for q in range(4):
        k.collective("AllGather", [G["hsrc"][q][:, :]], [G["hdst"][q][:, :]], GROUPS)


def emit_allgather_o(k, G, br):
    for q in range(4):
        k.collective("AllGather", [G["osrc"][br][q][:, :]], [G["odst"][br][q][:, :]], GROUPS)


def emit_ln0(k, banks, C, G):
    ntok = S * B // NCORES
    k.push()
    x = G["x"]
    g_b = k.sb("g_b", [128, D], F32)
    b_b = k.sb("b_b", [128, D], F32)
    k.dma("sp", g_b[:, :], bcast_row(G["ln_in_g"]))
    k.dma("sp", b_b[:, :], bcast_row(G["ln_in_b"]))
    hTsb = k.sb("hTsb", [128, 8, ntok], BF16)
    xs = [k.sb(f"x{i}", [128, D], F32) for i in range(2)]
    ys = [k.sb(f"y{i}", [128, D], F32) for i in range(2)]
    tmps = [k.sb(f"t{i}", [128, D], F32) for i in range(2)]
    sts = [k.sb(f"s{i}", [128, 4], F32) for i in range(2)]
    NT = ntok // 128
    recs = [Rec(k), Rec(k)]

    def ld(i):
        recs[i % 2].dma("sp", xs[i % 2][:, :], x[i * 128:(i + 1) * 128, :])

    ld(0)
    ld(1)
    for i in range(NT):
        kr = recs[i % 2]
        xt, yt, tt_, st = xs[i % 2], ys[i % 2], tmps[i % 2], sts[i % 2]
        layer_norm_tile(kr, xt[:, :], yt[:, :], g_b[:, :], b_b[:, :], tt_[:, :], st[:, :], eng_g="dve")
        if i + 2 < NT:
            ld(i + 2)
        kr.dma("sp", G["h_cur"][i * 128:(i + 1) * 128, :], yt[:, :])
        emit_publish_tile(kr, banks, C["ident"], yt, hTsb, i)
    replay_merged(k, recs[0].segs[0], recs[1].segs[0])
    emit_allgather_h(k, hTsb, G)
    k.pop()


GROUPS = [[0, 1, 2, 3], [4, 5, 6, 7]]

def emit_mla(k0, banks, C, W, G):
    T = S
    NBLK = T // 512
    wlat_d, gq_d, gkv_d, wuq_d, wuk_d, wuv_d = W["w_lat"], W["g_q"], W["g_kv"], W["w_uq"], W["w_uk"], W["w_uv"]
    pos_d = G["pos"]
    frq, sgn, esel, tri = C["frq"], C["sgn"], C["esel"], C["tri"]
    k = k0
    k.push()

    wlat = k.sb("wlat", [128, 8, 448], BF16)
    for kc in range(8):
        k.dma("pool", wlat[:, kc, :], wlat_d[kc * 128:(kc + 1) * 128, :])
    gq = k.sb("gq", [128, 2], F32)
    gkv = k.sb("gkv", [128, 1], F32)
    k.dma("sp", gq[:, :], gq_d[:, :])
    k.dma("sp", gkv[:, :], gkv_d[:, :])
    wtmp = k.sb("wtmp", [128, 2, 256], F32)
    wuq = k.sb("wuq", [128, 2, 256], BF16)
    for c in range(2):
        k.dma("sp", wtmp[:, c, :], wuq_d[c * 128:(c + 1) * 128, :])
    for c in range(2):
        k.ts("dve", wuq[:, c, :], wtmp[:, c, :], gq[:, c:c + 1], QK_SCALE, ALU.mult, ALU.mult)
    wtmp2 = k.sb("wtmp2", [128, 2, 128], F32)
    wuk = k.sb("wuk", [128, 128], BF16)
    wuv = k.sb("wuv", [128, 128], BF16)
    k.dma("sp", wtmp2[:, 0, :], wuk_d[:, :])
    k.dma("sp", wtmp2[:, 1, :], wuv_d[:, :])
    k.ts("dve", wuk[:, :], wtmp2[:, 0, :], gkv[:, 0:1], None, ALU.mult)
    k.ts("dve", wuv[:, :], wtmp2[:, 1, :], gkv[:, 0:1], None, ALU.mult)
    ones = k.sb("ones", [128, 128], F32)
    k.memset("dve", ones[:, :], 1.0)

    kT = [k.sb(f"kT{h}", [96, T], BF16) for h in range(2)]
    qT = [k.sb(f"qT{h}", [96, T], BF16) for h in range(2)]
    Vp = k.sb("Vp", [128, 2, T // 128, 128], BF16)
    kTv = [[V(kT[h].h[:, b * 512:(b + 1) * 512], Res(f"kT{h}_{b}")) for b in range(NBLK)] for h in range(2)]
    qTv = [[V(qT[h].h[:, b * 512:(b + 1) * 512], Res(f"qT{h}_{b}")) for b in range(NBLK)] for h in range(2)]
    Vpv = [V(Vp.h[:, :, b * 4:(b + 1) * 4, :], Res(f"Vp_{b}")) for b in range(NBLK)]
    for b in range(NBLK):
        k.memset("pool", Vpv[b], 1.0)
    mx = k.sb("mx", [128, 4], F32)
    k.memset("dve", mx[:, :], 0.0)
    negcb = [k.sb(f"negc{b}", [128, 2], F32) for b in range(NBLK)]

    hTb = [k.sb(f"hTb{i}", [128, 8, 512], BF16) for i in range(2)]
    posi = [k.sb(f"posi{i}", [128, 512], I32) for i in range(2)]
    ang = k.sb("ang", [128, 1024], F32)
    ni = k.sb("ni", [128, 1024], I32)
    nf = k.sb("nf", [128, 1024], F32)
    scs = [k.sb(f"scs{i}", [128, 1024], F32) for i in range(2)]
    cq_sb = [k.sb(f"cq_sb{i}", [128, 512], F32) for i in range(2)]
    sq_sb = [k.sb(f"sq_sb{i}", [128, 512], F32) for i in range(2)]
    ckv_sb = k.sb("ckv_sb", [128, 512], F32)
    sqkv = k.sb("sqkv", [128, 512], F32)
    rq = k.sb("rq", [128, 512], F32)
    rkv = k.sb("rkv", [128, 512], F32)
    cqn = [k.sb(f"cqn{i}", [128, 512], BF16) for i in range(2)]
    ckvn = k.sb("ckvn", [128, 512], BF16)
    t1 = k.sb("t1", [128, 512], F32)
    t2 = k.sb("t2", [128, 512], F32)
    nsq = k.sb("nsq", [96, 512], F32)
    mtmp = k.sb("mtmp", [128, 1], F32)
    osb = [k.sb(f"osb{i}", [128, 512], F32) for i in range(2)]
    ores = [k.sb(f"ores{i}", [64, 512], BF16) for i in range(2)]
    pT = [k.sb(f"pTx{i}", [128, 512], BF16) for i in range(7)]

    def load(blk):
        if blk < NBLK:
            col = slice(blk * 512, (blk + 1) * 512)
            for kc in range(8):
                k0.dma("sp", hTb[blk % 2][:, kc, :], G["hT_blk"](blk, kc))
            k0.dma("sp", posi[blk % 2][:, :], pos_d.v(pos_d.h[:, col].partition_broadcast(128)))

    rp = Rec(k0)
    k = rp
    nb = BankRR(banks[6:8])
    r6 = slice(64, 96)

    def rope_rows(dsts, bA, bB, cst, snt):
        k.stt("dve", t1[r6, :], bB[r6, :], sgn[r6, 0:1], snt, ALU.mult, ALU.mult)
        k.tt("dve", t2[r6, :], bA[r6, :], cst, ALU.mult)
        for d_ in dsts:
            k.tt("pool", d_[r6, :], t1[r6, :], t2[r6, :], ALU.add)

    def normsq(src, slot, running):
        k.act(nsq[:, :], src[0:96, :], AF.Square)
        bk = nb()
        k.mm(bk[:, :], ones[0:96, :], nsq[:, :])
        k.reduce("dve", mtmp[:, :], bk[:, :], ALU.max)
        if running:
            k.tt("dve", mx[:, slot:slot + 1], mx[:, slot:slot + 1], mtmp[:, :], ALU.max)
        else:
            k.copy("dve", mx[:, slot:slot + 1], mtmp[:, :])

    for blk in range(NBLK):
        k.mark()
        hb = hTb[blk % 2]
        pi_ = posi[blk % 2]
        sc_ = scs[blk % 2]
        snt, cst = sc_[r6, 0:512], sc_[r6, 512:1024]
        k.copy("dve", ang[r6, 0:512], pi_[r6, :])
        k.ts("dve", ang[r6, 0:512], ang[r6, 0:512], frq[r6, 0:1], None, ALU.mult)
        k.ts("dve", ang[r6, 512:1024], ang[r6, 0:512], float(np.pi / 2), None, ALU.add)
        k.ts("dve", nf[r6, :], ang[r6, :], 1.0 / TWO_PI, None, ALU.mult)
        k.copy("dve", ni[r6, :], nf[r6, :])
        k.copy("dve", nf[r6, :], ni[r6, :])
        k.stt("dve", nf[r6, :], nf[r6, :], -TWO_PI, ang[r6, :], ALU.mult, ALU.add)
        k.ts("dve", nf[r6, :], nf[r6, :], 3.1415925, -3.1415925, ALU.min, ALU.max)
        k.act(sc_[r6, :], nf[r6, :], AF.Sin)
        for c in range(2):
            bk = nb()
            for kc in range(8):
                k.mm(bk[:, :], wlat[:, kc, c * 128:(c + 1) * 128], hb[:, kc, :], start=(kc == 0), stop=(kc == 7))
            k.copy("act", cq_sb[c][:, :], bk[:, :])
            k.act(sq_sb[c][:, :], bk[:, :], AF.Square)
        bk = nb()
        for kc in range(8):
            k.mm(bk[:, :], wlat[:, kc, 256:384], hb[:, kc, :], start=(kc == 0), stop=(kc == 7))
        k.copy("act", ckv_sb[:, :], bk[:, :])
        k.act(sqkv[:, :], bk[:, :], AF.Square)
        bA = nb()
        for kc in range(8):
            k.mm(bA[0:96, :], wlat[:, kc, 320:416], hb[:, kc, :], start=(kc == 0), stop=(kc == 7))
        bB = nb()
        for kc in range(8):
            k.mm(bB[0:96, :], wlat[:, kc, 352:448], hb[:, kc, :], start=(kc == 0), stop=(kc == 7))
        rope_rows([kTv[0][blk], kTv[1][blk]], bA, bB, cst, snt)
        bk = nb()
        k.mm(bk[:, :], ones[:, :], sq_sb[0][:, :], start=True, stop=False)
        k.mm(bk[:, :], ones[:, :], sq_sb[1][:, :], start=False, stop=True)
        k.rstd_ln(rq[:, :], bk[:, :], 1.0 / 256, EPS)
        bk = nb()
        k.mm(bk[:, :], ones[:, :], sqkv[:, :])
        k.rstd_ln(rkv[:, :], bk[:, :], 1.0 / 128, EPS)
        for c in range(2):
            k.tt("dve", cqn[c][:, :], cq_sb[c][:, :], rq[:, :], ALU.mult)
        k.tt("pool", ckvn[:, :], ckv_sb[:, :], rkv[:, :], ALU.mult)
        for hd in range(2):
            bk = nb()
            k.mm(bk[0:64, :], wuk[:, hd * 64:(hd + 1) * 64], ckvn[:, :])
            k.copy("act", kTv[hd][blk][0:64, :], bk[0:64, :])
        bk = nb()
        for tt_ in range(4):
            k.mm(bk[:, tt_ * 128:(tt_ + 1) * 128], ckvn[:, tt_ * 128:(tt_ + 1) * 128], wuv[:, :],
                 start=True, stop=True, inc=(tt_ == 3))
        k.copy("act", Vpv[blk][:, :, :, 0:64],
               bk[:, :].f(lambda a: a.rearrange("p (t h d) -> p h t d", t=4, h=2)))
        for hd in range(2):
            bA = nb()
            for c in range(2):
                k.mm(bA[0:96, :], wuq[:, c, hd * 128:hd * 128 + 96], cqn[c][:, :], start=(c == 0), stop=(c == 1))
            bB = nb()
            for c in range(2):
                k.mm(bB[0:96, :], wuq[:, c, hd * 128 + 32:hd * 128 + 128], cqn[c][:, :], start=(c == 0), stop=(c == 1))
            k.copy("act", qTv[hd][blk][0:64, :], bA[0:64, :])
            rope_rows([qTv[hd][blk]], bA, bB, cst, snt)
        for hd in range(2):
            normsq(qTv[hd][blk], hd, False)
            normsq(kTv[hd][blk], 2 + hd, True)
        ng = negcb[blk]
        k.tt("dve", ng[:, :], mx[:, 0:2], mx[:, 2:4], ALU.mult)
        k.act(ng[:, :], ng[:, :], AF.Ln)
        k.act(ng[:, :], ng[:, :], AF.Exp, scale=0.5)
        k.ts("dve", ng[:, :], ng[:, :], -1.0, None, ALU.mult)

    ra = Rec(k0)
    k = ra
    s_banks = BankRR(banks[2:5])
    den_bank = banks[5]
    blocks = [(hd, qi, kb) for qi in range(NBLK) for hd in range(2) for kb in range(4 * qi + 4)]
    LOOKAHEAD = 6

    def stage1(i):
        hd, qi, kb = blocks[i]
        r = kb - 4 * qi
        c0 = 128 * r if r > 0 else 0
        sb_ = s_banks()
        pt = pT[i % 7]
        kblk, ko = kb // 4, (kb % 4) * 128
        k.mm(sb_[:, c0:512], kTv[hd][kblk][:, ko:ko + 128], qTv[hd][qi][:, c0:512])
        k.act(pt[:, c0:512], sb_[:, c0:512], AF.Exp, bias=negcb[qi][:, hd:hd + 1], scale=1.0)
        if r >= 0:
            k.tt("pool", pt[:, c0:c0 + 128], pt[:, c0:c0 + 128], tri[:, :], ALU.mult)
        return pt, c0

    def stage2(i, pt, c0):
        hd, qi, kb = blocks[i]
        nkb = 4 * qi + 4
        g = qi * 2 + hd
        oacc = banks[g % 2]
        k.mm(oacc[:, c0:512], Vpv[kb // 4][:, hd, kb % 4, :], pt[:, c0:512], start=(kb == 0), stop=(kb == nkb - 1))
        if kb == nkb - 1:
            ob = osb[g % 2]
            orr = ores[g % 2]
            k.copy("act", ob[:, :], oacc[:, :])
            k.op("dve", lambda e, ob=ob: e.reciprocal(ob[64:128, :].ap, ob[64:128, :].ap), [ob], [ob])
            k.mm(den_bank[0:64, :], esel[:, :], ob[:, :])
            k.tt("dve", orr[:, :], ob[0:64, :], den_bank[0:64, :], ALU.mult)
            k.dma("sp", G["osrc"][0][qi // 4][hd * 64:(hd + 1) * 64, (qi % 4) * 512:(qi % 4 + 1) * 512], orr[:, :])

    pend = []
    cur_qi = -1
    for i in range(len(blocks)):
        if blocks[i][1] != cur_qi:
            cur_qi = blocks[i][1]
            k.mark()
        pend.append((i,) + stage1(i))
        if len(pend) > LOOKAHEAD:
            stage2(*pend.pop(0))
    while pend:
        stage2(*pend.pop(0))

    k = k0
    assert len(rp.segs) == NBLK + 1 and len(ra.segs) == NBLK + 1 and not rp.segs[0] and not ra.segs[0]
    load(0)
    load(1)
    replay_merged(k, rp.segs[1])
    for qi in range(NBLK):
        load(qi + 2)
        replay_merged(k, ra.segs[1 + qi], rp.segs[2 + qi] if qi + 1 < NBLK else [])
    k.pop()


def emit_hg(k, banks, C, W, G, layer, hTb=None):
    T = S
    NBLK = T // 512
    w_d, lbr_d, lbc_d, on_d = W["w_hg"], G["lb_rows"], G["lb_cols"], W["hg_o_norm"]
    cU, cUrel, cW, cones, mbd, rowm, ident = C["cU"], C["cUrel"], C["cW"], C["cones"], C["maskbd"], C["rowmask"], C["ident"]
    nb = BankRR(banks)
    k.push()

    whg = k.sb("whg", [128, 8, 512], BF16)
    for kc in range(8):
        k.dma("pool", whg[:, kc, :], w_d[kc * 128:(kc + 1) * 128, :])
    gb = k.sb("gb", [128, 128], F32)
    k.dma("sp", gb[:, :], bcast_row(on_d))
    oTb = [k.sb(f"oTb{i}", [128, 512], BF16) for i in range(2)]

    def lower_bound(x, n, name):
        m = k.sb(name + "_m", [128, n], F32)
        e = k.sb(name + "_e", [128, DEPTH, n], F32)
        ssum = k.sb(name + "_s", [128, n], F32)
        lb = k.sb(name + "_lb", [128, n], F32)
        oml = k.sb(name + "_oml", [128, n], F32)
        k.copy("dve", m[:, :], x[:, 0, :])
        for i in range(1, DEPTH):
            k.tt("dve", m[:, :], m[:, :], x[:, i, :], ALU.max)
        for i in range(DEPTH):
            k.tt("dve", e[:, i, :], x[:, i, :], m[:, :], ALU.subtract)
        k.act(e[:, :, :], e[:, :, :], AF.Exp)
        k.copy("dve", ssum[:, :], e[:, 0, :])
        for i in range(1, DEPTH):
            k.tt("dve", ssum[:, :], ssum[:, :], e[:, i, :], ALU.add)
        k.op("dve", lambda en: en.reciprocal(ssum[:, :].ap, ssum[:, :].ap), [ssum], [ssum])
        for i in range(DEPTH):
            k.tt("dve", e[:, i, :], e[:, i, :], ssum[:, :], ALU.mult)
        k.copy("dve", lb[:, :], e[:, 0, :])
        for i in range(1, layer + 1):
            k.tt("dve", lb[:, :], lb[:, :], e[:, i, :], ALU.add)
        k.tt("dve", lb[:, :], lb[:, :], e[:, 0, :], ALU.subtract)
        k.ts("dve", oml[:, :], lb[:, :], -1.0, 1.0, ALU.mult, ALU.add)
        return lb, oml

    xr = k.sb("xr", [128, DEPTH, 128], F32)
    for i in range(DEPTH):
        k.dma("sp", xr[:, i, :], lbr_d.v(lbr_d.h[i:i + 1, :].partition_broadcast(128)))
    lb_b, oml_b = lower_bound(xr, 128, "lbr")
    xc = k.sb("xc", [128, DEPTH, 1], F32)
    k.dma("sp", xc[:, :, 0], lbc_d[:, :])
    lb_c, oml_c = lower_bound(xc, 1, "lbc")

    NS = 8
    Sf = [k.sb(f"Sf{i}", [128, 128], F32) for i in range(2)]
    Sb = [k.sb(f"Sb{i}", [128, 128], BF16) for i in range(NS)]
    k.memset("dve", Sf[0][:, :], 0.0)
    k.memset("dve", Sb[0][:, :], 0.0)
    Z = [k.sb(f"Z{i}", [128, 4, 128], BF16) for i in range(2)]
    for z in Z:
        k.memset("pool", z[:, :, :], 0.0)
    si = 0

    shared = hTb is not None
    if not shared:
        hTb = [k.sb(f"hTb{i}", [128, 8, 512], BF16) for i in range(2)]
    qTs = [k.sb(f"qTs{i}", [128, 512], F32) for i in range(2)]
    kTs = [k.sb(f"kTs{i}", [128, 512], F32) for i in range(2)]

    def dbl(name, shape, dt, n=2):
        return [k.sb(f"{name}{i}", shape, dt) for i in range(n)]

    sgx = dbl("sgx", [128, 384], F32)
    sg = [s_[:, 0:128] for s_ in sgx]
    uu = dbl("uu", [128, 128], F32)
    ff = dbl("ff", [128, 128], F32)
    ktm = dbl("ktm", [128, 128], F32)
    logf = dbl("logf", [128, 128], F32)
    vbf = dbl("vbf", [128, 128], BF16)
    sgate = dbl("sgate", [128, 128], F32)
    e1 = dbl("e1", [128, 128], F32)
    e2 = dbl("e2", [128, 128], F32)
    e3 = dbl("e3", [128, 128], F32)
    e4 = dbl("e4", [128, 128], F32)
    dl = dbl("dl", [128, 4], F32)
    qpT = dbl("qpT", [128, 128], BF16)
    kpT = dbl("kpT", [128, 128], BF16)
    kdp = dbl("kdp", [128, 4, 128], BF16)
    attm = dbl("attm", [128, 128], BF16)
    osq = dbl("osq", [128, 128], F32)
    ost = dbl("ost", [128, 2], F32)
    y1 = dbl("y1", [128, 128], F32)
    y2 = dbl("y2", [128, 128], F32)

    nt = 0
    for blk in range(NBLK):
        col = slice(blk * 512, (blk + 1) * 512)
        hb = hTb[blk % len(hTb)]
        if shared:
            k.mark()
        else:
            for kc in range(8):
                k.dma("sp", hb[:, kc, :], G["hT_blk"](blk, kc))
        qT_s, kT_s = qTs[blk % 2], kTs[blk % 2]
        bq = nb()
        for kc in range(8):
            k.mm(bq[:, :], whg[:, kc, 0:128], hb[:, kc, :], start=(kc == 0), stop=(kc == 7))
        k.act(qT_s[:, :], bq[:, :], AF.Exp, scale=-1.0)
        k.act(qT_s[:, :], qT_s[:, :], AF.Ln, bias=1.0, scale=1.0)
        k.act(qT_s[:, :], qT_s[:, :], AF.Exp, scale=-1.0)
        k.tt("dve", qT_s[:, :], bq[:, :], qT_s[:, :], ALU.mult)
        bz = nb()
        for kc in range(8):
            k.mm(bz[:, :], whg[:, kc, 128:256], hb[:, kc, :], start=(kc == 0), stop=(kc == 7))
        k.act(kT_s[:, :], bz[:, :], AF.Exp)
        k.act(kT_s[:, :], kT_s[:, :], AF.Ln, bias=1.0, scale=1.0)
        k.act(kT_s[:, :], kT_s[:, :], AF.Exp, scale=-1.0)
        k.ts("dve", kT_s[:, :], kT_s[:, :], oml_c[:, 0:1], None, ALU.mult)
        for tt_ in range(4):
            p = nt % 2
            nt += 1
            tcol = slice(tt_ * 128, (tt_ + 1) * 128)
            tok = slice(blk * 512 + tt_ * 128, blk * 512 + (tt_ + 1) * 128)
            btm = nb()
            for kc in range(8):
                k.mm(btm[:, 0:384], hb[:, kc, tcol], whg[:, kc, 128:512], start=(kc == 0), stop=(kc == 7))
            k.act(sgx[p][:, :], btm[:, 0:384], AF.Exp, scale=-1.0)
            k.copy("act", vbf[p][:, :], btm[:, 128:256])
            k.act(sgx[p][:, :], sgx[p][:, :], AF.Ln, bias=1.0, scale=1.0)
            k.act(sgx[p][:, :], sgx[p][:, :], AF.Exp, scale=-1.0)
            k.tt("dve", sgate[p][:, :], btm[:, 256:384], sgx[p][:, 256:384], ALU.mult)
            k.tt("dve", uu[p][:, :], sg[p][:, :], oml_b[:, :], ALU.mult)
            k.tt("pool", ff[p][:, :], uu[p][:, :], lb_b[:, :], ALU.add)
            k.tt("pool", ktm[p][:, :], oml_b[:, :], uu[p][:, :], ALU.subtract)
            k.ts("dve", ff[p][:, :], ff[p][:, :], 1e-30, None, ALU.max)
            k.act(logf[p][:, :], ff[p][:, :], AF.Ln)
            bc = nb()
            k.mm(bc[:, 0:128], logf[p][:, :], cU[:, :], inc=False)
            k.mm(bc[:, 128:256], logf[p][:, :], cUrel[:, :], inc=False)
            k.mm(bc[:, 256:384], cW[:, :], logf[p][:, :], inc=False)
            k.mm(bc[:, 384:388], logf[p][:, :], cones[:, :], inc=True)
            k.act(e1[p][:, :], bc[:, 0:128], AF.Exp)
            k.act(e2[p][:, :], bc[:, 128:256], AF.Exp)
            k.act(e3[p][:, :], bc[:, 128:256], AF.Exp, scale=-1.0)
            k.act(e4[p][:, :], bc[:, 256:384], AF.Exp)
            k.act(dl[p][:, :], bc[:, 384:388], AF.Exp)
            z = Z[p]
            zdiag = z.v(bass.AP(z.h, 0, [[512, 128], [160, 4], [1, 32]]))
            k.tt("dve", zdiag, qT_s[:, tcol].f(lambda a: a.rearrange("p (c x) -> p c x", c=4)),
                 e1[p][:, :].f(lambda a: a.rearrange("p (c x) -> p c x", c=4)), ALU.mult)
            k.tt("pool", qpT[p][:, :], qT_s[:, tcol], e2[p][:, :], ALU.mult)
            k.tt("pool", kpT[p][:, :], kT_s[:, tcol], e3[p][:, :], ALU.mult)
            for c in range(4):
                k.stt("dve" if c % 2 == 0 else "pool", kdp[p][:, c, :], ktm[p][:, :], rowm[:, c:c + 1], e4[p][:, :],
                      ALU.mult, ALU.mult) if c % 2 == 0 else None
            for c in range(4):
                if c % 2 == 1:
                    k.stt("dve", kdp[p][:, c, :], ktm[p][:, :], rowm[:, c:c + 1], e4[p][:, :], ALU.mult, ALU.mult)
            ba = nb()
            k.mm(ba[:, 0:128], kpT[p][:, :], qpT[p][:, :])
            k.tt("dve", attm[p][:, :], ba[:, 0:128], mbd[:, :], ALU.mult)
            bs = nb()
            for c in range(4):
                k.mm(bs[:, c * 128:(c + 1) * 128], kdp[p][:, c, :], vbf[p][:, :], inc=(c == 3))
            bo = nb()
            for c in range(4):
                k.mm(bo[:, 0:128], z[:, c, :], Sb[(si + c) % NS][:, :], start=(c == 0), stop=False, inc=False)
                s_old = Sf[(si + c) % 2]
                s_new = Sf[(si + c + 1) % 2]
                k.stt("dve", s_new[:, :], s_old[:, :], dl[p][:, c:c + 1], bs[:, c * 128:(c + 1) * 128],
                      ALU.mult, ALU.add)
                k.copy("pool", Sb[(si + c + 1) % NS][:, :], s_new[:, :])
            k.mm(bo[:, 0:128], attm[p][:, :], vbf[p][:, :], start=False, stop=True, inc=True)
            si += 4
            k.act(osq[p][:, :], bo[:, 0:128], AF.Square, accum_out=ost[p][:, 0:1])
            k.rstd_ln(ost[p][:, 1:2], ost[p][:, 0:1], 1.0 / 128, EPS)
            k.stt("dve", y1[p][:, :], bo[:, 0:128], ost[p][:, 1:2], gb[:, :], ALU.mult, ALU.mult)
            k.tt("pool", y2[p][:, :], y1[p][:, :], sgate[p][:, :], ALU.mult)
            bt = nb()
            k.transpose(bt[:, 0:128], y2[p][:, :], ident[:, :])
            k.copy("act", oTb[blk % 2][:, tcol], bt[:, 0:128])
        k.dma("sp", G["osrc"][2][blk // 4][:, (blk % 4) * 512:(blk % 4 + 1) * 512], oTb[blk % 2][:, :])
    k.pop()


def emit_dn(k, banks, C, W, G, hTb=None):
    T = S
    NBLK = T // 512
    w_d, cw_d, alog_d, dtb_d, on_d = W["w_dn"], W["conv_w"], W["a_log"], W["dt_bias"], W["dn_o_norm"]
    uinc, lpos, uneg, ident = C["uinc"], C["lpos_s"], C["uneg"], C["ident"]
    if hTb is not None:
        nb = BankRR(banks[:-DN_BACK_BANKS])
        nbb = BankRR(banks[-DN_BACK_BANKS:])
    else:
        nb = nbb = BankRR(banks)
    k.push()

    wdn = k.sb("wdn", [128, 8, 514], BF16)
    for kc in range(8):
        k.dma("pool", wdn[:, kc, :], w_d[kc * 128:(kc + 1) * 128, :])
    cw = k.sb("cw", [128, 3, 4], F32)
    k.dma("sp", cw[:, :, :], cw_d[:, :, :])
    gb = k.sb("gb", [128, 128], F32)
    k.dma("sp", gb[:, :], bcast_row(on_d))
    oTb = [k.sb(f"oTb{i}", [128, 512], BF16) for i in range(2)]
    sc = k.sb("sc", [128, 4], F32)
    k.dma("sp", sc[:, 0:1], bcast_row(alog_d))
    k.dma("sp", sc[:, 1:2], bcast_row(dtb_d))
    k.act(sc[:, 2:3], sc[:, 0:1], AF.Exp)
    k.ts("dve", sc[:, 2:3], sc[:, 2:3], -1.0, None, ALU.mult)
    ones = k.sb("ones", [128, 128], F32)
    k.memset("dve", ones[:, :], 1.0)
    ey = k.sb("ey", [128, 512], F32)

    Sf = [k.sb(f"S{i}", [128, 128], F32) for i in range(2)]
    k.memset("dve", Sf[0][:, :], 0.0)
    si = 0

    shared = hTb is not None
    if not shared:
        hTb = [k.sb(f"hTb{i}", [128, 8, 512], BF16) for i in range(2)]
    xh = [[k.sb(f"xh{w}_{i}", [128, 515], F32) for i in range(2)] for w in range(3)]
    for w in range(3):
        k.memset("dve", xh[w][1][:, 512:515], 0.0)
    cv = [k.sb(f"cv{w}", [128, 512], F32) for w in range(3)]
    sq = [k.sb(f"sq{w}", [128, 512], F32) for w in range(2)]
    rs = [k.sb(f"rs{w}", [128, 512], F32) for w in range(2)]

    def per_tile(name, shape, dt=F32):
        return [k.sb(f"{name}{i}", shape, dt) for i in range(4)]

    def per_tile2(name, shape, dt=F32):
        return [k.sb(f"{name}{i}", shape, dt) for i in range(8 if shared else 4)]

    tmcA = per_tile2("tmc", [128, 8])
    sgateA = per_tile2("sgate", [128, 128])
    r130 = per_tile("r130", [128, 130])
    ktm = per_tile("ktm", [128, 128])
    vb = per_tile("vb", [128, 128])
    gbc = per_tile("gbc", [128, 128])
    xm = per_tile("xm", [128, 128])
    ym = per_tile("ym", [128, 128])
    dec_s = per_tile("dec_s", [128, 128])
    decT = per_tile("decT", [128, 128])
    egb = per_tile("egb", [128, 128])
    Mt = per_tile("M", [128, 128])
    Nt = per_tile("N", [128, 128])
    Pa = per_tile("Pa", [128, 128])
    Pat = per_tile("Pat", [128, 128])
    Pb = per_tile("Pb", [128, 128])
    Pbt = per_tile("Pbt", [128, 128])
    Rr = per_tile("R", [128, 128])
    Rt = per_tile("Rt", [128, 128])
    qkTA = per_tile2("qkT", [128, 128])
    qdTA = per_tile2("qdT", [128, 128])
    kbg = per_tile("kbg", [128, 128])
    kdecA = per_tile2("kdec", [128, 128])
    u_sbA = per_tile2("u", [128, 128])
    wT_sbA = per_tile2("wT", [128, 128])
    vnew = per_tile("vnew", [128, 128])
    osq = per_tile("osq", [128, 128])
    ost = per_tile("ost", [128, 2])
    y1 = per_tile("y1", [128, 128])
    y2 = per_tile("y2", [128, 128])

    for blk in range(NBLK):
        col = slice(blk * 512, (blk + 1) * 512)
        hb = hTb[blk % len(hTb)]
        if shared:
            k.mark()
        else:
            for kc in range(8):
                k.dma("sp", hb[:, kc, :], G["hT_blk"](blk, kc))
        pb = (blk % 2) * 4 if shared else 0
        tmc, sgate, qkT, qdT, kdec, u_sb, wT_sb = (x[pb:pb + 4] for x in (tmcA, sgateA, qkTA, qdTA, kdecA, u_sbA, wT_sbA))
        for w in range(3):
            bk = nb()
            for kc in range(8):
                k.mm(bk[:, :], wdn[:, kc, w * 128:(w + 1) * 128], hb[:, kc, :], start=(kc == 0), stop=(kc == 7))
            cur, prv = xh[w][blk % 2], xh[w][(blk + 1) % 2]
            k.copy("act", cur[:, 3:515], bk[:, :])
            k.copy("act", cur[:, 0:3], prv[:, 512:515])
            y = cv[w]
            k.ts("dve", y[:, :], cur[:, 0:512], cw[:, w, 0:1], None, ALU.mult)
            for m in range(1, 4):
                k.stt("dve" if m != 2 else "dve", y[:, :], cur[:, m:m + 512], cw[:, w, m:m + 1], y[:, :], ALU.mult, ALU.add)
            k.act(ey[:, :], y[:, :], AF.Exp, scale=-1.0)
            k.act(ey[:, :], ey[:, :], AF.Ln, bias=1.0, scale=1.0)
            k.act(ey[:, :], ey[:, :], AF.Exp, scale=-1.0)
            k.tt("pool", y[:, :], y[:, :], ey[:, :], ALU.mult)
        for w in range(2):
            k.act(sq[w][:, :], cv[w][:, :], AF.Square)
            bk = nb()
            k.mm(bk[:, :], ones[:, :], sq[w][:, :])
            k.rstd_ln(rs[w][:, :], bk[:, :], 1.0, EPS)
            if w == 0:
                k.stt("pool", cv[w][:, :], cv[w][:, :], 1.0, rs[w][:, :], ALU.mult, ALU.mult) if False else None
        k.tt("pool", cv[0][:, :], cv[0][:, :], rs[0][:, :], ALU.mult)
        k.tt("pool", cv[1][:, :], cv[1][:, :], rs[1][:, :], ALU.mult)
        k.act(cv[0][:, :], cv[0][:, :], AF.Copy, scale=float(128 ** -0.5))
        qT_, kT_, vT_ = cv

        for t in range(4):
            tc_ = slice(t * 128, (t + 1) * 128)
            c = tmc[t]
            bk = nb()
            for kc in range(8):
                k.mm(bk[:, 0:130], hb[:, kc, tc_], wdn[:, kc, 384:514], start=(kc == 0), stop=(kc == 7))
            k.act(r130[t][:, :], bk[:, 0:130], AF.Exp, scale=-1.0)
            k.act(c[:, 1:2], bk[:, 1:2], AF.Exp, bias=sc[:, 1:2], scale=1.0)
            k.act(r130[t][:, :], r130[t][:, :], AF.Ln, bias=1.0, scale=1.0)
            k.act(r130[t][:, :], r130[t][:, :], AF.Exp, scale=-1.0)
            k.copy("dve", c[:, 0:1], r130[t][:, 0:1])
            k.tt("dve", sgate[t][:, :], bk[:, 2:130], r130[t][:, 2:130], ALU.mult)
            k.act(c[:, 1:2], c[:, 1:2], AF.Ln, bias=1.0, scale=1.0)
            k.tt("dve", c[:, 2:3], c[:, 1:2], sc[:, 2:3], ALU.mult)
            bk = nb()
            k.transpose(bk[:, 0:128], kT_[:, tc_], ident[:, :], inc=False)
            k.transpose(bk[:, 128:256], vT_[:, tc_], ident[:, :], inc=True)
            k.copy("act", ktm[t][:, :], bk[:, 0:128])
            k.act(vb[t][:, :], bk[:, 128:256], AF.Copy, scale=c[:, 0:1])
            k.ts("dve", gbc[t][:, :], ones[:, :], c[:, 2:3], None, ALU.mult)
            bk = nb()
            k.mm(bk[:, 0:128], gbc[t][:, :], uinc[:, :], inc=False)
            k.mm(bk[:, 128:129], uinc[:, :], c[:, 2:3], inc=True)
            k.copy("dve", c[:, 3:4], bk[:, 128:129])
            k.copy("dve", c[:, 7:8], bk[:, 127:128])
            k.stt("dve", xm[t][:, :], bk[:, 0:128], c[:, 3:4], lpos[:, :], ALU.subtract, ALU.max)
            k.stt("dve", ym[t][:, :], bk[:, 0:128], c[:, 3:4], uneg[:, :], ALU.subtract, ALU.min)
            k.act(egb[t][:, :], bk[:, 0:128], AF.Exp)
            k.act(dec_s[t][:, :], xm[t][:, :], AF.Exp, scale=-1.0)
            k.act(decT[t][:, :], ym[t][:, :], AF.Exp)
            k.act(c[:, 4:5], c[:, 3:4], AF.Exp)
            k.tt("dve", c[:, 4:5], c[:, 4:5], c[:, 0:1], ALU.mult)
            k.act(c[:, 5:6], c[:, 3:4], AF.Exp, bias=c[:, 7:8], scale=-1.0)
            k.act(c[:, 6:7], c[:, 7:8], AF.Exp)
            k.ts("pool", kbg[t][:, :], ktm[t][:, :], c[:, 4:5], None, ALU.mult) if False else None
            k.ts("dve", kbg[t][:, :], ktm[t][:, :], c[:, 4:5], None, ALU.mult)
            k.ts("dve", kdec[t][:, :], ktm[t][:, :], c[:, 5:6], None, ALU.mult)
            k.tt("pool", qdT[t][:, :], qT_[:, tc_], egb[t][:, :], ALU.mult)
            bk = nb()
            k.mm(bk[:, 0:128], kT_[:, tc_], kT_[:, tc_], inc=False)
            k.mm(bk[:, 128:256], kT_[:, tc_], qT_[:, tc_], inc=True)
            k.stt("dve", Mt[t][:, :], bk[:, 0:128], c[:, 0:1], dec_s[t][:, :], ALU.mult, ALU.mult)
            k.tt("dve", qkT[t][:, :], bk[:, 128:256], decT[t][:, :], ALU.mult)
            bk = nb()
            k.transpose(bk[:, 0:128], Mt[t][:, :], ident[:, :])
            k.copy("act", Nt[t][:, :], bk[:, 0:128])
            k.tt("pool", Rr[t][:, :], ident[:, :], Nt[t][:, :], ALU.subtract)
            k.tt("pool", Rt[t][:, :], ident[:, :], Mt[t][:, :], ALU.subtract)

        P = [Nt[t] for t in range(4)]
        Ptr = [Mt[t] for t in range(4)]
        for lvl in range(6):
            last = lvl == 5
            newP = Pa if lvl % 2 == 0 else Pb
            newPt = Pat if lvl % 2 == 0 else Pbt
            for t in range(4):
                bk = nb()
                k.mm(bk[:, 0:128], Ptr[t][:, :], P[t][:, :], inc=last)
                if not last:
                    k.mm(bk[:, 128:256], P[t][:, :], Ptr[t][:, :], inc=True)
                k.copy("act", newP[t][:, :], bk[:, 0:128])
                if not last:
                    k.copy("dve", newPt[t][:, :], bk[:, 128:256])
            for t in range(4):
                bk = nb()
                k.mm(bk[:, 0:128], Rt[t][:, :], newP[t][:, :], inc=last)
                if not last:
                    k.mm(bk[:, 128:256], newP[t][:, :], Rt[t][:, :], inc=True)
                k.tt("dve", Rr[t][:, :], Rr[t][:, :], bk[:, 0:128], ALU.add)
                if not last:
                    k.tt("dve", Rt[t][:, :], Rt[t][:, :], bk[:, 128:256], ALU.add)
            P = [newP[t] for t in range(4)]
            Ptr = [newPt[t] for t in range(4)]

        for t in range(4):
            bk = nb()
            k.mm(bk[:, 0:128], Rr[t][:, :], vb[t][:, :], inc=False)
            k.mm(bk[:, 128:256], kbg[t][:, :], Rr[t][:, :], inc=True)
            k.copy("act", u_sb[t][:, :], bk[:, 0:128])
            k.copy("dve", wT_sb[t][:, :], bk[:, 128:256])

        if shared:
            k.mark()
        for t in range(4):
            tok = slice(blk * 512 + t * 128, blk * 512 + (t + 1) * 128)
            c = tmc[t]
            s_old = Sf[si % 2]
            s_new = Sf[(si + 1) % 2]
            si += 1
            bk = nbb()
            k.mm(bk[:, 0:128], wT_sb[t][:, :], s_old[:, :])
            k.tt("dve", vnew[t][:, :], u_sb[t][:, :], bk[:, 0:128], ALU.subtract)
            bo = nbb()
            k.mm(bo[:, 0:128], qdT[t][:, :], s_old[:, :], start=True, stop=False, inc=False)
            k.mm(bo[:, 0:128], qkT[t][:, :], vnew[t][:, :], start=False, stop=True, inc=True)
            bs = nbb()
            k.mm(bs[:, 0:128], kdec[t][:, :], vnew[t][:, :])
            k.stt("dve", s_new[:, :], s_old[:, :], c[:, 6:7], bs[:, 0:128], ALU.mult, ALU.add)
            k.act(osq[t][:, :], bo[:, 0:128], AF.Square, accum_out=ost[t][:, 0:1])
            k.rstd_ln(ost[t][:, 1:2], ost[t][:, 0:1], 1.0 / 128, EPS)
            k.stt("dve", y1[t][:, :], bo[:, 0:128], ost[t][:, 1:2], gb[:, :], ALU.mult, ALU.mult)
            k.tt("pool", y2[t][:, :], y1[t][:, :], sgate[t][:, :], ALU.mult)
            bt = nbb()
            k.transpose(bt[:, 0:128], y2[t][:, :], ident[:, :])
            k.copy("act", oTb[blk % 2][:, t * 128:(t + 1) * 128], bt[:, 0:128])
        k.dma("sp", G["osrc"][1][blk // 4][:, (blk % 4) * 512:(blk % 4 + 1) * 512], oTb[blk % 2][:, :])
    k.pop()


def emit_stage_c(k, banks, C, W, G, last):
    ntok = S * B // NCORES
    NT = ntok // 128
    NB = ntok // 512
    upto = 9
    h_d = G["h_cur"]
    wg_d, wout_d = W["w_gates"], W["w_out"]
    wbr_d = [W["w_br_a"], W["w_br_b"], W["w_br_c"]]
    ln1g_d, ln1b_d, ln2g_d, ln2b_d = W["ln1_g"], W["ln1_b"], W["ln2_g"], W["ln2_b"]
    wr_d, br_d = W["w_router"], W["b_router"]
    ewg_d, ewu_d, ewd_d = W["exp_w_gate"], W["exp_w_up"], W["exp_w_down"]
    out_d = G["out"]
    ident = C["ident"]
    k.push()

    comb_all = k.sb("comb_all", [128, NT, 32], F32)
    mixT = k.sb("mixT", [128, 8, ntok], BF16)

    k.push()
    wg = k.sb("wg", [128, 8, 3 * D], BF16)
    wbr = k.sb("wbr", [128, 12, D], BF16)
    for kc in range(8):
        k.dma("pool", wg[:, kc, :], wg_d[kc * 128:(kc + 1) * 128, :])
    for br in range(3):
        for kc in range(4):
            k.dma("pool", wbr[:, br * 4 + kc, :], wbr_d[br][kc * 128:(kc + 1) * 128, :])
    hTb = [k.sb(f"hTb{i}", [128, 8, 512], BF16) for i in range(2)]
    oTb = [k.sb(f"oTb{i}", [128, 12, 512], BF16) for i in range(2)]
    sg = [k.sb(f"sg{i}", [128, 512], BF16) for i in range(2)]
    tmx = [k.sb(f"tmx{i}", [128, 512], F32) for i in range(2)]
    mixf = [k.sb(f"mixf{i}", [128, 512], F32) for i in range(2)]
    nb = 0
    for tb in range(NB):
        tsl = slice(tb * 512, (tb + 1) * 512)
        hb, ob = hTb[tb % 2], oTb[tb % 2]
        for kc in range(8):
            k.dma("sp", hb[:, kc, :], G["hsrc"][kc // 2][(kc % 2) * 128:(kc % 2 + 1) * 128, tsl])
        for br in range(3):
            for kc in range(4):
                k.dma("sp", ob[:, br * 4 + kc, :], G["o_own"](br, kc, tsl), extra_reads=G["o_own_res"](br))
        for r in range(8):
            mf = mixf[r % 2]
            for br in range(3):
                bg = banks[nb % 2]
                by = banks[2 + nb % 2]
                sgt = sg[nb % 2]
                tm = tmx[nb % 2]
                nb += 1
                col = br * D + r * 128
                for kc in range(8):
                    k.mm(bg[:, :], wg[:, kc, col:col + 128], hb[:, kc, :], start=(kc == 0), stop=(kc == 7))
                k.act(sgt[:, :], bg[:, :], AF.Sigmoid)
                for kc in range(4):
                    k.mm(by[:, :], wbr[:, br * 4 + kc, r * 128:(r + 1) * 128], ob[:, br * 4 + kc, :],
                         start=(kc == 0), stop=(kc == 3))
                if br == 0:
                    k.tt("dve", mf[:, :], by[:, :], sgt[:, :], ALU.mult)
                elif br == 1:
                    k.tt("dve", tm[:, :], by[:, :], sgt[:, :], ALU.mult)
                    k.tt("pool", mf[:, :], mf[:, :], tm[:, :], ALU.add)
                else:
                    k.tt("dve", tm[:, :], by[:, :], sgt[:, :], ALU.mult)
                    k.tt("pool", mixT[:, r, tsl], mf[:, :], tm[:, :], ALU.add)
    k.pop()

    acc = [k.sb(f"acc{i}", [128, D], F32) for i in range(NT)]
    h1T = k.sb("h1T", [128, 8, ntok], BF16)
    k.push()
    wout = k.sb("wout", [128, 8, D], BF16)
    for kc in range(8):
        k.dma("pool", wout[:, kc, :], wout_d[kc * 128:(kc + 1) * 128, :])
    wr = k.sb("wr", [128, 8, 36], F32)
    for kc in range(8):
        k.dma("sp", wr[:, kc, :], wr_d[kc * 128:(kc + 1) * 128, :])
    brb = k.sb("brb", [128, 36], F32)
    k.dma("sp", brb[:, :], bcast_row(br_d))
    g1 = k.sb("g1", [128, D], F32)
    b1 = k.sb("b1", [128, D], F32)
    k.dma("sp", g1[:, :], bcast_row(ln1g_d))
    k.dma("sp", b1[:, :], bcast_row(ln1b_d))
    hts = [k.sb(f"ht{i}", [128, D], F32) for i in range(2)]
    x1s = [k.sb(f"x1{i}", [128, D], F32) for i in range(2)]
    h1s = [k.sb(f"h1{i}", [128, D], F32) for i in range(2)]
    tmps = [k.sb(f"lt{i}", [128, D], F32) for i in range(2)]
    sts = [k.sb(f"ls{i}", [128, 4], F32) for i in range(2)]
    hTf = [k.sb(f"hTf{i}", [128, 8, 128], F32) for i in range(2)]
    rl = [k.sb(f"rl{i}", [128, 36], F32) for i in range(2)]
    rs = [k.sb(f"rs{i}", [128, 16], F32) for i in range(2)]
    elm = [k.sb(f"elm{i}", [128, 32], F32) for i in range(2)]
    elm2 = [k.sb(f"elm2{i}", [128, 32], F32) for i in range(2)]
    oh1 = [k.sb(f"oh1{i}", [128, 32], F32) for i in range(2)]
    oh2 = [k.sb(f"oh2{i}", [128, 32], F32) for i in range(2)]
    k_main = k
    recs = [Rec(k_main), Rec(k_main)]
    for t in range(NT):
        p = t % 2
        k = recs[p]
        tok = slice(t * 128, (t + 1) * 128)
        ht, x1, h1t, tmp, st = hts[p], x1s[p], h1s[p], tmps[p], sts[p]
        k.dma("sp", ht[:, :], h_d[tok, :])
        for half in range(2):
            bk = banks[half + 2 * p]
            for kc in range(8):
                k.mm(bk[:, :], mixT[:, kc, tok], wout[:, kc, half * 512:(half + 1) * 512],
                     start=(kc == 0), stop=(kc == 7))
            k.stt("dve", x1[:, half * 512:(half + 1) * 512], ht[:, half * 512:(half + 1) * 512], ALPHA,
                  bk[:, :], ALU.mult, ALU.add)
        layer_norm_tile(k, x1[:, :], h1t[:, :], g1[:, :], b1[:, :], tmp[:, :], st[:, :])
        k.act(acc[t][:, :], h1t[:, :], AF.Copy, scale=ALPHA)
        hf = hTf[p]
        for q4 in range(2):
            bk = banks[4 + q4 + 2 * p]
            for j in range(4):
                kc = q4 * 4 + j
                k.transpose(bk[:, j * 128:(j + 1) * 128], h1t[:, kc * 128:(kc + 1) * 128], ident[:, :],
                            inc=(j == 3))
            k.copy("dve", hf[:, q4 * 4:(q4 + 1) * 4, :],
                   bk[:, :].f(lambda a: a.rearrange("p (j t) -> p j t", j=4)))
        k.copy("act", h1T[:, :, tok], hf[:, :, :])
        bk = banks[2 * p]
        for kc in range(8):
            k.mm(bk[:, 0:36], hf[:, kc, :], wr[:, kc, :], start=(kc == 0), stop=(kc == 7))
        l, s_, em, em2, o1, o2 = rl[p], rs[p], elm[p], elm2[p], oh1[p], oh2[p]
        cb = comb_all[:, t, :]
        k.tt("dve", l[:, :], bk[:, 0:36], brb[:, :], ALU.add)
        k.reduce("dve", s_[:, 0:1], l[:, 0:4], ALU.max)
        k.ts("dve", s_[:, 1:2], s_[:, 0:1], -1.0, None, ALU.mult)
        k.act(s_[:, 8:12], l[:, 0:4], AF.Exp, bias=s_[:, 1:2], scale=1.0, accum_out=s_[:, 2:3])
        k.op("dve", lambda e, s_=s_: e.reciprocal(s_[:, 3:4].ap, s_[:, 2:3].ap), [s_], [s_])
        k.ts("dve", s_[:, 12:16], l[:, 0:4], s_[:, 0:1], None, ALU.is_equal)
        k.ts("dve", s_[:, 12:16], s_[:, 12:16], BIG, -BIG, ALU.mult, ALU.add)
        k.tt("dve", em[:, :].f(lambda a: a.rearrange("p (g e) -> p g e", g=4)),
             l[:, 4:36].f(lambda a: a.rearrange("p (g e) -> p g e", g=4)),
             s_[:, 12:16].f(lambda a: a.unsqueeze(2).broadcast_to([128, 4, 8])), ALU.add)
        k.reduce("dve", s_[:, 4:5], em[:, :], ALU.max)
        k.ts("dve", o1[:, :], em[:, :], s_[:, 4:5], None, ALU.is_equal)
        k.stt("dve", em2[:, :], o1[:, :], -BIG, em[:, :], ALU.mult, ALU.add)
        k.reduce("dve", s_[:, 5:6], em2[:, :], ALU.max)
        k.ts("dve", o2[:, :], em2[:, :], s_[:, 5:6], None, ALU.is_equal)
        k.tt("dve", s_[:, 6:7], s_[:, 5:6], s_[:, 4:5], ALU.subtract)
        k.act(s_[:, 6:7], s_[:, 6:7], AF.Exp)
        k.ts("dve", s_[:, 7:8], s_[:, 6:7], 1.0, None, ALU.add)
        k.op("dve", lambda e, s_=s_: e.reciprocal(s_[:, 7:8].ap, s_[:, 7:8].ap), [s_], [s_])
        k.tt("dve", s_[:, 7:8], s_[:, 7:8], s_[:, 3:4], ALU.mult)
        k.tt("dve", s_[:, 6:7], s_[:, 6:7], s_[:, 7:8], ALU.mult)
        k.ts("dve", cb, o1[:, :], s_[:, 7:8], None, ALU.mult)
        k.stt("dve", cb, o2[:, :], s_[:, 6:7], cb, ALU.mult, ALU.add)
    k = k_main
    replay_merged(k, recs[0].segs[0], recs[1].segs[0])
    k.pop()

    k.push()
    NW = 3
    ewg = [k.sb(f"ewg{i}", [128, 8, 256], BF16) for i in range(NW)]
    ewu = [k.sb(f"ewu{i}", [128, 8, 256], BF16) for i in range(NW)]
    ewd = [k.sb(f"ewd{i}", [128, 2, D], BF16) for i in range(NW)]
    sgs = [k.sb(f"sG{i}", [128, 512], BF16) for i in range(2)]
    hcs = [k.sb(f"Hc{i}", [128, 2, 512], BF16) for i in range(2)]
    n1 = 0
    n2 = [0]
    pending = None

    def down(e, tb, hc, wdt):
        for tt_ in range(4):
            t = tb * 4 + tt_
            for half in range(2):
                bO = banks[5 + n2[0] % 3]
                n2[0] += 1
                for fc in range(2):
                    k.mm(bO[:, :], hc[:, fc, tt_ * 128:(tt_ + 1) * 128], wdt[:, fc, half * 512:(half + 1) * 512],
                         start=(fc == 0), stop=(fc == 1))
                k.stt("dve", acc[t][:, half * 512:(half + 1) * 512], bO[:, :], comb_all[:, t, e:e + 1],
                      acc[t][:, half * 512:(half + 1) * 512], ALU.mult, ALU.add)

    for e in range(NEXP):
        wgt, wut, wdt = ewg[e % NW], ewu[e % NW], ewd[e % NW]
        k.dma("pool", wgt[:, :, :], ewg_d.v(ewg_d.h[e].rearrange("(kc p) f -> p kc f", p=128)))
        k.dma("pool", wut[:, :, :], ewu_d.v(ewu_d.h[e].rearrange("(kc p) f -> p kc f", p=128)))
        k.dma("pool", wdt[:, :, :], ewd_d.v(ewd_d.h[e].rearrange("(fc p) d -> p fc d", p=128)))
        for tb in range(NB):
            tsl = slice(tb * 512, (tb + 1) * 512)
            hc = hcs[tb % 2]
            for fc in range(2):
                bG = banks[1 + n1 % 2]
                bU = banks[3 + n1 % 2]
                sgt = sgs[n1 % 2]
                n1 += 1
                for kc in range(8):
                    k.mm(bG[:, :], wgt[:, kc, fc * 128:(fc + 1) * 128], h1T[:, kc, tsl], start=(kc == 0), stop=(kc == 7))
                for kc in range(8):
                    k.mm(bU[:, :], wut[:, kc, fc * 128:(fc + 1) * 128], h1T[:, kc, tsl], start=(kc == 0), stop=(kc == 7))
                k.act(sgt[:, :], bG[:, :], AF.Silu)
                k.tt("dve", hc[:, fc, :], bU[:, :], sgt[:, :], ALU.mult)
            if pending is not None:
                down(*pending)
            pending = (e, tb, hc, wdt)
    down(*pending)
    k.pop()

    k.push()
    g2 = k.sb("g2", [128, D], F32)
    b2 = k.sb("b2", [128, D], F32)
    k.dma("sp", g2[:, :], bcast_row(ln2g_d))
    k.dma("sp", b2[:, :], bcast_row(ln2b_d))
    ys = [k.sb(f"y{i}", [128, D], F32) for i in range(2)]
    tmps = [k.sb(f"lt{i}", [128, D], F32) for i in range(2)]
    sts = [k.sb(f"ls{i}", [128, 4], F32) for i in range(2)]
    if not last:
        hTsb = k.sb("hTsb", [128, 8, ntok], BF16)
    recs = [Rec(k), Rec(k)]
    for t in range(NT):
        p = t % 2
        kr = recs[p]
        layer_norm_tile(kr, acc[t][:, :], ys[p][:, :], g2[:, :], b2[:, :], tmps[p][:, :], sts[p][:, :], eng_g="dve")
        if last:
            kr.dma("sp", out_d[t * 128:(t + 1) * 128, :], ys[p][:, :])
        else:
            kr.dma("sp", h_d[t * 128:(t + 1) * 128, :], ys[p][:, :])
            emit_publish_tile(kr, banks, ident, ys[p], hTsb, t)
    replay_merged(k, recs[0].segs[0], recs[1].segs[0])
    if not last:
        emit_allgather_h(k, hTsb, G)
    k.pop()
    k.pop()


CONST_SHAPES = {"ident": [128, 128], "esel": [128, 64], "frq": [128, 1], "sgn": [128, 1],
                "cU": [128, 128], "cUrel": [128, 128], "cW": [128, 128], "cones": [128, 4], "maskbd": [128, 128],
                "rowmask": [128, 4], "uinc": [128, 128], "lpos_s": [128, 128], "uneg": [128, 128]}

LAYER_SHAPES = {
    "w_lat": [D, 448], "g_q": [128, 2], "g_kv": [128, 1], "w_uq": [256, 256], "w_uk": [128, 128], "w_uv": [128, 128],
    "w_hg": [D, 512], "hg_o_norm": [1, 128],
    "w_dn": [D, 514], "conv_w": [128, 3, 4], "a_log": [1, 1], "dt_bias": [1, 1], "dn_o_norm": [1, 128],
    "w_gates": [D, 3 * D], "w_br_a": [512, D], "w_br_b": [512, D], "w_br_c": [512, D], "w_out": [D, D],
    "ln1_g": [1, D], "ln1_b": [1, D], "ln2_g": [1, D], "ln2_b": [1, D],
    "w_router": [D, 36], "b_router": [1, 36],
    "exp_w_gate": [NEXP, D, 256], "exp_w_up": [NEXP, D, 256], "exp_w_down": [NEXP, 256, D],
}


def build_fused():
    nc = bass.Bass("TRN2", target_bir_lowering=False)
    k = K(nc)
    ntok = S * B // NCORES
    G = {}
    G["x"] = k.dram("x", [ntok, D], F32, "ExternalInput")
    G["ln_in_g"] = k.dram("ln_in_g", [1, D], F32, "ExternalInput")
    G["ln_in_b"] = k.dram("ln_in_b", [1, D], F32, "ExternalInput")
    G["pos"] = k.dram("pos", [1, S], I32, "ExternalInput")
    G["lb_rows"] = k.dram("lb_rows", [DEPTH, 128], F32, "ExternalInput")
    G["lb_cols"] = k.dram("lb_cols", [128, DEPTH], F32, "ExternalInput")
    rank_d = k.dram("rank", [1, 1], I32, "ExternalInput")
    tri_d = k.dram("tri", [128, 128], F32, "ExternalInput")
    G["out"] = k.dram("out", [ntok, D], F32, "ExternalOutput")
    cdram = {n: k.dram("c_" + n, shp, F32, "ExternalInput") for n, shp in CONST_SHAPES.items()}
    Ws = [{n: k.dram(f"L{l}_{n}", shp, F32, "ExternalInput") for n, shp in LAYER_SHAPES.items()} for l in range(DEPTH)]

    G["h_cur"] = k.dram("h_cur", [ntok, D], F32, "Internal")
    G["hsrc"] = [k.dram(f"hsrc{q}", [256, ntok], BF16, "Internal") for q in range(4)]
    G["hdst"] = [k.dram(f"hdst{q}", [4 * 256, ntok], BF16, "Internal") for q in range(4)]
    G["osrc"] = [[k.dram(f"osrc{br}_{q}", [128, ntok], BF16, "Internal") for q in range(4)] for br in range(3)]
    odst_full = [nc.dram_tensor(f"odst{br}", [4, 512, ntok], BF16, kind="Internal").ap() for br in range(3)]
    G["odst"] = [[T(odst_full[br][q], f"odst{br}_{q}") for q in range(4)] for br in range(3)]

    def hT_blk(blk, kc):
        r, t0, q = blk // 4, (blk % 4) * 512, kc // 2
        row0 = r * 256 + (kc % 2) * 128
        return G["hdst"][q][row0:row0 + 128, t0:t0 + 512]

    G["hT_blk"] = hT_blk

    reg = nc.sync.alloc_register("rank")
    nc.sync.reg_load(reg, rank_d.h[0:1, 0:1])
    rank_off = nc.sync.snap(reg, min_val=0, max_val=3)

    def o_own(br, kc, tsl):
        ap = odst_full[br][bass.ds(rank_off, 1), kc * 128:(kc + 1) * 128, tsl].rearrange("o p c -> (o p) c")
        return V(ap, G["odst"][br][0].res)

    G["o_own"] = o_own
    G["o_own_res"] = lambda br: [G["odst"][br][q].res for q in range(1, 4)]

    banks = [k.ps(f"bank{i}", [128, 512], F32) for i in range(8)]

    cres = Res("consts")
    C = {}
    for n, shp in CONST_SHAPES.items():
        C[n] = k.sb("c_" + n, shp, F32, res=cres)
        k.dma("sp", C[n][tuple(slice(None) for _ in shp)], cdram[n][tuple(slice(None) for _ in shp)])
    C["tri"] = k.sb("c_tri", [128, 128], BF16)
    k.dma("pool", C["tri"][:, :], tri_d[:, :])

    emit_ln0(k, banks, C, G)
    for l in range(DEPTH):
        W = Ws[l]
        emit_mla(k, banks, C, W, G)
        emit_allgather_o(k, G, 0)
        emit_dn_hg(k, banks, C, W, G, l)
        emit_stage_c(k, banks, C, W, G, last=(l == DEPTH - 1))
    k.finish([G["out"]])
    assert 5 + k.ndsem + k.ncoll <= 100, (k.ndsem, k.ncoll)
    build_fused.stats = (k.ndsem, k.ncoll, dict(k.tok))
    return nc


def layer_inputs(P, l, j):
    m = {}
    a = mla_inputs(None, np.zeros(1, np.int32), P, l, j)
    for n in ("w_lat", "g_q", "g_kv", "w_uq", "w_uk", "w_uv"):
        m[n] = a[n]
    hgi = hg_inputs(None, P, l, j)
    m["w_hg"] = hgi["w_hg"]
    m["hg_o_norm"] = hgi["o_norm"]
    dni = dn_inputs(None, P, l, j)
    for n in ("w_dn", "conv_w", "a_log", "dt_bias"):
        m[n] = dni[n]
    m["dn_o_norm"] = dni["o_norm"]
    w_in = P["w_in"][l]
    m.update({
        "w_gates": np.ascontiguousarray(w_in[:, 4520:]),
        "w_br_a": P["w_br_a"][l], "w_br_b": P["w_br_b"][l], "w_br_c": P["w_br_c"][l], "w_out": P["w_out"][l],
        "ln1_g": P["ln1_g"][l].reshape(1, D), "ln1_b": P["ln1_b"][l].reshape(1, D),
        "ln2_g": P["ln2_g"][l].reshape(1, D), "ln2_b": P["ln2_b"][l].reshape(1, D),
        "w_router": np.ascontiguousarray(np.concatenate([P["router_group_w"][l], P["router_expert_w"][l]], axis=1)),
        "b_router": np.concatenate([P["router_group_b"][l], P["router_expert_b"][l]]).reshape(1, 36),
        "exp_w_gate": P["exp_w_gate"][l], "exp_w_up": P["exp_w_up"][l], "exp_w_down": P["exp_w_down"][l],
    })
    return {f"L{l}_{n}": np.ascontiguousarray(v, dtype=np.float32) for n, v in m.items()}


def const_inputs():
    c = {}
    c.update(hg_consts())
    c.update(dn_consts())
    a = mla_inputs(None, np.zeros(1, np.int32), None, 0, 0, consts_only=True)
    c.update({n: a[n] for n in ("frq", "sgn", "esel")})
    out = {"c_" + n: np.ascontiguousarray(c[n], dtype=np.float32) for n in CONST_SHAPES}
    out["tri"] = a["tri"]
    return out


def kernel(**inputs):
    P = {k_: np.asarray(v) for k_, v in inputs.items()}
    x = np.asarray(P["x"], dtype=np.float32).reshape(B * S, D)
    pos = P["positions"]
    per = B * S // NCORES
    nc = build_fused()
    consts = const_inputs()
    in_maps = []
    for c in range(NCORES):
        b, j = c // 4, c % 4
        m = dict(consts)
        m["x"] = np.ascontiguousarray(x[c * per:(c + 1) * per])
        m["ln_in_g"] = P["ln_in_g"].reshape(1, D).astype(np.float32)
        m["ln_in_b"] = P["ln_in_b"].reshape(1, D).astype(np.float32)
        m["pos"] = np.ascontiguousarray(pos[b].reshape(1, S).astype(np.int32))
        lb = P["hg_lower_bounds"][:, j * 128:(j + 1) * 128].astype(np.float32)
        m["lb_rows"] = np.ascontiguousarray(lb)
        m["lb_cols"] = np.ascontiguousarray(lb.T)
        m["rank"] = np.array([[j]], np.int32)
        for l in range(DEPTH):
            m.update(layer_inputs(P, l, j))
        in_maps.append(m)
    res = run_bass_kernel_spmd(nc, in_maps, core_ids=list(range(NCORES)))
    out = np.concatenate([r["out"] for r in res.results], axis=0)
    return np.ascontiguousarray(out.reshape(B, S, D).astype(np.float32))
```
